# Optimizing a Trainium2 kernel written in Bass

```python
import jax, jax.numpy as jnp
from jax import lax
import numpy as np

D_MODEL = 1024
BATCH = 32
SEQ = 2048
DEPTH = 1

PLE_DIM = 256
ATT_HEADS = 8
ATT_KV_HEADS = 2
HEAD_DIM = 64
ATT_WIDTH = ATT_HEADS * HEAD_DIM
KV_WIDTH = ATT_KV_HEADS * HEAD_DIM
WINDOW = 128
BLOCK_Q = 128
ROPE_THETA = 10000.0
RWKV_HEADS = 8
RWKV_HEAD = 64
RWKV_WIDTH = RWKV_HEADS * RWKV_HEAD
DECAY_LORA = 64
AAA_LORA = 64
GATE_LORA = 128
RWKV_GN_EPS = 64e-5
ATT_COLS = ATT_WIDTH + 2 * KV_WIDTH
SHIFT_COLS = 3 * RWKV_WIDTH + DECAY_LORA + AAA_LORA + GATE_LORA
GATE_COLS = 2 * D_MODEL
IN_WIDTH = ATT_COLS + SHIFT_COLS + GATE_COLS
N_GROUPS = 4
EXPERTS_PER_GROUP = 8
N_EXPERTS = N_GROUPS * EXPERTS_PER_GROUP
TOP_K = 2
D_EXPERT = 512
MOE_BLOCK = 256
NORM_EPS = 1e-6
NEG_INF = -1e30

kernel_name = "hybrid_swa_rwkv7_hiermoe_block"


def rmsnorm(x, g):
    xf = x.astype(jnp.float32)
    y = xf * lax.rsqrt(jnp.mean(xf * xf, axis=-1, keepdims=True) + NORM_EPS)
    return (y * g.astype(jnp.float32)).astype(x.dtype)


def apply_rope(t, pos):
    half = t.shape[-1] // 2
    inv_freq = ROPE_THETA ** (-jnp.arange(half, dtype=jnp.float32) / half)
    ang = pos.astype(jnp.float32)[..., None] * inv_freq
    cos = jnp.cos(ang)[:, :, None, :]
    sin = jnp.sin(ang)[:, :, None, :]
    tf = t.astype(jnp.float32)
    t1, t2 = tf[..., :half], tf[..., half:]
    return jnp.concatenate([t1 * cos - t2 * sin, t2 * cos + t1 * sin], axis=-1).astype(t.dtype)


def sliding_window_attention(q, k, v, sinks):
    B, S, H, Dh = q.shape
    nb = S // BLOCK_Q
    grp = H // ATT_KV_HEADS
    qb = q.reshape(B, nb, BLOCK_Q, ATT_KV_HEADS, grp, Dh)
    kb = k.reshape(B, nb, BLOCK_Q, ATT_KV_HEADS, Dh)
    vb = v.reshape(B, nb, BLOCK_Q, ATT_KV_HEADS, Dh)
    prev = lambda t: jnp.concatenate([jnp.zeros_like(t[:, :1]), t[:, :-1]], axis=1)
    kk = jnp.concatenate([prev(kb), kb], axis=2)
    vv = jnp.concatenate([prev(vb), vb], axis=2)
    scores = jnp.einsum('bnqkgd,bnskd->bnkgqs', qb, kk,
                        preferred_element_type=jnp.float32) * (Dh ** -0.5)
    qi = jnp.arange(BLOCK_Q)[:, None]
    si = jnp.arange(2 * BLOCK_Q)[None, :]
    diff = qi + BLOCK_Q - si
    band = (diff >= 0) & (diff < WINDOW)
    exists = (jnp.arange(nb)[:, None, None] > 0) | (si >= BLOCK_Q)[None]
    valid = band[None] & exists
    scores = jnp.where(valid[None, :, None, None], scores, NEG_INF)
    sink = sinks.astype(jnp.float32).reshape(ATT_KV_HEADS, grp)[None, None, :, :, None, None]
    m = jnp.maximum(jnp.max(scores, axis=-1, keepdims=True), sink)
    e = jnp.exp(scores - m)
    probs = e / (jnp.sum(e, axis=-1, keepdims=True) + jnp.exp(sink - m))
    out = jnp.einsum('bnkgqs,bnskd->bnqkgd', probs.astype(v.dtype), vv)
    return out.reshape(B, S, H * Dh)


def token_shift(y, mu):
    prev = jnp.pad(y, ((0, 0), (1, 0), (0, 0)))[:, :-1]
    return y + (prev - y) * mu


def rwkv7_time_mix(r, k, v, xw, xa, xg, w0, w_decay_up, a0, w_aaa_up, w_gate_up,
                   k_k, k_a, r_k, ln_x_w, ln_x_b):
    B, S, C = r.shape
    H, N = RWKV_HEADS, RWKV_HEAD
    f32 = jnp.float32
    r, k, v = r.astype(f32), k.astype(f32), v.astype(f32)
    w = -jax.nn.softplus(-(w0.astype(f32) + jnp.tanh(xw.astype(f32)) @ w_decay_up.astype(f32))) - 0.5
    decay = jnp.exp(-jnp.exp(w))
    a = jax.nn.sigmoid(a0.astype(f32) + xa.astype(f32) @ w_aaa_up.astype(f32))
    g = jax.nn.sigmoid(xg.astype(f32)) @ w_gate_up.astype(f32)
    kk = (k * k_k.astype(f32)).reshape(B, S, H, N)
    kk = kk / jnp.maximum(jnp.sqrt(jnp.sum(kk * kk, axis=-1, keepdims=True)), 1e-12)
    k = k * (1.0 + (a - 1.0) * k_a.astype(f32))

    def step(state, inp):
        r_t, w_t, k_t, v_t, kk_t, a_t = inp
        sa = jnp.einsum('bhvk,bhk->bhv', state, -kk_t)
        state = (state * w_t[:, :, None, :]
                 + sa[..., None] * (kk_t * a_t)[:, :, None, :]
                 + v_t[..., None] * k_t[:, :, None, :])
        y_t = jnp.einsum('bhvk,bhk->bhv', state, r_t)
        return state, y_t

    to_seq = lambda t: t.reshape(B, S, H, N).transpose(1, 0, 2, 3)
    xs = (to_seq(r), to_seq(decay), to_seq(k), to_seq(v), kk.transpose(1, 0, 2, 3), to_seq(a))
    _, ys = lax.scan(step, jnp.zeros((B, H, N, N), f32), xs)
    y = ys.transpose(1, 0, 2, 3)
    mu = jnp.mean(y, axis=-1, keepdims=True)
    var = jnp.mean(jnp.square(y - mu), axis=-1, keepdims=True)
    y = ((y - mu) * lax.rsqrt(var + RWKV_GN_EPS)).reshape(B, S, C)
    y = y * ln_x_w.astype(f32) + ln_x_b.astype(f32)
    bonus = jnp.sum((r * k * r_k.astype(f32)).reshape(B, S, H, N), axis=-1, keepdims=True) * v.reshape(B, S, H, N)
    y = y + bonus.reshape(B, S, C)
    return y * g


def hierarchical_moe(h, w_group, b_group, w_expert, b_expert, w_gate_e, w_up_e, w_down_e):
    B, S, D = h.shape
    T = B * S
    xt = h.reshape(T, D)
    g_prob = jax.nn.softmax((xt @ w_group + b_group).astype(jnp.float32), axis=-1)
    g_w, g_idx = lax.top_k(g_prob, 1)
    e_logits = (xt @ w_expert + b_expert).astype(jnp.float32).reshape(T, N_GROUPS, EXPERTS_PER_GROUP)
    e_sel = jnp.take_along_axis(e_logits, g_idx[:, :, None], axis=1)[:, 0]
    e_w, e_idx = lax.top_k(jax.nn.softmax(e_sel, axis=-1), TOP_K)
    e_w = e_w / jnp.sum(e_w, axis=-1, keepdims=True)
    weights = (g_w * e_w).reshape(-1)
    experts = (g_idx * EXPERTS_PER_GROUP + e_idx).reshape(-1)
    tokens = jnp.repeat(jnp.arange(T, dtype=jnp.int32), TOP_K)
    A = T * TOP_K
    order = jnp.argsort(experts)
    se, stok, sw = experts[order], tokens[order], weights[order]
    counts = jnp.bincount(experts, length=N_EXPERTS)
    starts = jnp.cumsum(counts) - counts
    padded = (counts + MOE_BLOCK - 1) // MOE_BLOCK * MOE_BLOCK
    pad_ends = jnp.cumsum(padded)
    pad_starts = pad_ends - padded
    dest = pad_starts[se] + jnp.arange(A) - starts[se]
    n_blocks = -(-A // MOE_BLOCK) + N_EXPERTS
    P = n_blocks * MOE_BLOCK
    row_tok = jnp.zeros((P,), jnp.int32).at[dest].set(stok)
    row_w = jnp.zeros((P,), jnp.float32).at[dest].set(sw.astype(jnp.float32))
    block_e = jnp.minimum(jnp.searchsorted(pad_ends, jnp.arange(n_blocks) * MOE_BLOCK, side='right'),
                          N_EXPERTS - 1)
    xrows = xt[row_tok].reshape(n_blocks, MOE_BLOCK, D)

    def expert_block(args):
        xb, e = args
        hid = jax.nn.silu(xb @ w_gate_e[e]) * (xb @ w_up_e[e])
        return hid @ w_down_e[e]

    yrows = lax.map(expert_block, (xrows, block_e)).reshape(P, D)
    out = jax.ops.segment_sum(yrows * row_w[:, None].astype(yrows.dtype), row_tok, num_segments=T)
    return out.reshape(B, S, D)


def setup_inputs(seed: int = 0) -> dict:
    key = jax.random.key(seed)
    ks = jax.random.split(key, 32)
    nrm = lambda k, shape, s: jax.random.normal(k, shape, jnp.float32) * s
    gain = lambda k, shape: 1.0 + 0.02 * jax.random.normal(k, shape, jnp.float32)
    L, D = DEPTH, D_MODEL
    offsets = jax.random.randint(ks[2], (BATCH, 1), 0, 4096, dtype=jnp.int32)
    positions = jnp.arange(SEQ, dtype=jnp.int32)[None, :] + offsets
    return {
        "x": nrm(ks[0], (BATCH, SEQ, D), 1.0),
        "p": nrm(ks[1], (DEPTH, BATCH, SEQ, PLE_DIM), 1.0),
        "positions": positions,
        "ln_mix": gain(ks[3], (L, D)),
        "w_in": nrm(ks[4], (L, D, IN_WIDTH), D ** -0.5),
        "mu_shift": jax.random.uniform(ks[5], (L, SHIFT_COLS), jnp.float32),
        "w0": jax.random.uniform(ks[6], (L, RWKV_WIDTH), jnp.float32, -6.0, 1.0),
        "w_decay_up": nrm(ks[7], (L, DECAY_LORA, RWKV_WIDTH), 0.1),
        "a0": nrm(ks[8], (L, RWKV_WIDTH), 0.1),
        "w_aaa_up": nrm(ks[9], (L, AAA_LORA, RWKV_WIDTH), 0.1),
        "w_gate_up": nrm(ks[10], (L, GATE_LORA, RWKV_WIDTH), GATE_LORA ** -0.5),
        "k_k": 0.85 + 0.05 * jax.random.normal(ks[11], (L, RWKV_WIDTH), jnp.float32),
        "k_a": 1.0 + 0.05 * jax.random.normal(ks[12], (L, RWKV_WIDTH), jnp.float32),
        "r_k": nrm(ks[13], (L, RWKV_WIDTH), 0.1),
        "ln_x_w": gain(ks[14], (L, RWKV_WIDTH)),
        "ln_x_b": nrm(ks[15], (L, RWKV_WIDTH), 0.02),
        "sinks": nrm(ks[16], (L, ATT_HEADS), 1.0),
        "w_branch_att": nrm(ks[17], (L, ATT_WIDTH, D), ATT_WIDTH ** -0.5),
        "w_branch_rwkv": nrm(ks[18], (L, RWKV_WIDTH, D), RWKV_WIDTH ** -0.5),
        "w_out": nrm(ks[19], (L, D, D), D ** -0.5),
        "ln_moe": gain(ks[20], (L, D)),
        "w_group": nrm(ks[21], (L, D, N_GROUPS), D ** -0.5),
        "b_group": nrm(ks[22], (L, N_GROUPS), 0.01),
        "w_expert": nrm(ks[23], (L, D, N_EXPERTS), D ** -0.5),
        "b_expert": nrm(ks[24], (L, N_EXPERTS), 0.01),
        "w_gate_e": nrm(ks[25], (L, N_EXPERTS, D, D_EXPERT), D ** -0.5),
        "w_up_e": nrm(ks[26], (L, N_EXPERTS, D, D_EXPERT), D ** -0.5),
        "w_down_e": nrm(ks[27], (L, N_EXPERTS, D_EXPERT, D), D_EXPERT ** -0.5),
        "ln_ple": gain(ks[28], (L, D)),
        "w_ple_gate": nrm(ks[29], (L, D, D), D ** -0.5),
        "w_ple_proj": nrm(ks[30], (L, PLE_DIM, D), PLE_DIM ** -0.5),
        "ln_final": gain(ks[31], (D,)),
    }


def reference(x, p, positions, ln_mix, w_in, mu_shift, w0, w_decay_up, a0, w_aaa_up, w_gate_up,
              k_k, k_a, r_k, ln_x_w, ln_x_b, sinks, w_branch_att, w_branch_rwkv, w_out,
              ln_moe, w_group, b_group, w_expert, b_expert, w_gate_e, w_up_e, w_down_e,
              ln_ple, w_ple_gate, w_ple_proj, ln_final):
    B, S, D = x.shape
    for i in range(DEPTH):
        h = rmsnorm(x, ln_mix[i])
        z = h @ w_in[i]
        z_att = z[..., :ATT_COLS]
        z_rwkv = token_shift(z[..., ATT_COLS:ATT_COLS + SHIFT_COLS], mu_shift[i].astype(z.dtype))
        z_gate = z[..., ATT_COLS + SHIFT_COLS:]
        q = z_att[..., :ATT_WIDTH].reshape(B, S, ATT_HEADS, HEAD_DIM)
        k = z_att[..., ATT_WIDTH:ATT_WIDTH + KV_WIDTH].reshape(B, S, ATT_KV_HEADS, HEAD_DIM)
        v = z_att[..., ATT_WIDTH + KV_WIDTH:].reshape(B, S, ATT_KV_HEADS, HEAD_DIM)
        y_att = sliding_window_attention(apply_rope(q, positions), apply_rope(k, positions), v, sinks[i])
        c0 = RWKV_WIDTH
        r_b = z_rwkv[..., :c0]
        k_b = z_rwkv[..., c0:2 * c0]
        v_b = z_rwkv[..., 2 * c0:3 * c0]
        xw = z_rwkv[..., 3 * c0:3 * c0 + DECAY_LORA]
        xa = z_rwkv[..., 3 * c0 + DECAY_LORA:3 * c0 + DECAY_LORA + AAA_LORA]
        xg = z_rwkv[..., 3 * c0 + DECAY_LORA + AAA_LORA:]
        y_rwkv = rwkv7_time_mix(r_b, k_b, v_b, xw, xa, xg, w0[i], w_decay_up[i], a0[i], w_aaa_up[i],
                                w_gate_up[i], k_k[i], k_a[i], r_k[i], ln_x_w[i], ln_x_b[i]).astype(x.dtype)
        gates = jax.nn.sigmoid(z_gate.astype(jnp.float32)).astype(x.dtype)
        merged = gates[..., :D] * (y_att @ w_branch_att[i]) + gates[..., D:] * (y_rwkv @ w_branch_rwkv[i])
        x = x + merged @ w_out[i]
        x = x + hierarchical_moe(rmsnorm(x, ln_moe[i]), w_group[i], b_group[i], w_expert[i], b_expert[i],
                                 w_gate_e[i], w_up_e[i], w_down_e[i])
        ple_gate = jax.nn.sigmoid((rmsnorm(x, ln_ple[i]) @ w_ple_gate[i]).astype(jnp.float32)).astype(x.dtype)
        x = x + ple_gate * (p[i] @ w_ple_proj[i])
    return rmsnorm(x, ln_final)
```

```python
import numpy as np
import ml_dtypes
import concourse.bass as bass
import concourse.mybir as mybir
from concourse.bass_utils import run_bass_kernel_spmd

F32 = mybir.dt.float32
BF16 = mybir.dt.bfloat16
I32 = mybir.dt.int32
AF = mybir.ActivationFunctionType
ALU = mybir.AluOpType
AX = mybir.AxisListType

D = 1024
NE = 32
DE = 512
ENGS = ("pe", "act", "dve", "pool", "sp")
SYNC_SAME = ("act", "dve", "pool")


class _Rec:
    def __init__(self):
        self.call = None

    def __getattr__(self, name):
        def f(*a, **k):
            self.call = (name, a, k)
            return self
        return f


def _bind(fn):
    r = _Rec()
    fn(r)
    name, a, k = r.call
    return lambda e: getattr(e, name)(*a, **k)


import threading


class Weaver:
    def __init__(self):
        self.active = False

    def tick(self):
        if not self.active or threading.current_thread() is not self.cur_thread():
            return
        i = self.cur
        self.count[i] += 1
        if self.count[i] >= self.quota[i]:
            self.count[i] = 0
            self._handoff(i)

    def cur_thread(self):
        return self.threads[self.cur]

    def _next_live(self, i):
        n = len(self.threads)
        for d in range(1, n + 1):
            j = (i + d) % n
            if not self.done[j]:
                return j
        return None

    def _handoff(self, i):
        j = self._next_live(i)
        if j is None or j == i:
            return
        self.cur = j
        self.sems[j].release()
        self.sems[i].acquire()

    def run(self, fns, quota, seq=False):
        n = len(fns)
        if n == 1 or seq:
            for f in fns:
                f()
            return
        self.sems = [threading.Semaphore(0) for _ in range(n)]
        self.done = [False] * n
        self.count = [0] * n
        self.quota = list(quota)
        self.err = None
        fin = threading.Semaphore(0)

        def wrap(i):
            self.sems[i].acquire()
            try:
                fns[i]()
            except BaseException as ex:
                self.err = ex
            self.done[i] = True
            j = self._next_live(i)
            if j is None:
                fin.release()
            else:
                self.cur = j
                self.sems[j].release()

        self.threads = [threading.Thread(target=wrap, args=(i,)) for i in range(n)]
        for t in self.threads:
            t.start()
        self.active = True
        self.cur = 0
        self.sems[0].release()
        fin.acquire()
        self.active = False
        for t in self.threads:
            t.join()
        if self.err is not None:
            raise self.err


class Sched:
    def __init__(self):
        self.ops = {e: [] for e in ENGS}
        self.last_w = {}
        self.readers = {}
        self.waited = {e: {} for e in ENGS}
        self.dma_cnt = {}

    def _deps(self, eng, reads, writes):
        deps = []
        for r in reads:
            if r in self.last_w:
                deps.append((self.last_w[r], True))
        for w in writes:
            if w in self.last_w:
                deps.append((self.last_w[w], False))
            for t in self.readers.get(w, {}).values():
                deps.append((t, False))
        waits = []
        for d, raw in deps:
            if d[0] == "eng":
                if d[1] == eng and eng not in SYNC_SAME:
                    continue
                key = ("eng", d[1])
            else:
                key = ("dma", d[1])
            val = d[2]
            if self.waited[eng].get(key, -1) >= val:
                continue
            self.waited[eng][key] = val
            waits.append((key, val))
            if d[0] == "eng":
                self.ops[d[1]][val]["inc"] = True
        return waits

    def op(self, eng, fn, r=(), w=()):
        waits = self._deps(eng, r, w)
        idx = len(self.ops[eng])
        self.ops[eng].append(dict(fn=_bind(fn), waits=waits, inc=False, dma=None))
        tok = ("eng", eng, idx)
        for x in w:
            self.last_w[x] = tok
            self.readers[x] = {}
        for x in r:
            self.readers.setdefault(x, {})[("eng", eng)] = tok

    def dma(self, eng, fn, r=(), w=(), key=None):
        waits = self._deps(eng, r, w)
        prev = self.dma_cnt.get(key, 0)
        if prev and self.waited[eng].get(("dma", key), -1) < prev:
            self.waited[eng][("dma", key)] = prev
            waits.append((("dma", key), prev))
        cnt = prev + 16
        self.dma_cnt[key] = cnt
        self.ops[eng].append(dict(fn=_bind(fn), waits=waits, inc=False, dma=key))
        tok = ("dma", key, cnt)
        for x in w:
            self.last_w[x] = tok
            self.readers[x] = {}
        for x in r:
            self.readers.setdefault(x, {})[("dma", key)] = tok

    def barrier(self, nop_fns):
        self.nbar = getattr(self, "nbar", 0) + 1
        for e in ENGS:
            waits = []
            if e != "sp" and self.ops[e]:
                last = len(self.ops[e]) - 1
                while last >= 0 and (self.ops[e][last].get("bar") or self.ops[e][last]["dma"] is not None):
                    last -= 1
                if last >= 0:
                    self.ops[e][last]["inc"] = True
                    waits.append((("eng", e), last))
            if e == "sp":
                for k, c in self.dma_cnt.items():
                    if self.waited[e].get(("dma", k), -1) < c:
                        self.waited[e][("dma", k)] = c
                        waits.append((("dma", k), c))
            self.ops[e].append(dict(fn=nop_fns[e], waits=waits, inc=False, dma=None, bar=self.nbar))
        self.last_w = {}
        self.readers = {}

    def finish(self, nc, engines, sems, dma_sems):
        pref = {}
        for e in ENGS:
            c = 0
            arr = []
            for o in self.ops[e]:
                if o["inc"] and o["dma"] is None and not o.get("bar"):
                    c += 1
                arr.append(c)
            pref[e] = arr
        return pref


def build(cfg, debug=False):
    NSEQ, S, CAP = cfg["NSEQ"], cfg["S"], cfg["CAP"]
    TPS = S // 128
    NTILE = NSEQ * TPS
    TPC = NTILE * 128
    NSLOT = NE * CAP
    NST = NSLOT // 128
    CT = CAP // 128
    PH = cfg.get("phases", "1a,1b,2,3").split(",")
    CUT = cfg.get("cut", 99)

    nc = bass.Bass("TRN2", target_bir_lowering=False)
    dr = {}

    def din(name, shape, dt=F32):
        dr[name] = nc.dram_tensor(name, list(shape), dt, kind="ExternalInput").ap()
        return dr[name]

    def dscr(name, shape, dt=F32, out=False):
        dr[name] = nc.dram_tensor(name, list(shape), dt, kind=("ExternalOutput" if out else "Internal")).ap()
        return dr[name]

    x_d = din("x", [TPC, D])
    p_d = din("p", [TPC, 256])
    pos_d = din("posT", [128, NTILE], I32)
    win_d = din("w_in", [D, 4608])
    vec_d = din("vecs", [128, 70])
    cst_d = din("cst", [128, 832])
    wlora_d = din("wlora", [128, 512])
    wgu_d = din("wgu", [128, 512])
    wba_d = din("w_ba", [512, D])
    wbb_d = din("w_bb", [512, D])
    wout_d = din("w_out", [D, D])
    wr_d = din("w_r", [D, 36])
    br_d = din("b_r", [1, 36])
    wg_d = din("w_gate_e", [NE, D, DE])
    wu_d = din("w_up_e", [NE, D, DE])
    wd_d = din("w_down_e", [NE, DE, D])
    wpg_d = din("w_pg", [D, D])
    wpp_d = din("w_pp", [256, D])
    lnf_d = din("ln_final", [1, D])
    lnmoe_d = din("ln_moe_row", [1, D])
    out_d = dscr("out", [TPC, D], F32, out=True)
    yab_d = dscr("yab", [NTILE, 128, 8 * 128], BF16, out=debug)
    x1_d = dscr("x1s", [TPC, D], F32, out=debug)
    hs_d = dscr("hslots", [NSLOT, D], BF16, out=debug)
    ys_d = dscr("yslots", [NSLOT, D], F32, out=debug)
    if debug:
        rt_d = dscr("route", [128, NTILE * 4], F32, out=True)

    S_ = Sched()
    base0 = 229376 - int(nc.sbuf_bytes_remaining)
    base0 = (base0 + 63) // 64 * 64
    st = {"p": base0, "ph": None}

    def salloc(name, shape, dt):
        nb = int(np.prod(shape[1:])) * (4 if dt in (F32, I32) else 2)
        nb = (nb + 31) // 32 * 32
        t = nc.alloc_sbuf_tensor_at(name, list(shape), dt, offset=st["p"])
        st["p"] += nb
        assert st["p"] <= 229376 - 64, ("SBUF overflow", name, st["p"])
        return t

    psb = [nc.alloc_psum_tensor(f"psb{i}", [128, 1024], BF16) for i in range(2)]
    psf = [nc.alloc_psum_tensor(f"psf{i}", [128, 512], F32) for i in range(6)]
    rr = {"f": 0, "b": 0, "fA": 0, "fB": 0}
    tl = threading.local()

    def PS():
        pool = getattr(tl, "pool", None)
        if pool == "A":
            i = rr["fA"] % 2
            rr["fA"] += 1
        elif pool == "B":
            i = 2 + rr["fB"] % 4
            rr["fB"] += 1
        elif pool == "E":
            i = rr["fA"] % 3
            rr["fA"] += 1
        elif pool == "O":
            i = 3 + rr["fB"] % 3
            rr["fB"] += 1
        else:
            i = rr["f"] % 6
            rr["f"] += 1
        return psf[i], f"psf{i}"

    def PSB():
        pool = getattr(tl, "pool", None)
        if pool in ("A", "E"):
            i = 0
        elif pool in ("B", "O"):
            i = 1
        else:
            i = rr["b"] % 2
            rr["b"] += 1
        return psb[i], f"psb{i}"

    vec = salloc("vec", [128, 70], F32)
    cst = salloc("cstf", [128, 832], F32)
    identb = salloc("identb", [128, 128], BF16)
    bones = salloc("bones", [128, 128], BF16)
    onesb = salloc("onesb", [128, 128], BF16)
    onesf = salloc("onesf", [128, 128], F32)
    derived = salloc("derived", [128, 32], F32)
    posi = salloc("posi", [128, NTILE], I32)
    posf = salloc("posf", [128, NTILE], F32)
    slot_i = salloc("slot_i", [128, NTILE * 2], I32)
    rw_f = salloc("rw_f", [128, NTILE * 2], F32)
    scr = salloc("scr", [128, 64], F32)
    ph_base = st["p"]
    IDENTF = cst[:, 0:128]
    USTR = cst[:, 128:256]
    UINC = cst[:, 256:384]
    LSTR = cst[:, 384:512]
    INVF = cst[:, 512:576]
    OFFS = cst[:, 576:640]
    MASK2 = cst[:, 128:384]
    V_LNMIX, V_LNMOE, V_LNPLE, V_MU = 0, 8, 16, 24
    V_W0, V_A0, V_KK, V_KA, V_RK, V_LNW, V_LNB, V_SINK = 38, 42, 46, 50, 54, 58, 62, 66

    def bc(ap, shape):
        return ap.to_broadcast(list(shape))

    def vcol(c, n=1):
        return vec[:, c:c + n]

    WV = Weaver()
    GLOBAL_KEYS = {"vec", "cst", "identb", "bones", "onesb", "onesf", "derived", "posf", "posi",
                   "Wgt", "WbA", "WbB", "Wout", "Wr", "BR", "GMOE", "ustrb", "CNT", "hs_all", "ys_all",
                   "Wpg", "Wpp", "LNF", "WG0", "WG1", "WU0", "WU1", "WD0", "WD1"}
    GLOBAL_PREF = ("psf", "psb", "yab_d", "x1_d", "slot", "rw", "hs_sc", "out_d")

    def stream(sfx):
        def m(keys):
            return [k if (k in GLOBAL_KEYS or k.startswith(GLOBAL_PREF)) else k + sfx for k in keys]

        def op_(eng, fn, r=(), w=()):
            op(eng, fn, m(r), m(w))

        def dma_(eng, fn, r=(), w=(), key=None):
            dma(eng, fn, m(r), m(w), key)
        return op_, dma_

    def op(eng, fn, r=(), w=()):
        S_.op(eng, fn, r, w)
        WV.tick()
    op_glob = op

    def dma(eng, fn, r=(), w=(), key=None):
        S_.dma(eng, fn, r, w, key)
        WV.tick()

    dma("sp", lambda e: e.dma_start(out=vec[:], in_=vec_d), w=["vec"], key="vec")
    dma("sp", lambda e: e.dma_start(out=cst[:], in_=cst_d), w=["cst"], key="cst")
    dma("sp", lambda e: e.dma_start(out=posi[:], in_=pos_d), w=["posi"], key="posi")
    op("dve", lambda e: e.tensor_copy(out=identb[:], in_=IDENTF), r=["cst"], w=["identb"])
    op("dve", lambda e: e.tensor_copy(out=bones[:], in_=cst[:, 640:768]), r=["cst"], w=["bones"])
    op("dve", lambda e: e.memset(onesb[:], 1.0), w=["onesb"])
    op("dve", lambda e: e.memset(onesf[:], 1.0), w=["onesf"])
    op("dve", lambda e: e.tensor_copy(out=posf[:], in_=posi[:]), r=["posi"], w=["posf"])
    op("dve", lambda e: e.tensor_scalar(out=derived[:, 0:14], in0=vcol(V_MU, 14), scalar1=-1.0, scalar2=1.0,
                                        op0=ALU.mult, op1=ALU.add), r=["vec"], w=["derived"])
    op("dve", lambda e: e.tensor_scalar(out=derived[:, 14:18], in0=vcol(V_KA, 4), scalar1=-1.0, scalar2=1.0,
                                        op0=ALU.mult, op1=ALU.add), r=["vec"], w=["derived"])
    op("act", lambda e: e.activation(out=derived[:, 18:22], in_=vcol(V_SINK, 4), func=AF.Exp), r=["vec", "derived"],
       w=["derived"])
    OMU = lambda c, n=1: derived[:, c:c + n]
    OMKA = lambda c, n=1: derived[:, 14 + c:14 + c + n]
    ESINK = derived[:, 18:22]

    nop_fns = {
        "pe": lambda e: e.nop(), "act": lambda e: e.nop(), "dve": lambda e: e.nop(),
        "pool": lambda e: e.nop(), "sp": lambda e: e.nop(),
    }

    def phase_reset():
        S_.barrier(nop_fns)
        st["p"] = ph_base

    def load_cast_weight(dst, dst_key, src_ap, rows, cols, gcol, stage, stage_key, kchunks, eng_cycle):
        for kc in range(kchunks):
            sl = stage[kc % len(stage)]
            sk = stage_key[kc % len(stage)]
            dma("sp", lambda e, sl=sl, kc=kc: e.dma_start(out=sl[:, 0:cols], in_=src_ap[kc * 128:(kc + 1) * 128, :]),
                w=[sk], key=sk)
            eng = eng_cycle[kc % len(eng_cycle)]
            if gcol is None:
                op(eng, lambda e, sl=sl, kc=kc: e.tensor_copy(out=dst[:, kc, :], in_=sl[:, 0:cols]),
                   r=[sk], w=[dst_key])
            else:
                op(eng, lambda e, sl=sl, kc=kc: e.tensor_scalar(out=dst[:, kc, :], in0=sl[:, 0:cols],
                                                                 scalar1=vcol(gcol + kc), scalar2=None, op0=ALU.mult),
                   r=[sk, "vec"], w=[dst_key])

    def rms_to_hT(ti, xin, xin_key, hT, hT_key, tmp, normalize=True, op=None):
        op = op or op_glob
        junk, ss, rstd, xbf = tmp["junk"], tmp["ss"], tmp["rstd"], tmp["xbf"]
        op("act", lambda e: e.activation(out=junk[:], in_=xin[:], func=AF.Square, accum_out=ss[:, 0:1]),
           r=[xin_key], w=["junk", "ss"])
        op("dve", lambda e: e.tensor_scalar(out=ss[:, 1:2], in0=ss[:, 0:1], scalar1=1.0 / D, scalar2=1e-6,
                                            op0=ALU.mult, op1=ALU.add), r=["ss"], w=["ss1"])
        op("act", lambda e: e.activation(out=ss[:, 2:3], in_=ss[:, 1:2], func=AF.Sqrt), r=["ss1"], w=["ss2"])
        op("dve", lambda e: e.reciprocal(out=rstd[:, 0:1], in_=ss[:, 2:3]), r=["ss2"], w=["rstd"])
        if normalize:
            op("dve", lambda e: e.tensor_scalar(out=xbf[:], in0=xin[:], scalar1=rstd[:, 0:1], scalar2=None,
                                                op0=ALU.mult), r=[xin_key, "rstd"], w=["xbf"])
        else:
            op("pool", lambda e: e.tensor_copy(out=xbf[:], in_=xin[:]), r=[xin_key], w=["xbf"])
        pb, pk = PSB()
        for kc in range(8):
            op("pe", lambda e, kc=kc: e.transpose(pb[:, kc * 128:(kc + 1) * 128], xbf[:, kc * 128:(kc + 1) * 128],
                                                  identb[:]), r=["xbf", "identb"], w=[pk])
        op("act", lambda e: e.activation(out=hT[:].rearrange("p k t -> p (k t)"), in_=pb[:, :], func=AF.Copy),
           r=[pk], w=[hT_key])

    if "1a" in PH:
        Wqkv = salloc("Wqkv", [128, 8, 768], BF16)
        Wrw = salloc("Wrw", [128, 8, 1792], BF16)
        Wlora = salloc("Wlora", [128, 512], BF16)
        Wgu = salloc("Wgu", [128, 512], BF16)
        stg = [salloc("stgA", [128, 2560], F32)]
        for kc in range(8):
            dma("sp", lambda e, kc=kc: e.dma_start(out=stg[0][:, 0:2560], in_=win_d[kc * 128:(kc + 1) * 128, 0:2560]),
                w=["stgA"], key="stgA")
            op("dve", lambda e, kc=kc: e.tensor_scalar(out=Wqkv[:, kc, :], in0=stg[0][:, 0:768],
                                                       scalar1=vcol(V_LNMIX + kc), scalar2=None, op0=ALU.mult),
               r=["stgA", "vec"], w=["Wqkv"])
            op("pool", lambda e, kc=kc: e.tensor_scalar(out=Wrw[:, kc, :], in0=stg[0][:, 768:2560],
                                                        scalar1=vcol(V_LNMIX + kc), scalar2=1.0, op0=ALU.mult,
                                                        op1=ALU.mult),
               r=["stgA", "vec"], w=["Wrw"])
        dma("sp", lambda e: e.dma_start(out=stg[0][:, 0:512], in_=wlora_d), w=["stgA"], key="stgA")
        op("dve", lambda e: e.tensor_copy(out=Wlora[:], in_=stg[0][:, 0:512]), r=["stgA"], w=["Wlora"])
        dma("sp", lambda e: e.dma_start(out=stg[0][:, 512:1024], in_=wgu_d), w=["stgA"], key="stgA")
        op("dve", lambda e: e.tensor_copy(out=Wgu[:], in_=stg[0][:, 512:1024]), r=["stgA"], w=["Wgu"])

        xin = [salloc(f"xin{i}", [128, D], F32) for i in range(2)]
        tmp = dict(junk=salloc("junk", [128, D], BF16), ss=salloc("ss", [128, 4], F32),
                   rstd=salloc("rstd", [128, 1], F32), xbf=salloc("xbf", [128, D], BF16))
        hT = [salloc(f"hT{i}", [128, 8, 128], BF16) for i in range(2)]
        ropeT = salloc("ropeT", [128, 64], F32)
        ropeN = salloc("ropeN", [128, 64], I32)
        ropeF = salloc("ropeF", [128, 64], F32)
        ropeG = salloc("ropeG", [128, 64], F32)
        CS = salloc("CS", [128, 64], F32)
        ropA = salloc("ropA", [128, 640], F32)
        ropB = salloc("ropB", [128, 640], F32)
        qkr = salloc("qkr", [128, 640], BF16)
        qT = salloc("qT", [128, 4, 128], BF16)
        kTs = [salloc(f"kT{i}", [128, 128], BF16) for i in range(2)]
        vts = [salloc(f"vtok{i}", [128, 128], BF16) for i in range(2)]
        Eb = [salloc(f"Eb{i}", [128, 512], BF16) for i in range(4)]
        dent = salloc("dent", [128, 4, 128], F32)
        yab = [salloc(f"yabs{i}", [128, 8, 128], BF16) for i in range(2)]
        zb = [salloc(f"zb{i}", [128, 4, 129], F32) for i in range(2)]
        zt1 = salloc("zt1", [128, 4, 128], F32)
        zt2 = salloc("zt2", [128, 4, 128], F32)
        carry = salloc("carry", [128, 16], F32)
        Rr = salloc("Rr", [128, 4, 128], F32)
        Kr = salloc("Kr", [128, 4, 128], F32)
        Vr = salloc("Vr", [128, 4, 128], F32)
        XM = salloc("XM", [128, 2, 128], F32)
        LIN = salloc("LIN", [128, 128], BF16)
        SXG = salloc("SXG", [128, 128], BF16)
        SG = salloc("SG", [128, 4, 128], F32)
        Aa = salloc("Aa", [128, 4, 128], F32)
        Gg = salloc("Gg", [128, 4, 128], F32)
        LW = salloc("LW", [128, 4, 128], F32)
        CUM = salloc("CUM", [128, 4, 128], F32)
        CX = salloc("CX", [128, 4, 128], F32)
        E1 = salloc("E1", [128, 4, 128], F32)
        E2 = salloc("E2", [128, 4, 128], F32)
        E3 = salloc("E3", [128, 4, 128], F32)
        E4 = salloc("E4", [128, 4, 128], F32)
        KKR = salloc("KKR", [128, 4, 128], F32)
        SQb = salloc("SQb", [128, 4, 128], BF16)
        RN = salloc("RN", [128, 4, 128], F32)
        KK = salloc("KK", [128, 4, 128], F32)
        T1 = salloc("T1", [128, 4, 128], F32)
        KP = salloc("KP", [128, 4, 128], F32)
        Bb = salloc("Bb", [128, 4, 128], F32)
        AR = salloc("AR", [128, 4, 2, 128], BF16)
        BK = salloc("BK", [128, 4, 2, 128], BF16)
        BKS = salloc("BKS", [128, 4, 2, 128], BF16)
        ARm = [salloc(f"ARm{i}", [128, 4, 2, 128], BF16) for i in range(2)]
        BKm = [salloc(f"BKm{i}", [128, 4, 2, 128], BF16) for i in range(2)]
        RK = salloc("RK", [128, 4, 128], F32)
        RK2 = salloc("RK2", [128, 4, 128], BF16)
        BV = salloc("BV", [128, 4, 128], F32)
        VB = salloc("VB", [128, 4, 128], BF16)
        BKT = salloc("BKT", [128, 1024], BF16)
        VT = salloc("VT", [128, 512], BF16)
        MA = salloc("MA", [128, 8, 256], BF16)
        MB = salloc("MB", [128, 8, 256], BF16)
        Qm = [salloc(f"Qm{i}", [128, 8, 128], BF16) for i in range(2)]
        PX = [salloc(f"PX{i}", [128, 8, 256], BF16) for i in range(2)]
        XF = salloc("XF", [128, 8, 128], BF16)
        SF = salloc("SF", [128, 4, 64], F32)
        SBs = salloc("SBs", [128, 4, 64], BF16)
        TMPS = salloc("TMPS", [128, 4, 64], F32)
        RH = salloc("RH", [128, 512], BF16)
        UT = salloc("UT", [128, 512], BF16)
        Yf = salloc("Yf", [128, 4, 128], F32)
        YB = salloc("YB", [128, 4, 128], BF16)
        YSQ = salloc("YSQ", [128, 4, 128], BF16)
        MEAN = salloc("MEAN", [128, 4, 128], F32)
        M2 = salloc("M2", [128, 4, 128], F32)
        VAR = salloc("VAR", [128, 4, 128], F32)
        Dd = salloc("Dd", [128, 4, 128], F32)

        def f2(t):
            return t[:].rearrange("p a b -> p (a b)")

        def genA(ti):
            tl.pool = "A"
            tj = ti % TPS
            sl = ti % 2
            xk = f"xin{sl}"
            dma("sp", lambda e, ti=ti, sl=sl: e.dma_start(out=xin[sl][:], in_=x_d[ti * 128:(ti + 1) * 128, :]),
                w=[xk], key=xk)
            hk = f"hT{sl}"
            rms_to_hT(ti, xin[sl], xk, hT[sl], hk, tmp)
            h = hT[sl]
            if CUT <= 1:
                return
            pq, pqk = PS()
            pkv, pkvk = PS()
            for kc in range(8):
                op("pe", lambda e, kc=kc: e.matmul(pq[:, 0:512], h[:, kc, :], Wqkv[:, kc, 0:512],
                                                   start=(kc == 0), stop=(kc == 7)), r=[hk, "Wqkv"], w=[pqk])
            for kc in range(8):
                op("pe", lambda e, kc=kc: e.matmul(pkv[:, 0:256], h[:, kc, :], Wqkv[:, kc, 512:768],
                                                   start=(kc == 0), stop=(kc == 7)), r=[hk, "Wqkv"], w=[pkvk])
            op("dve", lambda e, ti=ti: e.scalar_tensor_tensor(out=ropeT[:], in0=INVF, scalar=posf[:, ti:ti + 1],
                                                              in1=OFFS, op0=ALU.mult, op1=ALU.add),
               r=["cst", "posf"], w=["ropeT"])
            op("dve", lambda e: e.tensor_copy(out=ropeN[:], in_=ropeT[:]), r=["ropeT"], w=["ropeN"])
            op("dve", lambda e: e.tensor_copy(out=ropeF[:], in_=ropeN[:]), r=["ropeN"], w=["ropeF"])
            op("dve", lambda e: e.tensor_tensor(out=ropeF[:], in0=ropeT[:], in1=ropeF[:], op=ALU.subtract),
               r=["ropeT", "ropeF"], w=["ropeF"])
            op("dve", lambda e: e.tensor_single_scalar(out=ropeG[:], in_=ropeF[:], scalar=0.5, op=ALU.is_gt),
               r=["ropeF"], w=["ropeG"])
            op("dve", lambda e: e.tensor_tensor(out=ropeF[:], in0=ropeF[:], in1=ropeG[:], op=ALU.subtract),
               r=["ropeF", "ropeG"], w=["ropeF"])
            op("act", lambda e: e.activation(out=CS[:], in_=ropeF[:], func=AF.Sin, scale=2.0 * np.pi),
               r=["ropeF"], w=["CS"])
            if CUT <= 2:
                return
            for (src, skey, c0, H) in ((pq, pqk, 0, 8), (pkv, pkvk, 512, 2)):
                W_ = H * 64
                s4 = src[:, 0:W_].rearrange("p (h t d) -> p h t d", h=H, t=2)
                A4 = ropA[:, c0:c0 + W_].rearrange("p (h t d) -> p h t d", h=H, t=2)
                B4 = ropB[:, c0:c0 + W_].rearrange("p (h t d) -> p h t d", h=H, t=2)
                O4 = qkr[:, c0:c0 + W_].rearrange("p (h t d) -> p h t d", h=H, t=2)
                cosb = CS[:, 32:64].unsqueeze(1).unsqueeze(1).to_broadcast([128, H, 2, 32])
                sinb = CS[:, 0:32].unsqueeze(1).to_broadcast([128, H, 32])
                op("dve", lambda e, s4=s4, A4=A4, cosb=cosb: e.tensor_tensor(out=A4, in0=s4, in1=cosb, op=ALU.mult),
                   r=[skey, "CS"], w=["ropA"])
                op("dve", lambda e, s4=s4, B4=B4, sinb=sinb: e.tensor_tensor(out=B4[:, :, 0, :], in0=s4[:, :, 1, :],
                                                                           in1=sinb, op=ALU.mult),
                   r=[skey, "CS"], w=["ropB"])
                op("dve", lambda e, s4=s4, B4=B4, sinb=sinb: e.tensor_tensor(out=B4[:, :, 1, :], in0=s4[:, :, 0, :],
                                                                           in1=sinb, op=ALU.mult),
                   r=[skey, "CS"], w=["ropB"])
                op("pool", lambda e, A4=A4, B4=B4, O4=O4: e.tensor_tensor(out=O4[:, :, 0, :], in0=A4[:, :, 0, :],
                                                                         in1=B4[:, :, 0, :], op=ALU.subtract),
                   r=["ropA", "ropB"], w=["qkr"])
                op("pool", lambda e, A4=A4, B4=B4, O4=O4: e.tensor_tensor(out=O4[:, :, 1, :], in0=A4[:, :, 1, :],
                                                                         in1=B4[:, :, 1, :], op=ALU.add),
                   r=["ropA", "ropB"], w=["qkr"])
            vk = f"vtok{sl}"
            kk_ = f"kT{sl}"
            op("act", lambda e, sl=sl: e.activation(out=vts[sl][:], in_=pkv[:, 128:256], func=AF.Copy),
               r=[pkvk], w=[vk])
            pb, pbk = PSB()
            for c in range(5):
                op("pe", lambda e, c=c: e.transpose(pb[:, c * 128:(c + 1) * 128], qkr[:, c * 128:(c + 1) * 128],
                                                    identb[:]), r=["qkr", "identb"], w=[pbk])
            op("act", lambda e: e.activation(out=f2(qT), in_=pb[:, 0:512], func=AF.Copy), r=[pbk], w=["qT"])
            op("act", lambda e, sl=sl: e.activation(out=kTs[sl][:], in_=pb[:, 512:640], func=AF.Copy),
               r=[pbk], w=[kk_])
            if CUT <= 3:
                return
            kbs = ([1 - sl] if tj > 0 else []) + [sl]
            ei = 0
            Euse = {}
            for g in range(2):
                for kb in kbs:
                    pe_, pek = PS()
                    op("pe", lambda e, g=g, kb=kb, pe_=pe_: e.matmul(
                        pe_[:, 0:512], kTs[kb][g * 64:(g + 1) * 64, :], qT[g * 64:(g + 1) * 64, :, :],
                        start=True, stop=True), r=[f"kT{kb}", "qT"], w=[pek])
                    Et = Eb[ei]
                    ek = f"Eb{ei}"
                    ei += 1
                    op("act", lambda e, Et=Et, pe_=pe_: e.activation(out=Et[:], in_=pe_[:, 0:512], func=AF.Exp,
                                                                    scale=0.125), r=[pek], w=[ek])
                    msk = UINC if kb == sl else LSTR
                    op("pool", lambda e, Et=Et, msk=msk: e.tensor_tensor(
                        out=Et[:].rearrange("p (c q) -> p c q", c=4), in0=Et[:].rearrange("p (c q) -> p c q", c=4),
                        in1=msk.unsqueeze(1).to_broadcast([128, 4, 128]), op=ALU.mult), r=[ek, "cst"], w=[ek])
                    Euse[(g, kb)] = (Et, ek)
            po, pok = PS()
            pd, pdk = PS()
            for g in range(2):
                for i, kb in enumerate(kbs):
                    Et, ek = Euse[(g, kb)]
                    op("pe", lambda e, g=g, kb=kb, Et=Et, i=i: e.matmul(
                        po[g * 64:(g + 1) * 64, 0:512], vts[kb][:, g * 64:(g + 1) * 64], Et[:],
                        start=(i == 0), stop=(i == len(kbs) - 1)), r=[f"vtok{kb}", ek], w=[pok])
                for i, kb in enumerate(kbs):
                    Et, ek = Euse[(g, kb)]
                    op("pe", lambda e, g=g, Et=Et, i=i: e.matmul(
                        pd[g * 64:(g + 1) * 64, 0:512], onesb[:, 0:64], Et[:],
                        start=(i == 0), stop=(i == len(kbs) - 1)), r=["onesb", ek], w=[pdk])
            ys = yab[sl]
            yk = f"yabs{sl}"
            op("dve", lambda e: e.tensor_tensor(out=dent[:], in0=pd[:, 0:512].rearrange("p (c q) -> p c q", c=4),
                                                in1=ESINK.unsqueeze(2).to_broadcast([128, 4, 128]), op=ALU.add),
               r=[pdk, "derived"], w=["dent"])
            op("dve", lambda e: e.reciprocal(out=dent[:], in_=dent[:]), r=["dent"], w=["dent"])
            op("dve", lambda e, ys=ys: e.tensor_tensor(out=ys[:, 0:4, :],
                                                       in0=po[:, 0:512].rearrange("p (c q) -> p c q", c=4),
                                                       in1=dent[:], op=ALU.mult), r=[pok, "dent"], w=[yk + "a"])


        def genBC(ti):
            tl.pool = "B"
            tj = ti % TPS
            sl = ti % 2
            hk = f"hT{sl}"
            h = hT[sl]
            ys = yab[sl]
            yk = f"yabs{sl}"
            if CUT <= 4:
                return
            if tj == 0:
                op("pool", lambda e: e.memset(carry[:], 0.0), w=["carry"])
                op("pool", lambda e: e.memset(SF[:], 0.0), w=["SF"])
                op("pool", lambda e: e.memset(SBs[:], 0.0), w=["SBs"])
            groups = [(0, 4, Rr, "Rr"), (4, 4, Kr, "Kr"), (8, 4, Vr, "Vr"), (12, 2, XM, "XM")]
            for gi, (z0, n, dst, dk) in enumerate(groups):
                pz, pzk = PS()
                for j in range(n):
                    zc = z0 + j
                    for kc in range(8):
                        op("pe", lambda e, j=j, zc=zc, kc=kc, pz=pz: e.matmul(
                            pz[:, j * 128:(j + 1) * 128], Wrw[:, kc, zc * 128:(zc + 1) * 128], h[:, kc, :],
                            start=(kc == 0), stop=(kc == 7)), r=[hk, "Wrw"], w=[pzk])
                zbt = zb[gi % 2]
                zk = f"zb{gi % 2}"
                op("act", lambda e, zbt=zbt, pz=pz, n=n: e.activation(
                    out=zbt[:, 0:n, 1:129], in_=pz[:, 0:n * 128].rearrange("p (c t) -> p c t", c=n), func=AF.Copy),
                   r=[pzk], w=[zk])
                op("pool", lambda e, zbt=zbt, z0=z0, n=n: e.tensor_copy(out=zbt[:, 0:n, 0], in_=carry[:, z0:z0 + n]),
                   r=["carry"], w=[zk])
                op("dve", lambda e, zbt=zbt, z0=z0, n=n: e.tensor_tensor(
                    out=zt1[:, 0:n, :], in0=zbt[:, 0:n, 0:128],
                    in1=vcol(V_MU + z0, n).unsqueeze(2).to_broadcast([128, n, 128]), op=ALU.mult),
                   r=[zk, "vec"], w=["zt1"])
                op("pool", lambda e, zbt=zbt, z0=z0, n=n: e.tensor_tensor(
                    out=zt2[:, 0:n, :], in0=zbt[:, 0:n, 1:129],
                    in1=OMU(z0, n).unsqueeze(2).to_broadcast([128, n, 128]), op=ALU.mult),
                   r=[zk, "derived"], w=["zt2"])
                op("dve", lambda e, dst=dst, n=n: e.tensor_tensor(out=dst[:, 0:n, :], in0=zt1[:, 0:n, :],
                                                                  in1=zt2[:, 0:n, :], op=ALU.add),
                   r=["zt1", "zt2"], w=[dk])
                op("pool", lambda e, zbt=zbt, z0=z0, n=n: e.tensor_copy(out=carry[:, z0:z0 + n], in_=zbt[:, 0:n, 128]),
                   r=[zk], w=["carry"])
            if CUT <= 5:
                return
            op("act", lambda e: e.activation(out=LIN[0:64, :], in_=XM[0:64, 0, :], func=AF.Tanh), r=["XM"], w=["LINa"])
            op("pool", lambda e: e.tensor_copy(out=LIN[64:128, :], in_=XM[64:128, 0, :]), r=["XM"], w=["LINb"])
            op("act", lambda e: e.activation(out=SXG[:], in_=XM[:, 1, :], func=AF.Sigmoid), r=["XM"], w=["SXG"])
            pu, puk = PS()
            pa, pak = PS()
            pg, pgk = PS()
            for cc in range(4):
                op("pe", lambda e, cc=cc: e.matmul(pu[:, cc * 128:(cc + 1) * 128], Wlora[0:64, cc * 128:(cc + 1) * 128],
                                                   LIN[0:64, :], start=True, stop=True),
                   r=["Wlora", "LINa"], w=[puk])
            for cc in range(4):
                op("pe", lambda e, cc=cc: e.matmul(pa[:, cc * 128:(cc + 1) * 128],
                                                   Wlora[64:128, cc * 128:(cc + 1) * 128],
                                                   LIN[64:128, :], start=True, stop=True),
                   r=["Wlora", "LINb"], w=[pak])
            for cc in range(4):
                op("pe", lambda e, cc=cc: e.matmul(pg[:, cc * 128:(cc + 1) * 128], Wgu[:, cc * 128:(cc + 1) * 128],
                                                   SXG[:], start=True, stop=True), r=["Wgu", "SXG"], w=[pgk])
            for cc in range(4):
                op("act", lambda e, cc=cc: e.activation(out=SG[:, cc, :], in_=pu[:, cc * 128:(cc + 1) * 128],
                                                        func=AF.Sigmoid, bias=vcol(V_W0 + cc)),
                   r=[puk, "vec"], w=["SG"])
            for cc in range(4):
                op("act", lambda e, cc=cc: e.activation(out=Aa[:, cc, :], in_=pa[:, cc * 128:(cc + 1) * 128],
                                                        func=AF.Sigmoid, bias=vcol(V_A0 + cc)),
                   r=[pak, "vec"], w=["Aa"])
            op("act", lambda e: e.activation(out=f2(Gg), in_=pg[:, 0:512], func=AF.Copy), r=[pgk], w=["Gg"])
            op("act", lambda e: e.activation(out=f2(LW), in_=f2(SG), func=AF.Copy, scale=-0.6065306597126334),
               r=["SG"], w=["LW"])
            for cc in range(4):
                op("dve", lambda e, cc=cc: e.tensor_tensor_scan(out=CUM[:, cc, :], data0=onesf[:], data1=LW[:, cc, :],
                                                                initial=0.0, op0=ALU.mult, op1=ALU.add),
                   r=["onesf", "LW"], w=["CUM"])
            op("pool", lambda e: e.tensor_tensor(out=f2(CX), in0=f2(CUM), in1=f2(LW), op=ALU.subtract),
               r=["CUM", "LW"], w=["CX"])
            op("act", lambda e: e.activation(out=f2(E1), in_=f2(CUM), func=AF.Exp), r=["CUM"], w=["E1"])
            op("act", lambda e: e.activation(out=f2(E2), in_=f2(CUM), func=AF.Exp, scale=-1.0), r=["CUM"], w=["E2"])
            op("act", lambda e: e.activation(out=f2(E3), in_=f2(CX), func=AF.Exp), r=["CX"], w=["E3"])
            for cc in range(4):
                op("act", lambda e, cc=cc: e.activation(out=E4[:, cc, :], in_=CUM[:, cc, :], func=AF.Exp, scale=-1.0,
                                                        bias=CUM[:, cc, 127:128]), r=["CUM"], w=["E4"])
            op("pool", lambda e: e.tensor_tensor(out=KKR[:], in0=Kr[:],
                                                 in1=vcol(V_KK, 4).unsqueeze(2).to_broadcast([128, 4, 128]),
                                                 op=ALU.mult), r=["Kr", "vec"], w=["KKR"])
            op("act", lambda e: e.activation(out=f2(SQb), in_=f2(KKR), func=AF.Square), r=["KKR"], w=["SQb"])
            pss, pssk = PS()
            op("pe", lambda e: e.matmul(pss[:, 0:512], bones[:], f2(SQb), start=True, stop=True),
               r=["bones", "SQb"], w=[pssk])
            op("act", lambda e: e.activation(out=f2(RN), in_=pss[:, 0:512], func=AF.Sqrt, bias=1e-24),
               r=[pssk], w=["RN"])
            op("dve", lambda e: e.reciprocal(out=f2(RN), in_=f2(RN)), r=["RN"], w=["RN"])
            op("pool", lambda e: e.tensor_tensor(out=f2(KK), in0=f2(KKR), in1=f2(RN), op=ALU.mult),
               r=["KKR", "RN"], w=["KK"])
            for cc in range(4):
                op("dve", lambda e, cc=cc: e.tensor_scalar(out=T1[:, cc, :], in0=Aa[:, cc, :],
                                                           scalar1=vcol(V_KA + cc), scalar2=OMKA(cc),
                                                           op0=ALU.mult, op1=ALU.add),
                   r=["Aa", "vec", "derived"], w=["T1"])
            op("pool", lambda e: e.tensor_tensor(out=f2(KP), in0=f2(Kr), in1=f2(T1), op=ALU.mult),
               r=["Kr", "T1"], w=["KP"])
            op("pool", lambda e: e.tensor_tensor(out=f2(Bb), in0=f2(KK), in1=f2(Aa), op=ALU.mult),
               r=["KK", "Aa"], w=["Bb"])
            op("dve", lambda e: e.scalar_tensor_tensor(out=AR[:, :, 0, :], in0=E3[:], scalar=-1.0, in1=KK[:],
                                                       op0=ALU.mult, op1=ALU.mult), r=["E3", "KK"], w=["AR"])
            op("pool", lambda e: e.tensor_tensor(out=AR[:, :, 1, :], in0=E1[:], in1=Rr[:], op=ALU.mult),
               r=["E1", "Rr", "AR"], w=["AR"])
            op("dve", lambda e: e.tensor_tensor(out=BK[:, :, 0, :], in0=E2[:], in1=Bb[:], op=ALU.mult),
               r=["E2", "Bb"], w=["BK"])
            op("pool", lambda e: e.tensor_tensor(out=BK[:, :, 1, :], in0=E2[:], in1=KP[:], op=ALU.mult),
               r=["E2", "KP", "BK"], w=["BK"])
            op("dve", lambda e: e.tensor_tensor(out=BKS[:, :, 0, :], in0=E4[:], in1=Bb[:], op=ALU.mult),
               r=["E4", "Bb"], w=["BKS"])
            op("pool", lambda e: e.tensor_tensor(out=BKS[:, :, 1, :], in0=E4[:], in1=KP[:], op=ALU.mult),
               r=["E4", "KP", "BKS"], w=["BKS"])
            for par in range(2):
                pmc = cst[:, 640 + 64 * par:641 + 64 * par]
                op("act", lambda e: e.activation(
                    out=ARm[par][:].rearrange("p a b c -> p (a b c)"), in_=AR[:].rearrange("p a b c -> p (a b c)"),
                    func=AF.Copy, scale=pmc), r=["AR", "cst"], w=[f"ARm{par}"])
                op("act" if par else "dve", (lambda e: e.activation(
                    out=BKm[par][:].rearrange("p a b c -> p (a b c)"), in_=BK[:].rearrange("p a b c -> p (a b c)"),
                    func=AF.Copy, scale=pmc)) if par else (lambda e: e.tensor_scalar(
                    out=BKm[par][:].rearrange("p a b c -> p (a b c)"), in0=BK[:].rearrange("p a b c -> p (a b c)"),
                    scalar1=pmc, scalar2=None, op0=ALU.mult)), r=["BK", "cst"], w=[f"BKm{par}"])
            op("pool", lambda e: e.tensor_tensor(out=f2(RK), in0=f2(Rr), in1=f2(KP), op=ALU.mult),
               r=["Rr", "KP"], w=["RK"])
            op("pool", lambda e: e.tensor_tensor(out=RK2[:], in0=RK[:],
                                                 in1=vcol(V_RK, 4).unsqueeze(2).to_broadcast([128, 4, 128]),
                                                 op=ALU.mult), r=["RK", "vec"], w=["RK2"])
            pbn, pbnk = PS()
            op("pe", lambda e: e.matmul(pbn[:, 0:512], bones[:], f2(RK2), start=True, stop=True),
               r=["bones", "RK2"], w=[pbnk])
            op("dve", lambda e: e.tensor_tensor(out=f2(BV), in0=pbn[:, 0:512], in1=f2(Vr), op=ALU.mult),
               r=[pbnk, "Vr"], w=["BV"])
            op("act", lambda e: e.activation(out=f2(VB), in_=f2(Vr), func=AF.Copy), r=["Vr"], w=["VB"])
            pb1, pb1k = PSB()
            for j in range(2):
                for cc in range(4):
                    op("pe", lambda e, j=j, cc=cc: e.transpose(pb1[:, j * 512 + cc * 128: j * 512 + (cc + 1) * 128],
                                                               BKS[:, cc, j, :], identb[:]),
                       r=["BKS", "identb"], w=[pb1k])
            op("act", lambda e: e.activation(out=BKT[:], in_=pb1[:, :], func=AF.Copy), r=[pb1k], w=["BKT"])
            pb2, pb2k = PSB()
            for cc in range(4):
                op("pe", lambda e, cc=cc: e.transpose(pb2[:, cc * 128:(cc + 1) * 128], VB[:, cc, :], identb[:]),
                   r=["VB", "identb"], w=[pb2k])
            op("act", lambda e: e.activation(out=VT[:], in_=pb2[:, 0:512], func=AF.Copy), r=[pb2k], w=["VT"])
            if CUT <= 6:
                return
            def inv_s0(hh):
                heads = list(range(4 * hh, 4 * hh + 4))
                hs = slice(4 * hh, 4 * hh + 4)
                mk2 = MASK2.unsqueeze(1).to_broadcast([128, 2, 256])
                for (which, dstM) in ((0, MA), (1, MB)):
                    pM_ = [PS(), PS()]
                    for i, hd in enumerate(heads):
                        cc = hd // 2
                        c0 = (i % 2) * 256
                        par = hd % 2
                        op("pe", lambda e: e.matmul(
                            pM_[i // 2][0][:, c0:c0 + 256], BKm[par][:, cc, which, :],
                            AR[:, cc, :, :].rearrange("p a t -> p (a t)"), start=True, stop=True),
                           r=[f"BKm{par}", "AR"], w=[pM_[i // 2][1]])
                    for b2 in range(2):
                        h2 = slice(4 * hh + 2 * b2, 4 * hh + 2 * b2 + 2)
                        op("dve", lambda e: e.tensor_tensor(
                            out=dstM[:, h2, :], in0=pM_[b2][0][:, 0:512].rearrange("p (h c) -> p h c", h=2), in1=mk2,
                            op=ALU.mult), r=[pM_[b2][1], "cst"], w=[("MA" if which == 0 else "MB") + str(hh)])
                pQ0 = PS()
                for i, hd in enumerate(heads):
                    cc = hd // 2
                    par = hd % 2
                    op("pe", lambda e: e.matmul(
                        pQ0[0][:, i * 128:(i + 1) * 128], ARm[par][:, cc, 0, :], BK[:, cc, 0, :], start=True, stop=True),
                       r=["BK", f"ARm{par}"], w=[pQ0[1]])
                op("dve", lambda e: e.tensor_tensor(
                    out=Qm[0][:, hs, :], in0=pQ0[0][:, 0:512].rearrange("p (h c) -> p h c", h=4),
                    in1=LSTR.unsqueeze(1).to_broadcast([128, 4, 128]), op=ALU.mult),
                   r=[pQ0[1], "cst"], w=[f"Q0_{hh}"])

            def inv_l0(hh):
                heads = list(range(4 * hh, 4 * hh + 4))
                hs = slice(4 * hh, 4 * hh + 4)
                pP = PS()
                pQn = PS()
                for i, hd in enumerate(heads):
                    op("pe", lambda e, i=i, hd=hd: e.matmul(pP[0][:, i * 128:(i + 1) * 128], Qm[0][:, hd, :],
                                                            MA[:, hd, 0:128], start=True, stop=True),
                       r=[f"Q0_{hh}", f"MA{hh}"], w=[pP[1]])
                    op("pe", lambda e, i=i, hd=hd: e.matmul(pQn[0][:, i * 128:(i + 1) * 128], MA[:, hd, 0:128],
                                                            Qm[0][:, hd, :], start=True, stop=True),
                       r=[f"Q0_{hh}", f"MA{hh}"], w=[pQn[1]])
                op("act", lambda e, hs=hs: e.activation(out=PX[1][:, hs, 0:128],
                                                        in_=pP[0][:, 0:512].rearrange("p (h c) -> p h c", h=4),
                                                        func=AF.Copy), r=[pP[1]], w=[f"PX1_{hh}"])
                op("act", lambda e, hs=hs: e.activation(out=Qm[1][:, hs, :],
                                                        in_=pQn[0][:, 0:512].rearrange("p (h c) -> p h c", h=4),
                                                        func=AF.Copy), r=[pQn[1]], w=[f"Q1_{hh}"])
                op("pool", lambda e, hs=hs: e.tensor_tensor(out=PX[1][:, hs, 128:256], in0=MA[:, hs, 0:128],
                                                            in1=IDENTF.unsqueeze(1).to_broadcast([128, 4, 128]),
                                                            op=ALU.add),
                   r=[f"MA{hh}", "cst", f"PX1_{hh}"], w=[f"PX1_{hh}"])

            def inv_lv(hh, lv):
                heads = list(range(4 * hh, 4 * hh + 4))
                hs = slice(4 * hh, 4 * hh + 4)
                if True:
                    cur, nxt = lv % 2, 1 - (lv % 2)
                    pA = [PS(), PS()]
                    pQn = PS()
                    for i, hd in enumerate(heads):
                        c0 = (i % 2) * 256
                        op("pe", lambda e, i=i, hd=hd, c0=c0, cur=cur: e.matmul(
                            pA[i // 2][0][:, c0:c0 + 256], Qm[cur][:, hd, :], PX[cur][:, hd, :], start=True, stop=True),
                           r=[f"Q{cur}_{hh}", f"PX{cur}_{hh}"], w=[pA[i // 2][1]])
                        op("pe", lambda e, i=i, hd=hd, cur=cur: e.matmul(
                            pQn[0][:, i * 128:(i + 1) * 128], PX[cur][:, hd, 0:128], Qm[cur][:, hd, :],
                            start=True, stop=True), r=[f"Q{cur}_{hh}", f"PX{cur}_{hh}"], w=[pQn[1]])
                    for b2 in range(2):
                        h2 = slice(4 * hh + 2 * b2, 4 * hh + 2 * b2 + 2)
                        v3 = pA[b2][0][:, 0:512].rearrange("p (h c) -> p h c", h=2)
                        op("act", lambda e, h2=h2, v3=v3, nxt=nxt: e.activation(out=PX[nxt][:, h2, 0:128],
                                                                               in_=v3[:, :, 0:128], func=AF.Copy),
                           r=[pA[b2][1]], w=[f"PX{nxt}_{hh}"])
                        op("dve", lambda e, h2=h2, v3=v3, nxt=nxt, cur=cur: e.tensor_tensor(
                            out=PX[nxt][:, h2, 128:256], in0=v3[:, :, 128:256], in1=PX[cur][:, h2, 128:256],
                            op=ALU.add), r=[pA[b2][1], f"PX{cur}_{hh}", f"PX{nxt}_{hh}"], w=[f"PX{nxt}_{hh}"])
                    op("act", lambda e, hs=hs, nxt=nxt, pQn=pQn: e.activation(
                        out=Qm[nxt][:, hs, :], in_=pQn[0][:, 0:512].rearrange("p (h c) -> p h c", h=4), func=AF.Copy),
                       r=[pQn[1]], w=[f"Q{nxt}_{hh}"])

            def inv_fin(hh):
                heads = list(range(4 * hh, 4 * hh + 4))
                hs = slice(4 * hh, 4 * hh + 4)
                pX = PS()
                for i, hd in enumerate(heads):
                    op("pe", lambda e, i=i, hd=hd: e.matmul(pX[0][:, i * 128:(i + 1) * 128], Qm[0][:, hd, :],
                                                            PX[0][:, hd, 128:256], start=True, stop=True),
                       r=[f"Q0_{hh}", f"PX0_{hh}"], w=[pX[1]])
                op("dve", lambda e, hs=hs, pX=pX: e.tensor_tensor(
                    out=XF[:, hs, :], in0=pX[0][:, 0:512].rearrange("p (h c) -> p h c", h=4),
                    in1=PX[0][:, hs, 128:256], op=ALU.add), r=[pX[1], f"PX0_{hh}"], w=[f"XF{hh}"])

            for hh in range(2):
                inv_s0(hh)
            for hh in range(2):
                inv_l0(hh)
            for lv in range(1, 6):
                for hh in range(2):
                    inv_lv(hh, lv)
            for hh in range(2):
                inv_fin(hh)
            if CUT <= 7:
                return
            pR = PS()
            for hd in range(8):
                cc = hd // 2
                pr = slice((hd % 2) * 64, (hd % 2) * 64 + 64)
                op("pe", lambda e: e.matmul(pR[0][:, hd * 64:(hd + 1) * 64], ARm[hd % 2][:, cc, 0, :],
                                            SBs[:, cc, :], start=True, stop=False),
                   r=[f"ARm{hd % 2}", "SBs"], w=[pR[1]])
                op("pe", lambda e, hd=hd: e.matmul(pR[0][:, hd * 64:(hd + 1) * 64], MB[:, hd, 0:128],
                                                   VT[:, hd * 64:(hd + 1) * 64], start=False, stop=True),
                   r=[f"MB{hd // 4}", "VT"], w=[pR[1]])
            op("act", lambda e: e.activation(out=RH[:], in_=pR[0][:, 0:512], func=AF.Copy), r=[pR[1]], w=["RH"])
            pU = PS()
            for hd in range(8):
                op("pe", lambda e, hd=hd: e.matmul(pU[0][:, hd * 64:(hd + 1) * 64], XF[:, hd, :],
                                                   RH[:, hd * 64:(hd + 1) * 64], start=True, stop=True),
                   r=[f"XF{hd // 4}", "RH"], w=[pU[1]])
            op("act", lambda e: e.activation(out=UT[:], in_=pU[0][:, 0:512], func=AF.Copy), r=[pU[1]], w=["UT"])
            pY = PS()
            pS_ = PS()
            for hd in range(8):
                cc = hd // 2
                pr = slice((hd % 2) * 64, (hd % 2) * 64 + 64)
                oy = pY[0][pr, cc * 128:(cc + 1) * 128]
                op("pe", lambda e: e.matmul(oy, SBs[:, cc, :], ARm[hd % 2][:, cc, 1, :],
                                            start=True, stop=False), r=["SBs", f"ARm{hd % 2}"], w=[pY[1]])
                op("pe", lambda e, oy=oy, hd=hd: e.matmul(oy, UT[:, hd * 64:(hd + 1) * 64], MA[:, hd, 128:256],
                                                          start=False, stop=False),
                   r=["UT", f"MA{hd // 4}"], w=[pY[1]])
                op("pe", lambda e, oy=oy, hd=hd: e.matmul(oy, VT[:, hd * 64:(hd + 1) * 64], MB[:, hd, 128:256],
                                                          start=False, stop=True),
                   r=["VT", f"MB{hd // 4}"], w=[pY[1]])
            for hd in range(8):
                cc = hd // 2
                pr = slice((hd % 2) * 64, (hd % 2) * 64 + 64)
                os_ = pS_[0][pr, cc * 64:(cc + 1) * 64]
                op("pe", lambda e, os_=os_, hd=hd: e.matmul(os_, BKT[:, hd * 64:(hd + 1) * 64],
                                                            UT[:, hd * 64:(hd + 1) * 64], start=True, stop=False),
                   r=["BKT", "UT"], w=[pS_[1]])
                op("pe", lambda e, os_=os_, hd=hd: e.matmul(os_, BKT[:, 512 + hd * 64:512 + (hd + 1) * 64],
                                                            VT[:, hd * 64:(hd + 1) * 64], start=False, stop=True),
                   r=["BKT", "VT"], w=[pS_[1]])
            op("act", lambda e: e.activation(out=f2(Yf), in_=pY[0][:, 0:512], func=AF.Copy), r=[pY[1]], w=["Yf"])
            op("dve", lambda e: e.tensor_tensor(out=TMPS[:], in0=SF[:],
                                                in1=E1[:, :, 127:128].to_broadcast([128, 4, 64]), op=ALU.mult),
               r=["SF", "E1"], w=["TMPS"])
            op("dve", lambda e: e.tensor_tensor(out=SF[:], in0=pS_[0][:, 0:256].rearrange("p (c v) -> p c v", c=4),
                                                in1=TMPS[:], op=ALU.add), r=[pS_[1], "TMPS"], w=["SF"])
            op("act", lambda e: e.activation(out=SBs[:], in_=SF[:], func=AF.Copy), r=["SF"], w=["SBs"])
            if CUT <= 8:
                return
            op("act", lambda e: e.activation(out=f2(YB), in_=pY[0][:, 0:512], func=AF.Copy), r=[pY[1]], w=["YB"])
            op("act", lambda e: e.activation(out=f2(YSQ), in_=pY[0][:, 0:512], func=AF.Square), r=[pY[1]], w=["YSQ"])
            pM = PS()
            pV = PS()
            op("pe", lambda e: e.matmul(pM[0][:, 0:512], bones[:], f2(YB), start=True, stop=True),
               r=["bones", "YB"], w=[pM[1]])
            op("pe", lambda e: e.matmul(pV[0][:, 0:512], bones[:], f2(YSQ), start=True, stop=True),
               r=["bones", "YSQ"], w=[pV[1]])
            op("act", lambda e: e.activation(out=f2(MEAN), in_=pM[0][:, 0:512], func=AF.Copy, scale=1.0 / 64),
               r=[pM[1]], w=["MEAN"])
            op("pool", lambda e: e.tensor_tensor(out=f2(M2), in0=f2(MEAN), in1=f2(MEAN), op=ALU.mult),
               r=["MEAN"], w=["M2"])
            op("dve", lambda e: e.scalar_tensor_tensor(out=f2(VAR), in0=pV[0][:, 0:512], scalar=1.0 / 64, in1=f2(M2),
                                                       op0=ALU.mult, op1=ALU.subtract), r=[pV[1], "M2"], w=["VAR"])
            op("act", lambda e: e.activation(out=f2(VAR), in_=f2(VAR), func=AF.Sqrt, bias=64e-5), r=["VAR"], w=["VAR"])
            op("dve", lambda e: e.reciprocal(out=f2(VAR), in_=f2(VAR)), r=["VAR"], w=["VAR"])
            op("pool", lambda e: e.tensor_tensor(out=f2(Dd), in0=f2(Yf), in1=f2(MEAN), op=ALU.subtract),
               r=["Yf", "MEAN"], w=["Dd"])
            op("pool", lambda e: e.tensor_tensor(out=f2(Dd), in0=f2(Dd), in1=f2(VAR), op=ALU.mult),
               r=["Dd", "VAR"], w=["Dd"])
            for cc in range(4):
                op("dve", lambda e, cc=cc: e.tensor_scalar(out=Dd[:, cc, :], in0=Dd[:, cc, :], scalar1=vcol(V_LNW + cc),
                                                           scalar2=vcol(V_LNB + cc), op0=ALU.mult, op1=ALU.add),
                   r=["Dd", "vec"], w=["Dd"])
            op("pool", lambda e: e.tensor_tensor(out=f2(Dd), in0=f2(Dd), in1=f2(BV), op=ALU.add),
               r=["Dd", "BV"], w=["Dd"])
            op("pool", lambda e, ys=ys: e.tensor_tensor(out=ys[:, 4:8, :], in0=Dd[:], in1=Gg[:], op=ALU.mult),
               r=["Dd", "Gg"], w=[yk + "b"])
            dma("sp", lambda e, ys=ys, ti=ti: e.dma_start(out=yab_d[ti], in_=ys[:].rearrange("p a b -> p (a b)")),
                r=[yk + "a", yk + "b"], w=[f"yab_d{ti}"], key=yk)

        genA(0)
        for ti in range(NTILE):
            fns = [lambda ti=ti: genBC(ti)]
            q = [cfg.get("qBC", 5)]
            if ti + 1 < NTILE:
                fns.append(lambda ti=ti: genA(ti + 1))
                q.append(1)
            WV.run(fns, q)
        tl.pool = None


    if "1b" in PH:
        phase_reset()
        Wgt = salloc("Wgt", [128, 8, 2048], BF16)
        WbA = salloc("WbA", [128, 4, 1024], BF16)
        WbB = salloc("WbB", [128, 4, 1024], BF16)
        Wout = salloc("Wout", [128, 8, 1024], BF16)
        Wr = salloc("Wr", [128, 8, 36], F32)
        BR = salloc("BR", [128, 36], F32)
        GMOE = salloc("GMOE", [128, D], F32)
        ustrb = salloc("ustrb", [128, 128], BF16)
        CNT = salloc("CNT", [128, 32], F32)
        stgB = [salloc(f"stgB{i}", [128, 2048], F32) for i in range(2)]
        sB = 0
        for kc in range(8):
            k_ = f"stgB{sB % 2}"
            t_ = stgB[sB % 2]
            sB += 1
            dma("sp", lambda e: e.dma_start(out=t_[:, 0:2048], in_=win_d[kc * 128:(kc + 1) * 128, 2560:4608]),
                w=[k_], key=k_)
            op("dve" if kc % 2 else "pool", lambda e: e.tensor_scalar(out=Wgt[:, kc, :], in0=t_[:, 0:2048],
                                                                      scalar1=vcol(V_LNMIX + kc), scalar2=1.0,
                                                                      op0=ALU.mult, op1=ALU.mult),
               r=[k_, "vec"], w=["Wgt"])
        for (dst, dk, src, nk) in ((WbA, "WbA", wba_d, 4), (WbB, "WbB", wbb_d, 4), (Wout, "Wout", wout_d, 8)):
            for kc in range(nk):
                k_ = f"stgB{sB % 2}"
                t_ = stgB[sB % 2]
                sB += 1
                dma("sp", lambda e: e.dma_start(out=t_[:, 0:1024], in_=src[kc * 128:(kc + 1) * 128, :]),
                    w=[k_], key=k_)
                op("dve" if kc % 2 else "pool", lambda e: e.tensor_copy(out=dst[:, kc, :], in_=t_[:, 0:1024]),
                   r=[k_], w=[dk])
        dma("sp", lambda e: e.dma_start(out=Wr[:], in_=wr_d.rearrange("(k p) n -> p k n", p=128)), w=["Wr"], key="Wr")
        for kc in range(8):
            op("dve", lambda e: e.tensor_scalar(out=Wr[:, kc, :], in0=Wr[:, kc, :], scalar1=vcol(V_LNMOE + kc),
                                                scalar2=None, op0=ALU.mult), r=["Wr", "vec"], w=["Wr"])
        dma("sp", lambda e: e.dma_start(out=BR[:], in_=br_d.to_broadcast([128, 36])), w=["BR"], key="BR")
        dma("sp", lambda e: e.dma_start(out=GMOE[:], in_=lnmoe_d.to_broadcast([128, D])), w=["GMOE"], key="GMOE")
        op("dve", lambda e: e.tensor_copy(out=ustrb[:], in_=USTR), r=["cst"], w=["ustrb"])
        op("dve", lambda e: e.memset(CNT[:], 0.0), w=["CNT"])
        ZR = salloc("ZR", [128, D], BF16)
        op("pool", lambda e: e.memset(ZR[:], 0.0), w=["ZR"])
        hs_v = hs_d.rearrange("(r p) d -> p r d", p=128)
        RCH = 8
        for r0 in range(0, NST, RCH):
            rn = min(RCH, NST - r0)
            dma("sp", lambda e: e.dma_start(out=hs_v[:, r0:r0 + rn, :],
                                            in_=ZR[:].unsqueeze(1).to_broadcast([128, rn, D])),
                r=["ZR"], w=["hs_all"], key="ZRst")

        xin = [salloc(f"xinb{i}", [128, D], F32) for i in range(2)]
        tmp = dict(junk=salloc("junkb", [128, D], BF16), ss=salloc("ssb", [128, 4], F32),
                   rstd=salloc("rstdb", [128, 1], F32), xbf=salloc("xbfb", [128, D], BF16))
        hT = [salloc(f"hTb{i}", [128, 8, 128], BF16) for i in range(2)]
        yin = [salloc(f"yin{i}", [128, 8, 128], BF16) for i in range(2)]
        GT = salloc("GT", [128, 2048], F32)
        MAf = salloc("MAf", [128, D], F32)
        MBf = salloc("MBf", [128, D], F32)
        MG = salloc("MG", [128, D], BF16)
        MGT = salloc("MGT", [128, 8, 128], BF16)
        X1 = [salloc(f"X1_{i}", [128, D], F32) for i in range(2)]
        HM = [salloc(f"HM{i}", [128, D], BF16) for i in range(2)]
        X1T = salloc("X1T", [128, 8, 128], F32)
        rs = salloc("rs", [128, 8], F32)
        LG = salloc("LG", [128, 36], F32)
        R_ = salloc("Rsm", [128, 16], F32)
        GOH = salloc("GOH", [128, 4], F32)
        EG = salloc("EG", [128, 4], F32)
        T48 = salloc("T48", [128, 4, 8], F32)
        ESEL = salloc("ESEL", [128, 8], F32)
        ES2 = salloc("ES2", [128, 8], F32)
        OHa = salloc("OHa", [128, 8], F32)
        OHb = salloc("OHb", [128, 8], F32)
        OH1 = salloc("OH1", [128, 4, 8], F32)
        OH2 = salloc("OH2", [128, 4, 8], F32)
        OHSb = salloc("OHSb", [128, 32], BF16)
        POS = salloc("POS", [128, 32], F32)
        PT2 = salloc("PT2", [128, 32], F32)
        SL = salloc("SL", [128, 2], F32)
        EOFF = cst[:, 768:800]

        def g2(t):
            return t[:].rearrange("p a b -> p (a b)")

        for ti in range(NTILE):
            sl = ti % 2
            xk = f"xinb{sl}"
            dma("sp", lambda e: e.dma_start(out=xin[sl][:], in_=x_d[ti * 128:(ti + 1) * 128, :]), w=[xk], key=xk)
            yk = f"yin{sl}"
            dma("sp", lambda e: e.dma_start(out=yin[sl][:].rearrange("p a b -> p (a b)"), in_=yab_d[ti]),
                r=[f"yab_d{ti}"], w=[yk], key=yk)
            hk = f"hTb{sl}"
            rms_to_hT(ti, xin[sl], xk, hT[sl], hk, tmp)
            h = hT[sl]
            for nb in range(4):
                pgt = PS()
                for kc in range(8):
                    op("pe", lambda e: e.matmul(pgt[0][:, 0:512], h[:, kc, :], Wgt[:, kc, nb * 512:(nb + 1) * 512],
                                                start=(kc == 0), stop=(kc == 7)), r=[hk, "Wgt"], w=[pgt[1]])
                op("act", lambda e: e.activation(out=GT[:, nb * 512:(nb + 1) * 512], in_=pgt[0][:, 0:512],
                                                 func=AF.Sigmoid), r=[pgt[1]], w=[f"GT{nb}"])
            for half in range(2):
                pba = PS()
                for c in range(4):
                    op("pe", lambda e: e.matmul(pba[0][:, 0:512], yin[sl][:, c, :],
                                                WbA[:, c, half * 512:(half + 1) * 512], start=(c == 0), stop=(c == 3)),
                       r=[yk, "WbA"], w=[pba[1]])
                op("dve", lambda e: e.tensor_tensor(out=MAf[:, half * 512:(half + 1) * 512], in0=pba[0][:, 0:512],
                                                    in1=GT[:, half * 512:(half + 1) * 512], op=ALU.mult),
                   r=[pba[1], f"GT{half}"], w=[f"MAf{half}"])
                pbb = PS()
                for c in range(4):
                    op("pe", lambda e: e.matmul(pbb[0][:, 0:512], yin[sl][:, 4 + c, :],
                                                WbB[:, c, half * 512:(half + 1) * 512], start=(c == 0), stop=(c == 3)),
                       r=[yk, "WbB"], w=[pbb[1]])
                op("dve", lambda e: e.tensor_tensor(out=MBf[:, half * 512:(half + 1) * 512], in0=pbb[0][:, 0:512],
                                                    in1=GT[:, 1024 + half * 512:1024 + (half + 1) * 512], op=ALU.mult),
                   r=[pbb[1], f"GT{2 + half}"], w=[f"MBf{half}"])
                op("pool", lambda e: e.tensor_tensor(out=MG[:, half * 512:(half + 1) * 512],
                                                     in0=MAf[:, half * 512:(half + 1) * 512],
                                                     in1=MBf[:, half * 512:(half + 1) * 512], op=ALU.add),
                   r=[f"MAf{half}", f"MBf{half}"], w=[f"MG{half}"])
            pb = PSB()
            for kc in range(8):
                op("pe", lambda e: e.transpose(pb[0][:, kc * 128:(kc + 1) * 128], MG[:, kc * 128:(kc + 1) * 128],
                                               identb[:]), r=["MG0", "MG1", "identb"], w=[pb[1]])
            op("act", lambda e: e.activation(out=g2(MGT), in_=pb[0][:, :], func=AF.Copy), r=[pb[1]], w=["MGT"])
            x1 = X1[sl]
            x1k = f"X1_{sl}"
            for half in range(2):
                po = PS()
                for kc in range(8):
                    op("pe", lambda e: e.matmul(po[0][:, 0:512], MGT[:, kc, :], Wout[:, kc, half * 512:(half + 1) * 512],
                                                start=(kc == 0), stop=(kc == 7)), r=["MGT", "Wout"], w=[po[1]])
                op("dve", lambda e: e.tensor_tensor(out=x1[:, half * 512:(half + 1) * 512], in0=po[0][:, 0:512],
                                                    in1=xin[sl][:, half * 512:(half + 1) * 512], op=ALU.add),
                   r=[po[1], xk], w=[x1k + f"h{half}"])
            dma("sp", lambda e: e.dma_start(out=x1_d[ti * 128:(ti + 1) * 128, :], in_=x1[:]),
                r=[x1k + "h0", x1k + "h1"], w=[f"x1_d{ti}"], key=x1k)
            op("act", lambda e: e.activation(out=tmp["junk"][:], in_=x1[:], func=AF.Square, accum_out=rs[:, 0:1]),
               r=[x1k + "h0", x1k + "h1"], w=["junk", "rs0"])
            op("dve", lambda e: e.tensor_scalar(out=rs[:, 1:2], in0=rs[:, 0:1], scalar1=1.0 / D, scalar2=1e-6,
                                                op0=ALU.mult, op1=ALU.add), r=["rs0"], w=["rs1"])
            op("act", lambda e: e.activation(out=rs[:, 2:3], in_=rs[:, 1:2], func=AF.Sqrt), r=["rs1"], w=["rs2"])
            op("dve", lambda e: e.reciprocal(out=rs[:, 3:4], in_=rs[:, 2:3]), r=["rs2"], w=["rs3"])
            hm = HM[sl]
            hmk = f"HM{sl}"
            op("dve", lambda e: e.scalar_tensor_tensor(out=hm[:], in0=x1[:], scalar=rs[:, 3:4], in1=GMOE[:],
                                                       op0=ALU.mult, op1=ALU.mult),
               r=[x1k + "h0", x1k + "h1", "rs3", "GMOE"], w=[hmk])
            for half in range(2):
                ptx = PS()
                for j in range(4):
                    kc = half * 4 + j
                    op("pe", lambda e: e.transpose(ptx[0][:, j * 128:(j + 1) * 128], x1[:, kc * 128:(kc + 1) * 128],
                                                   IDENTF), r=[x1k + "h0", x1k + "h1", "cst"], w=[ptx[1]])
                op("act", lambda e: e.activation(out=X1T[:, half * 4:half * 4 + 4, :].rearrange("p a b -> p (a b)"),
                                                 in_=ptx[0][:, 0:512], func=AF.Copy), r=[ptx[1]], w=[f"X1T{half}"])
            pl = PS()
            for kc in range(8):
                op("pe", lambda e: e.matmul(pl[0][:, 0:36], X1T[:, kc, :], Wr[:, kc, :], start=(kc == 0), stop=(kc == 7)),
                   r=["X1T0", "X1T1", "Wr"], w=[pl[1]])
            op("dve", lambda e: e.scalar_tensor_tensor(out=LG[:], in0=pl[0][:, 0:36], scalar=rs[:, 3:4], in1=BR[:],
                                                       op0=ALU.mult, op1=ALU.add), r=[pl[1], "rs3", "BR"], w=["LG"])
            V = lambda f, r, w: op("dve", f, r=r, w=w)
            V(lambda e: e.tensor_reduce(out=R_[:, 0:1], in_=LG[:, 0:4], axis=AX.X, op=ALU.max), ["LG"], ["R0"])
            V(lambda e: e.tensor_scalar(out=GOH[:], in0=LG[:, 0:4], scalar1=R_[:, 0:1], scalar2=None, op0=ALU.is_equal),
              ["LG", "R0"], ["GOH"])
            V(lambda e: e.tensor_scalar(out=R_[:, 1:2], in0=R_[:, 0:1], scalar1=-1.0, scalar2=None, op0=ALU.mult),
              ["R0"], ["R1"])
            op("act", lambda e: e.activation(out=EG[:], in_=LG[:, 0:4], func=AF.Exp, bias=R_[:, 1:2],
                                             accum_out=R_[:, 2:3]), r=["LG", "R1"], w=["EG", "R2"])
            V(lambda e: e.reciprocal(out=R_[:, 3:4], in_=R_[:, 2:3]), ["R2"], ["R3"])
            V(lambda e: e.tensor_tensor(out=T48[:], in0=LG[:, 4:36].rearrange("p (g x) -> p g x", g=4),
                                        in1=GOH[:].unsqueeze(2).to_broadcast([128, 4, 8]), op=ALU.mult),
              ["LG", "GOH"], ["T48"])
            V(lambda e: e.tensor_reduce(out=ESEL[:], in_=T48[:].rearrange("p g x -> p x g"), axis=AX.X, op=ALU.add),
              ["T48"], ["ESEL"])
            V(lambda e: e.tensor_reduce(out=R_[:, 4:5], in_=ESEL[:], axis=AX.X, op=ALU.max), ["ESEL"], ["R4"])
            V(lambda e: e.tensor_scalar(out=OHa[:], in0=ESEL[:], scalar1=R_[:, 4:5], scalar2=None, op0=ALU.is_equal),
              ["ESEL", "R4"], ["OHa"])
            V(lambda e: e.scalar_tensor_tensor(out=ES2[:], in0=OHa[:], scalar=-1e30, in1=ESEL[:], op0=ALU.mult,
                                               op1=ALU.add), ["OHa", "ESEL"], ["ES2"])
            V(lambda e: e.tensor_reduce(out=R_[:, 5:6], in_=ES2[:], axis=AX.X, op=ALU.max), ["ES2"], ["R5"])
            V(lambda e: e.tensor_scalar(out=OHb[:], in0=ES2[:], scalar1=R_[:, 5:6], scalar2=None, op0=ALU.is_equal),
              ["ES2", "R5"], ["OHb"])
            V(lambda e: e.tensor_tensor(out=R_[:, 6:7], in0=R_[:, 5:6], in1=R_[:, 4:5], op=ALU.subtract),
              ["R4", "R5"], ["R6"])
            op("act", lambda e: e.activation(out=R_[:, 7:8], in_=R_[:, 6:7], func=AF.Exp), r=["R6"], w=["R7"])
            V(lambda e: e.tensor_scalar(out=R_[:, 8:9], in0=R_[:, 7:8], scalar1=1.0, scalar2=None, op0=ALU.add),
              ["R7"], ["R8"])
            V(lambda e: e.reciprocal(out=R_[:, 9:10], in_=R_[:, 8:9]), ["R8"], ["R9"])
            V(lambda e: e.tensor_tensor(out=rw_f[:, 2 * ti:2 * ti + 1], in0=R_[:, 9:10], in1=R_[:, 3:4], op=ALU.mult),
              ["R9", "R3"], [f"rw{ti}a"])
            V(lambda e: e.tensor_tensor(out=rw_f[:, 2 * ti + 1:2 * ti + 2], in0=rw_f[:, 2 * ti:2 * ti + 1],
                                        in1=R_[:, 7:8], op=ALU.mult), [f"rw{ti}a", "R7"], [f"rw{ti}b"])
            gb = GOH[:].unsqueeze(2).to_broadcast([128, 4, 8])
            V(lambda e: e.tensor_tensor(out=OH1[:], in0=gb, in1=OHa[:].unsqueeze(1).to_broadcast([128, 4, 8]),
                                        op=ALU.mult), ["GOH", "OHa"], ["OH1"])
            V(lambda e: e.tensor_tensor(out=OH2[:], in0=gb, in1=OHb[:].unsqueeze(1).to_broadcast([128, 4, 8]),
                                        op=ALU.mult), ["GOH", "OHb"], ["OH2"])
            V(lambda e: e.tensor_tensor(out=OHSb[:], in0=g2(OH1), in1=g2(OH2), op=ALU.add), ["OH1", "OH2"], ["OHSb"])
            pc = PS()
            op("pe", lambda e: e.matmul(pc[0][:, 0:32], ustrb[:], OHSb[:], start=True, stop=True),
               r=["ustrb", "OHSb"], w=[pc[1]])
            op("pe", lambda e: e.matmul(pc[0][:, 32:64], onesb[:], OHSb[:], start=True, stop=True),
               r=["onesb", "OHSb"], w=[pc[1]])
            V(lambda e: e.tensor_tensor(out=POS[:], in0=pc[0][:, 0:32], in1=CNT[:], op=ALU.add), [pc[1], "CNT"], ["POS"])
            V(lambda e: e.tensor_tensor(out=CNT[:], in0=pc[0][:, 32:64], in1=CNT[:], op=ALU.add), [pc[1], "CNT"], ["CNT"])
            V(lambda e: e.tensor_scalar(out=POS[:], in0=POS[:], scalar1=float(CAP - 1), scalar2=None, op0=ALU.min),
              ["POS"], ["POS"])
            V(lambda e: e.tensor_tensor(out=POS[:], in0=POS[:], in1=EOFF, op=ALU.add), ["POS", "cst"], ["POS"])
            for j, OHx in enumerate((OH1, OH2)):
                V(lambda e: e.tensor_tensor(out=PT2[:], in0=POS[:], in1=g2(OHx), op=ALU.mult),
                  ["POS", f"OH{j + 1}"], ["PT2"])
                V(lambda e: e.tensor_reduce(out=SL[:, j:j + 1], in_=PT2[:], axis=AX.X, op=ALU.add), ["PT2"], [f"SL{j}"])
            V(lambda e: e.tensor_copy(out=slot_i[:, 2 * ti:2 * ti + 2], in_=SL[:]), ["SL0", "SL1"], [f"slot{ti}"])
            for j in range(2):
                dma("pool", lambda e: e.indirect_dma_start(
                    out=hs_d[:, :], out_offset=bass.IndirectOffsetOnAxis(ap=slot_i[:, 2 * ti + j:2 * ti + j + 1], axis=0),
                    in_=hm[:, :], in_offset=None), r=[hmk, f"slot{ti}", "hs_all"], w=[f"hs_sc{ti}_{j}"], key=f"sc{sl}{j}")
        if debug:
            RT = salloc("RT", [128, NTILE * 4], F32)
            op("dve", lambda e: e.tensor_copy(out=RT[:, 0:2 * NTILE], in_=slot_i[:]),
               r=[f"slot{t}" for t in range(NTILE)], w=["RT"])
            op("dve", lambda e: e.tensor_copy(out=RT[:, 2 * NTILE:4 * NTILE], in_=rw_f[:]),
               r=[f"rw{t}a" for t in range(NTILE)] + [f"rw{t}b" for t in range(NTILE)] + ["RT"], w=["RT"])
            dma("sp", lambda e: e.dma_start(out=rt_d, in_=RT[:]), r=["RT"], w=["rt_d"], key="RT")

    if "2" in PH:
        phase_reset()
        WG = [salloc(f"WG{i}", [128, 8, DE], BF16) for i in range(2)]
        WU = [salloc(f"WU{i}", [128, 8, DE], BF16) for i in range(2)]
        WD = [salloc(f"WD{i}", [128, 4, D], BF16) for i in range(2)]
        XG = [salloc(f"XG{i}", [128, D], BF16) for i in range(2)]
        XGT2 = [salloc(f"XGT{i}", [128, 8, 128], BF16) for i in range(2)]
        SGf2 = [salloc(f"SGf{i}", [128, 512], F32) for i in range(2)]
        HID2 = [salloc(f"HID{i}", [128, 512], BF16) for i in range(2)]
        YO = [salloc(f"YO{i}", [128, D], F32) for i in range(2)]
        all_sc = [f"hs_sc{t}_{j}" for t in range(NTILE) for j in range(2)] + ["hs_all"]

        def body2(ex, rt, it):
            b = ex % 2
            xb = it % 2
            tl.pool = "E" if xb == 0 else "O"
            op, dma = stream(f"@{xb}")
            XGT, SGf, HID = XGT2[xb], SGf2[xb], HID2[xb]
            if rt == 0:
                wdma(ex)
            body2b(ex, rt, it, b, xb, op, dma, XGT, SGf, HID)

        def wdma(ex):
            b = ex % 2
            dma("pool", lambda e: e.dma_start(out=WG[b][:], in_=wg_d[ex].rearrange("(p k) n -> p k n", k=8),
                                              max_dma_last_dim=8192), w=[f"WG{b}"], key=f"WG{b}")
            dma("pool", lambda e: e.dma_start(out=WU[b][:], in_=wu_d[ex].rearrange("(p k) n -> p k n", k=8),
                                              max_dma_last_dim=8192), w=[f"WU{b}"], key=f"WU{b}")
            dma("pool", lambda e: e.dma_start(out=WD[b][:], in_=wd_d[ex].rearrange("(k p) n -> p k n", p=128)),
                w=[f"WD{b}"], key=f"WD{b}")

        def body2b(ex, rt, it, b, xb, op, dma, XGT, SGf, HID):
            if True:
                row0 = ex * CAP + rt * 128
                xgk = f"XG{xb}"
                dma("sp", lambda e: e.dma_start(out=XG[xb][:], in_=hs_d[row0:row0 + 128, :]),
                    r=(all_sc if "1b" in PH else []), w=[xgk], key=xgk)
                pb = PSB()
                xv = XG[xb][:].rearrange("p (m j) -> p j m", j=8)
                for j in range(8):
                    op("pe", lambda e: e.transpose(pb[0][:, j * 128:(j + 1) * 128], xv[:, j, :], identb[:]),
                       r=[xgk, "identb"], w=[pb[1]])
                op("act", lambda e: e.activation(out=XGT[:].rearrange("p a b -> p (a b)"), in_=pb[0][:, :], func=AF.Copy),
                   r=[pb[1]], w=["XGT"])
                pG = PS()
                pU_ = PS()
                for hc in range(4):
                    for j in range(8):
                        op("pe", lambda e: e.matmul(pG[0][:, hc * 128:(hc + 1) * 128], WG[b][:, j, hc * 128:(hc + 1) * 128],
                                                    XGT[:, j, :], start=(j == 0), stop=(j == 7)),
                           r=[f"WG{b}", "XGT"], w=[pG[1]])
                for hc in range(4):
                    for j in range(8):
                        op("pe", lambda e: e.matmul(pU_[0][:, hc * 128:(hc + 1) * 128], WU[b][:, j, hc * 128:(hc + 1) * 128],
                                                    XGT[:, j, :], start=(j == 0), stop=(j == 7)),
                           r=[f"WU{b}", "XGT"], w=[pU_[1]])
                op("act", lambda e: e.activation(out=SGf[:], in_=pG[0][:, 0:512], func=AF.Silu), r=[pG[1]], w=["SGf"])
                op("dve", lambda e: e.tensor_tensor(out=HID[:], in0=pU_[0][:, 0:512], in1=SGf[:], op=ALU.mult),
                   r=[pU_[1], "SGf"], w=["HID"])
                yo = YO[xb]
                yok = f"YO{xb}"
                for half in range(2):
                    py = PS()
                    for hc in range(4):
                        op("pe", lambda e: e.matmul(py[0][:, 0:512], HID[:, hc * 128:(hc + 1) * 128],
                                                    WD[b][:, hc, half * 512:(half + 1) * 512], start=(hc == 0),
                                                    stop=(hc == 3)), r=["HID", f"WD{b}"], w=[py[1]])
                    op("act" if half else "dve",
                       (lambda e: e.activation(out=yo[:, 512:1024], in_=py[0][:, 0:512], func=AF.Copy)) if half else
                       (lambda e: e.tensor_copy(out=yo[:, 0:512], in_=py[0][:, 0:512])), r=[py[1]], w=[yok + f"h{half}"])
                dma("sp", lambda e: e.dma_start(out=ys_d[row0:row0 + 128, :], in_=yo[:]),
                    r=[yok + "h0", yok + "h1"], w=["ys_all"], key=yok)

        its = [(ex, rt) for ex in range(NE) for rt in range(CT)]
        for i0 in range(0, len(its), 2):
            fns = [lambda i0=i0: body2(its[i0][0], its[i0][1], i0)]
            if i0 + 1 < len(its):
                fns.append(lambda i0=i0: body2(its[i0 + 1][0], its[i0 + 1][1], i0 + 1))
            WV.run(fns, [1] * len(fns), seq=not cfg.get("weave23", False))
        tl.pool = None

    if "3" in PH:
        phase_reset()
        Wpg = salloc("Wpg", [128, 8, D], BF16)
        Wpp = salloc("Wpp", [128, 2, D], BF16)
        LNF = salloc("LNF", [128, D], F32)
        stgC = [salloc(f"stgC{i}", [128, D], F32) for i in range(2)]
        for kc in range(8):
            k_ = f"stgC{kc % 2}"
            t_ = stgC[kc % 2]
            dma("sp", lambda e: e.dma_start(out=t_[:], in_=wpg_d[kc * 128:(kc + 1) * 128, :]), w=[k_], key=k_)
            op("dve" if kc % 2 else "pool", lambda e: e.tensor_scalar(out=Wpg[:, kc, :], in0=t_[:],
                                                                      scalar1=vcol(V_LNPLE + kc), scalar2=1.0,
                                                                      op0=ALU.mult, op1=ALU.mult),
               r=[k_, "vec"], w=["Wpg"])
        for kc in range(2):
            k_ = f"stgC{kc % 2}"
            t_ = stgC[kc % 2]
            dma("sp", lambda e: e.dma_start(out=t_[:], in_=wpp_d[kc * 128:(kc + 1) * 128, :]), w=[k_], key=k_)
            op("dve", lambda e: e.tensor_copy(out=Wpp[:, kc, :], in_=t_[:]), r=[k_], w=["Wpp"])
        dma("sp", lambda e: e.dma_start(out=LNF[:], in_=lnf_d.to_broadcast([128, D])), w=["LNF"], key="LNF")
        X1c = [salloc(f"X1c{i}", [128, D], F32) for i in range(2)]
        Y1 = [salloc(f"Y1_{i}", [128, D], F32) for i in range(2)]
        Y2 = [salloc(f"Y2_{i}", [128, D], F32) for i in range(2)]
        Pin = [salloc(f"Pin{i}", [128, 256], F32) for i in range(2)]
        Pb2 = [salloc(f"Pb{i}", [128, 256], BF16) for i in range(2)]
        PTt2 = [salloc(f"PTt{i}", [128, 2, 128], BF16) for i in range(2)]
        X22 = [salloc(f"X2{i}", [128, D], F32) for i in range(2)]
        tmp2 = [dict(junk=salloc(f"junkc{i}", [128, D], BF16), ss=salloc(f"ssc{i}", [128, 4], F32),
                     rstd=salloc(f"rstdc{i}", [128, 1], F32), xbf=salloc(f"xbfc{i}", [128, D], BF16))
                for i in range(2)]
        hTc2 = [salloc(f"hTc{i}", [128, 8, 128], BF16) for i in range(2)]
        GP2 = [salloc(f"GP{i}", [128, D], F32) for i in range(2)]
        X32 = [salloc(f"X3{i}", [128, D], F32) for i in range(2)]
        rf2 = [salloc(f"rf{i}", [128, 4], F32) for i in range(2)]
        OUTt = [salloc(f"OUT{i}", [128, D], F32) for i in range(2)]

        def body3(ti):
            sl = ti % 2
            tl.pool = "E" if sl == 0 else "O"
            op, dma = stream(f"@{sl}")
            Pb, PTt, X2, tmp, hTc, GP, X3, rf = Pb2[sl], PTt2[sl], X22[sl], tmp2[sl], hTc2[sl], GP2[sl], X32[sl], rf2[sl]
            x1k, y1k, y2k, pk = f"X1c{sl}", f"Y1_{sl}", f"Y2_{sl}", f"Pin{sl}"
            dma("sp", lambda e: e.dma_start(out=X1c[sl][:], in_=x1_d[ti * 128:(ti + 1) * 128, :]),
                r=([f"x1_d{ti}"] if "1b" in PH else []), w=[x1k], key=x1k)
            dma("sp", lambda e: e.dma_start(out=Pin[sl][:], in_=p_d[ti * 128:(ti + 1) * 128, :]), w=[pk], key=pk)
            for (Yt, ykk, j) in ((Y1[sl], y1k, 0), (Y2[sl], y2k, 1)):
                dma("pool", lambda e: e.indirect_dma_start(
                    out=Yt[:, :], out_offset=None, in_=ys_d[:, :],
                    in_offset=bass.IndirectOffsetOnAxis(ap=slot_i[:, 2 * ti + j:2 * ti + j + 1], axis=0)),
                    r=(["ys_all", f"slot{ti}"] if "2" in PH else []), w=[ykk], key=ykk)
            op("dve", lambda e: e.scalar_tensor_tensor(out=X2[:], in0=Y1[sl][:], scalar=rw_f[:, 2 * ti:2 * ti + 1],
                                                       in1=X1c[sl][:], op0=ALU.mult, op1=ALU.add),
               r=[y1k, x1k, f"rw{ti}a"], w=["X2"])
            op("dve", lambda e: e.scalar_tensor_tensor(out=X2[:], in0=Y2[sl][:], scalar=rw_f[:, 2 * ti + 1:2 * ti + 2],
                                                       in1=X2[:], op0=ALU.mult, op1=ALU.add),
               r=[y2k, "X2", f"rw{ti}b"], w=["X2"])
            rms_to_hT(ti, X2, "X2", hTc, "hTc", tmp, normalize=False, op=op)
            op("pool", lambda e: e.tensor_copy(out=Pb[:], in_=Pin[sl][:]), r=[pk], w=["Pb"])
            pb = PSB()
            for kc in range(2):
                op("pe", lambda e: e.transpose(pb[0][:, kc * 128:(kc + 1) * 128], Pb[:, kc * 128:(kc + 1) * 128],
                                               identb[:]), r=["Pb", "identb"], w=[pb[1]])
            op("act", lambda e: e.activation(out=PTt[:].rearrange("p a b -> p (a b)"), in_=pb[0][:, 0:256],
                                             func=AF.Copy), r=[pb[1]], w=["PTt"])
            for half in range(2):
                pgm = PS()
                for kc in range(8):
                    op("pe", lambda e: e.matmul(pgm[0][:, 0:512], hTc[:, kc, :], Wpg[:, kc, half * 512:(half + 1) * 512],
                                                start=(kc == 0), stop=(kc == 7)), r=["hTc", "Wpg"], w=[pgm[1]])
                op("act", lambda e: e.activation(out=GP[:, half * 512:(half + 1) * 512], in_=pgm[0][:, 0:512],
                                                 func=AF.Sigmoid, scale=tmp["rstd"][:, 0:1]),
                   r=[pgm[1], "rstd"], w=[f"GP{half}"])
                ppm = PS()
                for kc in range(2):
                    op("pe", lambda e: e.matmul(ppm[0][:, 0:512], PTt[:, kc, :], Wpp[:, kc, half * 512:(half + 1) * 512],
                                                start=(kc == 0), stop=(kc == 1)), r=["PTt", "Wpp"], w=[ppm[1]])
                op("dve", lambda e: e.tensor_tensor(out=GP[:, half * 512:(half + 1) * 512], in0=ppm[0][:, 0:512],
                                                    in1=GP[:, half * 512:(half + 1) * 512], op=ALU.mult),
                   r=[ppm[1], f"GP{half}"], w=[f"GP{half}"])
                op("pool", lambda e: e.tensor_tensor(out=X3[:, half * 512:(half + 1) * 512],
                                                     in0=X2[:, half * 512:(half + 1) * 512],
                                                     in1=GP[:, half * 512:(half + 1) * 512], op=ALU.add),
                   r=["X2", f"GP{half}"], w=[f"X3{half}"])
            op("act", lambda e: e.activation(out=tmp["junk"][:], in_=X3[:], func=AF.Square, accum_out=rf[:, 0:1]),
               r=["X30", "X31"], w=["junk", "rf0"])
            op("dve", lambda e: e.tensor_scalar(out=rf[:, 1:2], in0=rf[:, 0:1], scalar1=1.0 / D, scalar2=1e-6,
                                                op0=ALU.mult, op1=ALU.add), r=["rf0"], w=["rf1"])
            op("act", lambda e: e.activation(out=rf[:, 2:3], in_=rf[:, 1:2], func=AF.Sqrt), r=["rf1"], w=["rf2"])
            op("dve", lambda e: e.reciprocal(out=rf[:, 3:4], in_=rf[:, 2:3]), r=["rf2"], w=["rf3"])
            ok = f"OUT{sl}"
            op("dve", lambda e: e.scalar_tensor_tensor(out=OUTt[sl][:], in0=X3[:], scalar=rf[:, 3:4], in1=LNF[:],
                                                       op0=ALU.mult, op1=ALU.mult), r=["X30", "X31", "rf3", "LNF"],
               w=[ok])
            dma("sp", lambda e: e.dma_start(out=out_d[ti * 128:(ti + 1) * 128, :], in_=OUTt[sl][:]),
                r=[ok], w=[f"out_d{ti}"], key=ok)

        for t0 in range(0, NTILE, 2):
            WV.run([lambda t0=t0: body3(t0), lambda t0=t0: body3(t0 + 1)], [1, 1], seq=not cfg.get("weave23", False))
        tl.pool = None

    S_.op("sp", nop_fns["sp"], r=[], w=[])
    return nc, S_


def emit(nc, S_):
    pref = S_.finish(nc, None, None, None)
    import contextlib
    with contextlib.ExitStack() as es:
        sems = {e: es.enter_context(nc.semaphore("s_" + e)) for e in ENGS}
        dsem = {k: es.enter_context(nc.semaphore("d_" + str(i))) for i, k in enumerate(S_.dma_cnt)}
        bsem = [es.enter_context(nc.semaphore(f"bar{i}")) for i in range(2)]
        block = es.enter_context(nc.Block())

        def run(ename, eh):
            for o in S_.ops[ename]:
                for (key, val) in o["waits"]:
                    if key[0] == "eng":
                        eh.wait_ge(sems[key[1]], pref[key[1]][val])
                    else:
                        eh.wait_ge(dsem[key[1]], val)
                ins = o["fn"](eh)
                if o.get("bar"):
                    nb = o["bar"]
                    ins.then_inc(bsem[nb % 2], 1)
                    eh.wait_ge(bsem[nb % 2], len(ENGS) * ((nb + 1) // 2 if nb % 2 else nb // 2))
                    continue
                if o["dma"] is not None:
                    ins.then_inc(dsem[o["dma"]], 16)
                elif o["inc"]:
                    ins.then_inc(sems[ename], 1)
            if ename == "sp":
                for k, c in S_.dma_cnt.items():
                    eh.wait_ge(dsem[k], c)

        @block.tensor
        def _(t):
            run("pe", t)

        @block.scalar
        def _(a):
            run("act", a)

        @block.vector
        def _(v):
            run("dve", v)

        @block.gpsimd
        def _(g):
            run("pool", g)

        @block.sync
        def _(s):
            run("sp", s)
    return nc


def _perm_q():
    idx = []
    for c in range(4):
        idx += list(range(c * 64, c * 64 + 64)) + list(range((c + 4) * 64, (c + 4) * 64 + 64))
    return np.array(idx)


def _consts(cap):
    c = np.zeros((128, 832), np.float32)
    c[:, 768:800] = (np.arange(32) * cap)[None, :]
    i = np.arange(128)
    c[:, 0:128] = np.eye(128)
    c[:, 128:256] = (i[:, None] < i[None, :])
    c[:, 256:384] = (i[:, None] <= i[None, :])
    c[:, 384:512] = (i[:, None] > i[None, :])
    invf = (10000.0 ** (-np.arange(32, dtype=np.float64) / 32.0)) / (2 * np.pi)
    c[:, 512:544] = invf[None, :]
    c[:, 544:576] = invf[None, :]
    c[:, 576:608] = 0.0
    c[:, 608:640] = 0.25
    c[:, 640:768] = ((i[:, None] // 64) == (i[None, :] // 64))
    return c


def prep_shared(inp, cap):
    f = lambda a: np.ascontiguousarray(np.asarray(a, dtype=np.float32))
    pq = _perm_q()
    w_in = f(inp["w_in"][0])
    cols = np.concatenate([pq, np.arange(512, 4608)])
    w_in = np.ascontiguousarray(w_in[:, cols])
    vec = np.zeros((128, 70), np.float32)
    vec[:, 0:8] = f(inp["ln_mix"][0]).reshape(8, 128).T
    vec[:, 8:16] = f(inp["ln_moe"][0]).reshape(8, 128).T
    vec[:, 16:24] = f(inp["ln_ple"][0]).reshape(8, 128).T
    vec[:, 24:38] = f(inp["mu_shift"][0]).reshape(14, 128).T
    for j, nm in enumerate(["w0", "a0", "k_k", "k_a", "r_k", "ln_x_w", "ln_x_b"]):
        vec[:, 38 + 4 * j:42 + 4 * j] = f(inp[nm][0]).reshape(4, 128).T
    sk = f(inp["sinks"][0])
    for c in range(4):
        vec[0:64, 66 + c] = sk[c]
        vec[64:128, 66 + c] = sk[c + 4]
    sh = dict(
        w_in=w_in, vecs=vec, cst=_consts(cap), ln_moe_row=f(inp['ln_moe'][0])[None, :],
        wlora=np.ascontiguousarray(np.concatenate([f(inp["w_decay_up"][0]), f(inp["w_aaa_up"][0])], 0)),
        wgu=f(inp["w_gate_up"][0]),
        w_ba=np.ascontiguousarray(f(inp["w_branch_att"][0])[pq, :]),
        w_bb=f(inp["w_branch_rwkv"][0]),
        w_out=f(inp["w_out"][0]),
        w_r=np.ascontiguousarray(np.concatenate([f(inp["w_group"][0]), f(inp["w_expert"][0])], 1)),
        b_r=np.ascontiguousarray(np.concatenate([f(inp["b_group"][0]), f(inp["b_expert"][0])])[None, :]),
        w_gate_e=f(inp["w_gate_e"][0]), w_up_e=f(inp["w_up_e"][0]), w_down_e=f(inp["w_down_e"][0]),
        w_pg=f(inp["w_ple_gate"][0]), w_pp=f(inp["w_ple_proj"][0]),
        ln_final=f(inp["ln_final"])[None, :],
    )
    return sh


def prep_core(inp, sh, b0, nseq):
    x = np.asarray(inp["x"], np.float32)[b0:b0 + nseq]
    S = x.shape[1]
    m = dict(sh)
    m["x"] = np.ascontiguousarray(x.reshape(nseq * S, D))
    m["p"] = np.ascontiguousarray(np.asarray(inp["p"], np.float32)[0, b0:b0 + nseq].reshape(nseq * S, 256))
    pos = np.asarray(inp["positions"], np.int32)[b0:b0 + nseq].reshape(-1)
    m["posT"] = np.ascontiguousarray(pos.reshape(-1, 128).T)
    return m


FULL_CFG = dict(NSEQ=4, S=2048, CAP=640)


def kernel(**inputs):
    cfg = FULL_CFG
    nc, S_ = build(cfg)
    emit(nc, S_)
    sh = prep_shared(inputs, cfg["CAP"])
    in_maps = [prep_core(inputs, sh, c * cfg["NSEQ"], cfg["NSEQ"]) for c in range(8)]
    res = run_bass_kernel_spmd(nc, in_maps, core_ids=list(range(8)))
    outs = [r["out"].reshape(cfg["NSEQ"], cfg["S"], D) for r in res.results]
    return np.concatenate(outs, 0).astype(np.float32)
```

```python
import numpy as np
import ml_dtypes
import concourse.bass as bass
import concourse.mybir as mybir
from concourse.bass_utils import run_bass_kernel_spmd

F32 = mybir.dt.float32
BF16 = mybir.dt.bfloat16
I32 = mybir.dt.int32
AF = mybir.ActivationFunctionType
ALU = mybir.AluOpType
AX = mybir.AxisListType

D = 1024
NE = 32
DE = 512
ENGS = ("pe", "act", "dve", "pool", "sp")
SYNC_SAME = ("act", "dve", "pool")


class _Rec:
    def __init__(self):
        self.call = None

    def __getattr__(self, name):
        def f(*a, **k):
            self.call = (name, a, k)
            return self
        return f


def _bind(fn):
    r = _Rec()
    fn(r)
    name, a, k = r.call
    return lambda e: getattr(e, name)(*a, **k)


import threading


class Weaver:
    def __init__(self):
        self.active = False

    def tick(self):
        if not self.active or threading.current_thread() is not self.cur_thread():
            return
        i = self.cur
        self.count[i] += 1
        if self.count[i] >= self.quota[i]:
            self.count[i] = 0
            self._handoff(i)

    def cur_thread(self):
        return self.threads[self.cur]

    def _next_live(self, i):
        n = len(self.threads)
        for d in range(1, n + 1):
            j = (i + d) % n
            if not self.done[j]:
                return j
        return None

    def _handoff(self, i):
        j = self._next_live(i)
        if j is None or j == i:
            return
        self.cur = j
        self.sems[j].release()
        self.sems[i].acquire()

    def run(self, fns, quota, seq=False):
        n = len(fns)
        if n == 1 or seq:
            for f in fns:
                f()
            return
        self.sems = [threading.Semaphore(0) for _ in range(n)]
        self.done = [False] * n
        self.count = [0] * n
        self.quota = list(quota)
        self.err = None
        fin = threading.Semaphore(0)

        def wrap(i):
            self.sems[i].acquire()
            try:
                fns[i]()
            except BaseException as ex:
                self.err = ex
            self.done[i] = True
            j = self._next_live(i)
            if j is None:
                fin.release()
            else:
                self.cur = j
                self.sems[j].release()

        self.threads = [threading.Thread(target=wrap, args=(i,)) for i in range(n)]
        for t in self.threads:
            t.start()
        self.active = True
        self.cur = 0
        self.sems[0].release()
        fin.acquire()
        self.active = False
        for t in self.threads:
            t.join()
        if self.err is not None:
            raise self.err


class Sched:
    def __init__(self):
        self.ops = {e: [] for e in ENGS}
        self.last_w = {}
        self.readers = {}
        self.waited = {e: {} for e in ENGS}
        self.dma_cnt = {}

    def _deps(self, eng, reads, writes):
        deps = []
        for r in reads:
            if r in self.last_w:
                deps.append((self.last_w[r], True))
        for w in writes:
            if w in self.last_w:
                deps.append((self.last_w[w], False))
            for t in self.readers.get(w, {}).values():
                deps.append((t, False))
        waits = []
        for d, raw in deps:
            if d[0] == "eng":
                if d[1] == eng and eng not in SYNC_SAME:
                    continue
                key = ("eng", d[1])
            else:
                key = ("dma", d[1])
            val = d[2]
            if self.waited[eng].get(key, -1) >= val:
                continue
            self.waited[eng][key] = val
            waits.append((key, val))
            if d[0] == "eng":
                self.ops[d[1]][val]["inc"] = True
        return waits

    def op(self, eng, fn, r=(), w=()):
        waits = self._deps(eng, r, w)
        idx = len(self.ops[eng])
        self.ops[eng].append(dict(fn=_bind(fn), waits=waits, inc=False, dma=None))
        tok = ("eng", eng, idx)
        for x in w:
            self.last_w[x] = tok
            self.readers[x] = {}
        for x in r:
            self.readers.setdefault(x, {})[("eng", eng)] = tok

    def dma(self, eng, fn, r=(), w=(), key=None):
        waits = self._deps(eng, r, w)
        prev = self.dma_cnt.get(key, 0)
        if prev and self.waited[eng].get(("dma", key), -1) < prev:
            self.waited[eng][("dma", key)] = prev
            waits.append((("dma", key), prev))
        cnt = prev + 16
        self.dma_cnt[key] = cnt
        self.ops[eng].append(dict(fn=_bind(fn), waits=waits, inc=False, dma=key))
        tok = ("dma", key, cnt)
        for x in w:
            self.last_w[x] = tok
            self.readers[x] = {}
        for x in r:
            self.readers.setdefault(x, {})[("dma", key)] = tok

    def barrier(self, nop_fns):
        self.nbar = getattr(self, "nbar", 0) + 1
        for e in ENGS:
            waits = []
            if e != "sp" and self.ops[e]:
                last = len(self.ops[e]) - 1
                while last >= 0 and (self.ops[e][last].get("bar") or self.ops[e][last]["dma"] is not None):
                    last -= 1
                if last >= 0:
                    self.ops[e][last]["inc"] = True
                    waits.append((("eng", e), last))
            if e == "sp":
                for k, c in self.dma_cnt.items():
                    if self.waited[e].get(("dma", k), -1) < c:
                        self.waited[e][("dma", k)] = c
                        waits.append((("dma", k), c))
            self.ops[e].append(dict(fn=nop_fns[e], waits=waits, inc=False, dma=None, bar=self.nbar))
        self.last_w = {}
        self.readers = {}

    def finish(self, nc, engines, sems, dma_sems):
        pref = {}
        for e in ENGS:
            c = 0
            arr = []
            for o in self.ops[e]:
                if o["inc"] and o["dma"] is None and not o.get("bar"):
                    c += 1
                arr.append(c)
            pref[e] = arr
        return pref


def build(cfg, debug=False):
    NSEQ, S, CAP = cfg["NSEQ"], cfg["S"], cfg["CAP"]
    TPS = S // 128
    NTILE = NSEQ * TPS
    TPC = NTILE * 128
    NSLOT = NE * CAP
    NST = NSLOT // 128
    CT = CAP // 128
    PH = cfg.get("phases", "1a,1b,2,3").split(",")
    CUT = cfg.get("cut", 99)

    nc = bass.Bass("TRN2", target_bir_lowering=False)
    dr = {}

    def din(name, shape, dt=F32):
        dr[name] = nc.dram_tensor(name, list(shape), dt, kind="ExternalInput").ap()
        return dr[name]

    def dscr(name, shape, dt=F32, out=False):
        dr[name] = nc.dram_tensor(name, list(shape), dt, kind=("ExternalOutput" if out else "Internal")).ap()
        return dr[name]

    x_d = din("x", [TPC, D])
    p_d = din("p", [TPC, 256])
    pos_d = din("posT", [128, NTILE], I32)
    win_d = din("w_in", [D, 4608])
    vec_d = din("vecs", [128, 70])
    cst_d = din("cst", [128, 832])
    wlora_d = din("wlora", [128, 512])
    wgu_d = din("wgu", [128, 512])
    wba_d = din("w_ba", [512, D])
    wbb_d = din("w_bb", [512, D])
    wout_d = din("w_out", [D, D])
    wr_d = din("w_r", [D, 36])
    br_d = din("b_r", [1, 36])
    wg_d = din("w_gate_e", [NE, D, DE])
    wu_d = din("w_up_e", [NE, D, DE])
    wd_d = din("w_down_e", [NE, DE, D])
    wpg_d = din("w_pg", [D, D])
    wpp_d = din("w_pp", [256, D])
    lnf_d = din("ln_final", [1, D])
    lnmoe_d = din("ln_moe_row", [1, D])
    out_d = dscr("out", [TPC, D], F32, out=True)
    yab_d = dscr("yab", [NTILE, 128, 8 * 128], BF16, out=debug)
    x1_d = dscr("x1s", [TPC, D], F32, out=debug)
    hs_d = dscr("hslots", [NSLOT, D], BF16, out=debug)
    ys_d = dscr("yslots", [NSLOT, D], F32, out=debug)
    if debug:
        rt_d = dscr("route", [128, NTILE * 4], F32, out=True)

    S_ = Sched()
    base0 = 229376 - int(nc.sbuf_bytes_remaining)
    base0 = (base0 + 63) // 64 * 64
    st = {"p": base0, "ph": None}

    def salloc(name, shape, dt):
        nb = int(np.prod(shape[1:])) * (4 if dt in (F32, I32) else 2)
        nb = (nb + 31) // 32 * 32
        t = nc.alloc_sbuf_tensor_at(name, list(shape), dt, offset=st["p"])
        st["p"] += nb
        assert st["p"] <= 229376 - 64, ("SBUF overflow", name, st["p"])
        return t

    psb = [nc.alloc_psum_tensor(f"psb{i}", [128, 1024], BF16) for i in range(2)]
    psf = [nc.alloc_psum_tensor(f"psf{i}", [128, 512], F32) for i in range(6)]
    rr = {"f": 0, "b": 0, "fA": 0, "fB": 0}
    tl = threading.local()

    def PS():
        pool = getattr(tl, "pool", None)
        if pool == "A":
            i = rr["fA"] % 2
            rr["fA"] += 1
        elif pool == "B":
            i = 2 + rr["fB"] % 4
            rr["fB"] += 1
        elif pool == "E":
            i = rr["fA"] % 3
            rr["fA"] += 1
        elif pool == "O":
            i = 3 + rr["fB"] % 3
            rr["fB"] += 1
        else:
            i = rr["f"] % 6
            rr["f"] += 1
        return psf[i], f"psf{i}"

    def PSB():
        pool = getattr(tl, "pool", None)
        if pool in ("A", "E"):
            i = 0
        elif pool in ("B", "O"):
            i = 1
        else:
            i = rr["b"] % 2
            rr["b"] += 1
        return psb[i], f"psb{i}"

    vec = salloc("vec", [128, 70], F32)
    cst = salloc("cstf", [128, 832], F32)
    identb = salloc("identb", [128, 128], BF16)
    bones = salloc("bones", [128, 128], BF16)
    onesb = salloc("onesb", [128, 128], BF16)
    onesf = salloc("onesf", [128, 128], F32)
    derived = salloc("derived", [128, 32], F32)
    posi = salloc("posi", [128, NTILE], I32)
    posf = salloc("posf", [128, NTILE], F32)
    slot_i = salloc("slot_i", [128, NTILE * 2], I32)
    rw_f = salloc("rw_f", [128, NTILE * 2], F32)
    scr = salloc("scr", [128, 64], F32)
    ph_base = st["p"]
    IDENTF = cst[:, 0:128]
    USTR = cst[:, 128:256]
    UINC = cst[:, 256:384]
    LSTR = cst[:, 384:512]
    INVF = cst[:, 512:576]
    OFFS = cst[:, 576:640]
    MASK2 = cst[:, 128:384]
    V_LNMIX, V_LNMOE, V_LNPLE, V_MU = 0, 8, 16, 24
    V_W0, V_A0, V_KK, V_KA, V_RK, V_LNW, V_LNB, V_SINK = 38, 42, 46, 50, 54, 58, 62, 66

    def bc(ap, shape):
        return ap.to_broadcast(list(shape))

    def vcol(c, n=1):
        return vec[:, c:c + n]

    WV = Weaver()
    GLOBAL_KEYS = {"vec", "cst", "identb", "bones", "onesb", "onesf", "derived", "posf", "posi",
                   "Wgt", "WbA", "WbB", "Wout", "Wr", "BR", "GMOE", "ustrb", "CNT", "hs_all", "ys_all",
                   "Wpg", "Wpp", "LNF", "WG0", "WG1", "WU0", "WU1", "WD0", "WD1"}
    GLOBAL_PREF = ("psf", "psb", "yab_d", "x1_d", "slot", "rw", "hs_sc", "out_d")

    def stream(sfx):
        def m(keys):
            return [k if (k in GLOBAL_KEYS or k.startswith(GLOBAL_PREF)) else k + sfx for k in keys]

        def op_(eng, fn, r=(), w=()):
            op(eng, fn, m(r), m(w))

        def dma_(eng, fn, r=(), w=(), key=None):
            dma(eng, fn, m(r), m(w), key)
        return op_, dma_

    def op(eng, fn, r=(), w=()):
        S_.op(eng, fn, r, w)
        WV.tick()
    op_glob = op

    def dma(eng, fn, r=(), w=(), key=None):
        S_.dma(eng, fn, r, w, key)
        WV.tick()

    dma("sp", lambda e: e.dma_start(out=vec[:], in_=vec_d), w=["vec"], key="vec")
    dma("sp", lambda e: e.dma_start(out=cst[:], in_=cst_d), w=["cst"], key="cst")
    dma("sp", lambda e: e.dma_start(out=posi[:], in_=pos_d), w=["posi"], key="posi")
    op("dve", lambda e: e.tensor_copy(out=identb[:], in_=IDENTF), r=["cst"], w=["identb"])
    op("dve", lambda e: e.tensor_copy(out=bones[:], in_=cst[:, 640:768]), r=["cst"], w=["bones"])
    op("dve", lambda e: e.memset(onesb[:], 1.0), w=["onesb"])
    op("dve", lambda e: e.memset(onesf[:], 1.0), w=["onesf"])
    op("dve", lambda e: e.tensor_copy(out=posf[:], in_=posi[:]), r=["posi"], w=["posf"])
    op("dve", lambda e: e.tensor_scalar(out=derived[:, 0:14], in0=vcol(V_MU, 14), scalar1=-1.0, scalar2=1.0,
                                        op0=ALU.mult, op1=ALU.add), r=["vec"], w=["derived"])
    op("dve", lambda e: e.tensor_scalar(out=derived[:, 14:18], in0=vcol(V_KA, 4), scalar1=-1.0, scalar2=1.0,
                                        op0=ALU.mult, op1=ALU.add), r=["vec"], w=["derived"])
    op("act", lambda e: e.activation(out=derived[:, 18:22], in_=vcol(V_SINK, 4), func=AF.Exp), r=["vec", "derived"],
       w=["derived"])
    OMU = lambda c, n=1: derived[:, c:c + n]
    OMKA = lambda c, n=1: derived[:, 14 + c:14 + c + n]
    ESINK = derived[:, 18:22]

    nop_fns = {
        "pe": lambda e: e.nop(), "act": lambda e: e.nop(), "dve": lambda e: e.nop(),
        "pool": lambda e: e.nop(), "sp": lambda e: e.nop(),
    }

    def phase_reset():
        S_.barrier(nop_fns)
        st["p"] = ph_base

    def load_cast_weight(dst, dst_key, src_ap, rows, cols, gcol, stage, stage_key, kchunks, eng_cycle):
        for kc in range(kchunks):
            sl = stage[kc % len(stage)]
            sk = stage_key[kc % len(stage)]
            dma("sp", lambda e, sl=sl, kc=kc: e.dma_start(out=sl[:, 0:cols], in_=src_ap[kc * 128:(kc + 1) * 128, :]),
                w=[sk], key=sk)
            eng = eng_cycle[kc % len(eng_cycle)]
            if gcol is None:
                op(eng, lambda e, sl=sl, kc=kc: e.tensor_copy(out=dst[:, kc, :], in_=sl[:, 0:cols]),
                   r=[sk], w=[dst_key])
            else:
                op(eng, lambda e, sl=sl, kc=kc: e.tensor_scalar(out=dst[:, kc, :], in0=sl[:, 0:cols],
                                                                 scalar1=vcol(gcol + kc), scalar2=None, op0=ALU.mult),
                   r=[sk, "vec"], w=[dst_key])

    def rms_to_hT(ti, xin, xin_key, hT, hT_key, tmp, normalize=True, op=None):
        op = op or op_glob
        junk, ss, rstd, xbf = tmp["junk"], tmp["ss"], tmp["rstd"], tmp["xbf"]
        op("act", lambda e: e.activation(out=junk[:], in_=xin[:], func=AF.Square, accum_out=ss[:, 0:1]),
           r=[xin_key], w=["junk", "ss"])
        op("dve", lambda e: e.tensor_scalar(out=ss[:, 1:2], in0=ss[:, 0:1], scalar1=1.0 / D, scalar2=1e-6,
                                            op0=ALU.mult, op1=ALU.add), r=["ss"], w=["ss1"])
        op("act", lambda e: e.activation(out=ss[:, 2:3], in_=ss[:, 1:2], func=AF.Sqrt), r=["ss1"], w=["ss2"])
        op("dve", lambda e: e.reciprocal(out=rstd[:, 0:1], in_=ss[:, 2:3]), r=["ss2"], w=["rstd"])
        if normalize:
            op("dve", lambda e: e.tensor_scalar(out=xbf[:], in0=xin[:], scalar1=rstd[:, 0:1], scalar2=None,
                                                op0=ALU.mult), r=[xin_key, "rstd"], w=["xbf"])
        else:
            op("pool", lambda e: e.tensor_copy(out=xbf[:], in_=xin[:]), r=[xin_key], w=["xbf"])
        pb, pk = PSB()
        for kc in range(8):
            op("pe", lambda e, kc=kc: e.transpose(pb[:, kc * 128:(kc + 1) * 128], xbf[:, kc * 128:(kc + 1) * 128],
                                                  identb[:]), r=["xbf", "identb"], w=[pk])
        op("act", lambda e: e.activation(out=hT[:].rearrange("p k t -> p (k t)"), in_=pb[:, :], func=AF.Copy),
           r=[pk], w=[hT_key])

    if "1a" in PH:
        Wqkv = salloc("Wqkv", [128, 8, 768], BF16)
        Wrw = salloc("Wrw", [128, 8, 1792], BF16)
        Wlora = salloc("Wlora", [128, 512], BF16)
        Wgu = salloc("Wgu", [128, 512], BF16)
        stg = [salloc("stgA", [128, 2560], F32)]
        for kc in range(8):
            dma("sp", lambda e, kc=kc: e.dma_start(out=stg[0][:, 0:2560], in_=win_d[kc * 128:(kc + 1) * 128, 0:2560]),
                w=["stgA"], key="stgA")
            op("dve", lambda e, kc=kc: e.tensor_scalar(out=Wqkv[:, kc, :], in0=stg[0][:, 0:768],
                                                       scalar1=vcol(V_LNMIX + kc), scalar2=None, op0=ALU.mult),
               r=["stgA", "vec"], w=["Wqkv"])
            op("pool", lambda e, kc=kc: e.tensor_scalar(out=Wrw[:, kc, :], in0=stg[0][:, 768:2560],
                                                        scalar1=vcol(V_LNMIX + kc), scalar2=1.0, op0=ALU.mult,
                                                        op1=ALU.mult),
               r=["stgA", "vec"], w=["Wrw"])
        dma("sp", lambda e: e.dma_start(out=stg[0][:, 0:512], in_=wlora_d), w=["stgA"], key="stgA")
        op("dve", lambda e: e.tensor_copy(out=Wlora[:], in_=stg[0][:, 0:512]), r=["stgA"], w=["Wlora"])
        dma("sp", lambda e: e.dma_start(out=stg[0][:, 512:1024], in_=wgu_d), w=["stgA"], key="stgA")
        op("dve", lambda e: e.tensor_copy(out=Wgu[:], in_=stg[0][:, 512:1024]), r=["stgA"], w=["Wgu"])

        xin = [salloc(f"xin{i}", [128, D], F32) for i in range(2)]
        tmp = dict(junk=salloc("junk", [128, D], BF16), ss=salloc("ss", [128, 4], F32),
                   rstd=salloc("rstd", [128, 1], F32), xbf=salloc("xbf", [128, D], BF16))
        hT = [salloc(f"hT{i}", [128, 8, 128], BF16) for i in range(2)]
        ropeT = salloc("ropeT", [128, 64], F32)
        ropeN = salloc("ropeN", [128, 64], I32)
        ropeF = salloc("ropeF", [128, 64], F32)
        ropeG = salloc("ropeG", [128, 64], F32)
        CS = salloc("CS", [128, 64], F32)
        ropA = salloc("ropA", [128, 640], F32)
        ropB = salloc("ropB", [128, 640], F32)
        qkr = salloc("qkr", [128, 640], BF16)
        qT = salloc("qT", [128, 4, 128], BF16)
        kTs = [salloc(f"kT{i}", [128, 128], BF16) for i in range(2)]
        vts = [salloc(f"vtok{i}", [128, 128], BF16) for i in range(2)]
        Eb = [salloc(f"Eb{i}", [128, 512], BF16) for i in range(4)]
        dent = salloc("dent", [128, 4, 128], F32)
        yab = [salloc(f"yabs{i}", [128, 8, 128], BF16) for i in range(2)]
        zb = [salloc(f"zb{i}", [128, 4, 129], F32) for i in range(2)]
        zt1 = salloc("zt1", [128, 4, 128], F32)
        zt2 = salloc("zt2", [128, 4, 128], F32)
        carry = salloc("carry", [128, 16], F32)
        Rr = salloc("Rr", [128, 4, 128], F32)
        Kr = salloc("Kr", [128, 4, 128], F32)
        Vr = salloc("Vr", [128, 4, 128], F32)
        XM = salloc("XM", [128, 2, 128], F32)
        LIN = salloc("LIN", [128, 128], BF16)
        SXG = salloc("SXG", [128, 128], BF16)
        SG = salloc("SG", [128, 4, 128], F32)
        Aa = salloc("Aa", [128, 4, 128], F32)
        Gg = salloc("Gg", [128, 4, 128], F32)
        LW = salloc("LW", [128, 4, 128], F32)
        CUM = salloc("CUM", [128, 4, 128], F32)
        CX = salloc("CX", [128, 4, 128], F32)
        E1 = salloc("E1", [128, 4, 128], F32)
        E2 = salloc("E2", [128, 4, 128], F32)
        E3 = salloc("E3", [128, 4, 128], F32)
        E4 = salloc("E4", [128, 4, 128], F32)
        KKR = salloc("KKR", [128, 4, 128], F32)
        SQb = salloc("SQb", [128, 4, 128], BF16)
        RN = salloc("RN", [128, 4, 128], F32)
        KK = salloc("KK", [128, 4, 128], F32)
        T1 = salloc("T1", [128, 4, 128], F32)
        KP = salloc("KP", [128, 4, 128], F32)
        Bb = salloc("Bb", [128, 4, 128], F32)
        AR = salloc("AR", [128, 4, 2, 128], BF16)
        BK = salloc("BK", [128, 4, 2, 128], BF16)
        BKS = salloc("BKS", [128, 4, 2, 128], BF16)
        ARm = [salloc(f"ARm{i}", [128, 4, 2, 128], BF16) for i in range(2)]
        BKm = [salloc(f"BKm{i}", [128, 4, 2, 128], BF16) for i in range(2)]
        RK = salloc("RK", [128, 4, 128], F32)
        RK2 = salloc("RK2", [128, 4, 128], BF16)
        BV = salloc("BV", [128, 4, 128], F32)
        VB = salloc("VB", [128, 4, 128], BF16)
        BKT = salloc("BKT", [128, 1024], BF16)
        VT = salloc("VT", [128, 512], BF16)
        MA = salloc("MA", [128, 8, 256], BF16)
        MB = salloc("MB", [128, 8, 256], BF16)
        Qm = [salloc(f"Qm{i}", [128, 8, 128], BF16) for i in range(2)]
        PX = [salloc(f"PX{i}", [128, 8, 256], BF16) for i in range(2)]
        XF = salloc("XF", [128, 8, 128], BF16)
        SF = salloc("SF", [128, 4, 64], F32)
        SBs = salloc("SBs", [128, 4, 64], BF16)
        TMPS = salloc("TMPS", [128, 4, 64], F32)
        RH = salloc("RH", [128, 512], BF16)
        UT = salloc("UT", [128, 512], BF16)
        Yf = salloc("Yf", [128, 4, 128], F32)
        YB = salloc("YB", [128, 4, 128], BF16)
        YSQ = salloc("YSQ", [128, 4, 128], BF16)
        MEAN = salloc("MEAN", [128, 4, 128], F32)
        M2 = salloc("M2", [128, 4, 128], F32)
        VAR = salloc("VAR", [128, 4, 128], F32)
        Dd = salloc("Dd", [128, 4, 128], F32)

        def f2(t):
            return t[:].rearrange("p a b -> p (a b)")

        def genA(ti):
            tl.pool = "A"
            tj = ti % TPS
            sl = ti % 2
            xk = f"xin{sl}"
            dma("sp", lambda e, ti=ti, sl=sl: e.dma_start(out=xin[sl][:], in_=x_d[ti * 128:(ti + 1) * 128, :]),
                w=[xk], key=xk)
            hk = f"hT{sl}"
            rms_to_hT(ti, xin[sl], xk, hT[sl], hk, tmp)
            h = hT[sl]
            if CUT <= 1:
                return
            pq, pqk = PS()
            pkv, pkvk = PS()
            for kc in range(8):
                op("pe", lambda e, kc=kc: e.matmul(pq[:, 0:512], h[:, kc, :], Wqkv[:, kc, 0:512],
                                                   start=(kc == 0), stop=(kc == 7)), r=[hk, "Wqkv"], w=[pqk])
            for kc in range(8):
                op("pe", lambda e, kc=kc: e.matmul(pkv[:, 0:256], h[:, kc, :], Wqkv[:, kc, 512:768],
                                                   start=(kc == 0), stop=(kc == 7)), r=[hk, "Wqkv"], w=[pkvk])
            op("dve", lambda e, ti=ti: e.scalar_tensor_tensor(out=ropeT[:], in0=INVF, scalar=posf[:, ti:ti + 1],
                                                              in1=OFFS, op0=ALU.mult, op1=ALU.add),
               r=["cst", "posf"], w=["ropeT"])
            op("dve", lambda e: e.tensor_copy(out=ropeN[:], in_=ropeT[:]), r=["ropeT"], w=["ropeN"])
            op("dve", lambda e: e.tensor_copy(out=ropeF[:], in_=ropeN[:]), r=["ropeN"], w=["ropeF"])
            op("dve", lambda e: e.tensor_tensor(out=ropeF[:], in0=ropeT[:], in1=ropeF[:], op=ALU.subtract),
               r=["ropeT", "ropeF"], w=["ropeF"])
            op("dve", lambda e: e.tensor_single_scalar(out=ropeG[:], in_=ropeF[:], scalar=0.5, op=ALU.is_gt),
               r=["ropeF"], w=["ropeG"])
            op("dve", lambda e: e.tensor_tensor(out=ropeF[:], in0=ropeF[:], in1=ropeG[:], op=ALU.subtract),
               r=["ropeF", "ropeG"], w=["ropeF"])
            op("act", lambda e: e.activation(out=CS[:], in_=ropeF[:], func=AF.Sin, scale=2.0 * np.pi),
               r=["ropeF"], w=["CS"])
            if CUT <= 2:
                return
            for (src, skey, c0, H) in ((pq, pqk, 0, 8), (pkv, pkvk, 512, 2)):
                W_ = H * 64
                s4 = src[:, 0:W_].rearrange("p (h t d) -> p h t d", h=H, t=2)
                A4 = ropA[:, c0:c0 + W_].rearrange("p (h t d) -> p h t d", h=H, t=2)
                B4 = ropB[:, c0:c0 + W_].rearrange("p (h t d) -> p h t d", h=H, t=2)
                O4 = qkr[:, c0:c0 + W_].rearrange("p (h t d) -> p h t d", h=H, t=2)
                cosb = CS[:, 32:64].unsqueeze(1).unsqueeze(1).to_broadcast([128, H, 2, 32])
                sinb = CS[:, 0:32].unsqueeze(1).to_broadcast([128, H, 32])
                op("dve", lambda e, s4=s4, A4=A4, cosb=cosb: e.tensor_tensor(out=A4, in0=s4, in1=cosb, op=ALU.mult),
                   r=[skey, "CS"], w=["ropA"])
                op("dve", lambda e, s4=s4, B4=B4, sinb=sinb: e.tensor_tensor(out=B4[:, :, 0, :], in0=s4[:, :, 1, :],
                                                                           in1=sinb, op=ALU.mult),
                   r=[skey, "CS"], w=["ropB"])
                op("dve", lambda e, s4=s4, B4=B4, sinb=sinb: e.tensor_tensor(out=B4[:, :, 1, :], in0=s4[:, :, 0, :],
                                                                           in1=sinb, op=ALU.mult),
                   r=[skey, "CS"], w=["ropB"])
                op("pool", lambda e, A4=A4, B4=B4, O4=O4: e.tensor_tensor(out=O4[:, :, 0, :], in0=A4[:, :, 0, :],
                                                                         in1=B4[:, :, 0, :], op=ALU.subtract),
                   r=["ropA", "ropB"], w=["qkr"])
                op("pool", lambda e, A4=A4, B4=B4, O4=O4: e.tensor_tensor(out=O4[:, :, 1, :], in0=A4[:, :, 1, :],
                                                                         in1=B4[:, :, 1, :], op=ALU.add),
                   r=["ropA", "ropB"], w=["qkr"])
            vk = f"vtok{sl}"
            kk_ = f"kT{sl}"
            op("act", lambda e, sl=sl: e.activation(out=vts[sl][:], in_=pkv[:, 128:256], func=AF.Copy),
               r=[pkvk], w=[vk])
            pb, pbk = PSB()
            for c in range(5):
                op("pe", lambda e, c=c: e.transpose(pb[:, c * 128:(c + 1) * 128], qkr[:, c * 128:(c + 1) * 128],
                                                    identb[:]), r=["qkr", "identb"], w=[pbk])
            op("act", lambda e: e.activation(out=f2(qT), in_=pb[:, 0:512], func=AF.Copy), r=[pbk], w=["qT"])
            op("act", lambda e, sl=sl: e.activation(out=kTs[sl][:], in_=pb[:, 512:640], func=AF.Copy),
               r=[pbk], w=[kk_])
            if CUT <= 3:
                return
            kbs = ([1 - sl] if tj > 0 else []) + [sl]
            ei = 0
            Euse = {}
            for g in range(2):
                for kb in kbs:
                    pe_, pek = PS()
                    op("pe", lambda e, g=g, kb=kb, pe_=pe_: e.matmul(
                        pe_[:, 0:512], kTs[kb][g * 64:(g + 1) * 64, :], qT[g * 64:(g + 1) * 64, :, :],
                        start=True, stop=True), r=[f"kT{kb}", "qT"], w=[pek])
                    Et = Eb[ei]
                    ek = f"Eb{ei}"
                    ei += 1
                    op("act", lambda e, Et=Et, pe_=pe_: e.activation(out=Et[:], in_=pe_[:, 0:512], func=AF.Exp,
                                                                    scale=0.125), r=[pek], w=[ek])
                    msk = UINC if kb == sl else LSTR
                    op("pool", lambda e, Et=Et, msk=msk: e.tensor_tensor(
                        out=Et[:].rearrange("p (c q) -> p c q", c=4), in0=Et[:].rearrange("p (c q) -> p c q", c=4),
                        in1=msk.unsqueeze(1).to_broadcast([128, 4, 128]), op=ALU.mult), r=[ek, "cst"], w=[ek])
                    Euse[(g, kb)] = (Et, ek)
            po, pok = PS()
            pd, pdk = PS()
            for g in range(2):
                for i, kb in enumerate(kbs):
                    Et, ek = Euse[(g, kb)]
                    op("pe", lambda e, g=g, kb=kb, Et=Et, i=i: e.matmul(
                        po[g * 64:(g + 1) * 64, 0:512], vts[kb][:, g * 64:(g + 1) * 64], Et[:],
                        start=(i == 0), stop=(i == len(kbs) - 1)), r=[f"vtok{kb}", ek], w=[pok])
                for i, kb in enumerate(kbs):
                    Et, ek = Euse[(g, kb)]
                    op("pe", lambda e, g=g, Et=Et, i=i: e.matmul(
                        pd[g * 64:(g + 1) * 64, 0:512], onesb[:, 0:64], Et[:],
                        start=(i == 0), stop=(i == len(kbs) - 1)), r=["onesb", ek], w=[pdk])
            ys = yab[sl]
            yk = f"yabs{sl}"
            op("dve", lambda e: e.tensor_tensor(out=dent[:], in0=pd[:, 0:512].rearrange("p (c q) -> p c q", c=4),
                                                in1=ESINK.unsqueeze(2).to_broadcast([128, 4, 128]), op=ALU.add),
               r=[pdk, "derived"], w=["dent"])
            op("dve", lambda e: e.reciprocal(out=dent[:], in_=dent[:]), r=["dent"], w=["dent"])
            op("dve", lambda e, ys=ys: e.tensor_tensor(out=ys[:, 0:4, :],
                                                       in0=po[:, 0:512].rearrange("p (c q) -> p c q", c=4),
                                                       in1=dent[:], op=ALU.mult), r=[pok, "dent"], w=[yk + "a"])


        def genBC(ti):
            tl.pool = "B"
            tj = ti % TPS
            sl = ti % 2
            hk = f"hT{sl}"
            h = hT[sl]
            ys = yab[sl]
            yk = f"yabs{sl}"
            if CUT <= 4:
                return
            if tj == 0:
                op("pool", lambda e: e.memset(carry[:], 0.0), w=["carry"])
                op("pool", lambda e: e.memset(SF[:], 0.0), w=["SF"])
                op("pool", lambda e: e.memset(SBs[:], 0.0), w=["SBs"])
            groups = [(0, 4, Rr, "Rr"), (4, 4, Kr, "Kr"), (8, 4, Vr, "Vr"), (12, 2, XM, "XM")]
            for gi, (z0, n, dst, dk) in enumerate(groups):
                pz, pzk = PS()
                for j in range(n):
                    zc = z0 + j
                    for kc in range(8):
                        op("pe", lambda e, j=j, zc=zc, kc=kc, pz=pz: e.matmul(
                            pz[:, j * 128:(j + 1) * 128], Wrw[:, kc, zc * 128:(zc + 1) * 128], h[:, kc, :],
                            start=(kc == 0), stop=(kc == 7)), r=[hk, "Wrw"], w=[pzk])
                zbt = zb[gi % 2]
                zk = f"zb{gi % 2}"
                op("act", lambda e, zbt=zbt, pz=pz, n=n: e.activation(
                    out=zbt[:, 0:n, 1:129], in_=pz[:, 0:n * 128].rearrange("p (c t) -> p c t", c=n), func=AF.Copy),
                   r=[pzk], w=[zk])
                op("pool", lambda e, zbt=zbt, z0=z0, n=n: e.tensor_copy(out=zbt[:, 0:n, 0], in_=carry[:, z0:z0 + n]),
                   r=["carry"], w=[zk])
                op("dve", lambda e, zbt=zbt, z0=z0, n=n: e.tensor_tensor(
                    out=zt1[:, 0:n, :], in0=zbt[:, 0:n, 0:128],
                    in1=vcol(V_MU + z0, n).unsqueeze(2).to_broadcast([128, n, 128]), op=ALU.mult),
                   r=[zk, "vec"], w=["zt1"])
                op("pool", lambda e, zbt=zbt, z0=z0, n=n: e.tensor_tensor(
                    out=zt2[:, 0:n, :], in0=zbt[:, 0:n, 1:129],
                    in1=OMU(z0, n).unsqueeze(2).to_broadcast([128, n, 128]), op=ALU.mult),
                   r=[zk, "derived"], w=["zt2"])
                op("dve", lambda e, dst=dst, n=n: e.tensor_tensor(out=dst[:, 0:n, :], in0=zt1[:, 0:n, :],
                                                                  in1=zt2[:, 0:n, :], op=ALU.add),
                   r=["zt1", "zt2"], w=[dk])
                op("pool", lambda e, zbt=zbt, z0=z0, n=n: e.tensor_copy(out=carry[:, z0:z0 + n], in_=zbt[:, 0:n, 128]),
                   r=[zk], w=["carry"])
            if CUT <= 5:
                return
            op("act", lambda e: e.activation(out=LIN[0:64, :], in_=XM[0:64, 0, :], func=AF.Tanh), r=["XM"], w=["LINa"])
            op("pool", lambda e: e.tensor_copy(out=LIN[64:128, :], in_=XM[64:128, 0, :]), r=["XM"], w=["LINb"])
            op("act", lambda e: e.activation(out=SXG[:], in_=XM[:, 1, :], func=AF.Sigmoid), r=["XM"], w=["SXG"])
            pu, puk = PS()
            pa, pak = PS()
            pg, pgk = PS()
            for cc in range(4):
                op("pe", lambda e, cc=cc: e.matmul(pu[:, cc * 128:(cc + 1) * 128], Wlora[0:64, cc * 128:(cc + 1) * 128],
                                                   LIN[0:64, :], start=True, stop=True),
                   r=["Wlora", "LINa"], w=[puk])
            for cc in range(4):
                op("pe", lambda e, cc=cc: e.matmul(pa[:, cc * 128:(cc + 1) * 128],
                                                   Wlora[64:128, cc * 128:(cc + 1) * 128],
                                                   LIN[64:128, :], start=True, stop=True),
                   r=["Wlora", "LINb"], w=[pak])
            for cc in range(4):
                op("pe", lambda e, cc=cc: e.matmul(pg[:, cc * 128:(cc + 1) * 128], Wgu[:, cc * 128:(cc + 1) * 128],
                                                   SXG[:], start=True, stop=True), r=["Wgu", "SXG"], w=[pgk])
            for cc in range(4):
                op("act", lambda e, cc=cc: e.activation(out=SG[:, cc, :], in_=pu[:, cc * 128:(cc + 1) * 128],
                                                        func=AF.Sigmoid, bias=vcol(V_W0 + cc)),
                   r=[puk, "vec"], w=["SG"])
            for cc in range(4):
                op("act", lambda e, cc=cc: e.activation(out=Aa[:, cc, :], in_=pa[:, cc * 128:(cc + 1) * 128],
                                                        func=AF.Sigmoid, bias=vcol(V_A0 + cc)),
                   r=[pak, "vec"], w=["Aa"])
            op("act", lambda e: e.activation(out=f2(Gg), in_=pg[:, 0:512], func=AF.Copy), r=[pgk], w=["Gg"])
            op("act", lambda e: e.activation(out=f2(LW), in_=f2(SG), func=AF.Copy, scale=-0.6065306597126334),
               r=["SG"], w=["LW"])
            for cc in range(4):
                op("dve", lambda e, cc=cc: e.tensor_tensor_scan(out=CUM[:, cc, :], data0=onesf[:], data1=LW[:, cc, :],
                                                                initial=0.0, op0=ALU.mult, op1=ALU.add),
                   r=["onesf", "LW"], w=["CUM"])
            op("pool", lambda e: e.tensor_tensor(out=f2(CX), in0=f2(CUM), in1=f2(LW), op=ALU.subtract),
               r=["CUM", "LW"], w=["CX"])
            op("act", lambda e: e.activation(out=f2(E1), in_=f2(CUM), func=AF.Exp), r=["CUM"], w=["E1"])
            op("act", lambda e: e.activation(out=f2(E2), in_=f2(CUM), func=AF.Exp, scale=-1.0), r=["CUM"], w=["E2"])
            op("act", lambda e: e.activation(out=f2(E3), in_=f2(CX), func=AF.Exp), r=["CX"], w=["E3"])
            for cc in range(4):
                op("act", lambda e, cc=cc: e.activation(out=E4[:, cc, :], in_=CUM[:, cc, :], func=AF.Exp, scale=-1.0,
                                                        bias=CUM[:, cc, 127:128]), r=["CUM"], w=["E4"])
            op("pool", lambda e: e.tensor_tensor(out=KKR[:], in0=Kr[:],
                                                 in1=vcol(V_KK, 4).unsqueeze(2).to_broadcast([128, 4, 128]),
                                                 op=ALU.mult), r=["Kr", "vec"], w=["KKR"])
            op("act", lambda e: e.activation(out=f2(SQb), in_=f2(KKR), func=AF.Square), r=["KKR"], w=["SQb"])
            pss, pssk = PS()
            op("pe", lambda e: e.matmul(pss[:, 0:512], bones[:], f2(SQb), start=True, stop=True),
               r=["bones", "SQb"], w=[pssk])
            op("act", lambda e: e.activation(out=f2(RN), in_=pss[:, 0:512], func=AF.Sqrt, bias=1e-24),
               r=[pssk], w=["RN"])
            op("dve", lambda e: e.reciprocal(out=f2(RN), in_=f2(RN)), r=["RN"], w=["RN"])
            op("pool", lambda e: e.tensor_tensor(out=f2(KK), in0=f2(KKR), in1=f2(RN), op=ALU.mult),
               r=["KKR", "RN"], w=["KK"])
            for cc in range(4):
                op("dve", lambda e, cc=cc: e.tensor_scalar(out=T1[:, cc, :], in0=Aa[:, cc, :],
                                                           scalar1=vcol(V_KA + cc), scalar2=OMKA(cc),
                                                           op0=ALU.mult, op1=ALU.add),
                   r=["Aa", "vec", "derived"], w=["T1"])
            op("pool", lambda e: e.tensor_tensor(out=f2(KP), in0=f2(Kr), in1=f2(T1), op=ALU.mult),
               r=["Kr", "T1"], w=["KP"])
            op("pool", lambda e: e.tensor_tensor(out=f2(Bb), in0=f2(KK), in1=f2(Aa), op=ALU.mult),
               r=["KK", "Aa"], w=["Bb"])
            op("dve", lambda e: e.scalar_tensor_tensor(out=AR[:, :, 0, :], in0=E3[:], scalar=-1.0, in1=KK[:],
                                                       op0=ALU.mult, op1=ALU.mult), r=["E3", "KK"], w=["AR"])
            op("pool", lambda e: e.tensor_tensor(out=AR[:, :, 1, :], in0=E1[:], in1=Rr[:], op=ALU.mult),
               r=["E1", "Rr", "AR"], w=["AR"])
            op("dve", lambda e: e.tensor_tensor(out=BK[:, :, 0, :], in0=E2[:], in1=Bb[:], op=ALU.mult),
               r=["E2", "Bb"], w=["BK"])
            op("pool", lambda e: e.tensor_tensor(out=BK[:, :, 1, :], in0=E2[:], in1=KP[:], op=ALU.mult),
               r=["E2", "KP", "BK"], w=["BK"])
            op("dve", lambda e: e.tensor_tensor(out=BKS[:, :, 0, :], in0=E4[:], in1=Bb[:], op=ALU.mult),
               r=["E4", "Bb"], w=["BKS"])
            op("pool", lambda e: e.tensor_tensor(out=BKS[:, :, 1, :], in0=E4[:], in1=KP[:], op=ALU.mult),
               r=["E4", "KP", "BKS"], w=["BKS"])
            for par in range(2):
                pmc = cst[:, 640 + 64 * par:641 + 64 * par]
                op("act", lambda e: e.activation(
                    out=ARm[par][:].rearrange("p a b c -> p (a b c)"), in_=AR[:].rearrange("p a b c -> p (a b c)"),
                    func=AF.Copy, scale=pmc), r=["AR", "cst"], w=[f"ARm{par}"])
                op("act" if par else "dve", (lambda e: e.activation(
                    out=BKm[par][:].rearrange("p a b c -> p (a b c)"), in_=BK[:].rearrange("p a b c -> p (a b c)"),
                    func=AF.Copy, scale=pmc)) if par else (lambda e: e.tensor_scalar(
                    out=BKm[par][:].rearrange("p a b c -> p (a b c)"), in0=BK[:].rearrange("p a b c -> p (a b c)"),
                    scalar1=pmc, scalar2=None, op0=ALU.mult)), r=["BK", "cst"], w=[f"BKm{par}"])
            op("pool", lambda e: e.tensor_tensor(out=f2(RK), in0=f2(Rr), in1=f2(KP), op=ALU.mult),
               r=["Rr", "KP"], w=["RK"])
            op("pool", lambda e: e.tensor_tensor(out=RK2[:], in0=RK[:],
                                                 in1=vcol(V_RK, 4).unsqueeze(2).to_broadcast([128, 4, 128]),
                                                 op=ALU.mult), r=["RK", "vec"], w=["RK2"])
            pbn, pbnk = PS()
            op("pe", lambda e: e.matmul(pbn[:, 0:512], bones[:], f2(RK2), start=True, stop=True),
               r=["bones", "RK2"], w=[pbnk])
            op("dve", lambda e: e.tensor_tensor(out=f2(BV), in0=pbn[:, 0:512], in1=f2(Vr), op=ALU.mult),
               r=[pbnk, "Vr"], w=["BV"])
            op("act", lambda e: e.activation(out=f2(VB), in_=f2(Vr), func=AF.Copy), r=["Vr"], w=["VB"])
            pb1, pb1k = PSB()
            for j in range(2):
                for cc in range(4):
                    op("pe", lambda e, j=j, cc=cc: e.transpose(pb1[:, j * 512 + cc * 128: j * 512 + (cc + 1) * 128],
                                                               BKS[:, cc, j, :], identb[:]),
                       r=["BKS", "identb"], w=[pb1k])
            op("act", lambda e: e.activation(out=BKT[:], in_=pb1[:, :], func=AF.Copy), r=[pb1k], w=["BKT"])
            pb2, pb2k = PSB()
            for cc in range(4):
                op("pe", lambda e, cc=cc: e.transpose(pb2[:, cc * 128:(cc + 1) * 128], VB[:, cc, :], identb[:]),
                   r=["VB", "identb"], w=[pb2k])
            op("act", lambda e: e.activation(out=VT[:], in_=pb2[:, 0:512], func=AF.Copy), r=[pb2k], w=["VT"])
            if CUT <= 6:
                return
            def inv_s0(hh):
                heads = list(range(4 * hh, 4 * hh + 4))
                hs = slice(4 * hh, 4 * hh + 4)
                mk2 = MASK2.unsqueeze(1).to_broadcast([128, 2, 256])
                for (which, dstM) in ((0, MA), (1, MB)):
                    pM_ = [PS(), PS()]
                    for i, hd in enumerate(heads):
                        cc = hd // 2
                        c0 = (i % 2) * 256
                        par = hd % 2
                        op("pe", lambda e: e.matmul(
                            pM_[i // 2][0][:, c0:c0 + 256], BKm[par][:, cc, which, :],
                            AR[:, cc, :, :].rearrange("p a t -> p (a t)"), start=True, stop=True),
                           r=[f"BKm{par}", "AR"], w=[pM_[i // 2][1]])
                    for b2 in range(2):
                        h2 = slice(4 * hh + 2 * b2, 4 * hh + 2 * b2 + 2)
                        op("dve", lambda e: e.tensor_tensor(
                            out=dstM[:, h2, :], in0=pM_[b2][0][:, 0:512].rearrange("p (h c) -> p h c", h=2), in1=mk2,
                            op=ALU.mult), r=[pM_[b2][1], "cst"], w=[("MA" if which == 0 else "MB") + str(hh)])
                pQ0 = PS()
                for i, hd in enumerate(heads):
                    cc = hd // 2
                    par = hd % 2
                    op("pe", lambda e: e.matmul(
                        pQ0[0][:, i * 128:(i + 1) * 128], ARm[par][:, cc, 0, :], BK[:, cc, 0, :], start=True, stop=True),
                       r=["BK", f"ARm{par}"], w=[pQ0[1]])
                op("dve", lambda e: e.tensor_tensor(
                    out=Qm[0][:, hs, :], in0=pQ0[0][:, 0:512].rearrange("p (h c) -> p h c", h=4),
                    in1=LSTR.unsqueeze(1).to_broadcast([128, 4, 128]), op=ALU.mult),
                   r=[pQ0[1], "cst"], w=[f"Q0_{hh}"])

            def inv_l0(hh):
                heads = list(range(4 * hh, 4 * hh + 4))
                hs = slice(4 * hh, 4 * hh + 4)
                pP = PS()
                pQn = PS()
                for i, hd in enumerate(heads):
                    op("pe", lambda e, i=i, hd=hd: e.matmul(pP[0][:, i * 128:(i + 1) * 128], Qm[0][:, hd, :],
                                                            MA[:, hd, 0:128], start=True, stop=True),
                       r=[f"Q0_{hh}", f"MA{hh}"], w=[pP[1]])
                    op("pe", lambda e, i=i, hd=hd: e.matmul(pQn[0][:, i * 128:(i + 1) * 128], MA[:, hd, 0:128],
                                                            Qm[0][:, hd, :], start=True, stop=True),
                       r=[f"Q0_{hh}", f"MA{hh}"], w=[pQn[1]])
                op("act", lambda e, hs=hs: e.activation(out=PX[1][:, hs, 0:128],
                                                        in_=pP[0][:, 0:512].rearrange("p (h c) -> p h c", h=4),
                                                        func=AF.Copy), r=[pP[1]], w=[f"PX1_{hh}"])
                op("act", lambda e, hs=hs: e.activation(out=Qm[1][:, hs, :],
                                                        in_=pQn[0][:, 0:512].rearrange("p (h c) -> p h c", h=4),
                                                        func=AF.Copy), r=[pQn[1]], w=[f"Q1_{hh}"])
                op("pool", lambda e, hs=hs: e.tensor_tensor(out=PX[1][:, hs, 128:256], in0=MA[:, hs, 0:128],
                                                            in1=IDENTF.unsqueeze(1).to_broadcast([128, 4, 128]),
                                                            op=ALU.add),
                   r=[f"MA{hh}", "cst", f"PX1_{hh}"], w=[f"PX1_{hh}"])

            def inv_lv(hh, lv):
                heads = list(range(4 * hh, 4 * hh + 4))
                hs = slice(4 * hh, 4 * hh + 4)
                if True:
                    cur, nxt = lv % 2, 1 - (lv % 2)
                    pA = [PS(), PS()]
                    pQn = PS()
                    for i, hd in enumerate(heads):
                        c0 = (i % 2) * 256
                        op("pe", lambda e, i=i, hd=hd, c0=c0, cur=cur: e.matmul(
                            pA[i // 2][0][:, c0:c0 + 256], Qm[cur][:, hd, :], PX[cur][:, hd, :], start=True, stop=True),
                           r=[f"Q{cur}_{hh}", f"PX{cur}_{hh}"], w=[pA[i // 2][1]])
                        op("pe", lambda e, i=i, hd=hd, cur=cur: e.matmul(
                            pQn[0][:, i * 128:(i + 1) * 128], PX[cur][:, hd, 0:128], Qm[cur][:, hd, :],
                            start=True, stop=True), r=[f"Q{cur}_{hh}", f"PX{cur}_{hh}"], w=[pQn[1]])
                    for b2 in range(2):
                        h2 = slice(4 * hh + 2 * b2, 4 * hh + 2 * b2 + 2)
                        v3 = pA[b2][0][:, 0:512].rearrange("p (h c) -> p h c", h=2)
                        op("act", lambda e, h2=h2, v3=v3, nxt=nxt: e.activation(out=PX[nxt][:, h2, 0:128],
                                                                               in_=v3[:, :, 0:128], func=AF.Copy),
                           r=[pA[b2][1]], w=[f"PX{nxt}_{hh}"])
                        op("dve", lambda e, h2=h2, v3=v3, nxt=nxt, cur=cur: e.tensor_tensor(
                            out=PX[nxt][:, h2, 128:256], in0=v3[:, :, 128:256], in1=PX[cur][:, h2, 128:256],
                            op=ALU.add), r=[pA[b2][1], f"PX{cur}_{hh}", f"PX{nxt}_{hh}"], w=[f"PX{nxt}_{hh}"])
                    op("act", lambda e, hs=hs, nxt=nxt, pQn=pQn: e.activation(
                        out=Qm[nxt][:, hs, :], in_=pQn[0][:, 0:512].rearrange("p (h c) -> p h c", h=4), func=AF.Copy),
                       r=[pQn[1]], w=[f"Q{nxt}_{hh}"])

            def inv_fin(hh):
                heads = list(range(4 * hh, 4 * hh + 4))
                hs = slice(4 * hh, 4 * hh + 4)
                pX = PS()
                for i, hd in enumerate(heads):
                    op("pe", lambda e, i=i, hd=hd: e.matmul(pX[0][:, i * 128:(i + 1) * 128], Qm[0][:, hd, :],
                                                            PX[0][:, hd, 128:256], start=True, stop=True),
                       r=[f"Q0_{hh}", f"PX0_{hh}"], w=[pX[1]])
                op("dve", lambda e, hs=hs, pX=pX: e.tensor_tensor(
                    out=XF[:, hs, :], in0=pX[0][:, 0:512].rearrange("p (h c) -> p h c", h=4),
                    in1=PX[0][:, hs, 128:256], op=ALU.add), r=[pX[1], f"PX0_{hh}"], w=[f"XF{hh}"])

            for hh in range(2):
                inv_s0(hh)
            for hh in range(2):
                inv_l0(hh)
            for lv in range(1, 6):
                for hh in range(2):
                    inv_lv(hh, lv)
            for hh in range(2):
                inv_fin(hh)
            if CUT <= 7:
                return
            pR = PS()
            for hd in range(8):
                cc = hd // 2
                pr = slice((hd % 2) * 64, (hd % 2) * 64 + 64)
                op("pe", lambda e: e.matmul(pR[0][:, hd * 64:(hd + 1) * 64], ARm[hd % 2][:, cc, 0, :],
                                            SBs[:, cc, :], start=True, stop=False),
                   r=[f"ARm{hd % 2}", "SBs"], w=[pR[1]])
                op("pe", lambda e, hd=hd: e.matmul(pR[0][:, hd * 64:(hd + 1) * 64], MB[:, hd, 0:128],
                                                   VT[:, hd * 64:(hd + 1) * 64], start=False, stop=True),
                   r=[f"MB{hd // 4}", "VT"], w=[pR[1]])
            op("act", lambda e: e.activation(out=RH[:], in_=pR[0][:, 0:512], func=AF.Copy), r=[pR[1]], w=["RH"])
            pU = PS()
            for hd in range(8):
                op("pe", lambda e, hd=hd: e.matmul(pU[0][:, hd * 64:(hd + 1) * 64], XF[:, hd, :],
                                                   RH[:, hd * 64:(hd + 1) * 64], start=True, stop=True),
                   r=[f"XF{hd // 4}", "RH"], w=[pU[1]])
            op("act", lambda e: e.activation(out=UT[:], in_=pU[0][:, 0:512], func=AF.Copy), r=[pU[1]], w=["UT"])
            pY = PS()
            pS_ = PS()
            for hd in range(8):
                cc = hd // 2
                pr = slice((hd % 2) * 64, (hd % 2) * 64 + 64)
                oy = pY[0][pr, cc * 128:(cc + 1) * 128]
                op("pe", lambda e: e.matmul(oy, SBs[:, cc, :], ARm[hd % 2][:, cc, 1, :],
                                            start=True, stop=False), r=["SBs", f"ARm{hd % 2}"], w=[pY[1]])
                op("pe", lambda e, oy=oy, hd=hd: e.matmul(oy, UT[:, hd * 64:(hd + 1) * 64], MA[:, hd, 128:256],
                                                          start=False, stop=False),
                   r=["UT", f"MA{hd // 4}"], w=[pY[1]])
                op("pe", lambda e, oy=oy, hd=hd: e.matmul(oy, VT[:, hd * 64:(hd + 1) * 64], MB[:, hd, 128:256],
                                                          start=False, stop=True),
                   r=["VT", f"MB{hd // 4}"], w=[pY[1]])
            for hd in range(8):
                cc = hd // 2
                pr = slice((hd % 2) * 64, (hd % 2) * 64 + 64)
                os_ = pS_[0][pr, cc * 64:(cc + 1) * 64]
                op("pe", lambda e, os_=os_, hd=hd: e.matmul(os_, BKT[:, hd * 64:(hd + 1) * 64],
                                                            UT[:, hd * 64:(hd + 1) * 64], start=True, stop=False),
                   r=["BKT", "UT"], w=[pS_[1]])
                op("pe", lambda e, os_=os_, hd=hd: e.matmul(os_, BKT[:, 512 + hd * 64:512 + (hd + 1) * 64],
                                                            VT[:, hd * 64:(hd + 1) * 64], start=False, stop=True),
                   r=["BKT", "VT"], w=[pS_[1]])
            op("act", lambda e: e.activation(out=f2(Yf), in_=pY[0][:, 0:512], func=AF.Copy), r=[pY[1]], w=["Yf"])
            op("dve", lambda e: e.tensor_tensor(out=TMPS[:], in0=SF[:],
                                                in1=E1[:, :, 127:128].to_broadcast([128, 4, 64]), op=ALU.mult),
               r=["SF", "E1"], w=["TMPS"])
            op("dve", lambda e: e.tensor_tensor(out=SF[:], in0=pS_[0][:, 0:256].rearrange("p (c v) -> p c v", c=4),
                                                in1=TMPS[:], op=ALU.add), r=[pS_[1], "TMPS"], w=["SF"])
            op("act", lambda e: e.activation(out=SBs[:], in_=SF[:], func=AF.Copy), r=["SF"], w=["SBs"])
            if CUT <= 8:
                return
            op("act", lambda e: e.activation(out=f2(YB), in_=pY[0][:, 0:512], func=AF.Copy), r=[pY[1]], w=["YB"])
            op("act", lambda e: e.activation(out=f2(YSQ), in_=pY[0][:, 0:512], func=AF.Square), r=[pY[1]], w=["YSQ"])
            pM = PS()
            pV = PS()
            op("pe", lambda e: e.matmul(pM[0][:, 0:512], bones[:], f2(YB), start=True, stop=True),
               r=["bones", "YB"], w=[pM[1]])
            op("pe", lambda e: e.matmul(pV[0][:, 0:512], bones[:], f2(YSQ), start=True, stop=True),
               r=["bones", "YSQ"], w=[pV[1]])
            op("act", lambda e: e.activation(out=f2(MEAN), in_=pM[0][:, 0:512], func=AF.Copy, scale=1.0 / 64),
               r=[pM[1]], w=["MEAN"])
            op("pool", lambda e: e.tensor_tensor(out=f2(M2), in0=f2(MEAN), in1=f2(MEAN), op=ALU.mult),
               r=["MEAN"], w=["M2"])
            op("dve", lambda e: e.scalar_tensor_tensor(out=f2(VAR), in0=pV[0][:, 0:512], scalar=1.0 / 64, in1=f2(M2),
                                                       op0=ALU.mult, op1=ALU.subtract), r=[pV[1], "M2"], w=["VAR"])
            op("act", lambda e: e.activation(out=f2(VAR), in_=f2(VAR), func=AF.Sqrt, bias=64e-5), r=["VAR"], w=["VAR"])
            op("dve", lambda e: e.reciprocal(out=f2(VAR), in_=f2(VAR)), r=["VAR"], w=["VAR"])
            op("pool", lambda e: e.tensor_tensor(out=f2(Dd), in0=f2(Yf), in1=f2(MEAN), op=ALU.subtract),
               r=["Yf", "MEAN"], w=["Dd"])
            op("pool", lambda e: e.tensor_tensor(out=f2(Dd), in0=f2(Dd), in1=f2(VAR), op=ALU.mult),
               r=["Dd", "VAR"], w=["Dd"])
            for cc in range(4):
                op("dve", lambda e, cc=cc: e.tensor_scalar(out=Dd[:, cc, :], in0=Dd[:, cc, :], scalar1=vcol(V_LNW + cc),
                                                           scalar2=vcol(V_LNB + cc), op0=ALU.mult, op1=ALU.add),
                   r=["Dd", "vec"], w=["Dd"])
            op("pool", lambda e: e.tensor_tensor(out=f2(Dd), in0=f2(Dd), in1=f2(BV), op=ALU.add),
               r=["Dd", "BV"], w=["Dd"])
            op("pool", lambda e, ys=ys: e.tensor_tensor(out=ys[:, 4:8, :], in0=Dd[:], in1=Gg[:], op=ALU.mult),
               r=["Dd", "Gg"], w=[yk + "b"])
            dma("sp", lambda e, ys=ys, ti=ti: e.dma_start(out=yab_d[ti], in_=ys[:].rearrange("p a b -> p (a b)")),
                r=[yk + "a", yk + "b"], w=[f"yab_d{ti}"], key=yk)

        genA(0)
        for ti in range(NTILE):
            fns = [lambda ti=ti: genBC(ti)]
            q = [cfg.get("qBC", 5)]
            if ti + 1 < NTILE:
                fns.append(lambda ti=ti: genA(ti + 1))
                q.append(1)
            WV.run(fns, q)
        tl.pool = None


    if "1b" in PH:
        phase_reset()
        Wgt = salloc("Wgt", [128, 8, 2048], BF16)
        WbA = salloc("WbA", [128, 4, 1024], BF16)
        WbB = salloc("WbB", [128, 4, 1024], BF16)
        Wout = salloc("Wout", [128, 8, 1024], BF16)
        Wr = salloc("Wr", [128, 8, 36], F32)
        BR = salloc("BR", [128, 36], F32)
        GMOE = salloc("GMOE", [128, D], F32)
        ustrb = salloc("ustrb", [128, 128], BF16)
        CNT = salloc("CNT", [128, 32], F32)
        stgB = [salloc(f"stgB{i}", [128, 2048], F32) for i in range(2)]
        sB = 0
        for kc in range(8):
            k_ = f"stgB{sB % 2}"
            t_ = stgB[sB % 2]
            sB += 1
            dma("sp", lambda e: e.dma_start(out=t_[:, 0:2048], in_=win_d[kc * 128:(kc + 1) * 128, 2560:4608]),
                w=[k_], key=k_)
            op("dve" if kc % 2 else "pool", lambda e: e.tensor_scalar(out=Wgt[:, kc, :], in0=t_[:, 0:2048],
                                                                      scalar1=vcol(V_LNMIX + kc), scalar2=1.0,
                                                                      op0=ALU.mult, op1=ALU.mult),
               r=[k_, "vec"], w=["Wgt"])
        for (dst, dk, src, nk) in ((WbA, "WbA", wba_d, 4), (WbB, "WbB", wbb_d, 4), (Wout, "Wout", wout_d, 8)):
            for kc in range(nk):
                k_ = f"stgB{sB % 2}"
                t_ = stgB[sB % 2]
                sB += 1
                dma("sp", lambda e: e.dma_start(out=t_[:, 0:1024], in_=src[kc * 128:(kc + 1) * 128, :]),
                    w=[k_], key=k_)
                op("dve" if kc % 2 else "pool", lambda e: e.tensor_copy(out=dst[:, kc, :], in_=t_[:, 0:1024]),
                   r=[k_], w=[dk])
        dma("sp", lambda e: e.dma_start(out=Wr[:], in_=wr_d.rearrange("(k p) n -> p k n", p=128)), w=["Wr"], key="Wr")
        for kc in range(8):
            op("dve", lambda e: e.tensor_scalar(out=Wr[:, kc, :], in0=Wr[:, kc, :], scalar1=vcol(V_LNMOE + kc),
                                                scalar2=None, op0=ALU.mult), r=["Wr", "vec"], w=["Wr"])
        dma("sp", lambda e: e.dma_start(out=BR[:], in_=br_d.to_broadcast([128, 36])), w=["BR"], key="BR")
        dma("sp", lambda e: e.dma_start(out=GMOE[:], in_=lnmoe_d.to_broadcast([128, D])), w=["GMOE"], key="GMOE")
        op("dve", lambda e: e.tensor_copy(out=ustrb[:], in_=USTR), r=["cst"], w=["ustrb"])
        op("dve", lambda e: e.memset(CNT[:], 0.0), w=["CNT"])
        ZR = salloc("ZR", [128, D], BF16)
        op("pool", lambda e: e.memset(ZR[:], 0.0), w=["ZR"])
        hs_v = hs_d.rearrange("(r p) d -> p r d", p=128)
        RCH = 8
        for r0 in range(0, NST, RCH):
            rn = min(RCH, NST - r0)
            dma("sp", lambda e: e.dma_start(out=hs_v[:, r0:r0 + rn, :],
                                            in_=ZR[:].unsqueeze(1).to_broadcast([128, rn, D])),
                r=["ZR"], w=["hs_all"], key="ZRst")

        xin = [salloc(f"xinb{i}", [128, D], F32) for i in range(2)]
        tmp = dict(junk=salloc("junkb", [128, D], BF16), ss=salloc("ssb", [128, 4], F32),
                   rstd=salloc("rstdb", [128, 1], F32), xbf=salloc("xbfb", [128, D], BF16))
        hT = [salloc(f"hTb{i}", [128, 8, 128], BF16) for i in range(2)]
        yin = [salloc(f"yin{i}", [128, 8, 128], BF16) for i in range(2)]
        GT = salloc("GT", [128, 2048], F32)
        MAf = salloc("MAf", [128, D], F32)
        MBf = salloc("MBf", [128, D], F32)
        MG = salloc("MG", [128, D], BF16)
        MGT = salloc("MGT", [128, 8, 128], BF16)
        X1 = [salloc(f"X1_{i}", [128, D], F32) for i in range(2)]
        HM = [salloc(f"HM{i}", [128, D], BF16) for i in range(2)]
        X1T = salloc("X1T", [128, 8, 128], F32)
        rs = salloc("rs", [128, 8], F32)
        LG = salloc("LG", [128, 36], F32)
        R_ = salloc("Rsm", [128, 16], F32)
        GOH = salloc("GOH", [128, 4], F32)
        EG = salloc("EG", [128, 4], F32)
        T48 = salloc("T48", [128, 4, 8], F32)
        ESEL = salloc("ESEL", [128, 8], F32)
        ES2 = salloc("ES2", [128, 8], F32)
        OHa = salloc("OHa", [128, 8], F32)
        OHb = salloc("OHb", [128, 8], F32)
        OH1 = salloc("OH1", [128, 4, 8], F32)
        OH2 = salloc("OH2", [128, 4, 8], F32)
        OHSb = salloc("OHSb", [128, 32], BF16)
        POS = salloc("POS", [128, 32], F32)
        PT2 = salloc("PT2", [128, 32], F32)
        SL = salloc("SL", [128, 2], F32)
        EOFF = cst[:, 768:800]

        def g2(t):
            return t[:].rearrange("p a b -> p (a b)")

        for ti in range(NTILE):
            sl = ti % 2
            xk = f"xinb{sl}"
            dma("sp", lambda e: e.dma_start(out=xin[sl][:], in_=x_d[ti * 128:(ti + 1) * 128, :]), w=[xk], key=xk)
            yk = f"yin{sl}"
            dma("sp", lambda e: e.dma_start(out=yin[sl][:].rearrange("p a b -> p (a b)"), in_=yab_d[ti]),
                r=[f"yab_d{ti}"], w=[yk], key=yk)
            hk = f"hTb{sl}"
            rms_to_hT(ti, xin[sl], xk, hT[sl], hk, tmp)
            h = hT[sl]
            for nb in range(4):
                pgt = PS()
                for kc in range(8):
                    op("pe", lambda e: e.matmul(pgt[0][:, 0:512], h[:, kc, :], Wgt[:, kc, nb * 512:(nb + 1) * 512],
                                                start=(kc == 0), stop=(kc == 7)), r=[hk, "Wgt"], w=[pgt[1]])
                op("act", lambda e: e.activation(out=GT[:, nb * 512:(nb + 1) * 512], in_=pgt[0][:, 0:512],
                                                 func=AF.Sigmoid), r=[pgt[1]], w=[f"GT{nb}"])
            for half in range(2):
                pba = PS()
                for c in range(4):
                    op("pe", lambda e: e.matmul(pba[0][:, 0:512], yin[sl][:, c, :],
                                                WbA[:, c, half * 512:(half + 1) * 512], start=(c == 0), stop=(c == 3)),
                       r=[yk, "WbA"], w=[pba[1]])
                op("dve", lambda e: e.tensor_tensor(out=MAf[:, half * 512:(half + 1) * 512], in0=pba[0][:, 0:512],
                                                    in1=GT[:, half * 512:(half + 1) * 512], op=ALU.mult),
                   r=[pba[1], f"GT{half}"], w=[f"MAf{half}"])
                pbb = PS()
                for c in range(4):
                    op("pe", lambda e: e.matmul(pbb[0][:, 0:512], yin[sl][:, 4 + c, :],
                                                WbB[:, c, half * 512:(half + 1) * 512], start=(c == 0), stop=(c == 3)),
                       r=[yk, "WbB"], w=[pbb[1]])
                op("dve", lambda e: e.tensor_tensor(out=MBf[:, half * 512:(half + 1) * 512], in0=pbb[0][:, 0:512],
                                                    in1=GT[:, 1024 + half * 512:1024 + (half + 1) * 512], op=ALU.mult),
                   r=[pbb[1], f"GT{2 + half}"], w=[f"MBf{half}"])
                op("pool", lambda e: e.tensor_tensor(out=MG[:, half * 512:(half + 1) * 512],
                                                     in0=MAf[:, half * 512:(half + 1) * 512],
                                                     in1=MBf[:, half * 512:(half + 1) * 512], op=ALU.add),
                   r=[f"MAf{half}", f"MBf{half}"], w=[f"MG{half}"])
            pb = PSB()
            for kc in range(8):
                op("pe", lambda e: e.transpose(pb[0][:, kc * 128:(kc + 1) * 128], MG[:, kc * 128:(kc + 1) * 128],
                                               identb[:]), r=["MG0", "MG1", "identb"], w=[pb[1]])
            op("act", lambda e: e.activation(out=g2(MGT), in_=pb[0][:, :], func=AF.Copy), r=[pb[1]], w=["MGT"])
            x1 = X1[sl]
            x1k = f"X1_{sl}"
            for half in range(2):
                po = PS()
                for kc in range(8):
                    op("pe", lambda e: e.matmul(po[0][:, 0:512], MGT[:, kc, :], Wout[:, kc, half * 512:(half + 1) * 512],
                                                start=(kc == 0), stop=(kc == 7)), r=["MGT", "Wout"], w=[po[1]])
                op("dve", lambda e: e.tensor_tensor(out=x1[:, half * 512:(half + 1) * 512], in0=po[0][:, 0:512],
                                                    in1=xin[sl][:, half * 512:(half + 1) * 512], op=ALU.add),
                   r=[po[1], xk], w=[x1k + f"h{half}"])
            dma("sp", lambda e: e.dma_start(out=x1_d[ti * 128:(ti + 1) * 128, :], in_=x1[:]),
                r=[x1k + "h0", x1k + "h1"], w=[f"x1_d{ti}"], key=x1k)
            op("act", lambda e: e.activation(out=tmp["junk"][:], in_=x1[:], func=AF.Square, accum_out=rs[:, 0:1]),
               r=[x1k + "h0", x1k + "h1"], w=["junk", "rs0"])
            op("dve", lambda e: e.tensor_scalar(out=rs[:, 1:2], in0=rs[:, 0:1], scalar1=1.0 / D, scalar2=1e-6,
                                                op0=ALU.mult, op1=ALU.add), r=["rs0"], w=["rs1"])
            op("act", lambda e: e.activation(out=rs[:, 2:3], in_=rs[:, 1:2], func=AF.Sqrt), r=["rs1"], w=["rs2"])
            op("dve", lambda e: e.reciprocal(out=rs[:, 3:4], in_=rs[:, 2:3]), r=["rs2"], w=["rs3"])
            hm = HM[sl]
            hmk = f"HM{sl}"
            op("dve", lambda e: e.scalar_tensor_tensor(out=hm[:], in0=x1[:], scalar=rs[:, 3:4], in1=GMOE[:],
                                                       op0=ALU.mult, op1=ALU.mult),
               r=[x1k + "h0", x1k + "h1", "rs3", "GMOE"], w=[hmk])
            for half in range(2):
                ptx = PS()
                for j in range(4):
                    kc = half * 4 + j
                    op("pe", lambda e: e.transpose(ptx[0][:, j * 128:(j + 1) * 128], x1[:, kc * 128:(kc + 1) * 128],
                                                   IDENTF), r=[x1k + "h0", x1k + "h1", "cst"], w=[ptx[1]])
                op("act", lambda e: e.activation(out=X1T[:, half * 4:half * 4 + 4, :].rearrange("p a b -> p (a b)"),
                                                 in_=ptx[0][:, 0:512], func=AF.Copy), r=[ptx[1]], w=[f"X1T{half}"])
            pl = PS()
            for kc in range(8):
                op("pe", lambda e: e.matmul(pl[0][:, 0:36], X1T[:, kc, :], Wr[:, kc, :], start=(kc == 0), stop=(kc == 7)),
                   r=["X1T0", "X1T1", "Wr"], w=[pl[1]])
            op("dve", lambda e: e.scalar_tensor_tensor(out=LG[:], in0=pl[0][:, 0:36], scalar=rs[:, 3:4], in1=BR[:],
                                                       op0=ALU.mult, op1=ALU.add), r=[pl[1], "rs3", "BR"], w=["LG"])
            V = lambda f, r, w: op("dve", f, r=r, w=w)
            V(lambda e: e.tensor_reduce(out=R_[:, 0:1], in_=LG[:, 0:4], axis=AX.X, op=ALU.max), ["LG"], ["R0"])
            V(lambda e: e.tensor_scalar(out=GOH[:], in0=LG[:, 0:4], scalar1=R_[:, 0:1], scalar2=None, op0=ALU.is_equal),
              ["LG", "R0"], ["GOH"])
            V(lambda e: e.tensor_scalar(out=R_[:, 1:2], in0=R_[:, 0:1], scalar1=-1.0, scalar2=None, op0=ALU.mult),
              ["R0"], ["R1"])
            op("act", lambda e: e.activation(out=EG[:], in_=LG[:, 0:4], func=AF.Exp, bias=R_[:, 1:2],
                                             accum_out=R_[:, 2:3]), r=["LG", "R1"], w=["EG", "R2"])
            V(lambda e: e.reciprocal(out=R_[:, 3:4], in_=R_[:, 2:3]), ["R2"], ["R3"])
            V(lambda e: e.tensor_tensor(out=T48[:], in0=LG[:, 4:36].rearrange("p (g x) -> p g x", g=4),
                                        in1=GOH[:].unsqueeze(2).to_broadcast([128, 4, 8]), op=ALU.mult),
              ["LG", "GOH"], ["T48"])
            V(lambda e: e.tensor_reduce(out=ESEL[:], in_=T48[:].rearrange("p g x -> p x g"), axis=AX.X, op=ALU.add),
              ["T48"], ["ESEL"])
            V(lambda e: e.tensor_reduce(out=R_[:, 4:5], in_=ESEL[:], axis=AX.X, op=ALU.max), ["ESEL"], ["R4"])
            V(lambda e: e.tensor_scalar(out=OHa[:], in0=ESEL[:], scalar1=R_[:, 4:5], scalar2=None, op0=ALU.is_equal),
              ["ESEL", "R4"], ["OHa"])
            V(lambda e: e.scalar_tensor_tensor(out=ES2[:], in0=OHa[:], scalar=-1e30, in1=ESEL[:], op0=ALU.mult,
                                               op1=ALU.add), ["OHa", "ESEL"], ["ES2"])
            V(lambda e: e.tensor_reduce(out=R_[:, 5:6], in_=ES2[:], axis=AX.X, op=ALU.max), ["ES2"], ["R5"])
            V(lambda e: e.tensor_scalar(out=OHb[:], in0=ES2[:], scalar1=R_[:, 5:6], scalar2=None, op0=ALU.is_equal),
              ["ES2", "R5"], ["OHb"])
            V(lambda e: e.tensor_tensor(out=R_[:, 6:7], in0=R_[:, 5:6], in1=R_[:, 4:5], op=ALU.subtract),
              ["R4", "R5"], ["R6"])
            op("act", lambda e: e.activation(out=R_[:, 7:8], in_=R_[:, 6:7], func=AF.Exp), r=["R6"], w=["R7"])
            V(lambda e: e.tensor_scalar(out=R_[:, 8:9], in0=R_[:, 7:8], scalar1=1.0, scalar2=None, op0=ALU.add),
              ["R7"], ["R8"])
            V(lambda e: e.reciprocal(out=R_[:, 9:10], in_=R_[:, 8:9]), ["R8"], ["R9"])
            V(lambda e: e.tensor_tensor(out=rw_f[:, 2 * ti:2 * ti + 1], in0=R_[:, 9:10], in1=R_[:, 3:4], op=ALU.mult),
              ["R9", "R3"], [f"rw{ti}a"])
            V(lambda e: e.tensor_tensor(out=rw_f[:, 2 * ti + 1:2 * ti + 2], in0=rw_f[:, 2 * ti:2 * ti + 1],
                                        in1=R_[:, 7:8], op=ALU.mult), [f"rw{ti}a", "R7"], [f"rw{ti}b"])
            gb = GOH[:].unsqueeze(2).to_broadcast([128, 4, 8])
            V(lambda e: e.tensor_tensor(out=OH1[:], in0=gb, in1=OHa[:].unsqueeze(1).to_broadcast([128, 4, 8]),
                                        op=ALU.mult), ["GOH", "OHa"], ["OH1"])
            V(lambda e: e.tensor_tensor(out=OH2[:], in0=gb, in1=OHb[:].unsqueeze(1).to_broadcast([128, 4, 8]),
                                        op=ALU.mult), ["GOH", "OHb"], ["OH2"])
            V(lambda e: e.tensor_tensor(out=OHSb[:], in0=g2(OH1), in1=g2(OH2), op=ALU.add), ["OH1", "OH2"], ["OHSb"])
            pc = PS()
            op("pe", lambda e: e.matmul(pc[0][:, 0:32], ustrb[:], OHSb[:], start=True, stop=True),
               r=["ustrb", "OHSb"], w=[pc[1]])
            op("pe", lambda e: e.matmul(pc[0][:, 32:64], onesb[:], OHSb[:], start=True, stop=True),
               r=["onesb", "OHSb"], w=[pc[1]])
            V(lambda e: e.tensor_tensor(out=POS[:], in0=pc[0][:, 0:32], in1=CNT[:], op=ALU.add), [pc[1], "CNT"], ["POS"])
            V(lambda e: e.tensor_tensor(out=CNT[:], in0=pc[0][:, 32:64], in1=CNT[:], op=ALU.add), [pc[1], "CNT"], ["CNT"])
            V(lambda e: e.tensor_scalar(out=POS[:], in0=POS[:], scalar1=float(CAP - 1), scalar2=None, op0=ALU.min),
              ["POS"], ["POS"])
            V(lambda e: e.tensor_tensor(out=POS[:], in0=POS[:], in1=EOFF, op=ALU.add), ["POS", "cst"], ["POS"])
            for j, OHx in enumerate((OH1, OH2)):
                V(lambda e: e.tensor_tensor(out=PT2[:], in0=POS[:], in1=g2(OHx), op=ALU.mult),
                  ["POS", f"OH{j + 1}"], ["PT2"])
                V(lambda e: e.tensor_reduce(out=SL[:, j:j + 1], in_=PT2[:], axis=AX.X, op=ALU.add), ["PT2"], [f"SL{j}"])
            V(lambda e: e.tensor_copy(out=slot_i[:, 2 * ti:2 * ti + 2], in_=SL[:]), ["SL0", "SL1"], [f"slot{ti}"])
            for j in range(2):
                dma("pool", lambda e: e.indirect_dma_start(
                    out=hs_d[:, :], out_offset=bass.IndirectOffsetOnAxis(ap=slot_i[:, 2 * ti + j:2 * ti + j + 1], axis=0),
                    in_=hm[:, :], in_offset=None), r=[hmk, f"slot{ti}", "hs_all"], w=[f"hs_sc{ti}_{j}"], key=f"sc{sl}{j}")
        if debug:
            RT = salloc("RT", [128, NTILE * 4], F32)
            op("dve", lambda e: e.tensor_copy(out=RT[:, 0:2 * NTILE], in_=slot_i[:]),
               r=[f"slot{t}" for t in range(NTILE)], w=["RT"])
            op("dve", lambda e: e.tensor_copy(out=RT[:, 2 * NTILE:4 * NTILE], in_=rw_f[:]),
               r=[f"rw{t}a" for t in range(NTILE)] + [f"rw{t}b" for t in range(NTILE)] + ["RT"], w=["RT"])
            dma("sp", lambda e: e.dma_start(out=rt_d, in_=RT[:]), r=["RT"], w=["rt_d"], key="RT")

    if "2" in PH:
        phase_reset()
        WG = [salloc(f"WG{i}", [128, 8, DE], BF16) for i in range(2)]
        WU = [salloc(f"WU{i}", [128, 8, DE], BF16) for i in range(2)]
        WD = [salloc(f"WD{i}", [128, 4, D], BF16) for i in range(2)]
        NB = 4
        XGb = [salloc(f"XGb{i}", [128, NB, D], BF16) for i in range(2)]
        XGT = salloc("XGT", [128, 8, NB * 128], BF16)
        SGf = [salloc(f"SGf{i}", [128, NB * 128], F32) for i in range(2)]
        HID = salloc("HID", [128, 4, NB * 128], BF16)
        YOb = [salloc(f"YOb{i}", [128, NB, D], F32) for i in range(2)]
        all_sc = [f"hs_sc{t}_{j}" for t in range(NTILE) for j in range(2)] + ["hs_all"]
        if CT <= 4:
            batches = [(0, CT)]
        elif CT == 5:
            batches = [(0, 3), (3, 2)]
        else:
            batches = [(r0, min(4, CT - r0)) for r0 in range(0, CT, 4)]
        it = 0
        for ex in range(NE):
            b = ex % 2
            dma("pool", lambda e: e.dma_start(out=WG[b][:], in_=wg_d[ex].rearrange("(p k) n -> p k n", k=8),
                                              max_dma_last_dim=8192), w=[f"WG{b}"], key=f"WG{b}")
            dma("pool", lambda e: e.dma_start(out=WU[b][:], in_=wu_d[ex].rearrange("(p k) n -> p k n", k=8),
                                              max_dma_last_dim=8192), w=[f"WU{b}"], key=f"WU{b}")
            dma("pool", lambda e: e.dma_start(out=WD[b][:], in_=wd_d[ex].rearrange("(k p) n -> p k n", p=128)),
                w=[f"WD{b}"], key=f"WD{b}")
            for (r0, nt) in batches:
                row0 = ex * CAP + r0 * 128
                xb = it % 2
                it += 1
                xgk = f"XGb{xb}"
                W_ = nt * 128
                dma("sp", lambda e: e.dma_start(
                    out=XGb[xb][:, 0:nt, :], in_=hs_d[row0:row0 + W_, :].rearrange("(t p) d -> p t d", p=128)),
                    r=(all_sc if "1b" in PH else []), w=[xgk], key=xgk)
                for t in range(nt):
                    pb = PSB()
                    xv = XGb[xb][:, t, :].rearrange("p (m j) -> p j m", j=8)
                    for j in range(8):
                        op("pe", lambda e: e.transpose(pb[0][:, j * 128:(j + 1) * 128], xv[:, j, :], identb[:]),
                           r=[xgk, "identb"], w=[pb[1]])
                    op("act", lambda e: e.activation(out=XGT[:, :, t * 128:(t + 1) * 128],
                                                     in_=pb[0][:, :].rearrange("p (j m) -> p j m", j=8), func=AF.Copy),
                       r=[pb[1]], w=[f"XGT{t}"])
                xgt_keys = [f"XGT{t}" for t in range(nt)]
                for hc in range(4):
                    pG = PS()
                    pU_ = PS()
                    for j in range(8):
                        op("pe", lambda e: e.matmul(pG[0][:, 0:W_], WG[b][:, j, hc * 128:(hc + 1) * 128],
                                                    XGT[:, j, 0:W_], start=(j == 0), stop=(j == 7)),
                           r=[f"WG{b}"] + xgt_keys, w=[pG[1]])
                    for j in range(8):
                        op("pe", lambda e: e.matmul(pU_[0][:, 0:W_], WU[b][:, j, hc * 128:(hc + 1) * 128],
                                                    XGT[:, j, 0:W_], start=(j == 0), stop=(j == 7)),
                           r=[f"WU{b}"] + xgt_keys, w=[pU_[1]])
                    sg = SGf[hc % 2]
                    sgk = f"SGf{hc % 2}"
                    op("act", lambda e: e.activation(out=sg[:, 0:W_], in_=pG[0][:, 0:W_], func=AF.Silu),
                       r=[pG[1]], w=[sgk])
                    op("dve", lambda e: e.tensor_tensor(out=HID[:, hc, 0:W_], in0=pU_[0][:, 0:W_], in1=sg[:, 0:W_],
                                                        op=ALU.mult), r=[pU_[1], sgk], w=[f"HID{hc}"])
                yok = f"YOb{xb}"
                for t in range(nt):
                    for half in range(2):
                        py = PS()
                        for hc in range(4):
                            op("pe", lambda e: e.matmul(py[0][:, 0:512], HID[:, hc, t * 128:(t + 1) * 128],
                                                        WD[b][:, hc, half * 512:(half + 1) * 512], start=(hc == 0),
                                                        stop=(hc == 3)), r=[f"HID{hc_}" for hc_ in range(4)] + [f"WD{b}"],
                               w=[py[1]])
                        if half:
                            op("act", lambda e: e.activation(out=YOb[xb][:, t, 512:1024], in_=py[0][:, 0:512],
                                                             func=AF.Copy), r=[py[1]], w=[yok + f"_{t}_1"])
                        else:
                            op("dve", lambda e: e.tensor_copy(out=YOb[xb][:, t, 0:512], in_=py[0][:, 0:512]),
                               r=[py[1]], w=[yok + f"_{t}_0"])
                dma("sp", lambda e: e.dma_start(
                    out=ys_d[row0:row0 + W_, :].rearrange("(t p) d -> p t d", p=128), in_=YOb[xb][:, 0:nt, :]),
                    r=[yok + f"_{t}_{hf}" for t in range(nt) for hf in range(2)], w=["ys_all"], key=yok)

    if "3" in PH:
        phase_reset()
        Wpg = salloc("Wpg", [128, 8, D], BF16)
        Wpp = salloc("Wpp", [128, 2, D], BF16)
        LNF = salloc("LNF", [128, D], F32)
        stgC = [salloc(f"stgC{i}", [128, D], F32) for i in range(2)]
        for kc in range(8):
            k_ = f"stgC{kc % 2}"
            t_ = stgC[kc % 2]
            dma("sp", lambda e: e.dma_start(out=t_[:], in_=wpg_d[kc * 128:(kc + 1) * 128, :]), w=[k_], key=k_)
            op("dve" if kc % 2 else "pool", lambda e: e.tensor_scalar(out=Wpg[:, kc, :], in0=t_[:],
                                                                      scalar1=vcol(V_LNPLE + kc), scalar2=1.0,
                                                                      op0=ALU.mult, op1=ALU.mult),
               r=[k_, "vec"], w=["Wpg"])
        for kc in range(2):
            k_ = f"stgC{kc % 2}"
            t_ = stgC[kc % 2]
            dma("sp", lambda e: e.dma_start(out=t_[:], in_=wpp_d[kc * 128:(kc + 1) * 128, :]), w=[k_], key=k_)
            op("dve", lambda e: e.tensor_copy(out=Wpp[:, kc, :], in_=t_[:]), r=[k_], w=["Wpp"])
        dma("sp", lambda e: e.dma_start(out=LNF[:], in_=lnf_d.to_broadcast([128, D])), w=["LNF"], key="LNF")
        X1c = [salloc(f"X1c{i}", [128, D], F32) for i in range(2)]
        Y1 = [salloc(f"Y1_{i}", [128, D], F32) for i in range(2)]
        Y2 = [salloc(f"Y2_{i}", [128, D], F32) for i in range(2)]
        Pin = [salloc(f"Pin{i}", [128, 256], F32) for i in range(2)]
        Pb2 = [salloc(f"Pb{i}", [128, 256], BF16) for i in range(2)]
        PTt2 = [salloc(f"PTt{i}", [128, 2, 128], BF16) for i in range(2)]
        X22 = [salloc(f"X2{i}", [128, D], F32) for i in range(2)]
        tmp2 = [dict(junk=salloc(f"junkc{i}", [128, D], BF16), ss=salloc(f"ssc{i}", [128, 4], F32),
                     rstd=salloc(f"rstdc{i}", [128, 1], F32), xbf=salloc(f"xbfc{i}", [128, D], BF16))
                for i in range(2)]
        hTc2 = [salloc(f"hTc{i}", [128, 8, 128], BF16) for i in range(2)]
        GP2 = [salloc(f"GP{i}", [128, D], F32) for i in range(2)]
        X32 = [salloc(f"X3{i}", [128, D], F32) for i in range(2)]
        rf2 = [salloc(f"rf{i}", [128, 4], F32) for i in range(2)]
        OUTt = [salloc(f"OUT{i}", [128, D], F32) for i in range(2)]

        def body3(ti):
            sl = ti % 2
            tl.pool = "E" if sl == 0 else "O"
            op, dma = stream(f"@{sl}")
            Pb, PTt, X2, tmp, hTc, GP, X3, rf = Pb2[sl], PTt2[sl], X22[sl], tmp2[sl], hTc2[sl], GP2[sl], X32[sl], rf2[sl]
            x1k, y1k, y2k, pk = f"X1c{sl}", f"Y1_{sl}", f"Y2_{sl}", f"Pin{sl}"
            dma("sp", lambda e: e.dma_start(out=X1c[sl][:], in_=x1_d[ti * 128:(ti + 1) * 128, :]),
                r=([f"x1_d{ti}"] if "1b" in PH else []), w=[x1k], key=x1k)
            dma("sp", lambda e: e.dma_start(out=Pin[sl][:], in_=p_d[ti * 128:(ti + 1) * 128, :]), w=[pk], key=pk)
            for (Yt, ykk, j) in ((Y1[sl], y1k, 0), (Y2[sl], y2k, 1)):
                dma("pool", lambda e: e.indirect_dma_start(
                    out=Yt[:, :], out_offset=None, in_=ys_d[:, :],
                    in_offset=bass.IndirectOffsetOnAxis(ap=slot_i[:, 2 * ti + j:2 * ti + j + 1], axis=0)),
                    r=(["ys_all", f"slot{ti}"] if "2" in PH else []), w=[ykk], key=ykk)
            op("dve", lambda e: e.scalar_tensor_tensor(out=X2[:], in0=Y1[sl][:], scalar=rw_f[:, 2 * ti:2 * ti + 1],
                                                       in1=X1c[sl][:], op0=ALU.mult, op1=ALU.add),
               r=[y1k, x1k, f"rw{ti}a"], w=["X2"])
            op("dve", lambda e: e.scalar_tensor_tensor(out=X2[:], in0=Y2[sl][:], scalar=rw_f[:, 2 * ti + 1:2 * ti + 2],
                                                       in1=X2[:], op0=ALU.mult, op1=ALU.add),
               r=[y2k, "X2", f"rw{ti}b"], w=["X2"])
            rms_to_hT(ti, X2, "X2", hTc, "hTc", tmp, normalize=False, op=op)
            op("pool", lambda e: e.tensor_copy(out=Pb[:], in_=Pin[sl][:]), r=[pk], w=["Pb"])
            pb = PSB()
            for kc in range(2):
                op("pe", lambda e: e.transpose(pb[0][:, kc * 128:(kc + 1) * 128], Pb[:, kc * 128:(kc + 1) * 128],
                                               identb[:]), r=["Pb", "identb"], w=[pb[1]])
            op("act", lambda e: e.activation(out=PTt[:].rearrange("p a b -> p (a b)"), in_=pb[0][:, 0:256],
                                             func=AF.Copy), r=[pb[1]], w=["PTt"])
            for half in range(2):
                pgm = PS()
                for kc in range(8):
                    op("pe", lambda e: e.matmul(pgm[0][:, 0:512], hTc[:, kc, :], Wpg[:, kc, half * 512:(half + 1) * 512],
                                                start=(kc == 0), stop=(kc == 7)), r=["hTc", "Wpg"], w=[pgm[1]])
                op("act", lambda e: e.activation(out=GP[:, half * 512:(half + 1) * 512], in_=pgm[0][:, 0:512],
                                                 func=AF.Sigmoid, scale=tmp["rstd"][:, 0:1]),
                   r=[pgm[1], "rstd"], w=[f"GP{half}"])
                ppm = PS()
                for kc in range(2):
                    op("pe", lambda e: e.matmul(ppm[0][:, 0:512], PTt[:, kc, :], Wpp[:, kc, half * 512:(half + 1) * 512],
                                                start=(kc == 0), stop=(kc == 1)), r=["PTt", "Wpp"], w=[ppm[1]])
                op("dve", lambda e: e.tensor_tensor(out=GP[:, half * 512:(half + 1) * 512], in0=ppm[0][:, 0:512],
                                                    in1=GP[:, half * 512:(half + 1) * 512], op=ALU.mult),
                   r=[ppm[1], f"GP{half}"], w=[f"GP{half}"])
                op("pool", lambda e: e.tensor_tensor(out=X3[:, half * 512:(half + 1) * 512],
                                                     in0=X2[:, half * 512:(half + 1) * 512],
                                                     in1=GP[:, half * 512:(half + 1) * 512], op=ALU.add),
                   r=["X2", f"GP{half}"], w=[f"X3{half}"])
            op("act", lambda e: e.activation(out=tmp["junk"][:], in_=X3[:], func=AF.Square, accum_out=rf[:, 0:1]),
               r=["X30", "X31"], w=["junk", "rf0"])
            op("dve", lambda e: e.tensor_scalar(out=rf[:, 1:2], in0=rf[:, 0:1], scalar1=1.0 / D, scalar2=1e-6,
                                                op0=ALU.mult, op1=ALU.add), r=["rf0"], w=["rf1"])
            op("act", lambda e: e.activation(out=rf[:, 2:3], in_=rf[:, 1:2], func=AF.Sqrt), r=["rf1"], w=["rf2"])
            op("dve", lambda e: e.reciprocal(out=rf[:, 3:4], in_=rf[:, 2:3]), r=["rf2"], w=["rf3"])
            ok = f"OUT{sl}"
            op("dve", lambda e: e.scalar_tensor_tensor(out=OUTt[sl][:], in0=X3[:], scalar=rf[:, 3:4], in1=LNF[:],
                                                       op0=ALU.mult, op1=ALU.mult), r=["X30", "X31", "rf3", "LNF"],
               w=[ok])
            dma("sp", lambda e: e.dma_start(out=out_d[ti * 128:(ti + 1) * 128, :], in_=OUTt[sl][:]),
                r=[ok], w=[f"out_d{ti}"], key=ok)

        for t0 in range(0, NTILE, 2):
            WV.run([lambda t0=t0: body3(t0), lambda t0=t0: body3(t0 + 1)], [1, 1], seq=not cfg.get("weave23", False))
        tl.pool = None

    S_.op("sp", nop_fns["sp"], r=[], w=[])
    return nc, S_


def emit(nc, S_):
    pref = S_.finish(nc, None, None, None)
    import contextlib
    with contextlib.ExitStack() as es:
        sems = {e: es.enter_context(nc.semaphore("s_" + e)) for e in ENGS}
        dsem = {k: es.enter_context(nc.semaphore("d_" + str(i))) for i, k in enumerate(S_.dma_cnt)}
        bsem = [es.enter_context(nc.semaphore(f"bar{i}")) for i in range(2)]
        block = es.enter_context(nc.Block())

        def run(ename, eh):
            for o in S_.ops[ename]:
                for (key, val) in o["waits"]:
                    if key[0] == "eng":
                        eh.wait_ge(sems[key[1]], pref[key[1]][val])
                    else:
                        eh.wait_ge(dsem[key[1]], val)
                ins = o["fn"](eh)
                if o.get("bar"):
                    nb = o["bar"]
                    ins.then_inc(bsem[nb % 2], 1)
                    eh.wait_ge(bsem[nb % 2], len(ENGS) * ((nb + 1) // 2 if nb % 2 else nb // 2))
                    continue
                if o["dma"] is not None:
                    ins.then_inc(dsem[o["dma"]], 16)
                elif o["inc"]:
                    ins.then_inc(sems[ename], 1)
            if ename == "sp":
                for k, c in S_.dma_cnt.items():
                    eh.wait_ge(dsem[k], c)

        @block.tensor
        def _(t):
            run("pe", t)

        @block.scalar
        def _(a):
            run("act", a)

        @block.vector
        def _(v):
            run("dve", v)

        @block.gpsimd
        def _(g):
            run("pool", g)

        @block.sync
        def _(s):
            run("sp", s)
    return nc


def _perm_q():
    idx = []
    for c in range(4):
        idx += list(range(c * 64, c * 64 + 64)) + list(range((c + 4) * 64, (c + 4) * 64 + 64))
    return np.array(idx)


def _consts(cap):
    c = np.zeros((128, 832), np.float32)
    c[:, 768:800] = (np.arange(32) * cap)[None, :]
    i = np.arange(128)
    c[:, 0:128] = np.eye(128)
    c[:, 128:256] = (i[:, None] < i[None, :])
    c[:, 256:384] = (i[:, None] <= i[None, :])
    c[:, 384:512] = (i[:, None] > i[None, :])
    invf = (10000.0 ** (-np.arange(32, dtype=np.float64) / 32.0)) / (2 * np.pi)
    c[:, 512:544] = invf[None, :]
    c[:, 544:576] = invf[None, :]
    c[:, 576:608] = 0.0
    c[:, 608:640] = 0.25
    c[:, 640:768] = ((i[:, None] // 64) == (i[None, :] // 64))
    return c


def prep_shared(inp, cap):
    f = lambda a: np.ascontiguousarray(np.asarray(a, dtype=np.float32))
    pq = _perm_q()
    w_in = f(inp["w_in"][0])
    cols = np.concatenate([pq, np.arange(512, 4608)])
    w_in = np.ascontiguousarray(w_in[:, cols])
    vec = np.zeros((128, 70), np.float32)
    vec[:, 0:8] = f(inp["ln_mix"][0]).reshape(8, 128).T
    vec[:, 8:16] = f(inp["ln_moe"][0]).reshape(8, 128).T
    vec[:, 16:24] = f(inp["ln_ple"][0]).reshape(8, 128).T
    vec[:, 24:38] = f(inp["mu_shift"][0]).reshape(14, 128).T
    for j, nm in enumerate(["w0", "a0", "k_k", "k_a", "r_k", "ln_x_w", "ln_x_b"]):
        vec[:, 38 + 4 * j:42 + 4 * j] = f(inp[nm][0]).reshape(4, 128).T
    sk = f(inp["sinks"][0])
    for c in range(4):
        vec[0:64, 66 + c] = sk[c]
        vec[64:128, 66 + c] = sk[c + 4]
    sh = dict(
        w_in=w_in, vecs=vec, cst=_consts(cap), ln_moe_row=f(inp['ln_moe'][0])[None, :],
        wlora=np.ascontiguousarray(np.concatenate([f(inp["w_decay_up"][0]), f(inp["w_aaa_up"][0])], 0)),
        wgu=f(inp["w_gate_up"][0]),
        w_ba=np.ascontiguousarray(f(inp["w_branch_att"][0])[pq, :]),
        w_bb=f(inp["w_branch_rwkv"][0]),
        w_out=f(inp["w_out"][0]),
        w_r=np.ascontiguousarray(np.concatenate([f(inp["w_group"][0]), f(inp["w_expert"][0])], 1)),
        b_r=np.ascontiguousarray(np.concatenate([f(inp["b_group"][0]), f(inp["b_expert"][0])])[None, :]),
        w_gate_e=f(inp["w_gate_e"][0]), w_up_e=f(inp["w_up_e"][0]), w_down_e=f(inp["w_down_e"][0]),
        w_pg=f(inp["w_ple_gate"][0]), w_pp=f(inp["w_ple_proj"][0]),
        ln_final=f(inp["ln_final"])[None, :],
    )
    return sh


def prep_core(inp, sh, b0, nseq):
    x = np.asarray(inp["x"], np.float32)[b0:b0 + nseq]
    S = x.shape[1]
    m = dict(sh)
    m["x"] = np.ascontiguousarray(x.reshape(nseq * S, D))
    m["p"] = np.ascontiguousarray(np.asarray(inp["p"], np.float32)[0, b0:b0 + nseq].reshape(nseq * S, 256))
    pos = np.asarray(inp["positions"], np.int32)[b0:b0 + nseq].reshape(-1)
    m["posT"] = np.ascontiguousarray(pos.reshape(-1, 128).T)
    return m


FULL_CFG = dict(NSEQ=4, S=2048, CAP=640)


def kernel(**inputs):
    cfg = FULL_CFG
    nc, S_ = build(cfg)
    emit(nc, S_)
    sh = prep_shared(inputs, cfg["CAP"])
    in_maps = [prep_core(inputs, sh, c * cfg["NSEQ"], cfg["NSEQ"]) for c in range(8)]
    res = run_bass_kernel_spmd(nc, in_maps, core_ids=list(range(8)))
    outs = [r["out"].reshape(cfg["NSEQ"], cfg["S"], D) for r in res.results]
    return np.concatenate(outs, 0).astype(np.float32)
```

```python
import numpy as np
import ml_dtypes
import concourse.bass as bass
import concourse.mybir as mybir
from concourse.bass_utils import run_bass_kernel_spmd

F32 = mybir.dt.float32
BF16 = mybir.dt.bfloat16
I32 = mybir.dt.int32
AF = mybir.ActivationFunctionType
ALU = mybir.AluOpType
AX = mybir.AxisListType

D = 1024
NE = 32
DE = 512
ENGS = ("pe", "act", "dve", "pool", "sp")
SYNC_SAME = ("act", "dve", "pool")


class _Rec:
    def __init__(self):
        self.call = None

    def __getattr__(self, name):
        def f(*a, **k):
            self.call = (name, a, k)
            return self
        return f


def _bind(fn):
    r = _Rec()
    fn(r)
    name, a, k = r.call
    return lambda e: getattr(e, name)(*a, **k)


import threading


class Weaver:
    def __init__(self):
        self.active = False

    def tick(self):
        if not self.active or threading.current_thread() is not self.cur_thread():
            return
        i = self.cur
        self.count[i] += 1
        if self.count[i] >= self.quota[i]:
            self.count[i] = 0
            self._handoff(i)

    def cur_thread(self):
        return self.threads[self.cur]

    def _next_live(self, i):
        n = len(self.threads)
        for d in range(1, n + 1):
            j = (i + d) % n
            if not self.done[j]:
                return j
        return None

    def _handoff(self, i):
        j = self._next_live(i)
        if j is None or j == i:
            return
        self.cur = j
        self.sems[j].release()
        self.sems[i].acquire()

    def run(self, fns, quota, seq=False):
        n = len(fns)
        if n == 1 or seq:
            for f in fns:
                f()
            return
        self.sems = [threading.Semaphore(0) for _ in range(n)]
        self.done = [False] * n
        self.count = [0] * n
        self.quota = list(quota)
        self.err = None
        fin = threading.Semaphore(0)

        def wrap(i):
            self.sems[i].acquire()
            try:
                fns[i]()
            except BaseException as ex:
                self.err = ex
            self.done[i] = True
            j = self._next_live(i)
            if j is None:
                fin.release()
            else:
                self.cur = j
                self.sems[j].release()

        self.threads = [threading.Thread(target=wrap, args=(i,)) for i in range(n)]
        for t in self.threads:
            t.start()
        self.active = True
        self.cur = 0
        self.sems[0].release()
        fin.acquire()
        self.active = False
        for t in self.threads:
            t.join()
        if self.err is not None:
            raise self.err


class Sched:
    def __init__(self):
        self.ops = {e: [] for e in ENGS}
        self.last_w = {}
        self.readers = {}
        self.waited = {e: {} for e in ENGS}
        self.dma_cnt = {}

    def _deps(self, eng, reads, writes):
        deps = []
        for r in reads:
            if r in self.last_w:
                deps.append((self.last_w[r], True))
        for w in writes:
            if w in self.last_w:
                deps.append((self.last_w[w], False))
            for t in self.readers.get(w, {}).values():
                deps.append((t, False))
        waits = []
        for d, raw in deps:
            if d[0] == "eng":
                if d[1] == eng and eng not in SYNC_SAME:
                    continue
                key = ("eng", d[1])
            else:
                key = ("dma", d[1])
            val = d[2]
            if self.waited[eng].get(key, -1) >= val:
                continue
            self.waited[eng][key] = val
            waits.append((key, val))
            if d[0] == "eng":
                self.ops[d[1]][val]["inc"] = True
        return waits

    def op(self, eng, fn, r=(), w=()):
        waits = self._deps(eng, r, w)
        idx = len(self.ops[eng])
        self.ops[eng].append(dict(fn=_bind(fn), waits=waits, inc=False, dma=None))
        tok = ("eng", eng, idx)
        for x in w:
            self.last_w[x] = tok
            self.readers[x] = {}
        for x in r:
            self.readers.setdefault(x, {})[("eng", eng)] = tok

    def dma(self, eng, fn, r=(), w=(), key=None):
        waits = self._deps(eng, r, w)
        prev = self.dma_cnt.get(key, 0)
        if prev and self.waited[eng].get(("dma", key), -1) < prev:
            self.waited[eng][("dma", key)] = prev
            waits.append((("dma", key), prev))
        cnt = prev + 16
        self.dma_cnt[key] = cnt
        self.ops[eng].append(dict(fn=_bind(fn), waits=waits, inc=False, dma=key))
        tok = ("dma", key, cnt)
        for x in w:
            self.last_w[x] = tok
            self.readers[x] = {}
        for x in r:
            self.readers.setdefault(x, {})[("dma", key)] = tok

    def barrier(self, nop_fns):
        self.nbar = getattr(self, "nbar", 0) + 1
        for e in ENGS:
            waits = []
            if e != "sp" and self.ops[e]:
                last = len(self.ops[e]) - 1
                while last >= 0 and (self.ops[e][last].get("bar") or self.ops[e][last]["dma"] is not None):
                    last -= 1
                if last >= 0:
                    self.ops[e][last]["inc"] = True
                    waits.append((("eng", e), last))
            if e == "sp":
                for k, c in self.dma_cnt.items():
                    if self.waited[e].get(("dma", k), -1) < c:
                        self.waited[e][("dma", k)] = c
                        waits.append((("dma", k), c))
            self.ops[e].append(dict(fn=nop_fns[e], waits=waits, inc=False, dma=None, bar=self.nbar))
        self.last_w = {}
        self.readers = {}

    def finish(self, nc, engines, sems, dma_sems):
        pref = {}
        for e in ENGS:
            c = 0
            arr = []
            for o in self.ops[e]:
                if o["inc"] and o["dma"] is None and not o.get("bar"):
                    c += 1
                arr.append(c)
            pref[e] = arr
        return pref


def build(cfg, debug=False):
    NSEQ, S, CAP = cfg["NSEQ"], cfg["S"], cfg["CAP"]
    TPS = S // 128
    NTILE = NSEQ * TPS
    TPC = NTILE * 128
    NSLOT = NE * CAP
    NST = NSLOT // 128
    CT = CAP // 128
    PH = cfg.get("phases", "1a,1b,2,3").split(",")
    CUT = cfg.get("cut", 99)

    nc = bass.Bass("TRN2", target_bir_lowering=False)
    dr = {}

    def din(name, shape, dt=F32):
        dr[name] = nc.dram_tensor(name, list(shape), dt, kind="ExternalInput").ap()
        return dr[name]

    def dscr(name, shape, dt=F32, out=False):
        dr[name] = nc.dram_tensor(name, list(shape), dt, kind=("ExternalOutput" if out else "Internal")).ap()
        return dr[name]

    x_d = din("x", [TPC, D])
    p_d = din("p", [TPC, 256])
    pos_d = din("posT", [128, NTILE], I32)
    win_d = din("w_in", [D, 4608])
    vec_d = din("vecs", [128, 70])
    cst_d = din("cst", [128, 832])
    wlora_d = din("wlora", [128, 512])
    wgu_d = din("wgu", [128, 512])
    wba_d = din("w_ba", [512, D])
    wbb_d = din("w_bb", [512, D])
    wout_d = din("w_out", [D, D])
    wr_d = din("w_r", [D, 36])
    br_d = din("b_r", [1, 36])
    wg_d = din("w_gate_e", [NE, D, DE])
    wu_d = din("w_up_e", [NE, D, DE])
    wd_d = din("w_down_e", [NE, DE, D])
    wpg_d = din("w_pg", [D, D])
    wpp_d = din("w_pp", [256, D])
    lnf_d = din("ln_final", [1, D])
    lnmoe_d = din("ln_moe_row", [1, D])
    out_d = dscr("out", [TPC, D], F32, out=True)
    yab_d = dscr("yab", [NTILE, 128, 8 * 128], BF16, out=debug)
    x1_d = dscr("x1s", [TPC, D], F32, out=debug)
    hs_d = dscr("hslots", [NSLOT, D], BF16, out=debug)
    ys_d = dscr("yslots", [NSLOT, D], F32, out=debug)
    if debug:
        rt_d = dscr("route", [128, NTILE * 4], F32, out=True)

    S_ = Sched()
    base0 = 229376 - int(nc.sbuf_bytes_remaining)
    base0 = (base0 + 63) // 64 * 64
    st = {"p": base0, "ph": None}

    def salloc(name, shape, dt):
        nb = int(np.prod(shape[1:])) * (4 if dt in (F32, I32) else 2)
        nb = (nb + 31) // 32 * 32
        t = nc.alloc_sbuf_tensor_at(name, list(shape), dt, offset=st["p"])
        st["p"] += nb
        assert st["p"] <= 229376 - 64, ("SBUF overflow", name, st["p"])
        return t

    psb = [nc.alloc_psum_tensor(f"psb{i}", [128, 1024], BF16) for i in range(2)]
    psf = [nc.alloc_psum_tensor(f"psf{i}", [128, 512], F32) for i in range(6)]
    rr = {"f": 0, "b": 0, "fA": 0, "fB": 0}
    tl = threading.local()

    def PS():
        pool = getattr(tl, "pool", None)
        if pool == "A":
            i = rr["fA"] % 2
            rr["fA"] += 1
        elif pool == "B":
            i = 2 + rr["fB"] % 4
            rr["fB"] += 1
        elif pool == "E":
            i = rr["fA"] % 3
            rr["fA"] += 1
        elif pool == "O":
            i = 3 + rr["fB"] % 3
            rr["fB"] += 1
        else:
            i = rr["f"] % 6
            rr["f"] += 1
        return psf[i], f"psf{i}"

    def PSB():
        pool = getattr(tl, "pool", None)
        if pool in ("A", "E"):
            i = 0
        elif pool in ("B", "O"):
            i = 1
        else:
            i = rr["b"] % 2
            rr["b"] += 1
        return psb[i], f"psb{i}"

    vec = salloc("vec", [128, 70], F32)
    cst = salloc("cstf", [128, 832], F32)
    identb = salloc("identb", [128, 128], BF16)
    bones = salloc("bones", [128, 128], BF16)
    onesb = salloc("onesb", [128, 128], BF16)
    onesf = salloc("onesf", [128, 128], F32)
    derived = salloc("derived", [128, 32], F32)
    posi = salloc("posi", [128, NTILE], I32)
    posf = salloc("posf", [128, NTILE], F32)
    slot_i = salloc("slot_i", [128, NTILE * 2], I32)
    rw_f = salloc("rw_f", [128, NTILE * 2], F32)
    scr = salloc("scr", [128, 64], F32)
    ph_base = st["p"]
    IDENTF = cst[:, 0:128]
    USTR = cst[:, 128:256]
    UINC = cst[:, 256:384]
    LSTR = cst[:, 384:512]
    INVF = cst[:, 512:576]
    OFFS = cst[:, 576:640]
    MASK2 = cst[:, 128:384]
    V_LNMIX, V_LNMOE, V_LNPLE, V_MU = 0, 8, 16, 24
    V_W0, V_A0, V_KK, V_KA, V_RK, V_LNW, V_LNB, V_SINK = 38, 42, 46, 50, 54, 58, 62, 66

    def bc(ap, shape):
        return ap.to_broadcast(list(shape))

    def vcol(c, n=1):
        return vec[:, c:c + n]

    WV = Weaver()
    GLOBAL_KEYS = {"vec", "cst", "identb", "bones", "onesb", "onesf", "derived", "posf", "posi",
                   "Wgt", "WbA", "WbB", "Wout", "Wr", "BR", "GMOE", "ustrb", "CNT", "hs_all", "ys_all",
                   "Wpg", "Wpp", "LNF", "WG0", "WG1", "WU0", "WU1", "WD0", "WD1"}
    GLOBAL_PREF = ("psf", "psb", "yab_d", "x1_d", "slot", "rw", "hs_sc", "out_d")

    def stream(sfx):
        def m(keys):
            return [k if (k in GLOBAL_KEYS or k.startswith(GLOBAL_PREF)) else k + sfx for k in keys]

        def op_(eng, fn, r=(), w=()):
            op(eng, fn, m(r), m(w))

        def dma_(eng, fn, r=(), w=(), key=None):
            dma(eng, fn, m(r), m(w), key)
        return op_, dma_

    def op(eng, fn, r=(), w=()):
        S_.op(eng, fn, r, w)
        WV.tick()
    op_glob = op

    def dma(eng, fn, r=(), w=(), key=None):
        S_.dma(eng, fn, r, w, key)
        WV.tick()

    dma("sp", lambda e: e.dma_start(out=vec[:], in_=vec_d), w=["vec"], key="vec")
    dma("sp", lambda e: e.dma_start(out=cst[:], in_=cst_d), w=["cst"], key="cst")
    dma("sp", lambda e: e.dma_start(out=posi[:], in_=pos_d), w=["posi"], key="posi")
    op("dve", lambda e: e.tensor_copy(out=identb[:], in_=IDENTF), r=["cst"], w=["identb"])
    op("dve", lambda e: e.tensor_copy(out=bones[:], in_=cst[:, 640:768]), r=["cst"], w=["bones"])
    op("dve", lambda e: e.memset(onesb[:], 1.0), w=["onesb"])
    op("dve", lambda e: e.memset(onesf[:], 1.0), w=["onesf"])
    op("dve", lambda e: e.tensor_copy(out=posf[:], in_=posi[:]), r=["posi"], w=["posf"])
    op("dve", lambda e: e.tensor_scalar(out=derived[:, 0:14], in0=vcol(V_MU, 14), scalar1=-1.0, scalar2=1.0,
                                        op0=ALU.mult, op1=ALU.add), r=["vec"], w=["derived"])
    op("dve", lambda e: e.tensor_scalar(out=derived[:, 14:18], in0=vcol(V_KA, 4), scalar1=-1.0, scalar2=1.0,
                                        op0=ALU.mult, op1=ALU.add), r=["vec"], w=["derived"])
    op("act", lambda e: e.activation(out=derived[:, 18:22], in_=vcol(V_SINK, 4), func=AF.Exp), r=["vec", "derived"],
       w=["derived"])
    OMU = lambda c, n=1: derived[:, c:c + n]
    OMKA = lambda c, n=1: derived[:, 14 + c:14 + c + n]
    ESINK = derived[:, 18:22]

    nop_fns = {
        "pe": lambda e: e.nop(), "act": lambda e: e.nop(), "dve": lambda e: e.nop(),
        "pool": lambda e: e.nop(), "sp": lambda e: e.nop(),
    }

    def phase_reset():
        S_.barrier(nop_fns)
        st["p"] = ph_base

    def load_cast_weight(dst, dst_key, src_ap, rows, cols, gcol, stage, stage_key, kchunks, eng_cycle):
        for kc in range(kchunks):
            sl = stage[kc % len(stage)]
            sk = stage_key[kc % len(stage)]
            dma("sp", lambda e, sl=sl, kc=kc: e.dma_start(out=sl[:, 0:cols], in_=src_ap[kc * 128:(kc + 1) * 128, :]),
                w=[sk], key=sk)
            eng = eng_cycle[kc % len(eng_cycle)]
            if gcol is None:
                op(eng, lambda e, sl=sl, kc=kc: e.tensor_copy(out=dst[:, kc, :], in_=sl[:, 0:cols]),
                   r=[sk], w=[dst_key])
            else:
                op(eng, lambda e, sl=sl, kc=kc: e.tensor_scalar(out=dst[:, kc, :], in0=sl[:, 0:cols],
                                                                 scalar1=vcol(gcol + kc), scalar2=None, op0=ALU.mult),
                   r=[sk, "vec"], w=[dst_key])

    def rms_to_hT(ti, xin, xin_key, hT, hT_key, tmp, normalize=True, op=None):
        op = op or op_glob
        junk, ss, rstd, xbf = tmp["junk"], tmp["ss"], tmp["rstd"], tmp["xbf"]
        op("act", lambda e: e.activation(out=junk[:], in_=xin[:], func=AF.Square, accum_out=ss[:, 0:1]),
           r=[xin_key], w=["junk", "ss"])
        op("dve", lambda e: e.tensor_scalar(out=ss[:, 1:2], in0=ss[:, 0:1], scalar1=1.0 / D, scalar2=1e-6,
                                            op0=ALU.mult, op1=ALU.add), r=["ss"], w=["ss1"])
        op("act", lambda e: e.activation(out=ss[:, 2:3], in_=ss[:, 1:2], func=AF.Sqrt), r=["ss1"], w=["ss2"])
        op("dve", lambda e: e.reciprocal(out=rstd[:, 0:1], in_=ss[:, 2:3]), r=["ss2"], w=["rstd"])
        if normalize:
            op("dve", lambda e: e.tensor_scalar(out=xbf[:], in0=xin[:], scalar1=rstd[:, 0:1], scalar2=None,
                                                op0=ALU.mult), r=[xin_key, "rstd"], w=["xbf"])
        else:
            op("pool", lambda e: e.tensor_copy(out=xbf[:], in_=xin[:]), r=[xin_key], w=["xbf"])
        pb, pk = PSB()
        for kc in range(8):
            op("pe", lambda e, kc=kc: e.transpose(pb[:, kc * 128:(kc + 1) * 128], xbf[:, kc * 128:(kc + 1) * 128],
                                                  identb[:]), r=["xbf", "identb"], w=[pk])
        op("act", lambda e: e.activation(out=hT[:].rearrange("p k t -> p (k t)"), in_=pb[:, :], func=AF.Copy),
           r=[pk], w=[hT_key])

    if "1a" in PH:
        Wqkv = salloc("Wqkv", [128, 8, 768], BF16)
        Wrw = salloc("Wrw", [128, 8, 1792], BF16)
        Wlora = salloc("Wlora", [128, 512], BF16)
        Wgu = salloc("Wgu", [128, 512], BF16)
        stg = [salloc("stgA", [128, 2560], F32)]
        for kc in range(8):
            dma("sp", lambda e, kc=kc: e.dma_start(out=stg[0][:, 0:2560], in_=win_d[kc * 128:(kc + 1) * 128, 0:2560]),
                w=["stgA"], key="stgA")
            op("dve", lambda e, kc=kc: e.tensor_scalar(out=Wqkv[:, kc, :], in0=stg[0][:, 0:768],
                                                       scalar1=vcol(V_LNMIX + kc), scalar2=None, op0=ALU.mult),
               r=["stgA", "vec"], w=["Wqkv"])
            op("pool", lambda e, kc=kc: e.tensor_scalar(out=Wrw[:, kc, :], in0=stg[0][:, 768:2560],
                                                        scalar1=vcol(V_LNMIX + kc), scalar2=1.0, op0=ALU.mult,
                                                        op1=ALU.mult),
               r=["stgA", "vec"], w=["Wrw"])
        dma("sp", lambda e: e.dma_start(out=stg[0][:, 0:512], in_=wlora_d), w=["stgA"], key="stgA")
        op("dve", lambda e: e.tensor_copy(out=Wlora[:], in_=stg[0][:, 0:512]), r=["stgA"], w=["Wlora"])
        dma("sp", lambda e: e.dma_start(out=stg[0][:, 512:1024], in_=wgu_d), w=["stgA"], key="stgA")
        op("dve", lambda e: e.tensor_copy(out=Wgu[:], in_=stg[0][:, 512:1024]), r=["stgA"], w=["Wgu"])

        xin = [salloc(f"xin{i}", [128, D], F32) for i in range(2)]
        tmp = dict(junk=salloc("junk", [128, D], BF16), ss=salloc("ss", [128, 4], F32),
                   rstd=salloc("rstd", [128, 1], F32), xbf=salloc("xbf", [128, D], BF16))
        hT = [salloc(f"hT{i}", [128, 8, 128], BF16) for i in range(2)]
        ropeT = salloc("ropeT", [128, 64], F32)
        ropeN = salloc("ropeN", [128, 64], I32)
        ropeF = salloc("ropeF", [128, 64], F32)
        ropeG = salloc("ropeG", [128, 64], F32)
        CS = salloc("CS", [128, 64], F32)
        ropA = salloc("ropA", [128, 640], F32)
        ropB = salloc("ropB", [128, 640], F32)
        qkr = salloc("qkr", [128, 640], BF16)
        qT = salloc("qT", [128, 4, 128], BF16)
        kTs = [salloc(f"kT{i}", [128, 128], BF16) for i in range(2)]
        vts = [salloc(f"vtok{i}", [128, 128], BF16) for i in range(2)]
        Eb = [salloc(f"Eb{i}", [128, 512], BF16) for i in range(4)]
        dent = salloc("dent", [128, 4, 128], F32)
        yab = [salloc(f"yabs{i}", [128, 8, 128], BF16) for i in range(2)]
        zb = [salloc(f"zb{i}", [128, 4, 129], F32) for i in range(2)]
        zt1 = salloc("zt1", [128, 4, 128], F32)
        zt2 = salloc("zt2", [128, 4, 128], F32)
        carry = salloc("carry", [128, 16], F32)
        Rr = salloc("Rr", [128, 4, 128], F32)
        Kr = salloc("Kr", [128, 4, 128], F32)
        Vr = salloc("Vr", [128, 4, 128], F32)
        XM = salloc("XM", [128, 2, 128], F32)
        LIN = salloc("LIN", [128, 128], BF16)
        SXG = salloc("SXG", [128, 128], BF16)
        SG = salloc("SG", [128, 4, 128], F32)
        Aa = salloc("Aa", [128, 4, 128], F32)
        Gg = salloc("Gg", [128, 4, 128], F32)
        LW = salloc("LW", [128, 4, 128], F32)
        CUM = salloc("CUM", [128, 4, 128], F32)
        CX = salloc("CX", [128, 4, 128], F32)
        E1 = salloc("E1", [128, 4, 128], F32)
        E2 = salloc("E2", [128, 4, 128], F32)
        E3 = salloc("E3", [128, 4, 128], F32)
        E4 = salloc("E4", [128, 4, 128], F32)
        KKR = salloc("KKR", [128, 4, 128], F32)
        SQb = salloc("SQb", [128, 4, 128], BF16)
        RN = salloc("RN", [128, 4, 128], F32)
        KK = salloc("KK", [128, 4, 128], F32)
        T1 = salloc("T1", [128, 4, 128], F32)
        KP = salloc("KP", [128, 4, 128], F32)
        Bb = salloc("Bb", [128, 4, 128], F32)
        AR = salloc("AR", [128, 4, 2, 128], BF16)
        BK = salloc("BK", [128, 4, 2, 128], BF16)
        BKS = salloc("BKS", [128, 4, 2, 128], BF16)
        ARm = [salloc(f"ARm{i}", [128, 4, 2, 128], BF16) for i in range(2)]
        BKm = [salloc(f"BKm{i}", [128, 4, 2, 128], BF16) for i in range(2)]
        RK = salloc("RK", [128, 4, 128], F32)
        RK2 = salloc("RK2", [128, 4, 128], BF16)
        BV = salloc("BV", [128, 4, 128], F32)
        VB = salloc("VB", [128, 4, 128], BF16)
        BKT = salloc("BKT", [128, 1024], BF16)
        VT = salloc("VT", [128, 512], BF16)
        MA = salloc("MA", [128, 8, 256], BF16)
        MB = salloc("MB", [128, 8, 256], BF16)
        Qm = [salloc(f"Qm{i}", [128, 8, 128], BF16) for i in range(2)]
        PX = [salloc(f"PX{i}", [128, 8, 256], BF16) for i in range(2)]
        XF = salloc("XF", [128, 8, 128], BF16)
        SF = salloc("SF", [128, 4, 64], F32)
        SBs = salloc("SBs", [128, 4, 64], BF16)
        TMPS = salloc("TMPS", [128, 4, 64], F32)
        RH = salloc("RH", [128, 512], BF16)
        UT = salloc("UT", [128, 512], BF16)
        Yf = salloc("Yf", [128, 4, 128], F32)
        YB = salloc("YB", [128, 4, 128], BF16)
        YSQ = salloc("YSQ", [128, 4, 128], BF16)
        MEAN = salloc("MEAN", [128, 4, 128], F32)
        M2 = salloc("M2", [128, 4, 128], F32)
        VAR = salloc("VAR", [128, 4, 128], F32)
        Dd = salloc("Dd", [128, 4, 128], F32)

        def f2(t):
            return t[:].rearrange("p a b -> p (a b)")

        def genA(ti):
            tl.pool = "A"
            tj = ti % TPS
            sl = ti % 2
            xk = f"xin{sl}"
            dma("sp", lambda e, ti=ti, sl=sl: e.dma_start(out=xin[sl][:], in_=x_d[ti * 128:(ti + 1) * 128, :]),
                w=[xk], key=xk)
            hk = f"hT{sl}"
            rms_to_hT(ti, xin[sl], xk, hT[sl], hk, tmp)
            h = hT[sl]
            if CUT <= 1:
                return
            pq, pqk = PS()
            pkv, pkvk = PS()
            for kc in range(8):
                op("pe", lambda e, kc=kc: e.matmul(pq[:, 0:512], h[:, kc, :], Wqkv[:, kc, 0:512],
                                                   start=(kc == 0), stop=(kc == 7)), r=[hk, "Wqkv"], w=[pqk])
            for kc in range(8):
                op("pe", lambda e, kc=kc: e.matmul(pkv[:, 0:256], h[:, kc, :], Wqkv[:, kc, 512:768],
                                                   start=(kc == 0), stop=(kc == 7)), r=[hk, "Wqkv"], w=[pkvk])
            op("dve", lambda e, ti=ti: e.scalar_tensor_tensor(out=ropeT[:], in0=INVF, scalar=posf[:, ti:ti + 1],
                                                              in1=OFFS, op0=ALU.mult, op1=ALU.add),
               r=["cst", "posf"], w=["ropeT"])
            op("dve", lambda e: e.tensor_copy(out=ropeN[:], in_=ropeT[:]), r=["ropeT"], w=["ropeN"])
            op("dve", lambda e: e.tensor_copy(out=ropeF[:], in_=ropeN[:]), r=["ropeN"], w=["ropeF"])
            op("dve", lambda e: e.tensor_tensor(out=ropeF[:], in0=ropeT[:], in1=ropeF[:], op=ALU.subtract),
               r=["ropeT", "ropeF"], w=["ropeF"])
            op("dve", lambda e: e.tensor_single_scalar(out=ropeG[:], in_=ropeF[:], scalar=0.5, op=ALU.is_gt),
               r=["ropeF"], w=["ropeG"])
            op("dve", lambda e: e.tensor_tensor(out=ropeF[:], in0=ropeF[:], in1=ropeG[:], op=ALU.subtract),
               r=["ropeF", "ropeG"], w=["ropeF"])
            op("act", lambda e: e.activation(out=CS[:], in_=ropeF[:], func=AF.Sin, scale=2.0 * np.pi),
               r=["ropeF"], w=["CS"])
            if CUT <= 2:
                return
            for (src, skey, c0, H) in ((pq, pqk, 0, 8), (pkv, pkvk, 512, 2)):
                W_ = H * 64
                s4 = src[:, 0:W_].rearrange("p (h t d) -> p h t d", h=H, t=2)
                A4 = ropA[:, c0:c0 + W_].rearrange("p (h t d) -> p h t d", h=H, t=2)
                B4 = ropB[:, c0:c0 + W_].rearrange("p (h t d) -> p h t d", h=H, t=2)
                O4 = qkr[:, c0:c0 + W_].rearrange("p (h t d) -> p h t d", h=H, t=2)
                cosb = CS[:, 32:64].unsqueeze(1).unsqueeze(1).to_broadcast([128, H, 2, 32])
                sinb = CS[:, 0:32].unsqueeze(1).to_broadcast([128, H, 32])
                op("dve", lambda e, s4=s4, A4=A4, cosb=cosb: e.tensor_tensor(out=A4, in0=s4, in1=cosb, op=ALU.mult),
                   r=[skey, "CS"], w=["ropA"])
                op("dve", lambda e, s4=s4, B4=B4, sinb=sinb: e.tensor_tensor(out=B4[:, :, 0, :], in0=s4[:, :, 1, :],
                                                                           in1=sinb, op=ALU.mult),
                   r=[skey, "CS"], w=["ropB"])
                op("dve", lambda e, s4=s4, B4=B4, sinb=sinb: e.tensor_tensor(out=B4[:, :, 1, :], in0=s4[:, :, 0, :],
                                                                           in1=sinb, op=ALU.mult),
                   r=[skey, "CS"], w=["ropB"])
                op("pool", lambda e, A4=A4, B4=B4, O4=O4: e.tensor_tensor(out=O4[:, :, 0, :], in0=A4[:, :, 0, :],
                                                                         in1=B4[:, :, 0, :], op=ALU.subtract),
                   r=["ropA", "ropB"], w=["qkr"])
                op("pool", lambda e, A4=A4, B4=B4, O4=O4: e.tensor_tensor(out=O4[:, :, 1, :], in0=A4[:, :, 1, :],
                                                                         in1=B4[:, :, 1, :], op=ALU.add),
                   r=["ropA", "ropB"], w=["qkr"])
            vk = f"vtok{sl}"
            kk_ = f"kT{sl}"
            op("act", lambda e, sl=sl: e.activation(out=vts[sl][:], in_=pkv[:, 128:256], func=AF.Copy),
               r=[pkvk], w=[vk])
            pb, pbk = PSB()
            for c in range(5):
                op("pe", lambda e, c=c: e.transpose(pb[:, c * 128:(c + 1) * 128], qkr[:, c * 128:(c + 1) * 128],
                                                    identb[:]), r=["qkr", "identb"], w=[pbk])
            op("act", lambda e: e.activation(out=f2(qT), in_=pb[:, 0:512], func=AF.Copy), r=[pbk], w=["qT"])
            op("act", lambda e, sl=sl: e.activation(out=kTs[sl][:], in_=pb[:, 512:640], func=AF.Copy),
               r=[pbk], w=[kk_])
            if CUT <= 3:
                return
            kbs = ([1 - sl] if tj > 0 else []) + [sl]
            ei = 0
            Euse = {}
            for g in range(2):
                for kb in kbs:
                    pe_, pek = PS()
                    op("pe", lambda e, g=g, kb=kb, pe_=pe_: e.matmul(
                        pe_[:, 0:512], kTs[kb][g * 64:(g + 1) * 64, :], qT[g * 64:(g + 1) * 64, :, :],
                        start=True, stop=True), r=[f"kT{kb}", "qT"], w=[pek])
                    Et = Eb[ei]
                    ek = f"Eb{ei}"
                    ei += 1
                    op("act", lambda e, Et=Et, pe_=pe_: e.activation(out=Et[:], in_=pe_[:, 0:512], func=AF.Exp,
                                                                    scale=0.125), r=[pek], w=[ek])
                    msk = UINC if kb == sl else LSTR
                    op("pool", lambda e, Et=Et, msk=msk: e.tensor_tensor(
                        out=Et[:].rearrange("p (c q) -> p c q", c=4), in0=Et[:].rearrange("p (c q) -> p c q", c=4),
                        in1=msk.unsqueeze(1).to_broadcast([128, 4, 128]), op=ALU.mult), r=[ek, "cst"], w=[ek])
                    Euse[(g, kb)] = (Et, ek)
            po, pok = PS()
            pd, pdk = PS()
            for g in range(2):
                for i, kb in enumerate(kbs):
                    Et, ek = Euse[(g, kb)]
                    op("pe", lambda e, g=g, kb=kb, Et=Et, i=i: e.matmul(
                        po[g * 64:(g + 1) * 64, 0:512], vts[kb][:, g * 64:(g + 1) * 64], Et[:],
                        start=(i == 0), stop=(i == len(kbs) - 1)), r=[f"vtok{kb}", ek], w=[pok])
                for i, kb in enumerate(kbs):
                    Et, ek = Euse[(g, kb)]
                    op("pe", lambda e, g=g, Et=Et, i=i: e.matmul(
                        pd[g * 64:(g + 1) * 64, 0:512], onesb[:, 0:64], Et[:],
                        start=(i == 0), stop=(i == len(kbs) - 1)), r=["onesb", ek], w=[pdk])
            ys = yab[sl]
            yk = f"yabs{sl}"
            op("dve", lambda e: e.tensor_tensor(out=dent[:], in0=pd[:, 0:512].rearrange("p (c q) -> p c q", c=4),
                                                in1=ESINK.unsqueeze(2).to_broadcast([128, 4, 128]), op=ALU.add),
               r=[pdk, "derived"], w=["dent"])
            op("dve", lambda e: e.reciprocal(out=dent[:], in_=dent[:]), r=["dent"], w=["dent"])
            op("dve", lambda e, ys=ys: e.tensor_tensor(out=ys[:, 0:4, :],
                                                       in0=po[:, 0:512].rearrange("p (c q) -> p c q", c=4),
                                                       in1=dent[:], op=ALU.mult), r=[pok, "dent"], w=[yk + "a"])


        def genBC(ti):
            tl.pool = "B"
            tj = ti % TPS
            sl = ti % 2
            hk = f"hT{sl}"
            h = hT[sl]
            ys = yab[sl]
            yk = f"yabs{sl}"
            if CUT <= 4:
                return
            if tj == 0:
                op("pool", lambda e: e.memset(carry[:], 0.0), w=["carry"])
                op("pool", lambda e: e.memset(SF[:], 0.0), w=["SF"])
                op("pool", lambda e: e.memset(SBs[:], 0.0), w=["SBs"])
            groups = [(0, 4, Rr, "Rr"), (4, 4, Kr, "Kr"), (8, 4, Vr, "Vr"), (12, 2, XM, "XM")]
            for gi, (z0, n, dst, dk) in enumerate(groups):
                pz, pzk = PS()
                for j in range(n):
                    zc = z0 + j
                    for kc in range(8):
                        op("pe", lambda e, j=j, zc=zc, kc=kc, pz=pz: e.matmul(
                            pz[:, j * 128:(j + 1) * 128], Wrw[:, kc, zc * 128:(zc + 1) * 128], h[:, kc, :],
                            start=(kc == 0), stop=(kc == 7)), r=[hk, "Wrw"], w=[pzk])
                zbt = zb[gi % 2]
                zk = f"zb{gi % 2}"
                op("act", lambda e, zbt=zbt, pz=pz, n=n: e.activation(
                    out=zbt[:, 0:n, 1:129], in_=pz[:, 0:n * 128].rearrange("p (c t) -> p c t", c=n), func=AF.Copy),
                   r=[pzk], w=[zk])
                op("pool", lambda e, zbt=zbt, z0=z0, n=n: e.tensor_copy(out=zbt[:, 0:n, 0], in_=carry[:, z0:z0 + n]),
                   r=["carry"], w=[zk])
                op("dve", lambda e, zbt=zbt, z0=z0, n=n: e.tensor_tensor(
                    out=zt1[:, 0:n, :], in0=zbt[:, 0:n, 0:128],
                    in1=vcol(V_MU + z0, n).unsqueeze(2).to_broadcast([128, n, 128]), op=ALU.mult),
                   r=[zk, "vec"], w=["zt1"])
                op("pool", lambda e, zbt=zbt, z0=z0, n=n: e.tensor_tensor(
                    out=zt2[:, 0:n, :], in0=zbt[:, 0:n, 1:129],
                    in1=OMU(z0, n).unsqueeze(2).to_broadcast([128, n, 128]), op=ALU.mult),
                   r=[zk, "derived"], w=["zt2"])
                op("dve", lambda e, dst=dst, n=n: e.tensor_tensor(out=dst[:, 0:n, :], in0=zt1[:, 0:n, :],
                                                                  in1=zt2[:, 0:n, :], op=ALU.add),
                   r=["zt1", "zt2"], w=[dk])
                op("pool", lambda e, zbt=zbt, z0=z0, n=n: e.tensor_copy(out=carry[:, z0:z0 + n], in_=zbt[:, 0:n, 128]),
                   r=[zk], w=["carry"])
            if CUT <= 5:
                return
            op("act", lambda e: e.activation(out=LIN[0:64, :], in_=XM[0:64, 0, :], func=AF.Tanh), r=["XM"], w=["LINa"])
            op("pool", lambda e: e.tensor_copy(out=LIN[64:128, :], in_=XM[64:128, 0, :]), r=["XM"], w=["LINb"])
            op("act", lambda e: e.activation(out=SXG[:], in_=XM[:, 1, :], func=AF.Sigmoid), r=["XM"], w=["SXG"])
            pu, puk = PS()
            pa, pak = PS()
            pg, pgk = PS()
            for cc in range(4):
                op("pe", lambda e, cc=cc: e.matmul(pu[:, cc * 128:(cc + 1) * 128], Wlora[0:64, cc * 128:(cc + 1) * 128],
                                                   LIN[0:64, :], start=True, stop=True),
                   r=["Wlora", "LINa"], w=[puk])
            for cc in range(4):
                op("pe", lambda e, cc=cc: e.matmul(pa[:, cc * 128:(cc + 1) * 128],
                                                   Wlora[64:128, cc * 128:(cc + 1) * 128],
                                                   LIN[64:128, :], start=True, stop=True),
                   r=["Wlora", "LINb"], w=[pak])
            for cc in range(4):
                op("pe", lambda e, cc=cc: e.matmul(pg[:, cc * 128:(cc + 1) * 128], Wgu[:, cc * 128:(cc + 1) * 128],
                                                   SXG[:], start=True, stop=True), r=["Wgu", "SXG"], w=[pgk])
            for cc in range(4):
                op("act", lambda e, cc=cc: e.activation(out=SG[:, cc, :], in_=pu[:, cc * 128:(cc + 1) * 128],
                                                        func=AF.Sigmoid, bias=vcol(V_W0 + cc)),
                   r=[puk, "vec"], w=["SG"])
            for cc in range(4):
                op("act", lambda e, cc=cc: e.activation(out=Aa[:, cc, :], in_=pa[:, cc * 128:(cc + 1) * 128],
                                                        func=AF.Sigmoid, bias=vcol(V_A0 + cc)),
                   r=[pak, "vec"], w=["Aa"])
            op("act", lambda e: e.activation(out=f2(Gg), in_=pg[:, 0:512], func=AF.Copy), r=[pgk], w=["Gg"])
            op("act", lambda e: e.activation(out=f2(LW), in_=f2(SG), func=AF.Copy, scale=-0.6065306597126334),
               r=["SG"], w=["LW"])
            for cc in range(4):
                op("dve", lambda e, cc=cc: e.tensor_tensor_scan(out=CUM[:, cc, :], data0=onesf[:], data1=LW[:, cc, :],
                                                                initial=0.0, op0=ALU.mult, op1=ALU.add),
                   r=["onesf", "LW"], w=["CUM"])
            op("dve", lambda e: e.tensor_tensor(out=f2(CX), in0=f2(CUM), in1=f2(LW), op=ALU.subtract),
               r=["CUM", "LW"], w=["CX"])
            op("act", lambda e: e.activation(out=f2(E1), in_=f2(CUM), func=AF.Exp), r=["CUM"], w=["E1"])
            op("act", lambda e: e.activation(out=f2(E2), in_=f2(CUM), func=AF.Exp, scale=-1.0), r=["CUM"], w=["E2"])
            op("act", lambda e: e.activation(out=f2(E3), in_=f2(CX), func=AF.Exp), r=["CX"], w=["E3"])
            for cc in range(4):
                op("act", lambda e, cc=cc: e.activation(out=E4[:, cc, :], in_=CUM[:, cc, :], func=AF.Exp, scale=-1.0,
                                                        bias=CUM[:, cc, 127:128]), r=["CUM"], w=["E4"])
            op("dve", lambda e: e.tensor_tensor(out=KKR[:], in0=Kr[:],
                                                 in1=vcol(V_KK, 4).unsqueeze(2).to_broadcast([128, 4, 128]),
                                                 op=ALU.mult), r=["Kr", "vec"], w=["KKR"])
            op("act", lambda e: e.activation(out=f2(SQb), in_=f2(KKR), func=AF.Square), r=["KKR"], w=["SQb"])
            pss, pssk = PS()
            op("pe", lambda e: e.matmul(pss[:, 0:512], bones[:], f2(SQb), start=True, stop=True),
               r=["bones", "SQb"], w=[pssk])
            op("act", lambda e: e.activation(out=f2(RN), in_=pss[:, 0:512], func=AF.Ln, bias=1e-19),
               r=[pssk], w=["RN"])
            op("act", lambda e: e.activation(out=f2(RN), in_=f2(RN), func=AF.Exp, scale=-0.5), r=["RN"], w=["RN"])
            op("dve", lambda e: e.tensor_tensor(out=f2(KK), in0=f2(KKR), in1=f2(RN), op=ALU.mult),
               r=["KKR", "RN"], w=["KK"])
            for cc in range(4):
                op("dve", lambda e, cc=cc: e.tensor_scalar(out=T1[:, cc, :], in0=Aa[:, cc, :],
                                                           scalar1=vcol(V_KA + cc), scalar2=OMKA(cc),
                                                           op0=ALU.mult, op1=ALU.add),
                   r=["Aa", "vec", "derived"], w=["T1"])
            op("dve", lambda e: e.tensor_tensor(out=f2(KP), in0=f2(Kr), in1=f2(T1), op=ALU.mult),
               r=["Kr", "T1"], w=["KP"])
            op("dve", lambda e: e.tensor_tensor(out=f2(Bb), in0=f2(KK), in1=f2(Aa), op=ALU.mult),
               r=["KK", "Aa"], w=["Bb"])
            op("dve", lambda e: e.scalar_tensor_tensor(out=AR[:, :, 0, :], in0=E3[:], scalar=-1.0, in1=KK[:],
                                                       op0=ALU.mult, op1=ALU.mult), r=["E3", "KK"], w=["AR"])
            op("dve", lambda e: e.tensor_tensor(out=AR[:, :, 1, :], in0=E1[:], in1=Rr[:], op=ALU.mult),
               r=["E1", "Rr", "AR"], w=["AR"])
            op("dve", lambda e: e.tensor_tensor(out=BK[:, :, 0, :], in0=E2[:], in1=Bb[:], op=ALU.mult),
               r=["E2", "Bb"], w=["BK"])
            op("dve", lambda e: e.tensor_tensor(out=BK[:, :, 1, :], in0=E2[:], in1=KP[:], op=ALU.mult),
               r=["E2", "KP", "BK"], w=["BK"])
            op("dve", lambda e: e.tensor_tensor(out=BKS[:, :, 0, :], in0=E4[:], in1=Bb[:], op=ALU.mult),
               r=["E4", "Bb"], w=["BKS"])
            op("dve", lambda e: e.tensor_tensor(out=BKS[:, :, 1, :], in0=E4[:], in1=KP[:], op=ALU.mult),
               r=["E4", "KP", "BKS"], w=["BKS"])
            for par in range(2):
                pmc = cst[:, 640 + 64 * par:641 + 64 * par]
                op("act", lambda e: e.activation(
                    out=ARm[par][:].rearrange("p a b c -> p (a b c)"), in_=AR[:].rearrange("p a b c -> p (a b c)"),
                    func=AF.Copy, scale=pmc), r=["AR", "cst"], w=[f"ARm{par}"])
                op("act" if par else "dve", (lambda e: e.activation(
                    out=BKm[par][:].rearrange("p a b c -> p (a b c)"), in_=BK[:].rearrange("p a b c -> p (a b c)"),
                    func=AF.Copy, scale=pmc)) if par else (lambda e: e.tensor_scalar(
                    out=BKm[par][:].rearrange("p a b c -> p (a b c)"), in0=BK[:].rearrange("p a b c -> p (a b c)"),
                    scalar1=pmc, scalar2=None, op0=ALU.mult)), r=["BK", "cst"], w=[f"BKm{par}"])
            op("pool", lambda e: e.tensor_tensor(out=f2(RK), in0=f2(Rr), in1=f2(KP), op=ALU.mult),
               r=["Rr", "KP"], w=["RK"])
            op("pool", lambda e: e.tensor_tensor(out=RK2[:], in0=RK[:],
                                                 in1=vcol(V_RK, 4).unsqueeze(2).to_broadcast([128, 4, 128]),
                                                 op=ALU.mult), r=["RK", "vec"], w=["RK2"])
            pbn, pbnk = PS()
            op("pe", lambda e: e.matmul(pbn[:, 0:512], bones[:], f2(RK2), start=True, stop=True),
               r=["bones", "RK2"], w=[pbnk])
            op("dve", lambda e: e.tensor_tensor(out=f2(BV), in0=pbn[:, 0:512], in1=f2(Vr), op=ALU.mult),
               r=[pbnk, "Vr"], w=["BV"])
            op("act", lambda e: e.activation(out=f2(VB), in_=f2(Vr), func=AF.Copy), r=["Vr"], w=["VB"])
            pb1, pb1k = PSB()
            for j in range(2):
                for cc in range(4):
                    op("pe", lambda e, j=j, cc=cc: e.transpose(pb1[:, j * 512 + cc * 128: j * 512 + (cc + 1) * 128],
                                                               BKS[:, cc, j, :], identb[:]),
                       r=["BKS", "identb"], w=[pb1k])
            op("act", lambda e: e.activation(out=BKT[:], in_=pb1[:, :], func=AF.Copy), r=[pb1k], w=["BKT"])
            pb2, pb2k = PSB()
            for cc in range(4):
                op("pe", lambda e, cc=cc: e.transpose(pb2[:, cc * 128:(cc + 1) * 128], VB[:, cc, :], identb[:]),
                   r=["VB", "identb"], w=[pb2k])
            op("act", lambda e: e.activation(out=VT[:], in_=pb2[:, 0:512], func=AF.Copy), r=[pb2k], w=["VT"])
            if CUT <= 6:
                return
            def inv_s0(hh):
                heads = list(range(4 * hh, 4 * hh + 4))
                hs = slice(4 * hh, 4 * hh + 4)
                mk2 = MASK2.unsqueeze(1).to_broadcast([128, 2, 256])
                for (which, dstM) in ((0, MA), (1, MB)):
                    pM_ = [PS(), PS()]
                    for i, hd in enumerate(heads):
                        cc = hd // 2
                        c0 = (i % 2) * 256
                        par = hd % 2
                        op("pe", lambda e: e.matmul(
                            pM_[i // 2][0][:, c0:c0 + 256], BKm[par][:, cc, which, :],
                            AR[:, cc, :, :].rearrange("p a t -> p (a t)"), start=True, stop=True),
                           r=[f"BKm{par}", "AR"], w=[pM_[i // 2][1]])
                    for b2 in range(2):
                        h2 = slice(4 * hh + 2 * b2, 4 * hh + 2 * b2 + 2)
                        op("dve", lambda e: e.tensor_tensor(
                            out=dstM[:, h2, :], in0=pM_[b2][0][:, 0:512].rearrange("p (h c) -> p h c", h=2), in1=mk2,
                            op=ALU.mult), r=[pM_[b2][1], "cst"], w=[("MA" if which == 0 else "MB") + str(hh)])
                pQ0 = PS()
                for i, hd in enumerate(heads):
                    cc = hd // 2
                    par = hd % 2
                    op("pe", lambda e: e.matmul(
                        pQ0[0][:, i * 128:(i + 1) * 128], ARm[par][:, cc, 0, :], BK[:, cc, 0, :], start=True, stop=True),
                       r=["BK", f"ARm{par}"], w=[pQ0[1]])
                op("dve", lambda e: e.tensor_tensor(
                    out=Qm[0][:, hs, :], in0=pQ0[0][:, 0:512].rearrange("p (h c) -> p h c", h=4),
                    in1=LSTR.unsqueeze(1).to_broadcast([128, 4, 128]), op=ALU.mult),
                   r=[pQ0[1], "cst"], w=[f"Q0_{hh}"])

            def inv_l0(hh):
                heads = list(range(4 * hh, 4 * hh + 4))
                hs = slice(4 * hh, 4 * hh + 4)
                pP = PS()
                pQn = PS()
                for i, hd in enumerate(heads):
                    op("pe", lambda e, i=i, hd=hd: e.matmul(pP[0][:, i * 128:(i + 1) * 128], Qm[0][:, hd, :],
                                                            MA[:, hd, 0:128], start=True, stop=True),
                       r=[f"Q0_{hh}", f"MA{hh}"], w=[pP[1]])
                    op("pe", lambda e, i=i, hd=hd: e.matmul(pQn[0][:, i * 128:(i + 1) * 128], MA[:, hd, 0:128],
                                                            Qm[0][:, hd, :], start=True, stop=True),
                       r=[f"Q0_{hh}", f"MA{hh}"], w=[pQn[1]])
                op("act", lambda e, hs=hs: e.activation(out=PX[1][:, hs, 0:128],
                                                        in_=pP[0][:, 0:512].rearrange("p (h c) -> p h c", h=4),
                                                        func=AF.Copy), r=[pP[1]], w=[f"PX1_{hh}"])
                op("act", lambda e, hs=hs: e.activation(out=Qm[1][:, hs, :],
                                                        in_=pQn[0][:, 0:512].rearrange("p (h c) -> p h c", h=4),
                                                        func=AF.Copy), r=[pQn[1]], w=[f"Q1_{hh}"])
                op("pool", lambda e, hs=hs: e.tensor_tensor(out=PX[1][:, hs, 128:256], in0=MA[:, hs, 0:128],
                                                            in1=IDENTF.unsqueeze(1).to_broadcast([128, 4, 128]),
                                                            op=ALU.add),
                   r=[f"MA{hh}", "cst", f"PX1_{hh}"], w=[f"PX1_{hh}"])

            def inv_lv(hh, lv):
                heads = list(range(4 * hh, 4 * hh + 4))
                hs = slice(4 * hh, 4 * hh + 4)
                if True:
                    cur, nxt = lv % 2, 1 - (lv % 2)
                    pA = [PS(), PS()]
                    pQn = PS()
                    for i, hd in enumerate(heads):
                        c0 = (i % 2) * 256
                        op("pe", lambda e, i=i, hd=hd, c0=c0, cur=cur: e.matmul(
                            pA[i // 2][0][:, c0:c0 + 256], Qm[cur][:, hd, :], PX[cur][:, hd, :], start=True, stop=True),
                           r=[f"Q{cur}_{hh}", f"PX{cur}_{hh}"], w=[pA[i // 2][1]])
                        op("pe", lambda e, i=i, hd=hd, cur=cur: e.matmul(
                            pQn[0][:, i * 128:(i + 1) * 128], PX[cur][:, hd, 0:128], Qm[cur][:, hd, :],
                            start=True, stop=True), r=[f"Q{cur}_{hh}", f"PX{cur}_{hh}"], w=[pQn[1]])
                    for b2 in range(2):
                        h2 = slice(4 * hh + 2 * b2, 4 * hh + 2 * b2 + 2)
                        v3 = pA[b2][0][:, 0:512].rearrange("p (h c) -> p h c", h=2)
                        op("act", lambda e, h2=h2, v3=v3, nxt=nxt: e.activation(out=PX[nxt][:, h2, 0:128],
                                                                               in_=v3[:, :, 0:128], func=AF.Copy),
                           r=[pA[b2][1]], w=[f"PX{nxt}_{hh}"])
                        op("dve", lambda e, h2=h2, v3=v3, nxt=nxt, cur=cur: e.tensor_tensor(
                            out=PX[nxt][:, h2, 128:256], in0=v3[:, :, 128:256], in1=PX[cur][:, h2, 128:256],
                            op=ALU.add), r=[pA[b2][1], f"PX{cur}_{hh}", f"PX{nxt}_{hh}"], w=[f"PX{nxt}_{hh}"])
                    op("act", lambda e, hs=hs, nxt=nxt, pQn=pQn: e.activation(
                        out=Qm[nxt][:, hs, :], in_=pQn[0][:, 0:512].rearrange("p (h c) -> p h c", h=4), func=AF.Copy),
                       r=[pQn[1]], w=[f"Q{nxt}_{hh}"])

            def inv_fin(hh):
                heads = list(range(4 * hh, 4 * hh + 4))
                hs = slice(4 * hh, 4 * hh + 4)
                pX = PS()
                for i, hd in enumerate(heads):
                    op("pe", lambda e, i=i, hd=hd: e.matmul(pX[0][:, i * 128:(i + 1) * 128], Qm[0][:, hd, :],
                                                            PX[0][:, hd, 128:256], start=True, stop=True),
                       r=[f"Q0_{hh}", f"PX0_{hh}"], w=[pX[1]])
                op("dve", lambda e, hs=hs, pX=pX: e.tensor_tensor(
                    out=XF[:, hs, :], in0=pX[0][:, 0:512].rearrange("p (h c) -> p h c", h=4),
                    in1=PX[0][:, hs, 128:256], op=ALU.add), r=[pX[1], f"PX0_{hh}"], w=[f"XF{hh}"])

            for hh in range(2):
                inv_s0(hh)
            for hh in range(2):
                inv_l0(hh)
            for lv in range(1, 6):
                for hh in range(2):
                    inv_lv(hh, lv)
            for hh in range(2):
                inv_fin(hh)
            if CUT <= 7:
                return
            pR = PS()
            for hd in range(8):
                cc = hd // 2
                pr = slice((hd % 2) * 64, (hd % 2) * 64 + 64)
                op("pe", lambda e: e.matmul(pR[0][:, hd * 64:(hd + 1) * 64], ARm[hd % 2][:, cc, 0, :],
                                            SBs[:, cc, :], start=True, stop=False),
                   r=[f"ARm{hd % 2}", "SBs"], w=[pR[1]])
                op("pe", lambda e, hd=hd: e.matmul(pR[0][:, hd * 64:(hd + 1) * 64], MB[:, hd, 0:128],
                                                   VT[:, hd * 64:(hd + 1) * 64], start=False, stop=True),
                   r=[f"MB{hd // 4}", "VT"], w=[pR[1]])
            op("act", lambda e: e.activation(out=RH[:], in_=pR[0][:, 0:512], func=AF.Copy), r=[pR[1]], w=["RH"])
            pU = PS()
            for hd in range(8):
                op("pe", lambda e, hd=hd: e.matmul(pU[0][:, hd * 64:(hd + 1) * 64], XF[:, hd, :],
                                                   RH[:, hd * 64:(hd + 1) * 64], start=True, stop=True),
                   r=[f"XF{hd // 4}", "RH"], w=[pU[1]])
            op("act", lambda e: e.activation(out=UT[:], in_=pU[0][:, 0:512], func=AF.Copy), r=[pU[1]], w=["UT"])
            pY = PS()
            pS_ = PS()
            for hd in range(8):
                cc = hd // 2
                pr = slice((hd % 2) * 64, (hd % 2) * 64 + 64)
                oy = pY[0][pr, cc * 128:(cc + 1) * 128]
                op("pe", lambda e: e.matmul(oy, SBs[:, cc, :], ARm[hd % 2][:, cc, 1, :],
                                            start=True, stop=False), r=["SBs", f"ARm{hd % 2}"], w=[pY[1]])
                op("pe", lambda e, oy=oy, hd=hd: e.matmul(oy, UT[:, hd * 64:(hd + 1) * 64], MA[:, hd, 128:256],
                                                          start=False, stop=False),
                   r=["UT", f"MA{hd // 4}"], w=[pY[1]])
                op("pe", lambda e, oy=oy, hd=hd: e.matmul(oy, VT[:, hd * 64:(hd + 1) * 64], MB[:, hd, 128:256],
                                                          start=False, stop=True),
                   r=["VT", f"MB{hd // 4}"], w=[pY[1]])
            for hd in range(8):
                cc = hd // 2
                pr = slice((hd % 2) * 64, (hd % 2) * 64 + 64)
                os_ = pS_[0][pr, cc * 64:(cc + 1) * 64]
                op("pe", lambda e, os_=os_, hd=hd: e.matmul(os_, BKT[:, hd * 64:(hd + 1) * 64],
                                                            UT[:, hd * 64:(hd + 1) * 64], start=True, stop=False),
                   r=["BKT", "UT"], w=[pS_[1]])
                op("pe", lambda e, os_=os_, hd=hd: e.matmul(os_, BKT[:, 512 + hd * 64:512 + (hd + 1) * 64],
                                                            VT[:, hd * 64:(hd + 1) * 64], start=False, stop=True),
                   r=["BKT", "VT"], w=[pS_[1]])
            op("act", lambda e: e.activation(out=f2(Yf), in_=pY[0][:, 0:512], func=AF.Copy), r=[pY[1]], w=["Yf"])
            op("dve", lambda e: e.tensor_tensor(out=TMPS[:], in0=SF[:],
                                                in1=E1[:, :, 127:128].to_broadcast([128, 4, 64]), op=ALU.mult),
               r=["SF", "E1"], w=["TMPS"])
            op("dve", lambda e: e.tensor_tensor(out=SF[:], in0=pS_[0][:, 0:256].rearrange("p (c v) -> p c v", c=4),
                                                in1=TMPS[:], op=ALU.add), r=[pS_[1], "TMPS"], w=["SF"])
            op("act", lambda e: e.activation(out=SBs[:], in_=SF[:], func=AF.Copy), r=["SF"], w=["SBs"])
            if CUT <= 8:
                return
            op("act", lambda e: e.activation(out=f2(YB), in_=pY[0][:, 0:512], func=AF.Copy), r=[pY[1]], w=["YB"])
            op("act", lambda e: e.activation(out=f2(YSQ), in_=pY[0][:, 0:512], func=AF.Square), r=[pY[1]], w=["YSQ"])
            pM = PS()
            pV = PS()
            op("pe", lambda e: e.matmul(pM[0][:, 0:512], bones[:], f2(YB), start=True, stop=True),
               r=["bones", "YB"], w=[pM[1]])
            op("pe", lambda e: e.matmul(pV[0][:, 0:512], bones[:], f2(YSQ), start=True, stop=True),
               r=["bones", "YSQ"], w=[pV[1]])
            op("act", lambda e: e.activation(out=f2(MEAN), in_=pM[0][:, 0:512], func=AF.Copy, scale=1.0 / 64),
               r=[pM[1]], w=["MEAN"])
            op("pool", lambda e: e.tensor_tensor(out=f2(M2), in0=f2(MEAN), in1=f2(MEAN), op=ALU.mult),
               r=["MEAN"], w=["M2"])
            op("dve", lambda e: e.scalar_tensor_tensor(out=f2(VAR), in0=pV[0][:, 0:512], scalar=1.0 / 64, in1=f2(M2),
                                                       op0=ALU.mult, op1=ALU.subtract), r=[pV[1], "M2"], w=["VAR"])
            op("act", lambda e: e.activation(out=f2(VAR), in_=f2(VAR), func=AF.Ln, bias=64e-5), r=["VAR"], w=["VAR"])
            op("act", lambda e: e.activation(out=f2(VAR), in_=f2(VAR), func=AF.Exp, scale=-0.5), r=["VAR"], w=["VAR"])
            op("pool", lambda e: e.tensor_tensor(out=f2(Dd), in0=f2(Yf), in1=f2(MEAN), op=ALU.subtract),
               r=["Yf", "MEAN"], w=["Dd"])
            op("pool", lambda e: e.tensor_tensor(out=f2(Dd), in0=f2(Dd), in1=f2(VAR), op=ALU.mult),
               r=["Dd", "VAR"], w=["Dd"])
            for cc in range(4):
                op("dve", lambda e, cc=cc: e.tensor_scalar(out=Dd[:, cc, :], in0=Dd[:, cc, :], scalar1=vcol(V_LNW + cc),
                                                           scalar2=vcol(V_LNB + cc), op0=ALU.mult, op1=ALU.add),
                   r=["Dd", "vec"], w=["Dd"])
            op("pool", lambda e: e.tensor_tensor(out=f2(Dd), in0=f2(Dd), in1=f2(BV), op=ALU.add),
               r=["Dd", "BV"], w=["Dd"])
            op("pool", lambda e, ys=ys: e.tensor_tensor(out=ys[:, 4:8, :], in0=Dd[:], in1=Gg[:], op=ALU.mult),
               r=["Dd", "Gg"], w=[yk + "b"])
            dma("sp", lambda e, ys=ys, ti=ti: e.dma_start(out=yab_d[ti], in_=ys[:].rearrange("p a b -> p (a b)")),
                r=[yk + "a", yk + "b"], w=[f"yab_d{ti}"], key=yk)

        genA(0)
        for ti in range(NTILE):
            fns = [lambda ti=ti: genBC(ti)]
            q = [cfg.get("qBC", 5)]
            if ti + 1 < NTILE:
                fns.append(lambda ti=ti: genA(ti + 1))
                q.append(1)
            WV.run(fns, q)
        tl.pool = None


    if "1b" in PH:
        phase_reset()
        Wgt = salloc("Wgt", [128, 8, 2048], BF16)
        WbA = salloc("WbA", [128, 4, 1024], BF16)
        WbB = salloc("WbB", [128, 4, 1024], BF16)
        Wout = salloc("Wout", [128, 8, 1024], BF16)
        Wr = salloc("Wr", [128, 8, 36], F32)
        BR = salloc("BR", [128, 36], F32)
        GMOE = salloc("GMOE", [128, D], F32)
        ustrb = salloc("ustrb", [128, 128], BF16)
        CNT = salloc("CNT", [128, 32], F32)
        stgB = [salloc(f"stgB{i}", [128, 2048], F32) for i in range(2)]
        sB = 0
        for kc in range(8):
            k_ = f"stgB{sB % 2}"
            t_ = stgB[sB % 2]
            sB += 1
            dma("sp", lambda e: e.dma_start(out=t_[:, 0:2048], in_=win_d[kc * 128:(kc + 1) * 128, 2560:4608]),
                w=[k_], key=k_)
            op("dve" if kc % 2 else "pool", lambda e: e.tensor_scalar(out=Wgt[:, kc, :], in0=t_[:, 0:2048],
                                                                      scalar1=vcol(V_LNMIX + kc), scalar2=1.0,
                                                                      op0=ALU.mult, op1=ALU.mult),
               r=[k_, "vec"], w=["Wgt"])
        for (dst, dk, src, nk) in ((WbA, "WbA", wba_d, 4), (WbB, "WbB", wbb_d, 4), (Wout, "Wout", wout_d, 8)):
            for kc in range(nk):
                k_ = f"stgB{sB % 2}"
                t_ = stgB[sB % 2]
                sB += 1
                dma("sp", lambda e: e.dma_start(out=t_[:, 0:1024], in_=src[kc * 128:(kc + 1) * 128, :]),
                    w=[k_], key=k_)
                op("dve" if kc % 2 else "pool", lambda e: e.tensor_copy(out=dst[:, kc, :], in_=t_[:, 0:1024]),
                   r=[k_], w=[dk])
        dma("sp", lambda e: e.dma_start(out=Wr[:], in_=wr_d.rearrange("(k p) n -> p k n", p=128)), w=["Wr"], key="Wr")
        for kc in range(8):
            op("dve", lambda e: e.tensor_scalar(out=Wr[:, kc, :], in0=Wr[:, kc, :], scalar1=vcol(V_LNMOE + kc),
                                                scalar2=None, op0=ALU.mult), r=["Wr", "vec"], w=["Wr"])
        dma("sp", lambda e: e.dma_start(out=BR[:], in_=br_d.to_broadcast([128, 36])), w=["BR"], key="BR")
        dma("sp", lambda e: e.dma_start(out=GMOE[:], in_=lnmoe_d.to_broadcast([128, D])), w=["GMOE"], key="GMOE")
        op("dve", lambda e: e.tensor_copy(out=ustrb[:], in_=USTR), r=["cst"], w=["ustrb"])
        op("dve", lambda e: e.memset(CNT[:], 0.0), w=["CNT"])
        ZR = salloc("ZR", [128, D], BF16)
        op("pool", lambda e: e.memset(ZR[:], 0.0), w=["ZR"])
        hs_v = hs_d.rearrange("(r p) d -> p r d", p=128)
        RCH = 8
        for r0 in range(0, NST, RCH):
            rn = min(RCH, NST - r0)
            dma("sp", lambda e: e.dma_start(out=hs_v[:, r0:r0 + rn, :],
                                            in_=ZR[:].unsqueeze(1).to_broadcast([128, rn, D])),
                r=["ZR"], w=["hs_all"], key="ZRst")

        xin = [salloc(f"xinb{i}", [128, D], F32) for i in range(2)]
        tmp = dict(junk=salloc("junkb", [128, D], BF16), ss=salloc("ssb", [128, 4], F32),
                   rstd=salloc("rstdb", [128, 1], F32), xbf=salloc("xbfb", [128, D], BF16))
        hT = [salloc(f"hTb{i}", [128, 8, 128], BF16) for i in range(2)]
        yin = [salloc(f"yin{i}", [128, 8, 128], BF16) for i in range(2)]
        GT = salloc("GT", [128, 2048], F32)
        MAf = salloc("MAf", [128, D], F32)
        MBf = salloc("MBf", [128, D], F32)
        MG = salloc("MG", [128, D], BF16)
        MGT = salloc("MGT", [128, 8, 128], BF16)
        X1 = [salloc(f"X1_{i}", [128, D], F32) for i in range(2)]
        HM = [salloc(f"HM{i}", [128, D], BF16) for i in range(2)]
        X1T = salloc("X1T", [128, 8, 128], F32)
        rs = salloc("rs", [128, 8], F32)
        LG = salloc("LG", [128, 36], F32)
        R_ = salloc("Rsm", [128, 16], F32)
        GOH = salloc("GOH", [128, 4], F32)
        EG = salloc("EG", [128, 4], F32)
        T48 = salloc("T48", [128, 4, 8], F32)
        ESEL = salloc("ESEL", [128, 8], F32)
        ES2 = salloc("ES2", [128, 8], F32)
        OHa = salloc("OHa", [128, 8], F32)
        OHb = salloc("OHb", [128, 8], F32)
        OH1 = salloc("OH1", [128, 4, 8], F32)
        OH2 = salloc("OH2", [128, 4, 8], F32)
        OHSb = salloc("OHSb", [128, 32], BF16)
        POS = salloc("POS", [128, 32], F32)
        PT2 = salloc("PT2", [128, 32], F32)
        SL = salloc("SL", [128, 2], F32)
        EOFF = cst[:, 768:800]

        def g2(t):
            return t[:].rearrange("p a b -> p (a b)")

        for ti in range(NTILE):
            sl = ti % 2
            xk = f"xinb{sl}"
            dma("sp", lambda e: e.dma_start(out=xin[sl][:], in_=x_d[ti * 128:(ti + 1) * 128, :]), w=[xk], key=xk)
            yk = f"yin{sl}"
            dma("sp", lambda e: e.dma_start(out=yin[sl][:].rearrange("p a b -> p (a b)"), in_=yab_d[ti]),
                r=[f"yab_d{ti}"], w=[yk], key=yk)
            hk = f"hTb{sl}"
            rms_to_hT(ti, xin[sl], xk, hT[sl], hk, tmp)
            h = hT[sl]
            for nb in range(4):
                pgt = PS()
                for kc in range(8):
                    op("pe", lambda e: e.matmul(pgt[0][:, 0:512], h[:, kc, :], Wgt[:, kc, nb * 512:(nb + 1) * 512],
                                                start=(kc == 0), stop=(kc == 7)), r=[hk, "Wgt"], w=[pgt[1]])
                op("act", lambda e: e.activation(out=GT[:, nb * 512:(nb + 1) * 512], in_=pgt[0][:, 0:512],
                                                 func=AF.Sigmoid), r=[pgt[1]], w=[f"GT{nb}"])
            for half in range(2):
                pba = PS()
                for c in range(4):
                    op("pe", lambda e: e.matmul(pba[0][:, 0:512], yin[sl][:, c, :],
                                                WbA[:, c, half * 512:(half + 1) * 512], start=(c == 0), stop=(c == 3)),
                       r=[yk, "WbA"], w=[pba[1]])
                op("dve", lambda e: e.tensor_tensor(out=MAf[:, half * 512:(half + 1) * 512], in0=pba[0][:, 0:512],
                                                    in1=GT[:, half * 512:(half + 1) * 512], op=ALU.mult),
                   r=[pba[1], f"GT{half}"], w=[f"MAf{half}"])
                pbb = PS()
                for c in range(4):
                    op("pe", lambda e: e.matmul(pbb[0][:, 0:512], yin[sl][:, 4 + c, :],
                                                WbB[:, c, half * 512:(half + 1) * 512], start=(c == 0), stop=(c == 3)),
                       r=[yk, "WbB"], w=[pbb[1]])
                op("dve", lambda e: e.tensor_tensor(out=MBf[:, half * 512:(half + 1) * 512], in0=pbb[0][:, 0:512],
                                                    in1=GT[:, 1024 + half * 512:1024 + (half + 1) * 512], op=ALU.mult),
                   r=[pbb[1], f"GT{2 + half}"], w=[f"MBf{half}"])
                op("pool", lambda e: e.tensor_tensor(out=MG[:, half * 512:(half + 1) * 512],
                                                     in0=MAf[:, half * 512:(half + 1) * 512],
                                                     in1=MBf[:, half * 512:(half + 1) * 512], op=ALU.add),
                   r=[f"MAf{half}", f"MBf{half}"], w=[f"MG{half}"])
            pb = PSB()
            for kc in range(8):
                op("pe", lambda e: e.transpose(pb[0][:, kc * 128:(kc + 1) * 128], MG[:, kc * 128:(kc + 1) * 128],
                                               identb[:]), r=["MG0", "MG1", "identb"], w=[pb[1]])
            op("act", lambda e: e.activation(out=g2(MGT), in_=pb[0][:, :], func=AF.Copy), r=[pb[1]], w=["MGT"])
            x1 = X1[sl]
            x1k = f"X1_{sl}"
            for half in range(2):
                po = PS()
                for kc in range(8):
                    op("pe", lambda e: e.matmul(po[0][:, 0:512], MGT[:, kc, :], Wout[:, kc, half * 512:(half + 1) * 512],
                                                start=(kc == 0), stop=(kc == 7)), r=["MGT", "Wout"], w=[po[1]])
                op("dve", lambda e: e.tensor_tensor(out=x1[:, half * 512:(half + 1) * 512], in0=po[0][:, 0:512],
                                                    in1=xin[sl][:, half * 512:(half + 1) * 512], op=ALU.add),
                   r=[po[1], xk], w=[x1k + f"h{half}"])
            dma("sp", lambda e: e.dma_start(out=x1_d[ti * 128:(ti + 1) * 128, :], in_=x1[:]),
                r=[x1k + "h0", x1k + "h1"], w=[f"x1_d{ti}"], key=x1k)
            op("act", lambda e: e.activation(out=tmp["junk"][:], in_=x1[:], func=AF.Square, accum_out=rs[:, 0:1]),
               r=[x1k + "h0", x1k + "h1"], w=["junk", "rs0"])
            op("dve", lambda e: e.tensor_scalar(out=rs[:, 1:2], in0=rs[:, 0:1], scalar1=1.0 / D, scalar2=1e-6,
                                                op0=ALU.mult, op1=ALU.add), r=["rs0"], w=["rs1"])
            op("act", lambda e: e.activation(out=rs[:, 2:3], in_=rs[:, 1:2], func=AF.Sqrt), r=["rs1"], w=["rs2"])
            op("dve", lambda e: e.reciprocal(out=rs[:, 3:4], in_=rs[:, 2:3]), r=["rs2"], w=["rs3"])
            hm = HM[sl]
            hmk = f"HM{sl}"
            op("dve", lambda e: e.scalar_tensor_tensor(out=hm[:], in0=x1[:], scalar=rs[:, 3:4], in1=GMOE[:],
                                                       op0=ALU.mult, op1=ALU.mult),
               r=[x1k + "h0", x1k + "h1", "rs3", "GMOE"], w=[hmk])
            for half in range(2):
                ptx = PS()
                for j in range(4):
                    kc = half * 4 + j
                    op("pe", lambda e: e.transpose(ptx[0][:, j * 128:(j + 1) * 128], x1[:, kc * 128:(kc + 1) * 128],
                                                   IDENTF), r=[x1k + "h0", x1k + "h1", "cst"], w=[ptx[1]])
                op("act", lambda e: e.activation(out=X1T[:, half * 4:half * 4 + 4, :].rearrange("p a b -> p (a b)"),
                                                 in_=ptx[0][:, 0:512], func=AF.Copy), r=[ptx[1]], w=[f"X1T{half}"])
            pl = PS()
            for kc in range(8):
                op("pe", lambda e: e.matmul(pl[0][:, 0:36], X1T[:, kc, :], Wr[:, kc, :], start=(kc == 0), stop=(kc == 7)),
                   r=["X1T0", "X1T1", "Wr"], w=[pl[1]])
            op("dve", lambda e: e.scalar_tensor_tensor(out=LG[:], in0=pl[0][:, 0:36], scalar=rs[:, 3:4], in1=BR[:],
                                                       op0=ALU.mult, op1=ALU.add), r=[pl[1], "rs3", "BR"], w=["LG"])
            V = lambda f, r, w: op("dve", f, r=r, w=w)
            V(lambda e: e.tensor_reduce(out=R_[:, 0:1], in_=LG[:, 0:4], axis=AX.X, op=ALU.max), ["LG"], ["R0"])
            V(lambda e: e.tensor_scalar(out=GOH[:], in0=LG[:, 0:4], scalar1=R_[:, 0:1], scalar2=None, op0=ALU.is_equal),
              ["LG", "R0"], ["GOH"])
            V(lambda e: e.tensor_scalar(out=R_[:, 1:2], in0=R_[:, 0:1], scalar1=-1.0, scalar2=None, op0=ALU.mult),
              ["R0"], ["R1"])
            op("act", lambda e: e.activation(out=EG[:], in_=LG[:, 0:4], func=AF.Exp, bias=R_[:, 1:2],
                                             accum_out=R_[:, 2:3]), r=["LG", "R1"], w=["EG", "R2"])
            V(lambda e: e.reciprocal(out=R_[:, 3:4], in_=R_[:, 2:3]), ["R2"], ["R3"])
            V(lambda e: e.tensor_tensor(out=T48[:], in0=LG[:, 4:36].rearrange("p (g x) -> p g x", g=4),
                                        in1=GOH[:].unsqueeze(2).to_broadcast([128, 4, 8]), op=ALU.mult),
              ["LG", "GOH"], ["T48"])
            V(lambda e: e.tensor_reduce(out=ESEL[:], in_=T48[:].rearrange("p g x -> p x g"), axis=AX.X, op=ALU.add),
              ["T48"], ["ESEL"])
            V(lambda e: e.tensor_reduce(out=R_[:, 4:5], in_=ESEL[:], axis=AX.X, op=ALU.max), ["ESEL"], ["R4"])
            V(lambda e: e.tensor_scalar(out=OHa[:], in0=ESEL[:], scalar1=R_[:, 4:5], scalar2=None, op0=ALU.is_equal),
              ["ESEL", "R4"], ["OHa"])
            V(lambda e: e.scalar_tensor_tensor(out=ES2[:], in0=OHa[:], scalar=-1e30, in1=ESEL[:], op0=ALU.mult,
                                               op1=ALU.add), ["OHa", "ESEL"], ["ES2"])
            V(lambda e: e.tensor_reduce(out=R_[:, 5:6], in_=ES2[:], axis=AX.X, op=ALU.max), ["ES2"], ["R5"])
            V(lambda e: e.tensor_scalar(out=OHb[:], in0=ES2[:], scalar1=R_[:, 5:6], scalar2=None, op0=ALU.is_equal),
              ["ES2", "R5"], ["OHb"])
            V(lambda e: e.tensor_tensor(out=R_[:, 6:7], in0=R_[:, 5:6], in1=R_[:, 4:5], op=ALU.subtract),
              ["R4", "R5"], ["R6"])
            op("act", lambda e: e.activation(out=R_[:, 7:8], in_=R_[:, 6:7], func=AF.Exp), r=["R6"], w=["R7"])
            V(lambda e: e.tensor_scalar(out=R_[:, 8:9], in0=R_[:, 7:8], scalar1=1.0, scalar2=None, op0=ALU.add),
              ["R7"], ["R8"])
            V(lambda e: e.reciprocal(out=R_[:, 9:10], in_=R_[:, 8:9]), ["R8"], ["R9"])
            V(lambda e: e.tensor_tensor(out=rw_f[:, 2 * ti:2 * ti + 1], in0=R_[:, 9:10], in1=R_[:, 3:4], op=ALU.mult),
              ["R9", "R3"], [f"rw{ti}a"])
            V(lambda e: e.tensor_tensor(out=rw_f[:, 2 * ti + 1:2 * ti + 2], in0=rw_f[:, 2 * ti:2 * ti + 1],
                                        in1=R_[:, 7:8], op=ALU.mult), [f"rw{ti}a", "R7"], [f"rw{ti}b"])
            gb = GOH[:].unsqueeze(2).to_broadcast([128, 4, 8])
            V(lambda e: e.tensor_tensor(out=OH1[:], in0=gb, in1=OHa[:].unsqueeze(1).to_broadcast([128, 4, 8]),
                                        op=ALU.mult), ["GOH", "OHa"], ["OH1"])
            V(lambda e: e.tensor_tensor(out=OH2[:], in0=gb, in1=OHb[:].unsqueeze(1).to_broadcast([128, 4, 8]),
                                        op=ALU.mult), ["GOH", "OHb"], ["OH2"])
            V(lambda e: e.tensor_tensor(out=OHSb[:], in0=g2(OH1), in1=g2(OH2), op=ALU.add), ["OH1", "OH2"], ["OHSb"])
            pc = PS()
            op("pe", lambda e: e.matmul(pc[0][:, 0:32], ustrb[:], OHSb[:], start=True, stop=True),
               r=["ustrb", "OHSb"], w=[pc[1]])
            op("pe", lambda e: e.matmul(pc[0][:, 32:64], onesb[:], OHSb[:], start=True, stop=True),
               r=["onesb", "OHSb"], w=[pc[1]])
            V(lambda e: e.tensor_tensor(out=POS[:], in0=pc[0][:, 0:32], in1=CNT[:], op=ALU.add), [pc[1], "CNT"], ["POS"])
            V(lambda e: e.tensor_tensor(out=CNT[:], in0=pc[0][:, 32:64], in1=CNT[:], op=ALU.add), [pc[1], "CNT"], ["CNT"])
            V(lambda e: e.tensor_scalar(out=POS[:], in0=POS[:], scalar1=float(CAP - 1), scalar2=None, op0=ALU.min),
              ["POS"], ["POS"])
            V(lambda e: e.tensor_tensor(out=POS[:], in0=POS[:], in1=EOFF, op=ALU.add), ["POS", "cst"], ["POS"])
            for j, OHx in enumerate((OH1, OH2)):
                V(lambda e: e.tensor_tensor(out=PT2[:], in0=POS[:], in1=g2(OHx), op=ALU.mult),
                  ["POS", f"OH{j + 1}"], ["PT2"])
                V(lambda e: e.tensor_reduce(out=SL[:, j:j + 1], in_=PT2[:], axis=AX.X, op=ALU.add), ["PT2"], [f"SL{j}"])
            V(lambda e: e.tensor_copy(out=slot_i[:, 2 * ti:2 * ti + 2], in_=SL[:]), ["SL0", "SL1"], [f"slot{ti}"])
            for j in range(2):
                dma("pool", lambda e: e.indirect_dma_start(
                    out=hs_d[:, :], out_offset=bass.IndirectOffsetOnAxis(ap=slot_i[:, 2 * ti + j:2 * ti + j + 1], axis=0),
                    in_=hm[:, :], in_offset=None), r=[hmk, f"slot{ti}", "hs_all"], w=[f"hs_sc{ti}_{j}"], key=f"sc{sl}{j}")
        if debug:
            RT = salloc("RT", [128, NTILE * 4], F32)
            op("dve", lambda e: e.tensor_copy(out=RT[:, 0:2 * NTILE], in_=slot_i[:]),
               r=[f"slot{t}" for t in range(NTILE)], w=["RT"])
            op("dve", lambda e: e.tensor_copy(out=RT[:, 2 * NTILE:4 * NTILE], in_=rw_f[:]),
               r=[f"rw{t}a" for t in range(NTILE)] + [f"rw{t}b" for t in range(NTILE)] + ["RT"], w=["RT"])
            dma("sp", lambda e: e.dma_start(out=rt_d, in_=RT[:]), r=["RT"], w=["rt_d"], key="RT")

    if "2" in PH:
        phase_reset()
        WG = [salloc(f"WG{i}", [128, 8, DE], BF16) for i in range(2)]
        WU = [salloc(f"WU{i}", [128, 8, DE], BF16) for i in range(2)]
        WD = [salloc(f"WD{i}", [128, 4, D], BF16) for i in range(2)]
        NB = 4
        XGb = [salloc(f"XGb{i}", [128, NB, D], BF16) for i in range(2)]
        XGT = salloc("XGT", [128, 8, NB * 128], BF16)
        SGf = [salloc(f"SGf{i}", [128, NB * 128], F32) for i in range(2)]
        HID = salloc("HID", [128, 4, NB * 128], BF16)
        YOb = [salloc(f"YOb{i}", [128, NB, D], F32) for i in range(2)]
        all_sc = [f"hs_sc{t}_{j}" for t in range(NTILE) for j in range(2)] + ["hs_all"]
        if CT <= 4:
            batches = [(0, CT)]
        elif CT == 5:
            batches = [(0, 3), (3, 2)]
        else:
            batches = [(r0, min(4, CT - r0)) for r0 in range(0, CT, 4)]
        it = 0
        for ex in range(NE):
            b = ex % 2
            dma("pool", lambda e: e.dma_start(out=WG[b][:], in_=wg_d[ex].rearrange("(p k) n -> p k n", k=8),
                                              max_dma_last_dim=8192), w=[f"WG{b}"], key=f"WG{b}")
            dma("pool", lambda e: e.dma_start(out=WU[b][:], in_=wu_d[ex].rearrange("(p k) n -> p k n", k=8),
                                              max_dma_last_dim=8192), w=[f"WU{b}"], key=f"WU{b}")
            dma("pool", lambda e: e.dma_start(out=WD[b][:], in_=wd_d[ex].rearrange("(k p) n -> p k n", p=128)),
                w=[f"WD{b}"], key=f"WD{b}")
            for (r0, nt) in batches:
                row0 = ex * CAP + r0 * 128
                xb = it % 2
                it += 1
                xgk = f"XGb{xb}"
                W_ = nt * 128
                dma("sp", lambda e: e.dma_start(
                    out=XGb[xb][:, 0:nt, :], in_=hs_d[row0:row0 + W_, :].rearrange("(t p) d -> p t d", p=128)),
                    r=(all_sc if "1b" in PH else []), w=[xgk], key=xgk)
                for t in range(nt):
                    pb = PSB()
                    xv = XGb[xb][:, t, :].rearrange("p (m j) -> p j m", j=8)
                    for j in range(8):
                        op("pe", lambda e: e.transpose(pb[0][:, j * 128:(j + 1) * 128], xv[:, j, :], identb[:]),
                           r=[xgk, "identb"], w=[pb[1]])
                    op("act", lambda e: e.activation(out=XGT[:, :, t * 128:(t + 1) * 128],
                                                     in_=pb[0][:, :].rearrange("p (j m) -> p j m", j=8), func=AF.Copy),
                       r=[pb[1]], w=[f"XGT{t}"])
                xgt_keys = [f"XGT{t}" for t in range(nt)]
                for hc in range(4):
                    pG = PS()
                    pU_ = PS()
                    for j in range(8):
                        op("pe", lambda e: e.matmul(pG[0][:, 0:W_], WG[b][:, j, hc * 128:(hc + 1) * 128],
                                                    XGT[:, j, 0:W_], start=(j == 0), stop=(j == 7)),
                           r=[f"WG{b}"] + xgt_keys, w=[pG[1]])
                    for j in range(8):
                        op("pe", lambda e: e.matmul(pU_[0][:, 0:W_], WU[b][:, j, hc * 128:(hc + 1) * 128],
                                                    XGT[:, j, 0:W_], start=(j == 0), stop=(j == 7)),
                           r=[f"WU{b}"] + xgt_keys, w=[pU_[1]])
                    sg = SGf[hc % 2]
                    sgk = f"SGf{hc % 2}"
                    op("act", lambda e: e.activation(out=sg[:, 0:W_], in_=pG[0][:, 0:W_], func=AF.Silu),
                       r=[pG[1]], w=[sgk])
                    op("dve", lambda e: e.tensor_tensor(out=HID[:, hc, 0:W_], in0=pU_[0][:, 0:W_], in1=sg[:, 0:W_],
                                                        op=ALU.mult), r=[pU_[1], sgk], w=[f"HID{hc}"])
                yok = f"YOb{xb}"
                for t in range(nt):
                    for half in range(2):
                        py = PS()
                        for hc in range(4):
                            op("pe", lambda e: e.matmul(py[0][:, 0:512], HID[:, hc, t * 128:(t + 1) * 128],
                                                        WD[b][:, hc, half * 512:(half + 1) * 512], start=(hc == 0),
                                                        stop=(hc == 3)), r=[f"HID{hc_}" for hc_ in range(4)] + [f"WD{b}"],
                               w=[py[1]])
                        if half:
                            op("act", lambda e: e.activation(out=YOb[xb][:, t, 512:1024], in_=py[0][:, 0:512],
                                                             func=AF.Copy), r=[py[1]], w=[yok + f"_{t}_1"])
                        else:
                            op("dve", lambda e: e.tensor_copy(out=YOb[xb][:, t, 0:512], in_=py[0][:, 0:512]),
                               r=[py[1]], w=[yok + f"_{t}_0"])
                dma("sp", lambda e: e.dma_start(
                    out=ys_d[row0:row0 + W_, :].rearrange("(t p) d -> p t d", p=128), in_=YOb[xb][:, 0:nt, :]),
                    r=[yok + f"_{t}_{hf}" for t in range(nt) for hf in range(2)], w=["ys_all"], key=yok)

    if "3" in PH:
        phase_reset()
        Wpg = salloc("Wpg", [128, 8, D], BF16)
        Wpp = salloc("Wpp", [128, 2, D], BF16)
        LNF = salloc("LNF", [128, D], F32)
        stgC = [salloc(f"stgC{i}", [128, D], F32) for i in range(2)]
        for kc in range(8):
            k_ = f"stgC{kc % 2}"
            t_ = stgC[kc % 2]
            dma("sp", lambda e: e.dma_start(out=t_[:], in_=wpg_d[kc * 128:(kc + 1) * 128, :]), w=[k_], key=k_)
            op("dve" if kc % 2 else "pool", lambda e: e.tensor_scalar(out=Wpg[:, kc, :], in0=t_[:],
                                                                      scalar1=vcol(V_LNPLE + kc), scalar2=1.0,
                                                                      op0=ALU.mult, op1=ALU.mult),
               r=[k_, "vec"], w=["Wpg"])
        for kc in range(2):
            k_ = f"stgC{kc % 2}"
            t_ = stgC[kc % 2]
            dma("sp", lambda e: e.dma_start(out=t_[:], in_=wpp_d[kc * 128:(kc + 1) * 128, :]), w=[k_], key=k_)
            op("dve", lambda e: e.tensor_copy(out=Wpp[:, kc, :], in_=t_[:]), r=[k_], w=["Wpp"])
        dma("sp", lambda e: e.dma_start(out=LNF[:], in_=lnf_d.to_broadcast([128, D])), w=["LNF"], key="LNF")
        X1c = [salloc(f"X1c{i}", [128, D], F32) for i in range(2)]
        Y1 = [salloc(f"Y1_{i}", [128, D], F32) for i in range(2)]
        Y2 = [salloc(f"Y2_{i}", [128, D], F32) for i in range(2)]
        Pin = [salloc(f"Pin{i}", [128, 256], F32) for i in range(2)]
        Pb2 = [salloc(f"Pb{i}", [128, 256], BF16) for i in range(2)]
        PTt2 = [salloc(f"PTt{i}", [128, 2, 128], BF16) for i in range(2)]
        X22 = [salloc(f"X2{i}", [128, D], F32) for i in range(2)]
        tmp2 = [dict(junk=salloc(f"junkc{i}", [128, D], BF16), ss=salloc(f"ssc{i}", [128, 4], F32),
                     rstd=salloc(f"rstdc{i}", [128, 1], F32), xbf=salloc(f"xbfc{i}", [128, D], BF16))
                for i in range(2)]
        hTc2 = [salloc(f"hTc{i}", [128, 8, 128], BF16) for i in range(2)]
        GP2 = [salloc(f"GP{i}", [128, D], F32) for i in range(2)]
        X32 = [salloc(f"X3{i}", [128, D], F32) for i in range(2)]
        rf2 = [salloc(f"rf{i}", [128, 4], F32) for i in range(2)]
        OUTt = [salloc(f"OUT{i}", [128, D], F32) for i in range(2)]

        def body3(ti):
            sl = ti % 2
            tl.pool = "E" if sl == 0 else "O"
            op, dma = stream(f"@{sl}")
            Pb, PTt, X2, tmp, hTc, GP, X3, rf = Pb2[sl], PTt2[sl], X22[sl], tmp2[sl], hTc2[sl], GP2[sl], X32[sl], rf2[sl]
            x1k, y1k, y2k, pk = f"X1c{sl}", f"Y1_{sl}", f"Y2_{sl}", f"Pin{sl}"
            dma("sp", lambda e: e.dma_start(out=X1c[sl][:], in_=x1_d[ti * 128:(ti + 1) * 128, :]),
                r=([f"x1_d{ti}"] if "1b" in PH else []), w=[x1k], key=x1k)
            dma("sp", lambda e: e.dma_start(out=Pin[sl][:], in_=p_d[ti * 128:(ti + 1) * 128, :]), w=[pk], key=pk)
            for (Yt, ykk, j) in ((Y1[sl], y1k, 0), (Y2[sl], y2k, 1)):
                dma("pool", lambda e: e.indirect_dma_start(
                    out=Yt[:, :], out_offset=None, in_=ys_d[:, :],
                    in_offset=bass.IndirectOffsetOnAxis(ap=slot_i[:, 2 * ti + j:2 * ti + j + 1], axis=0)),
                    r=(["ys_all", f"slot{ti}"] if "2" in PH else []), w=[ykk], key=ykk)
            op("dve", lambda e: e.scalar_tensor_tensor(out=X2[:], in0=Y1[sl][:], scalar=rw_f[:, 2 * ti:2 * ti + 1],
                                                       in1=X1c[sl][:], op0=ALU.mult, op1=ALU.add),
               r=[y1k, x1k, f"rw{ti}a"], w=["X2"])
            op("dve", lambda e: e.scalar_tensor_tensor(out=X2[:], in0=Y2[sl][:], scalar=rw_f[:, 2 * ti + 1:2 * ti + 2],
                                                       in1=X2[:], op0=ALU.mult, op1=ALU.add),
               r=[y2k, "X2", f"rw{ti}b"], w=["X2"])
            rms_to_hT(ti, X2, "X2", hTc, "hTc", tmp, normalize=False, op=op)
            op("pool", lambda e: e.tensor_copy(out=Pb[:], in_=Pin[sl][:]), r=[pk], w=["Pb"])
            pb = PSB()
            for kc in range(2):
                op("pe", lambda e: e.transpose(pb[0][:, kc * 128:(kc + 1) * 128], Pb[:, kc * 128:(kc + 1) * 128],
                                               identb[:]), r=["Pb", "identb"], w=[pb[1]])
            op("act", lambda e: e.activation(out=PTt[:].rearrange("p a b -> p (a b)"), in_=pb[0][:, 0:256],
                                             func=AF.Copy), r=[pb[1]], w=["PTt"])
            for half in range(2):
                pgm = PS()
                for kc in range(8):
                    op("pe", lambda e: e.matmul(pgm[0][:, 0:512], hTc[:, kc, :], Wpg[:, kc, half * 512:(half + 1) * 512],
                                                start=(kc == 0), stop=(kc == 7)), r=["hTc", "Wpg"], w=[pgm[1]])
                op("act", lambda e: e.activation(out=GP[:, half * 512:(half + 1) * 512], in_=pgm[0][:, 0:512],
                                                 func=AF.Sigmoid, scale=tmp["rstd"][:, 0:1]),
                   r=[pgm[1], "rstd"], w=[f"GP{half}"])
                ppm = PS()
                for kc in range(2):
                    op("pe", lambda e: e.matmul(ppm[0][:, 0:512], PTt[:, kc, :], Wpp[:, kc, half * 512:(half + 1) * 512],
                                                start=(kc == 0), stop=(kc == 1)), r=["PTt", "Wpp"], w=[ppm[1]])
                op("dve", lambda e: e.tensor_tensor(out=GP[:, half * 512:(half + 1) * 512], in0=ppm[0][:, 0:512],
                                                    in1=GP[:, half * 512:(half + 1) * 512], op=ALU.mult),
                   r=[ppm[1], f"GP{half}"], w=[f"GP{half}"])
                op("pool", lambda e: e.tensor_tensor(out=X3[:, half * 512:(half + 1) * 512],
                                                     in0=X2[:, half * 512:(half + 1) * 512],
                                                     in1=GP[:, half * 512:(half + 1) * 512], op=ALU.add),
                   r=["X2", f"GP{half}"], w=[f"X3{half}"])
            op("act", lambda e: e.activation(out=tmp["junk"][:], in_=X3[:], func=AF.Square, accum_out=rf[:, 0:1]),
               r=["X30", "X31"], w=["junk", "rf0"])
            op("dve", lambda e: e.tensor_scalar(out=rf[:, 1:2], in0=rf[:, 0:1], scalar1=1.0 / D, scalar2=1e-6,
                                                op0=ALU.mult, op1=ALU.add), r=["rf0"], w=["rf1"])
            op("act", lambda e: e.activation(out=rf[:, 2:3], in_=rf[:, 1:2], func=AF.Sqrt), r=["rf1"], w=["rf2"])
            op("dve", lambda e: e.reciprocal(out=rf[:, 3:4], in_=rf[:, 2:3]), r=["rf2"], w=["rf3"])
            ok = f"OUT{sl}"
            op("dve", lambda e: e.scalar_tensor_tensor(out=OUTt[sl][:], in0=X3[:], scalar=rf[:, 3:4], in1=LNF[:],
                                                       op0=ALU.mult, op1=ALU.mult), r=["X30", "X31", "rf3", "LNF"],
               w=[ok])
            dma("sp", lambda e: e.dma_start(out=out_d[ti * 128:(ti + 1) * 128, :], in_=OUTt[sl][:]),
                r=[ok], w=[f"out_d{ti}"], key=ok)

        for t0 in range(0, NTILE, 2):
            WV.run([lambda t0=t0: body3(t0), lambda t0=t0: body3(t0 + 1)], [1, 1], seq=not cfg.get("weave23", False))
        tl.pool = None

    S_.op("sp", nop_fns["sp"], r=[], w=[])
    return nc, S_


def emit(nc, S_):
    pref = S_.finish(nc, None, None, None)
    import contextlib
    with contextlib.ExitStack() as es:
        sems = {e: es.enter_context(nc.semaphore("s_" + e)) for e in ENGS}
        dsem = {k: es.enter_context(nc.semaphore("d_" + str(i))) for i, k in enumerate(S_.dma_cnt)}
        bsem = [es.enter_context(nc.semaphore(f"bar{i}")) for i in range(2)]
        block = es.enter_context(nc.Block())

        def run(ename, eh):
            for o in S_.ops[ename]:
                for (key, val) in o["waits"]:
                    if key[0] == "eng":
                        eh.wait_ge(sems[key[1]], pref[key[1]][val])
                    else:
                        eh.wait_ge(dsem[key[1]], val)
                ins = o["fn"](eh)
                if o.get("bar"):
                    nb = o["bar"]
                    ins.then_inc(bsem[nb % 2], 1)
                    eh.wait_ge(bsem[nb % 2], len(ENGS) * ((nb + 1) // 2 if nb % 2 else nb // 2))
                    continue
                if o["dma"] is not None:
                    ins.then_inc(dsem[o["dma"]], 16)
                elif o["inc"]:
                    ins.then_inc(sems[ename], 1)
            if ename == "sp":
                for k, c in S_.dma_cnt.items():
                    eh.wait_ge(dsem[k], c)

        @block.tensor
        def _(t):
            run("pe", t)

        @block.scalar
        def _(a):
            run("act", a)

        @block.vector
        def _(v):
            run("dve", v)

        @block.gpsimd
        def _(g):
            run("pool", g)

        @block.sync
        def _(s):
            run("sp", s)
    return nc


def _perm_q():
    idx = []
    for c in range(4):
        idx += list(range(c * 64, c * 64 + 64)) + list(range((c + 4) * 64, (c + 4) * 64 + 64))
    return np.array(idx)


def _consts(cap):
    c = np.zeros((128, 832), np.float32)
    c[:, 768:800] = (np.arange(32) * cap)[None, :]
    i = np.arange(128)
    c[:, 0:128] = np.eye(128)
    c[:, 128:256] = (i[:, None] < i[None, :])
    c[:, 256:384] = (i[:, None] <= i[None, :])
    c[:, 384:512] = (i[:, None] > i[None, :])
    invf = (10000.0 ** (-np.arange(32, dtype=np.float64) / 32.0)) / (2 * np.pi)
    c[:, 512:544] = invf[None, :]
    c[:, 544:576] = invf[None, :]
    c[:, 576:608] = 0.0
    c[:, 608:640] = 0.25
    c[:, 640:768] = ((i[:, None] // 64) == (i[None, :] // 64))
    return c


def prep_shared(inp, cap):
    f = lambda a: np.ascontiguousarray(np.asarray(a, dtype=np.float32))
    pq = _perm_q()
    w_in = f(inp["w_in"][0])
    cols = np.concatenate([pq, np.arange(512, 4608)])
    w_in = np.ascontiguousarray(w_in[:, cols])
    vec = np.zeros((128, 70), np.float32)
    vec[:, 0:8] = f(inp["ln_mix"][0]).reshape(8, 128).T
    vec[:, 8:16] = f(inp["ln_moe"][0]).reshape(8, 128).T
    vec[:, 16:24] = f(inp["ln_ple"][0]).reshape(8, 128).T
    vec[:, 24:38] = f(inp["mu_shift"][0]).reshape(14, 128).T
    for j, nm in enumerate(["w0", "a0", "k_k", "k_a", "r_k", "ln_x_w", "ln_x_b"]):
        vec[:, 38 + 4 * j:42 + 4 * j] = f(inp[nm][0]).reshape(4, 128).T
    sk = f(inp["sinks"][0])
    for c in range(4):
        vec[0:64, 66 + c] = sk[c]
        vec[64:128, 66 + c] = sk[c + 4]
    sh = dict(
        w_in=w_in, vecs=vec, cst=_consts(cap), ln_moe_row=f(inp['ln_moe'][0])[None, :],
        wlora=np.ascontiguousarray(np.concatenate([f(inp["w_decay_up"][0]), f(inp["w_aaa_up"][0])], 0)),
        wgu=f(inp["w_gate_up"][0]),
        w_ba=np.ascontiguousarray(f(inp["w_branch_att"][0])[pq, :]),
        w_bb=f(inp["w_branch_rwkv"][0]),
        w_out=f(inp["w_out"][0]),
        w_r=np.ascontiguousarray(np.concatenate([f(inp["w_group"][0]), f(inp["w_expert"][0])], 1)),
        b_r=np.ascontiguousarray(np.concatenate([f(inp["b_group"][0]), f(inp["b_expert"][0])])[None, :]),
        w_gate_e=f(inp["w_gate_e"][0]), w_up_e=f(inp["w_up_e"][0]), w_down_e=f(inp["w_down_e"][0]),
        w_pg=f(inp["w_ple_gate"][0]), w_pp=f(inp["w_ple_proj"][0]),
        ln_final=f(inp["ln_final"])[None, :],
    )
    return sh


def prep_core(inp, sh, b0, nseq):
    x = np.asarray(inp["x"], np.float32)[b0:b0 + nseq]
    S = x.shape[1]
    m = dict(sh)
    m["x"] = np.ascontiguousarray(x.reshape(nseq * S, D))
    m["p"] = np.ascontiguousarray(np.asarray(inp["p"], np.float32)[0, b0:b0 + nseq].reshape(nseq * S, 256))
    pos = np.asarray(inp["positions"], np.int32)[b0:b0 + nseq].reshape(-1)
    m["posT"] = np.ascontiguousarray(pos.reshape(-1, 128).T)
    return m


FULL_CFG = dict(NSEQ=4, S=2048, CAP=640)


def kernel(**inputs):
    cfg = FULL_CFG
    nc, S_ = build(cfg)
    emit(nc, S_)
    sh = prep_shared(inputs, cfg["CAP"])
    in_maps = [prep_core(inputs, sh, c * cfg["NSEQ"], cfg["NSEQ"]) for c in range(8)]
    res = run_bass_kernel_spmd(nc, in_maps, core_ids=list(range(8)))
    outs = [r["out"].reshape(cfg["NSEQ"], cfg["S"], D) for r in res.results]
    return np.concatenate(outs, 0).astype(np.float32)
```

```python
import numpy as np
import ml_dtypes
import concourse.bass as bass
import concourse.mybir as mybir
from concourse.bass_utils import run_bass_kernel_spmd

F32 = mybir.dt.float32
BF16 = mybir.dt.bfloat16
I32 = mybir.dt.int32
AF = mybir.ActivationFunctionType
ALU = mybir.AluOpType
AX = mybir.AxisListType

D = 1024
NE = 32
DE = 512
ENGS = ("pe", "act", "dve", "pool", "sp")
SYNC_SAME = ("act", "dve", "pool")


class _Rec:
    def __init__(self):
        self.call = None

    def __getattr__(self, name):
        def f(*a, **k):
            self.call = (name, a, k)
            return self
        return f


def _bind(fn):
    r = _Rec()
    fn(r)
    name, a, k = r.call
    return lambda e: getattr(e, name)(*a, **k)


import threading


class Weaver:
    def __init__(self):
        self.active = False

    def tick(self):
        if not self.active or threading.current_thread() is not self.cur_thread():
            return
        i = self.cur
        self.count[i] += 1
        if self.count[i] >= self.quota[i]:
            self.count[i] = 0
            self._handoff(i)

    def cur_thread(self):
        return self.threads[self.cur]

    def _next_live(self, i):
        n = len(self.threads)
        for d in range(1, n + 1):
            j = (i + d) % n
            if not self.done[j]:
                return j
        return None

    def _handoff(self, i):
        j = self._next_live(i)
        if j is None or j == i:
            return
        self.cur = j
        self.sems[j].release()
        self.sems[i].acquire()

    def run(self, fns, quota, seq=False):
        n = len(fns)
        if n == 1 or seq:
            for f in fns:
                f()
            return
        self.sems = [threading.Semaphore(0) for _ in range(n)]
        self.done = [False] * n
        self.count = [0] * n
        self.quota = list(quota)
        self.err = None
        fin = threading.Semaphore(0)

        def wrap(i):
            self.sems[i].acquire()
            try:
                fns[i]()
            except BaseException as ex:
                self.err = ex
            self.done[i] = True
            j = self._next_live(i)
            if j is None:
                fin.release()
            else:
                self.cur = j
                self.sems[j].release()

        self.threads = [threading.Thread(target=wrap, args=(i,)) for i in range(n)]
        for t in self.threads:
            t.start()
        self.active = True
        self.cur = 0
        self.sems[0].release()
        fin.acquire()
        self.active = False
        for t in self.threads:
            t.join()
        if self.err is not None:
            raise self.err


class Sched:
    def __init__(self):
        self.ops = {e: [] for e in ENGS}
        self.last_w = {}
        self.readers = {}
        self.waited = {e: {} for e in ENGS}
        self.dma_cnt = {}

    def _deps(self, eng, reads, writes):
        deps = []
        for r in reads:
            if r in self.last_w:
                deps.append((self.last_w[r], True))
        for w in writes:
            if w in self.last_w:
                deps.append((self.last_w[w], False))
            for t in self.readers.get(w, {}).values():
                deps.append((t, False))
        waits = []
        for d, raw in deps:
            if d[0] == "eng":
                if d[1] == eng and eng not in SYNC_SAME:
                    continue
                key = ("eng", d[1])
            else:
                key = ("dma", d[1])
            val = d[2]
            if self.waited[eng].get(key, -1) >= val:
                continue
            self.waited[eng][key] = val
            waits.append((key, val))
            if d[0] == "eng":
                self.ops[d[1]][val]["inc"] = True
        return waits

    def op(self, eng, fn, r=(), w=()):
        waits = self._deps(eng, r, w)
        idx = len(self.ops[eng])
        self.ops[eng].append(dict(fn=_bind(fn), waits=waits, inc=False, dma=None))
        tok = ("eng", eng, idx)
        for x in w:
            self.last_w[x] = tok
            self.readers[x] = {}
        for x in r:
            self.readers.setdefault(x, {})[("eng", eng)] = tok

    def dma(self, eng, fn, r=(), w=(), key=None):
        waits = self._deps(eng, r, w)
        prev = self.dma_cnt.get(key, 0)
        if prev and self.waited[eng].get(("dma", key), -1) < prev:
            self.waited[eng][("dma", key)] = prev
            waits.append((("dma", key), prev))
        cnt = prev + 16
        self.dma_cnt[key] = cnt
        self.ops[eng].append(dict(fn=_bind(fn), waits=waits, inc=False, dma=key))
        tok = ("dma", key, cnt)
        for x in w:
            self.last_w[x] = tok
            self.readers[x] = {}
        for x in r:
            self.readers.setdefault(x, {})[("dma", key)] = tok

    def barrier(self, nop_fns):
        self.nbar = getattr(self, "nbar", 0) + 1
        for e in ENGS:
            waits = []
            if e != "sp" and self.ops[e]:
                last = len(self.ops[e]) - 1
                while last >= 0 and (self.ops[e][last].get("bar") or self.ops[e][last]["dma"] is not None):
                    last -= 1
                if last >= 0:
                    self.ops[e][last]["inc"] = True
                    waits.append((("eng", e), last))
            if e == "sp":
                for k, c in self.dma_cnt.items():
                    if self.waited[e].get(("dma", k), -1) < c:
                        self.waited[e][("dma", k)] = c
                        waits.append((("dma", k), c))
            self.ops[e].append(dict(fn=nop_fns[e], waits=waits, inc=False, dma=None, bar=self.nbar))
        self.last_w = {}
        self.readers = {}

    def finish(self, nc, engines, sems, dma_sems):
        pref = {}
        for e in ENGS:
            c = 0
            arr = []
            for o in self.ops[e]:
                if o["inc"] and o["dma"] is None and not o.get("bar"):
                    c += 1
                arr.append(c)
            pref[e] = arr
        return pref


def build(cfg, debug=False):
    NSEQ, S, CAP = cfg["NSEQ"], cfg["S"], cfg["CAP"]
    TPS = S // 128
    NTILE = NSEQ * TPS
    TPC = NTILE * 128
    NSLOT = NE * CAP
    NST = NSLOT // 128
    CT = CAP // 128
    PH = cfg.get("phases", "1a,1b,2,3").split(",")
    CUT = cfg.get("cut", 99)

    nc = bass.Bass("TRN2", target_bir_lowering=False)
    dr = {}

    def din(name, shape, dt=F32):
        dr[name] = nc.dram_tensor(name, list(shape), dt, kind="ExternalInput").ap()
        return dr[name]

    def dscr(name, shape, dt=F32, out=False):
        dr[name] = nc.dram_tensor(name, list(shape), dt, kind=("ExternalOutput" if out else "Internal")).ap()
        return dr[name]

    x_d = din("x", [TPC, D])
    p_d = din("p", [TPC, 256])
    pos_d = din("posT", [128, NTILE], I32)
    win_d = din("w_in", [D, 4608])
    vec_d = din("vecs", [128, 70])
    cst_d = din("cst", [128, 832])
    wlora_d = din("wlora", [128, 512])
    wgu_d = din("wgu", [128, 512])
    wba_d = din("w_ba", [512, D])
    wbb_d = din("w_bb", [512, D])
    wout_d = din("w_out", [D, D])
    wr_d = din("w_r", [D, 36])
    br_d = din("b_r", [1, 36])
    wg_d = din("w_gate_e", [NE, D, DE])
    wu_d = din("w_up_e", [NE, D, DE])
    wd_d = din("w_down_e", [NE, DE, D])
    wpg_d = din("w_pg", [D, D])
    wpp_d = din("w_pp", [256, D])
    lnf_d = din("ln_final", [1, D])
    lnmoe_d = din("ln_moe_row", [1, D])
    out_d = dscr("out", [TPC, D], F32, out=True)
    yab_d = dscr("yab", [NTILE, 128, 8 * 128], BF16, out=debug)
    x1_d = dscr("x1s", [TPC, D], F32, out=debug)
    hs_d = dscr("hslots", [NSLOT, D], BF16, out=debug)
    ys_d = dscr("yslots", [NSLOT, D], F32, out=debug)
    if debug:
        rt_d = dscr("route", [128, NTILE * 4], F32, out=True)

    S_ = Sched()
    base0 = 229376 - int(nc.sbuf_bytes_remaining)
    base0 = (base0 + 63) // 64 * 64
    st = {"p": base0, "ph": None}

    def salloc(name, shape, dt):
        nb = int(np.prod(shape[1:])) * (4 if dt in (F32, I32) else 2)
        nb = (nb + 31) // 32 * 32
        t = nc.alloc_sbuf_tensor_at(name, list(shape), dt, offset=st["p"])
        st["p"] += nb
        assert st["p"] <= 229376 - 64, ("SBUF overflow", name, st["p"])
        return t

    psb = [nc.alloc_psum_tensor(f"psb{i}", [128, 1024], BF16) for i in range(2)]
    psf = [nc.alloc_psum_tensor(f"psf{i}", [128, 512], F32) for i in range(6)]
    rr = {"f": 0, "b": 0, "fA": 0, "fB": 0}
    tl = threading.local()

    def PS():
        pool = getattr(tl, "pool", None)
        if pool == "A":
            i = rr["fA"] % 2
            rr["fA"] += 1
        elif pool == "B":
            i = 2 + rr["fB"] % 4
            rr["fB"] += 1
        elif pool == "E":
            i = rr["fA"] % 3
            rr["fA"] += 1
        elif pool == "O":
            i = 3 + rr["fB"] % 3
            rr["fB"] += 1
        else:
            i = rr["f"] % 6
            rr["f"] += 1
        return psf[i], f"psf{i}"

    def PSB():
        pool = getattr(tl, "pool", None)
        if pool in ("A", "E"):
            i = 0
        elif pool in ("B", "O"):
            i = 1
        else:
            i = rr["b"] % 2
            rr["b"] += 1
        return psb[i], f"psb{i}"

    vec = salloc("vec", [128, 70], F32)
    cst = salloc("cstf", [128, 832], F32)
    identb = salloc("identb", [128, 128], BF16)
    bones = salloc("bones", [128, 128], BF16)
    onesb = salloc("onesb", [128, 128], BF16)
    onesf = salloc("onesf", [128, 128], F32)
    derived = salloc("derived", [128, 32], F32)
    posi = salloc("posi", [128, NTILE], I32)
    posf = salloc("posf", [128, NTILE], F32)
    slot_i = salloc("slot_i", [128, NTILE * 2], I32)
    rw_f = salloc("rw_f", [128, NTILE * 2], F32)
    scr = salloc("scr", [128, 64], F32)
    ph_base = st["p"]
    IDENTF = cst[:, 0:128]
    USTR = cst[:, 128:256]
    UINC = cst[:, 256:384]
    LSTR = cst[:, 384:512]
    INVF = cst[:, 512:576]
    OFFS = cst[:, 576:640]
    MASK2 = cst[:, 128:384]
    V_LNMIX, V_LNMOE, V_LNPLE, V_MU = 0, 8, 16, 24
    V_W0, V_A0, V_KK, V_KA, V_RK, V_LNW, V_LNB, V_SINK = 38, 42, 46, 50, 54, 58, 62, 66

    def bc(ap, shape):
        return ap.to_broadcast(list(shape))

    def vcol(c, n=1):
        return vec[:, c:c + n]

    WV = Weaver()
    GLOBAL_KEYS = {"vec", "cst", "identb", "bones", "onesb", "onesf", "derived", "posf", "posi",
                   "Wgt", "WbA", "WbB", "Wout", "Wr", "BR", "GMOE", "ustrb", "CNT", "hs_all", "ys_all",
                   "Wpg", "Wpp", "LNF", "WG0", "WG1", "WU0", "WU1", "WD0", "WD1"}
    GLOBAL_PREF = ("psf", "psb", "yab_d", "x1_d", "slot", "rw", "hs_sc", "out_d")

    def stream(sfx):
        def m(keys):
            return [k if (k in GLOBAL_KEYS or k.startswith(GLOBAL_PREF)) else k + sfx for k in keys]

        def op_(eng, fn, r=(), w=()):
            op(eng, fn, m(r), m(w))

        def dma_(eng, fn, r=(), w=(), key=None):
            dma(eng, fn, m(r), m(w), key)
        return op_, dma_

    def op(eng, fn, r=(), w=()):
        S_.op(eng, fn, r, w)
        WV.tick()
    op_glob = op

    def dma(eng, fn, r=(), w=(), key=None):
        S_.dma(eng, fn, r, w, key)
        WV.tick()

    dma("sp", lambda e: e.dma_start(out=vec[:], in_=vec_d), w=["vec"], key="vec")
    dma("sp", lambda e: e.dma_start(out=cst[:], in_=cst_d), w=["cst"], key="cst")
    dma("sp", lambda e: e.dma_start(out=posi[:], in_=pos_d), w=["posi"], key="posi")
    op("dve", lambda e: e.tensor_copy(out=identb[:], in_=IDENTF), r=["cst"], w=["identb"])
    op("dve", lambda e: e.tensor_copy(out=bones[:], in_=cst[:, 640:768]), r=["cst"], w=["bones"])
    op("dve", lambda e: e.memset(onesb[:], 1.0), w=["onesb"])
    op("dve", lambda e: e.memset(onesf[:], 1.0), w=["onesf"])
    op("dve", lambda e: e.tensor_copy(out=posf[:], in_=posi[:]), r=["posi"], w=["posf"])
    op("dve", lambda e: e.tensor_scalar(out=derived[:, 0:14], in0=vcol(V_MU, 14), scalar1=-1.0, scalar2=1.0,
                                        op0=ALU.mult, op1=ALU.add), r=["vec"], w=["derived"])
    op("dve", lambda e: e.tensor_scalar(out=derived[:, 14:18], in0=vcol(V_KA, 4), scalar1=-1.0, scalar2=1.0,
                                        op0=ALU.mult, op1=ALU.add), r=["vec"], w=["derived"])
    op("act", lambda e: e.activation(out=derived[:, 18:22], in_=vcol(V_SINK, 4), func=AF.Exp), r=["vec", "derived"],
       w=["derived"])
    OMU = lambda c, n=1: derived[:, c:c + n]
    OMKA = lambda c, n=1: derived[:, 14 + c:14 + c + n]
    ESINK = derived[:, 18:22]

    nop_fns = {
        "pe": lambda e: e.nop(), "act": lambda e: e.nop(), "dve": lambda e: e.nop(),
        "pool": lambda e: e.nop(), "sp": lambda e: e.nop(),
    }

    def phase_reset():
        S_.barrier(nop_fns)
        st["p"] = ph_base

    def load_cast_weight(dst, dst_key, src_ap, rows, cols, gcol, stage, stage_key, kchunks, eng_cycle):
        for kc in range(kchunks):
            sl = stage[kc % len(stage)]
            sk = stage_key[kc % len(stage)]
            dma("sp", lambda e, sl=sl, kc=kc: e.dma_start(out=sl[:, 0:cols], in_=src_ap[kc * 128:(kc + 1) * 128, :]),
                w=[sk], key=sk)
            eng = eng_cycle[kc % len(eng_cycle)]
            if gcol is None:
                op(eng, lambda e, sl=sl, kc=kc: e.tensor_copy(out=dst[:, kc, :], in_=sl[:, 0:cols]),
                   r=[sk], w=[dst_key])
            else:
                op(eng, lambda e, sl=sl, kc=kc: e.tensor_scalar(out=dst[:, kc, :], in0=sl[:, 0:cols],
                                                                 scalar1=vcol(gcol + kc), scalar2=None, op0=ALU.mult),
                   r=[sk, "vec"], w=[dst_key])

    def rms_to_hT(ti, xin, xin_key, hT, hT_key, tmp, normalize=True, op=None):
        op = op or op_glob
        junk, ss, rstd, xbf = tmp["junk"], tmp["ss"], tmp["rstd"], tmp["xbf"]
        op("act", lambda e: e.activation(out=junk[:], in_=xin[:], func=AF.Square, accum_out=ss[:, 0:1]),
           r=[xin_key], w=["junk", "ss"])
        op("dve", lambda e: e.tensor_scalar(out=ss[:, 1:2], in0=ss[:, 0:1], scalar1=1.0 / D, scalar2=1e-6,
                                            op0=ALU.mult, op1=ALU.add), r=["ss"], w=["ss1"])
        op("act", lambda e: e.activation(out=ss[:, 2:3], in_=ss[:, 1:2], func=AF.Sqrt), r=["ss1"], w=["ss2"])
        op("dve", lambda e: e.reciprocal(out=rstd[:, 0:1], in_=ss[:, 2:3]), r=["ss2"], w=["rstd"])
        if normalize:
            op("dve", lambda e: e.tensor_scalar(out=xbf[:], in0=xin[:], scalar1=rstd[:, 0:1], scalar2=None,
                                                op0=ALU.mult), r=[xin_key, "rstd"], w=["xbf"])
        else:
            op("pool", lambda e: e.tensor_copy(out=xbf[:], in_=xin[:]), r=[xin_key], w=["xbf"])
        pb, pk = PSB()
        for kc in range(8):
            op("pe", lambda e, kc=kc: e.transpose(pb[:, kc * 128:(kc + 1) * 128], xbf[:, kc * 128:(kc + 1) * 128],
                                                  identb[:]), r=["xbf", "identb"], w=[pk])
        op("act", lambda e: e.activation(out=hT[:].rearrange("p k t -> p (k t)"), in_=pb[:, :], func=AF.Copy),
           r=[pk], w=[hT_key])

    if "1a" in PH:
        Wqkv = salloc("Wqkv", [128, 8, 768], BF16)
        Wrw = salloc("Wrw", [128, 8, 1792], BF16)
        Wlora = salloc("Wlora", [128, 512], BF16)
        Wgu = salloc("Wgu", [128, 512], BF16)
        stg = [salloc("stgA", [128, 2560], F32)]
        for kc in range(8):
            dma("sp", lambda e, kc=kc: e.dma_start(out=stg[0][:, 0:2560], in_=win_d[kc * 128:(kc + 1) * 128, 0:2560]),
                w=["stgA"], key="stgA")
            op("dve", lambda e, kc=kc: e.tensor_scalar(out=Wqkv[:, kc, :], in0=stg[0][:, 0:768],
                                                       scalar1=vcol(V_LNMIX + kc), scalar2=None, op0=ALU.mult),
               r=["stgA", "vec"], w=["Wqkv"])
            op("pool", lambda e, kc=kc: e.tensor_scalar(out=Wrw[:, kc, :], in0=stg[0][:, 768:2560],
                                                        scalar1=vcol(V_LNMIX + kc), scalar2=1.0, op0=ALU.mult,
                                                        op1=ALU.mult),
               r=["stgA", "vec"], w=["Wrw"])
        dma("sp", lambda e: e.dma_start(out=stg[0][:, 0:512], in_=wlora_d), w=["stgA"], key="stgA")
        op("dve", lambda e: e.tensor_copy(out=Wlora[:], in_=stg[0][:, 0:512]), r=["stgA"], w=["Wlora"])
        dma("sp", lambda e: e.dma_start(out=stg[0][:, 512:1024], in_=wgu_d), w=["stgA"], key="stgA")
        op("dve", lambda e: e.tensor_copy(out=Wgu[:], in_=stg[0][:, 512:1024]), r=["stgA"], w=["Wgu"])

        xin = [salloc(f"xin{i}", [128, D], F32) for i in range(2)]
        tmp = dict(junk=salloc("junk", [128, D], BF16), ss=salloc("ss", [128, 4], F32),
                   rstd=salloc("rstd", [128, 1], F32), xbf=salloc("xbf", [128, D], BF16))
        hT = [salloc(f"hT{i}", [128, 8, 128], BF16) for i in range(2)]
        ropeT = salloc("ropeT", [128, 64], F32)
        ropeN = salloc("ropeN", [128, 64], I32)
        ropeF = salloc("ropeF", [128, 64], F32)
        ropeG = salloc("ropeG", [128, 64], F32)
        CS = salloc("CS", [128, 64], F32)
        ropA = salloc("ropA", [128, 640], F32)
        ropB = salloc("ropB", [128, 640], F32)
        qkr = salloc("qkr", [128, 640], BF16)
        qT = salloc("qT", [128, 4, 128], BF16)
        kTs = [salloc(f"kT{i}", [128, 128], BF16) for i in range(2)]
        vts = [salloc(f"vtok{i}", [128, 128], BF16) for i in range(2)]
        Eb = [salloc(f"Eb{i}", [128, 512], BF16) for i in range(4)]
        dent = salloc("dent", [128, 4, 128], F32)
        yab = [salloc(f"yabs{i}", [128, 8, 128], BF16) for i in range(2)]
        zb = [salloc(f"zb{i}", [128, 4, 129], F32) for i in range(2)]
        zt1 = salloc("zt1", [128, 4, 128], F32)
        zt2 = salloc("zt2", [128, 4, 128], F32)
        carry = salloc("carry", [128, 16], F32)
        Rr = salloc("Rr", [128, 4, 128], F32)
        Kr = salloc("Kr", [128, 4, 128], F32)
        Vr = salloc("Vr", [128, 4, 128], F32)
        XM = salloc("XM", [128, 2, 128], F32)
        LIN = salloc("LIN", [128, 128], BF16)
        SXG = salloc("SXG", [128, 128], BF16)
        SG = salloc("SG", [128, 4, 128], F32)
        Aa = salloc("Aa", [128, 4, 128], F32)
        Gg = salloc("Gg", [128, 4, 128], F32)
        LW = salloc("LW", [128, 4, 128], F32)
        CUM = salloc("CUM", [128, 4, 128], F32)
        CX = salloc("CX", [128, 4, 128], F32)
        E1 = salloc("E1", [128, 4, 128], F32)
        E2 = salloc("E2", [128, 4, 128], F32)
        E3 = salloc("E3", [128, 4, 128], F32)
        E4 = salloc("E4", [128, 4, 128], F32)
        KKR = salloc("KKR", [128, 4, 128], F32)
        SQb = salloc("SQb", [128, 4, 128], BF16)
        RN = salloc("RN", [128, 4, 128], F32)
        KK = salloc("KK", [128, 4, 128], F32)
        T1 = salloc("T1", [128, 4, 128], F32)
        KP = salloc("KP", [128, 4, 128], F32)
        Bb = salloc("Bb", [128, 4, 128], F32)
        AR = salloc("AR", [128, 4, 2, 128], BF16)
        BK = salloc("BK", [128, 4, 2, 128], BF16)
        BKS = salloc("BKS", [128, 4, 2, 128], BF16)
        ARm = [salloc(f"ARm{i}", [128, 4, 2, 128], BF16) for i in range(2)]
        BKm = [salloc(f"BKm{i}", [128, 4, 2, 128], BF16) for i in range(2)]
        RK = salloc("RK", [128, 4, 128], F32)
        RK2 = salloc("RK2", [128, 4, 128], BF16)
        BV = salloc("BV", [128, 4, 128], F32)
        VB = salloc("VB", [128, 4, 128], BF16)
        BKT = salloc("BKT", [128, 1024], BF16)
        VT = salloc("VT", [128, 512], BF16)
        MA = salloc("MA", [128, 8, 256], BF16)
        MB = salloc("MB", [128, 8, 256], BF16)
        Qm = [salloc(f"Qm{i}", [128, 8, 128], BF16) for i in range(2)]
        PX = [salloc(f"PX{i}", [128, 8, 256], BF16) for i in range(2)]
        XF = salloc("XF", [128, 8, 128], BF16)
        SF = salloc("SF", [128, 4, 64], F32)
        SBs = salloc("SBs", [128, 4, 64], BF16)
        TMPS = salloc("TMPS", [128, 4, 64], F32)
        RH = salloc("RH", [128, 512], BF16)
        UT = salloc("UT", [128, 512], BF16)
        Yf = salloc("Yf", [128, 4, 128], F32)
        YB = salloc("YB", [128, 4, 128], BF16)
        YSQ = salloc("YSQ", [128, 4, 128], BF16)
        MEAN = salloc("MEAN", [128, 4, 128], F32)
        M2 = salloc("M2", [128, 4, 128], F32)
        VAR = salloc("VAR", [128, 4, 128], F32)
        Dd = salloc("Dd", [128, 4, 128], F32)

        def f2(t):
            return t[:].rearrange("p a b -> p (a b)")

        def genA(ti):
            tl.pool = "A"
            tj = ti % TPS
            sl = ti % 2
            xk = f"xin{sl}"
            dma("sp", lambda e, ti=ti, sl=sl: e.dma_start(out=xin[sl][:], in_=x_d[ti * 128:(ti + 1) * 128, :]),
                w=[xk], key=xk)
            hk = f"hT{sl}"
            rms_to_hT(ti, xin[sl], xk, hT[sl], hk, tmp)
            h = hT[sl]
            if CUT <= 1:
                return
            pq, pqk = PS()
            pkv, pkvk = PS()
            for kc in range(8):
                op("pe", lambda e, kc=kc: e.matmul(pq[:, 0:512], h[:, kc, :], Wqkv[:, kc, 0:512],
                                                   start=(kc == 0), stop=(kc == 7)), r=[hk, "Wqkv"], w=[pqk])
            for kc in range(8):
                op("pe", lambda e, kc=kc: e.matmul(pkv[:, 0:256], h[:, kc, :], Wqkv[:, kc, 512:768],
                                                   start=(kc == 0), stop=(kc == 7)), r=[hk, "Wqkv"], w=[pkvk])
            op("dve", lambda e, ti=ti: e.scalar_tensor_tensor(out=ropeT[:], in0=INVF, scalar=posf[:, ti:ti + 1],
                                                              in1=OFFS, op0=ALU.mult, op1=ALU.add),
               r=["cst", "posf"], w=["ropeT"])
            op("dve", lambda e: e.tensor_copy(out=ropeN[:], in_=ropeT[:]), r=["ropeT"], w=["ropeN"])
            op("dve", lambda e: e.tensor_copy(out=ropeF[:], in_=ropeN[:]), r=["ropeN"], w=["ropeF"])
            op("dve", lambda e: e.tensor_tensor(out=ropeF[:], in0=ropeT[:], in1=ropeF[:], op=ALU.subtract),
               r=["ropeT", "ropeF"], w=["ropeF"])
            op("dve", lambda e: e.tensor_single_scalar(out=ropeG[:], in_=ropeF[:], scalar=0.5, op=ALU.is_gt),
               r=["ropeF"], w=["ropeG"])
            op("dve", lambda e: e.tensor_tensor(out=ropeF[:], in0=ropeF[:], in1=ropeG[:], op=ALU.subtract),
               r=["ropeF", "ropeG"], w=["ropeF"])
            op("act", lambda e: e.activation(out=CS[:], in_=ropeF[:], func=AF.Sin, scale=2.0 * np.pi),
               r=["ropeF"], w=["CS"])
            if CUT <= 2:
                return
            for (src, skey, c0, H) in ((pq, pqk, 0, 8), (pkv, pkvk, 512, 2)):
                W_ = H * 64
                s4 = src[:, 0:W_].rearrange("p (h t d) -> p h t d", h=H, t=2)
                A4 = ropA[:, c0:c0 + W_].rearrange("p (h t d) -> p h t d", h=H, t=2)
                B4 = ropB[:, c0:c0 + W_].rearrange("p (h t d) -> p h t d", h=H, t=2)
                O4 = qkr[:, c0:c0 + W_].rearrange("p (h t d) -> p h t d", h=H, t=2)
                cosb = CS[:, 32:64].unsqueeze(1).unsqueeze(1).to_broadcast([128, H, 2, 32])
                sinb = CS[:, 0:32].unsqueeze(1).to_broadcast([128, H, 32])
                op("dve", lambda e, s4=s4, A4=A4, cosb=cosb: e.tensor_tensor(out=A4, in0=s4, in1=cosb, op=ALU.mult),
                   r=[skey, "CS"], w=["ropA"])
                op("dve", lambda e, s4=s4, B4=B4, sinb=sinb: e.tensor_tensor(out=B4[:, :, 0, :], in0=s4[:, :, 1, :],
                                                                           in1=sinb, op=ALU.mult),
                   r=[skey, "CS"], w=["ropB"])
                op("dve", lambda e, s4=s4, B4=B4, sinb=sinb: e.tensor_tensor(out=B4[:, :, 1, :], in0=s4[:, :, 0, :],
                                                                           in1=sinb, op=ALU.mult),
                   r=[skey, "CS"], w=["ropB"])
                op("pool", lambda e, A4=A4, B4=B4, O4=O4: e.tensor_tensor(out=O4[:, :, 0, :], in0=A4[:, :, 0, :],
                                                                         in1=B4[:, :, 0, :], op=ALU.subtract),
                   r=["ropA", "ropB"], w=["qkr"])
                op("pool", lambda e, A4=A4, B4=B4, O4=O4: e.tensor_tensor(out=O4[:, :, 1, :], in0=A4[:, :, 1, :],
                                                                         in1=B4[:, :, 1, :], op=ALU.add),
                   r=["ropA", "ropB"], w=["qkr"])
            vk = f"vtok{sl}"
            kk_ = f"kT{sl}"
            op("act", lambda e, sl=sl: e.activation(out=vts[sl][:], in_=pkv[:, 128:256], func=AF.Copy),
               r=[pkvk], w=[vk])
            pb, pbk = PSB()
            for c in range(5):
                op("pe", lambda e, c=c: e.transpose(pb[:, c * 128:(c + 1) * 128], qkr[:, c * 128:(c + 1) * 128],
                                                    identb[:]), r=["qkr", "identb"], w=[pbk])
            op("act", lambda e: e.activation(out=f2(qT), in_=pb[:, 0:512], func=AF.Copy), r=[pbk], w=["qT"])
            op("act", lambda e, sl=sl: e.activation(out=kTs[sl][:], in_=pb[:, 512:640], func=AF.Copy),
               r=[pbk], w=[kk_])
            if CUT <= 3:
                return
            kbs = ([1 - sl] if tj > 0 else []) + [sl]
            ei = 0
            Euse = {}
            for g in range(2):
                for kb in kbs:
                    pe_, pek = PS()
                    op("pe", lambda e, g=g, kb=kb, pe_=pe_: e.matmul(
                        pe_[:, 0:512], kTs[kb][g * 64:(g + 1) * 64, :], qT[g * 64:(g + 1) * 64, :, :],
                        start=True, stop=True), r=[f"kT{kb}", "qT"], w=[pek])
                    Et = Eb[ei]
                    ek = f"Eb{ei}"
                    ei += 1
                    op("act", lambda e, Et=Et, pe_=pe_: e.activation(out=Et[:], in_=pe_[:, 0:512], func=AF.Exp,
                                                                    scale=0.125), r=[pek], w=[ek])
                    msk = UINC if kb == sl else LSTR
                    op("pool", lambda e, Et=Et, msk=msk: e.tensor_tensor(
                        out=Et[:].rearrange("p (c q) -> p c q", c=4), in0=Et[:].rearrange("p (c q) -> p c q", c=4),
                        in1=msk.unsqueeze(1).to_broadcast([128, 4, 128]), op=ALU.mult), r=[ek, "cst"], w=[ek])
                    Euse[(g, kb)] = (Et, ek)
            po, pok = PS()
            pd, pdk = PS()
            for g in range(2):
                for i, kb in enumerate(kbs):
                    Et, ek = Euse[(g, kb)]
                    op("pe", lambda e, g=g, kb=kb, Et=Et, i=i: e.matmul(
                        po[g * 64:(g + 1) * 64, 0:512], vts[kb][:, g * 64:(g + 1) * 64], Et[:],
                        start=(i == 0), stop=(i == len(kbs) - 1)), r=[f"vtok{kb}", ek], w=[pok])
                for i, kb in enumerate(kbs):
                    Et, ek = Euse[(g, kb)]
                    op("pe", lambda e, g=g, Et=Et, i=i: e.matmul(
                        pd[g * 64:(g + 1) * 64, 0:512], onesb[:, 0:64], Et[:],
                        start=(i == 0), stop=(i == len(kbs) - 1)), r=["onesb", ek], w=[pdk])
            ys = yab[sl]
            yk = f"yabs{sl}"
            op("dve", lambda e: e.tensor_tensor(out=dent[:], in0=pd[:, 0:512].rearrange("p (c q) -> p c q", c=4),
                                                in1=ESINK.unsqueeze(2).to_broadcast([128, 4, 128]), op=ALU.add),
               r=[pdk, "derived"], w=["dent"])
            op("act", lambda e: e.activation(out=f2(dent), in_=f2(dent), func=AF.Ln), r=["dent"], w=["dent"])
            op("act", lambda e: e.activation(out=f2(dent), in_=f2(dent), func=AF.Exp, scale=-1.0), r=["dent"], w=["dent"])
            op("dve", lambda e, ys=ys: e.tensor_tensor(out=ys[:, 0:4, :],
                                                       in0=po[:, 0:512].rearrange("p (c q) -> p c q", c=4),
                                                       in1=dent[:], op=ALU.mult), r=[pok, "dent"], w=[yk + "a"])


        def genBC(ti):
            tl.pool = "B"
            tj = ti % TPS
            sl = ti % 2
            hk = f"hT{sl}"
            h = hT[sl]
            ys = yab[sl]
            yk = f"yabs{sl}"
            if CUT <= 4:
                return
            if tj == 0:
                op("pool", lambda e: e.memset(carry[:], 0.0), w=["carry"])
                op("pool", lambda e: e.memset(SF[:], 0.0), w=["SF"])
                op("pool", lambda e: e.memset(SBs[:], 0.0), w=["SBs"])
            groups = [(0, 4, Rr, "Rr"), (4, 4, Kr, "Kr"), (8, 4, Vr, "Vr"), (12, 2, XM, "XM")]
            for gi, (z0, n, dst, dk) in enumerate(groups):
                pz, pzk = PS()
                for j in range(n):
                    zc = z0 + j
                    for kc in range(8):
                        op("pe", lambda e, j=j, zc=zc, kc=kc, pz=pz: e.matmul(
                            pz[:, j * 128:(j + 1) * 128], Wrw[:, kc, zc * 128:(zc + 1) * 128], h[:, kc, :],
                            start=(kc == 0), stop=(kc == 7)), r=[hk, "Wrw"], w=[pzk])
                zbt = zb[gi % 2]
                zk = f"zb{gi % 2}"
                op("act", lambda e, zbt=zbt, pz=pz, n=n: e.activation(
                    out=zbt[:, 0:n, 1:129], in_=pz[:, 0:n * 128].rearrange("p (c t) -> p c t", c=n), func=AF.Copy),
                   r=[pzk], w=[zk])
                op("pool", lambda e, zbt=zbt, z0=z0, n=n: e.tensor_copy(out=zbt[:, 0:n, 0], in_=carry[:, z0:z0 + n]),
                   r=["carry"], w=[zk])
                op("dve", lambda e, zbt=zbt, z0=z0, n=n: e.tensor_tensor(
                    out=zt1[:, 0:n, :], in0=zbt[:, 0:n, 0:128],
                    in1=vcol(V_MU + z0, n).unsqueeze(2).to_broadcast([128, n, 128]), op=ALU.mult),
                   r=[zk, "vec"], w=["zt1"])
                op("pool", lambda e, zbt=zbt, z0=z0, n=n: e.tensor_tensor(
                    out=zt2[:, 0:n, :], in0=zbt[:, 0:n, 1:129],
                    in1=OMU(z0, n).unsqueeze(2).to_broadcast([128, n, 128]), op=ALU.mult),
                   r=[zk, "derived"], w=["zt2"])
                op("dve", lambda e, dst=dst, n=n: e.tensor_tensor(out=dst[:, 0:n, :], in0=zt1[:, 0:n, :],
                                                                  in1=zt2[:, 0:n, :], op=ALU.add),
                   r=["zt1", "zt2"], w=[dk])
                op("pool", lambda e, zbt=zbt, z0=z0, n=n: e.tensor_copy(out=carry[:, z0:z0 + n], in_=zbt[:, 0:n, 128]),
                   r=[zk], w=["carry"])
            if CUT <= 5:
                return
            op("act", lambda e: e.activation(out=LIN[0:64, :], in_=XM[0:64, 0, :], func=AF.Tanh), r=["XM"], w=["LINa"])
            op("pool", lambda e: e.tensor_copy(out=LIN[64:128, :], in_=XM[64:128, 0, :]), r=["XM"], w=["LINb"])
            op("act", lambda e: e.activation(out=SXG[:], in_=XM[:, 1, :], func=AF.Sigmoid), r=["XM"], w=["SXG"])
            pu, puk = PS()
            pa, pak = PS()
            pg, pgk = PS()
            for cc in range(4):
                op("pe", lambda e, cc=cc: e.matmul(pu[:, cc * 128:(cc + 1) * 128], Wlora[0:64, cc * 128:(cc + 1) * 128],
                                                   LIN[0:64, :], start=True, stop=True),
                   r=["Wlora", "LINa"], w=[puk])
            for cc in range(4):
                op("pe", lambda e, cc=cc: e.matmul(pa[:, cc * 128:(cc + 1) * 128],
                                                   Wlora[64:128, cc * 128:(cc + 1) * 128],
                                                   LIN[64:128, :], start=True, stop=True),
                   r=["Wlora", "LINb"], w=[pak])
            for cc in range(4):
                op("pe", lambda e, cc=cc: e.matmul(pg[:, cc * 128:(cc + 1) * 128], Wgu[:, cc * 128:(cc + 1) * 128],
                                                   SXG[:], start=True, stop=True), r=["Wgu", "SXG"], w=[pgk])
            for cc in range(4):
                op("act", lambda e, cc=cc: e.activation(out=SG[:, cc, :], in_=pu[:, cc * 128:(cc + 1) * 128],
                                                        func=AF.Sigmoid, bias=vcol(V_W0 + cc)),
                   r=[puk, "vec"], w=["SG"])
            for cc in range(4):
                op("act", lambda e, cc=cc: e.activation(out=Aa[:, cc, :], in_=pa[:, cc * 128:(cc + 1) * 128],
                                                        func=AF.Sigmoid, bias=vcol(V_A0 + cc)),
                   r=[pak, "vec"], w=["Aa"])
            op("act", lambda e: e.activation(out=f2(Gg), in_=pg[:, 0:512], func=AF.Copy), r=[pgk], w=["Gg"])
            op("act", lambda e: e.activation(out=f2(LW), in_=f2(SG), func=AF.Copy, scale=-0.6065306597126334),
               r=["SG"], w=["LW"])
            for cc in range(4):
                op("dve", lambda e, cc=cc: e.tensor_tensor_scan(out=CUM[:, cc, :], data0=onesf[:], data1=LW[:, cc, :],
                                                                initial=0.0, op0=ALU.mult, op1=ALU.add),
                   r=["onesf", "LW"], w=["CUM"])
            op("dve", lambda e: e.tensor_tensor(out=f2(CX), in0=f2(CUM), in1=f2(LW), op=ALU.subtract),
               r=["CUM", "LW"], w=["CX"])
            op("act", lambda e: e.activation(out=f2(E1), in_=f2(CUM), func=AF.Exp), r=["CUM"], w=["E1"])
            op("act", lambda e: e.activation(out=f2(E2), in_=f2(CUM), func=AF.Exp, scale=-1.0), r=["CUM"], w=["E2"])
            op("act", lambda e: e.activation(out=f2(E3), in_=f2(CX), func=AF.Exp), r=["CX"], w=["E3"])
            for cc in range(4):
                op("act", lambda e, cc=cc: e.activation(out=E4[:, cc, :], in_=CUM[:, cc, :], func=AF.Exp, scale=-1.0,
                                                        bias=CUM[:, cc, 127:128]), r=["CUM"], w=["E4"])
            op("dve", lambda e: e.tensor_tensor(out=KKR[:], in0=Kr[:],
                                                 in1=vcol(V_KK, 4).unsqueeze(2).to_broadcast([128, 4, 128]),
                                                 op=ALU.mult), r=["Kr", "vec"], w=["KKR"])
            op("act", lambda e: e.activation(out=f2(SQb), in_=f2(KKR), func=AF.Square), r=["KKR"], w=["SQb"])
            pss, pssk = PS()
            op("pe", lambda e: e.matmul(pss[:, 0:512], bones[:], f2(SQb), start=True, stop=True),
               r=["bones", "SQb"], w=[pssk])
            op("act", lambda e: e.activation(out=f2(RN), in_=pss[:, 0:512], func=AF.Ln, bias=1e-19),
               r=[pssk], w=["RN"])
            op("act", lambda e: e.activation(out=f2(RN), in_=f2(RN), func=AF.Exp, scale=-0.5), r=["RN"], w=["RN"])
            op("dve", lambda e: e.tensor_tensor(out=f2(KK), in0=f2(KKR), in1=f2(RN), op=ALU.mult),
               r=["KKR", "RN"], w=["KK"])
            for cc in range(4):
                op("dve", lambda e, cc=cc: e.tensor_scalar(out=T1[:, cc, :], in0=Aa[:, cc, :],
                                                           scalar1=vcol(V_KA + cc), scalar2=OMKA(cc),
                                                           op0=ALU.mult, op1=ALU.add),
                   r=["Aa", "vec", "derived"], w=["T1"])
            op("dve", lambda e: e.tensor_tensor(out=f2(KP), in0=f2(Kr), in1=f2(T1), op=ALU.mult),
               r=["Kr", "T1"], w=["KP"])
            op("dve", lambda e: e.tensor_tensor(out=f2(Bb), in0=f2(KK), in1=f2(Aa), op=ALU.mult),
               r=["KK", "Aa"], w=["Bb"])
            op("dve", lambda e: e.scalar_tensor_tensor(out=AR[:, :, 0, :], in0=E3[:], scalar=-1.0, in1=KK[:],
                                                       op0=ALU.mult, op1=ALU.mult), r=["E3", "KK"], w=["AR"])
            op("dve", lambda e: e.tensor_tensor(out=AR[:, :, 1, :], in0=E1[:], in1=Rr[:], op=ALU.mult),
               r=["E1", "Rr", "AR"], w=["AR"])
            op("dve", lambda e: e.tensor_tensor(out=BK[:, :, 0, :], in0=E2[:], in1=Bb[:], op=ALU.mult),
               r=["E2", "Bb"], w=["BK"])
            op("dve", lambda e: e.tensor_tensor(out=BK[:, :, 1, :], in0=E2[:], in1=KP[:], op=ALU.mult),
               r=["E2", "KP", "BK"], w=["BK"])
            op("dve", lambda e: e.tensor_tensor(out=BKS[:, :, 0, :], in0=E4[:], in1=Bb[:], op=ALU.mult),
               r=["E4", "Bb"], w=["BKS"])
            op("dve", lambda e: e.tensor_tensor(out=BKS[:, :, 1, :], in0=E4[:], in1=KP[:], op=ALU.mult),
               r=["E4", "KP", "BKS"], w=["BKS"])
            for par in range(2):
                pmc = cst[:, 640 + 64 * par:641 + 64 * par]
                op("act", lambda e: e.activation(
                    out=ARm[par][:].rearrange("p a b c -> p (a b c)"), in_=AR[:].rearrange("p a b c -> p (a b c)"),
                    func=AF.Copy, scale=pmc), r=["AR", "cst"], w=[f"ARm{par}"])
                op("act" if par else "dve", (lambda e: e.activation(
                    out=BKm[par][:].rearrange("p a b c -> p (a b c)"), in_=BK[:].rearrange("p a b c -> p (a b c)"),
                    func=AF.Copy, scale=pmc)) if par else (lambda e: e.tensor_scalar(
                    out=BKm[par][:].rearrange("p a b c -> p (a b c)"), in0=BK[:].rearrange("p a b c -> p (a b c)"),
                    scalar1=pmc, scalar2=None, op0=ALU.mult)), r=["BK", "cst"], w=[f"BKm{par}"])
            op("pool", lambda e: e.tensor_tensor(out=f2(RK), in0=f2(Rr), in1=f2(KP), op=ALU.mult),
               r=["Rr", "KP"], w=["RK"])
            op("pool", lambda e: e.tensor_tensor(out=RK2[:], in0=RK[:],
                                                 in1=vcol(V_RK, 4).unsqueeze(2).to_broadcast([128, 4, 128]),
                                                 op=ALU.mult), r=["RK", "vec"], w=["RK2"])
            pbn, pbnk = PS()
            op("pe", lambda e: e.matmul(pbn[:, 0:512], bones[:], f2(RK2), start=True, stop=True),
               r=["bones", "RK2"], w=[pbnk])
            op("dve", lambda e: e.tensor_tensor(out=f2(BV), in0=pbn[:, 0:512], in1=f2(Vr), op=ALU.mult),
               r=[pbnk, "Vr"], w=["BV"])
            op("act", lambda e: e.activation(out=f2(VB), in_=f2(Vr), func=AF.Copy), r=["Vr"], w=["VB"])
            pb1, pb1k = PSB()
            for j in range(2):
                for cc in range(4):
                    op("pe", lambda e, j=j, cc=cc: e.transpose(pb1[:, j * 512 + cc * 128: j * 512 + (cc + 1) * 128],
                                                               BKS[:, cc, j, :], identb[:]),
                       r=["BKS", "identb"], w=[pb1k])
            op("act", lambda e: e.activation(out=BKT[:], in_=pb1[:, :], func=AF.Copy), r=[pb1k], w=["BKT"])
            pb2, pb2k = PSB()
            for cc in range(4):
                op("pe", lambda e, cc=cc: e.transpose(pb2[:, cc * 128:(cc + 1) * 128], VB[:, cc, :], identb[:]),
                   r=["VB", "identb"], w=[pb2k])
            op("act", lambda e: e.activation(out=VT[:], in_=pb2[:, 0:512], func=AF.Copy), r=[pb2k], w=["VT"])
            if CUT <= 6:
                return
            def inv_s0(hh):
                heads = list(range(4 * hh, 4 * hh + 4))
                hs = slice(4 * hh, 4 * hh + 4)
                mk2 = MASK2.unsqueeze(1).to_broadcast([128, 2, 256])
                for (which, dstM) in ((0, MA), (1, MB)):
                    pM_ = [PS(), PS()]
                    for i, hd in enumerate(heads):
                        cc = hd // 2
                        c0 = (i % 2) * 256
                        par = hd % 2
                        op("pe", lambda e: e.matmul(
                            pM_[i // 2][0][:, c0:c0 + 256], BKm[par][:, cc, which, :],
                            AR[:, cc, :, :].rearrange("p a t -> p (a t)"), start=True, stop=True),
                           r=[f"BKm{par}", "AR"], w=[pM_[i // 2][1]])
                    for b2 in range(2):
                        h2 = slice(4 * hh + 2 * b2, 4 * hh + 2 * b2 + 2)
                        op("dve", lambda e: e.tensor_tensor(
                            out=dstM[:, h2, :], in0=pM_[b2][0][:, 0:512].rearrange("p (h c) -> p h c", h=2), in1=mk2,
                            op=ALU.mult), r=[pM_[b2][1], "cst"], w=[("MA" if which == 0 else "MB") + str(hh)])
                pQ0 = PS()
                for i, hd in enumerate(heads):
                    cc = hd // 2
                    par = hd % 2
                    op("pe", lambda e: e.matmul(
                        pQ0[0][:, i * 128:(i + 1) * 128], ARm[par][:, cc, 0, :], BK[:, cc, 0, :], start=True, stop=True),
                       r=["BK", f"ARm{par}"], w=[pQ0[1]])
                op("dve", lambda e: e.tensor_tensor(
                    out=Qm[0][:, hs, :], in0=pQ0[0][:, 0:512].rearrange("p (h c) -> p h c", h=4),
                    in1=LSTR.unsqueeze(1).to_broadcast([128, 4, 128]), op=ALU.mult),
                   r=[pQ0[1], "cst"], w=[f"Q0_{hh}"])

            def inv_l0(hh):
                heads = list(range(4 * hh, 4 * hh + 4))
                hs = slice(4 * hh, 4 * hh + 4)
                pP = PS()
                pQn = PS()
                for i, hd in enumerate(heads):
                    op("pe", lambda e, i=i, hd=hd: e.matmul(pP[0][:, i * 128:(i + 1) * 128], Qm[0][:, hd, :],
                                                            MA[:, hd, 0:128], start=True, stop=True),
                       r=[f"Q0_{hh}", f"MA{hh}"], w=[pP[1]])
                    op("pe", lambda e, i=i, hd=hd: e.matmul(pQn[0][:, i * 128:(i + 1) * 128], MA[:, hd, 0:128],
                                                            Qm[0][:, hd, :], start=True, stop=True),
                       r=[f"Q0_{hh}", f"MA{hh}"], w=[pQn[1]])
                op("act", lambda e, hs=hs: e.activation(out=PX[1][:, hs, 0:128],
                                                        in_=pP[0][:, 0:512].rearrange("p (h c) -> p h c", h=4),
                                                        func=AF.Copy), r=[pP[1]], w=[f"PX1_{hh}"])
                op("act", lambda e, hs=hs: e.activation(out=Qm[1][:, hs, :],
                                                        in_=pQn[0][:, 0:512].rearrange("p (h c) -> p h c", h=4),
                                                        func=AF.Copy), r=[pQn[1]], w=[f"Q1_{hh}"])
                op("pool", lambda e, hs=hs: e.tensor_tensor(out=PX[1][:, hs, 128:256], in0=MA[:, hs, 0:128],
                                                            in1=IDENTF.unsqueeze(1).to_broadcast([128, 4, 128]),
                                                            op=ALU.add),
                   r=[f"MA{hh}", "cst", f"PX1_{hh}"], w=[f"PX1_{hh}"])

            def inv_lv(hh, lv):
                heads = list(range(4 * hh, 4 * hh + 4))
                hs = slice(4 * hh, 4 * hh + 4)
                if True:
                    cur, nxt = lv % 2, 1 - (lv % 2)
                    pA = [PS(), PS()]
                    pQn = PS()
                    for i, hd in enumerate(heads):
                        c0 = (i % 2) * 256
                        op("pe", lambda e, i=i, hd=hd, c0=c0, cur=cur: e.matmul(
                            pA[i // 2][0][:, c0:c0 + 256], Qm[cur][:, hd, :], PX[cur][:, hd, :], start=True, stop=True),
                           r=[f"Q{cur}_{hh}", f"PX{cur}_{hh}"], w=[pA[i // 2][1]])
                        op("pe", lambda e, i=i, hd=hd, cur=cur: e.matmul(
                            pQn[0][:, i * 128:(i + 1) * 128], PX[cur][:, hd, 0:128], Qm[cur][:, hd, :],
                            start=True, stop=True), r=[f"Q{cur}_{hh}", f"PX{cur}_{hh}"], w=[pQn[1]])
                    for b2 in range(2):
                        h2 = slice(4 * hh + 2 * b2, 4 * hh + 2 * b2 + 2)
                        v3 = pA[b2][0][:, 0:512].rearrange("p (h c) -> p h c", h=2)
                        op("act", lambda e, h2=h2, v3=v3, nxt=nxt: e.activation(out=PX[nxt][:, h2, 0:128],
                                                                               in_=v3[:, :, 0:128], func=AF.Copy),
                           r=[pA[b2][1]], w=[f"PX{nxt}_{hh}"])
                        op("dve", lambda e, h2=h2, v3=v3, nxt=nxt, cur=cur: e.tensor_tensor(
                            out=PX[nxt][:, h2, 128:256], in0=v3[:, :, 128:256], in1=PX[cur][:, h2, 128:256],
                            op=ALU.add), r=[pA[b2][1], f"PX{cur}_{hh}", f"PX{nxt}_{hh}"], w=[f"PX{nxt}_{hh}"])
                    op("act", lambda e, hs=hs, nxt=nxt, pQn=pQn: e.activation(
                        out=Qm[nxt][:, hs, :], in_=pQn[0][:, 0:512].rearrange("p (h c) -> p h c", h=4), func=AF.Copy),
                       r=[pQn[1]], w=[f"Q{nxt}_{hh}"])

            def inv_fin(hh):
                heads = list(range(4 * hh, 4 * hh + 4))
                hs = slice(4 * hh, 4 * hh + 4)
                pX = PS()
                for i, hd in enumerate(heads):
                    op("pe", lambda e, i=i, hd=hd: e.matmul(pX[0][:, i * 128:(i + 1) * 128], Qm[0][:, hd, :],
                                                            PX[0][:, hd, 128:256], start=True, stop=True),
                       r=[f"Q0_{hh}", f"PX0_{hh}"], w=[pX[1]])
                op("dve", lambda e, hs=hs, pX=pX: e.tensor_tensor(
                    out=XF[:, hs, :], in0=pX[0][:, 0:512].rearrange("p (h c) -> p h c", h=4),
                    in1=PX[0][:, hs, 128:256], op=ALU.add), r=[pX[1], f"PX0_{hh}"], w=[f"XF{hh}"])

            for hh in range(2):
                inv_s0(hh)
            for hh in range(2):
                inv_l0(hh)
            for lv in range(1, 6):
                for hh in range(2):
                    inv_lv(hh, lv)
            for hh in range(2):
                inv_fin(hh)
            if CUT <= 7:
                return
            pR = PS()
            for hd in range(8):
                cc = hd // 2
                pr = slice((hd % 2) * 64, (hd % 2) * 64 + 64)
                op("pe", lambda e: e.matmul(pR[0][:, hd * 64:(hd + 1) * 64], ARm[hd % 2][:, cc, 0, :],
                                            SBs[:, cc, :], start=True, stop=False),
                   r=[f"ARm{hd % 2}", "SBs"], w=[pR[1]])
                op("pe", lambda e, hd=hd: e.matmul(pR[0][:, hd * 64:(hd + 1) * 64], MB[:, hd, 0:128],
                                                   VT[:, hd * 64:(hd + 1) * 64], start=False, stop=True),
                   r=[f"MB{hd // 4}", "VT"], w=[pR[1]])
            op("act", lambda e: e.activation(out=RH[:], in_=pR[0][:, 0:512], func=AF.Copy), r=[pR[1]], w=["RH"])
            pU = PS()
            for hd in range(8):
                op("pe", lambda e, hd=hd: e.matmul(pU[0][:, hd * 64:(hd + 1) * 64], XF[:, hd, :],
                                                   RH[:, hd * 64:(hd + 1) * 64], start=True, stop=True),
                   r=[f"XF{hd // 4}", "RH"], w=[pU[1]])
            op("act", lambda e: e.activation(out=UT[:], in_=pU[0][:, 0:512], func=AF.Copy), r=[pU[1]], w=["UT"])
            pY = PS()
            pS_ = PS()
            for hd in range(8):
                cc = hd // 2
                pr = slice((hd % 2) * 64, (hd % 2) * 64 + 64)
                oy = pY[0][pr, cc * 128:(cc + 1) * 128]
                op("pe", lambda e: e.matmul(oy, SBs[:, cc, :], ARm[hd % 2][:, cc, 1, :],
                                            start=True, stop=False), r=["SBs", f"ARm{hd % 2}"], w=[pY[1]])
                op("pe", lambda e, oy=oy, hd=hd: e.matmul(oy, UT[:, hd * 64:(hd + 1) * 64], MA[:, hd, 128:256],
                                                          start=False, stop=False),
                   r=["UT", f"MA{hd // 4}"], w=[pY[1]])
                op("pe", lambda e, oy=oy, hd=hd: e.matmul(oy, VT[:, hd * 64:(hd + 1) * 64], MB[:, hd, 128:256],
                                                          start=False, stop=True),
                   r=["VT", f"MB{hd // 4}"], w=[pY[1]])
            for hd in range(8):
                cc = hd // 2
                pr = slice((hd % 2) * 64, (hd % 2) * 64 + 64)
                os_ = pS_[0][pr, cc * 64:(cc + 1) * 64]
                op("pe", lambda e, os_=os_, hd=hd: e.matmul(os_, BKT[:, hd * 64:(hd + 1) * 64],
                                                            UT[:, hd * 64:(hd + 1) * 64], start=True, stop=False),
                   r=["BKT", "UT"], w=[pS_[1]])
                op("pe", lambda e, os_=os_, hd=hd: e.matmul(os_, BKT[:, 512 + hd * 64:512 + (hd + 1) * 64],
                                                            VT[:, hd * 64:(hd + 1) * 64], start=False, stop=True),
                   r=["BKT", "VT"], w=[pS_[1]])
            op("act", lambda e: e.activation(out=f2(Yf), in_=pY[0][:, 0:512], func=AF.Copy), r=[pY[1]], w=["Yf"])
            op("dve", lambda e: e.tensor_tensor(out=TMPS[:], in0=SF[:],
                                                in1=E1[:, :, 127:128].to_broadcast([128, 4, 64]), op=ALU.mult),
               r=["SF", "E1"], w=["TMPS"])
            op("dve", lambda e: e.tensor_tensor(out=SF[:], in0=pS_[0][:, 0:256].rearrange("p (c v) -> p c v", c=4),
                                                in1=TMPS[:], op=ALU.add), r=[pS_[1], "TMPS"], w=["SF"])
            op("act", lambda e: e.activation(out=SBs[:], in_=SF[:], func=AF.Copy), r=["SF"], w=["SBs"])
            if CUT <= 8:
                return
            op("act", lambda e: e.activation(out=f2(YB), in_=pY[0][:, 0:512], func=AF.Copy), r=[pY[1]], w=["YB"])
            op("act", lambda e: e.activation(out=f2(YSQ), in_=pY[0][:, 0:512], func=AF.Square), r=[pY[1]], w=["YSQ"])
            pM = PS()
            pV = PS()
            op("pe", lambda e: e.matmul(pM[0][:, 0:512], bones[:], f2(YB), start=True, stop=True),
               r=["bones", "YB"], w=[pM[1]])
            op("pe", lambda e: e.matmul(pV[0][:, 0:512], bones[:], f2(YSQ), start=True, stop=True),
               r=["bones", "YSQ"], w=[pV[1]])
            op("act", lambda e: e.activation(out=f2(MEAN), in_=pM[0][:, 0:512], func=AF.Copy, scale=1.0 / 64),
               r=[pM[1]], w=["MEAN"])
            op("pool", lambda e: e.tensor_tensor(out=f2(M2), in0=f2(MEAN), in1=f2(MEAN), op=ALU.mult),
               r=["MEAN"], w=["M2"])
            op("dve", lambda e: e.scalar_tensor_tensor(out=f2(VAR), in0=pV[0][:, 0:512], scalar=1.0 / 64, in1=f2(M2),
                                                       op0=ALU.mult, op1=ALU.subtract), r=[pV[1], "M2"], w=["VAR"])
            op("act", lambda e: e.activation(out=f2(VAR), in_=f2(VAR), func=AF.Ln, bias=64e-5), r=["VAR"], w=["VAR"])
            op("act", lambda e: e.activation(out=f2(VAR), in_=f2(VAR), func=AF.Exp, scale=-0.5), r=["VAR"], w=["VAR"])
            op("dve", lambda e: e.tensor_tensor(out=f2(Dd), in0=f2(Yf), in1=f2(MEAN), op=ALU.subtract),
               r=["Yf", "MEAN"], w=["Dd"])
            op("dve", lambda e: e.tensor_tensor(out=f2(Dd), in0=f2(Dd), in1=f2(VAR), op=ALU.mult),
               r=["Dd", "VAR"], w=["Dd"])
            for cc in range(4):
                op("dve", lambda e, cc=cc: e.tensor_scalar(out=Dd[:, cc, :], in0=Dd[:, cc, :], scalar1=vcol(V_LNW + cc),
                                                           scalar2=vcol(V_LNB + cc), op0=ALU.mult, op1=ALU.add),
                   r=["Dd", "vec"], w=["Dd"])
            op("dve", lambda e: e.tensor_tensor(out=f2(Dd), in0=f2(Dd), in1=f2(BV), op=ALU.add),
               r=["Dd", "BV"], w=["Dd"])
            op("dve", lambda e, ys=ys: e.tensor_tensor(out=ys[:, 4:8, :], in0=Dd[:], in1=Gg[:], op=ALU.mult),
               r=["Dd", "Gg"], w=[yk + "b"])
            dma("sp", lambda e, ys=ys, ti=ti: e.dma_start(out=yab_d[ti], in_=ys[:].rearrange("p a b -> p (a b)")),
                r=[yk + "a", yk + "b"], w=[f"yab_d{ti}"], key=yk)

        genA(0)
        for ti in range(NTILE):
            fns = [lambda ti=ti: genBC(ti)]
            q = [cfg.get("qBC", 5)]
            if ti + 1 < NTILE:
                fns.append(lambda ti=ti: genA(ti + 1))
                q.append(1)
            WV.run(fns, q)
        tl.pool = None


    if "1b" in PH:
        phase_reset()
        Wgt = salloc("Wgt", [128, 8, 2048], BF16)
        WbA = salloc("WbA", [128, 4, 1024], BF16)
        WbB = salloc("WbB", [128, 4, 1024], BF16)
        Wout = salloc("Wout", [128, 8, 1024], BF16)
        Wr = salloc("Wr", [128, 8, 36], F32)
        BR = salloc("BR", [128, 36], F32)
        GMOE = salloc("GMOE", [128, D], F32)
        ustrb = salloc("ustrb", [128, 128], BF16)
        CNT = salloc("CNT", [128, 32], F32)
        stgB = [salloc(f"stgB{i}", [128, 2048], F32) for i in range(2)]
        sB = 0
        for kc in range(8):
            k_ = f"stgB{sB % 2}"
            t_ = stgB[sB % 2]
            sB += 1
            dma("sp", lambda e: e.dma_start(out=t_[:, 0:2048], in_=win_d[kc * 128:(kc + 1) * 128, 2560:4608]),
                w=[k_], key=k_)
            op("dve" if kc % 2 else "pool", lambda e: e.tensor_scalar(out=Wgt[:, kc, :], in0=t_[:, 0:2048],
                                                                      scalar1=vcol(V_LNMIX + kc), scalar2=1.0,
                                                                      op0=ALU.mult, op1=ALU.mult),
               r=[k_, "vec"], w=["Wgt"])
        for (dst, dk, src, nk) in ((WbA, "WbA", wba_d, 4), (WbB, "WbB", wbb_d, 4), (Wout, "Wout", wout_d, 8)):
            for kc in range(nk):
                k_ = f"stgB{sB % 2}"
                t_ = stgB[sB % 2]
                sB += 1
                dma("sp", lambda e: e.dma_start(out=t_[:, 0:1024], in_=src[kc * 128:(kc + 1) * 128, :]),
                    w=[k_], key=k_)
                op("dve" if kc % 2 else "pool", lambda e: e.tensor_copy(out=dst[:, kc, :], in_=t_[:, 0:1024]),
                   r=[k_], w=[dk])
        dma("sp", lambda e: e.dma_start(out=Wr[:], in_=wr_d.rearrange("(k p) n -> p k n", p=128)), w=["Wr"], key="Wr")
        for kc in range(8):
            op("dve", lambda e: e.tensor_scalar(out=Wr[:, kc, :], in0=Wr[:, kc, :], scalar1=vcol(V_LNMOE + kc),
                                                scalar2=None, op0=ALU.mult), r=["Wr", "vec"], w=["Wr"])
        dma("sp", lambda e: e.dma_start(out=BR[:], in_=br_d.to_broadcast([128, 36])), w=["BR"], key="BR")
        dma("sp", lambda e: e.dma_start(out=GMOE[:], in_=lnmoe_d.to_broadcast([128, D])), w=["GMOE"], key="GMOE")
        op("dve", lambda e: e.tensor_copy(out=ustrb[:], in_=USTR), r=["cst"], w=["ustrb"])
        op("dve", lambda e: e.memset(CNT[:], 0.0), w=["CNT"])
        ZR = salloc("ZR", [128, D], BF16)
        op("pool", lambda e: e.memset(ZR[:], 0.0), w=["ZR"])
        hs_v = hs_d.rearrange("(r p) d -> p r d", p=128)
        RCH = 8
        for r0 in range(0, NST, RCH):
            rn = min(RCH, NST - r0)
            dma("sp", lambda e: e.dma_start(out=hs_v[:, r0:r0 + rn, :],
                                            in_=ZR[:].unsqueeze(1).to_broadcast([128, rn, D])),
                r=["ZR"], w=["hs_all"], key="ZRst")

        xin = [salloc(f"xinb{i}", [128, D], F32) for i in range(2)]
        tmp = dict(junk=salloc("junkb", [128, D], BF16), ss=salloc("ssb", [128, 4], F32),
                   rstd=salloc("rstdb", [128, 1], F32), xbf=salloc("xbfb", [128, D], BF16))
        hT = [salloc(f"hTb{i}", [128, 8, 128], BF16) for i in range(2)]
        yin = [salloc(f"yin{i}", [128, 8, 128], BF16) for i in range(2)]
        GT = salloc("GT", [128, 2048], F32)
        MAf = salloc("MAf", [128, D], F32)
        MBf = salloc("MBf", [128, D], F32)
        MG = salloc("MG", [128, D], BF16)
        MGT = salloc("MGT", [128, 8, 128], BF16)
        X1 = [salloc(f"X1_{i}", [128, D], F32) for i in range(2)]
        HM = [salloc(f"HM{i}", [128, D], BF16) for i in range(2)]
        X1T = salloc("X1T", [128, 8, 128], F32)
        rs = salloc("rs", [128, 8], F32)
        LG = salloc("LG", [128, 36], F32)
        R_ = salloc("Rsm", [128, 16], F32)
        GOH = salloc("GOH", [128, 4], F32)
        EG = salloc("EG", [128, 4], F32)
        T48 = salloc("T48", [128, 4, 8], F32)
        ESEL = salloc("ESEL", [128, 8], F32)
        ES2 = salloc("ES2", [128, 8], F32)
        OHa = salloc("OHa", [128, 8], F32)
        OHb = salloc("OHb", [128, 8], F32)
        OH1 = salloc("OH1", [128, 4, 8], F32)
        OH2 = salloc("OH2", [128, 4, 8], F32)
        OHSb = salloc("OHSb", [128, 32], BF16)
        POS = salloc("POS", [128, 32], F32)
        PT2 = salloc("PT2", [128, 32], F32)
        SL = salloc("SL", [128, 2], F32)
        EOFF = cst[:, 768:800]

        def g2(t):
            return t[:].rearrange("p a b -> p (a b)")

        for ti in range(NTILE):
            sl = ti % 2
            xk = f"xinb{sl}"
            dma("sp", lambda e: e.dma_start(out=xin[sl][:], in_=x_d[ti * 128:(ti + 1) * 128, :]), w=[xk], key=xk)
            yk = f"yin{sl}"
            dma("sp", lambda e: e.dma_start(out=yin[sl][:].rearrange("p a b -> p (a b)"), in_=yab_d[ti]),
                r=[f"yab_d{ti}"], w=[yk], key=yk)
            hk = f"hTb{sl}"
            rms_to_hT(ti, xin[sl], xk, hT[sl], hk, tmp)
            h = hT[sl]
            for nb in range(4):
                pgt = PS()
                for kc in range(8):
                    op("pe", lambda e: e.matmul(pgt[0][:, 0:512], h[:, kc, :], Wgt[:, kc, nb * 512:(nb + 1) * 512],
                                                start=(kc == 0), stop=(kc == 7)), r=[hk, "Wgt"], w=[pgt[1]])
                op("act", lambda e: e.activation(out=GT[:, nb * 512:(nb + 1) * 512], in_=pgt[0][:, 0:512],
                                                 func=AF.Sigmoid), r=[pgt[1]], w=[f"GT{nb}"])
            for half in range(2):
                pba = PS()
                for c in range(4):
                    op("pe", lambda e: e.matmul(pba[0][:, 0:512], yin[sl][:, c, :],
                                                WbA[:, c, half * 512:(half + 1) * 512], start=(c == 0), stop=(c == 3)),
                       r=[yk, "WbA"], w=[pba[1]])
                op("dve", lambda e: e.tensor_tensor(out=MAf[:, half * 512:(half + 1) * 512], in0=pba[0][:, 0:512],
                                                    in1=GT[:, half * 512:(half + 1) * 512], op=ALU.mult),
                   r=[pba[1], f"GT{half}"], w=[f"MAf{half}"])
                pbb = PS()
                for c in range(4):
                    op("pe", lambda e: e.matmul(pbb[0][:, 0:512], yin[sl][:, 4 + c, :],
                                                WbB[:, c, half * 512:(half + 1) * 512], start=(c == 0), stop=(c == 3)),
                       r=[yk, "WbB"], w=[pbb[1]])
                op("dve", lambda e: e.tensor_tensor(out=MBf[:, half * 512:(half + 1) * 512], in0=pbb[0][:, 0:512],
                                                    in1=GT[:, 1024 + half * 512:1024 + (half + 1) * 512], op=ALU.mult),
                   r=[pbb[1], f"GT{2 + half}"], w=[f"MBf{half}"])
                op("pool", lambda e: e.tensor_tensor(out=MG[:, half * 512:(half + 1) * 512],
                                                     in0=MAf[:, half * 512:(half + 1) * 512],
                                                     in1=MBf[:, half * 512:(half + 1) * 512], op=ALU.add),
                   r=[f"MAf{half}", f"MBf{half}"], w=[f"MG{half}"])
            pb = PSB()
            for kc in range(8):
                op("pe", lambda e: e.transpose(pb[0][:, kc * 128:(kc + 1) * 128], MG[:, kc * 128:(kc + 1) * 128],
                                               identb[:]), r=["MG0", "MG1", "identb"], w=[pb[1]])
            op("act", lambda e: e.activation(out=g2(MGT), in_=pb[0][:, :], func=AF.Copy), r=[pb[1]], w=["MGT"])
            x1 = X1[sl]
            x1k = f"X1_{sl}"
            for half in range(2):
                po = PS()
                for kc in range(8):
                    op("pe", lambda e: e.matmul(po[0][:, 0:512], MGT[:, kc, :], Wout[:, kc, half * 512:(half + 1) * 512],
                                                start=(kc == 0), stop=(kc == 7)), r=["MGT", "Wout"], w=[po[1]])
                op("dve", lambda e: e.tensor_tensor(out=x1[:, half * 512:(half + 1) * 512], in0=po[0][:, 0:512],
                                                    in1=xin[sl][:, half * 512:(half + 1) * 512], op=ALU.add),
                   r=[po[1], xk], w=[x1k + f"h{half}"])
            dma("sp", lambda e: e.dma_start(out=x1_d[ti * 128:(ti + 1) * 128, :], in_=x1[:]),
                r=[x1k + "h0", x1k + "h1"], w=[f"x1_d{ti}"], key=x1k)
            op("act", lambda e: e.activation(out=tmp["junk"][:], in_=x1[:], func=AF.Square, accum_out=rs[:, 0:1]),
               r=[x1k + "h0", x1k + "h1"], w=["junk", "rs0"])
            op("dve", lambda e: e.tensor_scalar(out=rs[:, 1:2], in0=rs[:, 0:1], scalar1=1.0 / D, scalar2=1e-6,
                                                op0=ALU.mult, op1=ALU.add), r=["rs0"], w=["rs1"])
            op("act", lambda e: e.activation(out=rs[:, 2:3], in_=rs[:, 1:2], func=AF.Sqrt), r=["rs1"], w=["rs2"])
            op("dve", lambda e: e.reciprocal(out=rs[:, 3:4], in_=rs[:, 2:3]), r=["rs2"], w=["rs3"])
            hm = HM[sl]
            hmk = f"HM{sl}"
            op("dve", lambda e: e.scalar_tensor_tensor(out=hm[:], in0=x1[:], scalar=rs[:, 3:4], in1=GMOE[:],
                                                       op0=ALU.mult, op1=ALU.mult),
               r=[x1k + "h0", x1k + "h1", "rs3", "GMOE"], w=[hmk])
            for half in range(2):
                ptx = PS()
                for j in range(4):
                    kc = half * 4 + j
                    op("pe", lambda e: e.transpose(ptx[0][:, j * 128:(j + 1) * 128], x1[:, kc * 128:(kc + 1) * 128],
                                                   IDENTF), r=[x1k + "h0", x1k + "h1", "cst"], w=[ptx[1]])
                op("act", lambda e: e.activation(out=X1T[:, half * 4:half * 4 + 4, :].rearrange("p a b -> p (a b)"),
                                                 in_=ptx[0][:, 0:512], func=AF.Copy), r=[ptx[1]], w=[f"X1T{half}"])
            pl = PS()
            for kc in range(8):
                op("pe", lambda e: e.matmul(pl[0][:, 0:36], X1T[:, kc, :], Wr[:, kc, :], start=(kc == 0), stop=(kc == 7)),
                   r=["X1T0", "X1T1", "Wr"], w=[pl[1]])
            op("dve", lambda e: e.scalar_tensor_tensor(out=LG[:], in0=pl[0][:, 0:36], scalar=rs[:, 3:4], in1=BR[:],
                                                       op0=ALU.mult, op1=ALU.add), r=[pl[1], "rs3", "BR"], w=["LG"])
            V = lambda f, r, w: op("dve", f, r=r, w=w)
            V(lambda e: e.tensor_reduce(out=R_[:, 0:1], in_=LG[:, 0:4], axis=AX.X, op=ALU.max), ["LG"], ["R0"])
            V(lambda e: e.tensor_scalar(out=GOH[:], in0=LG[:, 0:4], scalar1=R_[:, 0:1], scalar2=None, op0=ALU.is_equal),
              ["LG", "R0"], ["GOH"])
            V(lambda e: e.tensor_scalar(out=R_[:, 1:2], in0=R_[:, 0:1], scalar1=-1.0, scalar2=None, op0=ALU.mult),
              ["R0"], ["R1"])
            op("act", lambda e: e.activation(out=EG[:], in_=LG[:, 0:4], func=AF.Exp, bias=R_[:, 1:2],
                                             accum_out=R_[:, 2:3]), r=["LG", "R1"], w=["EG", "R2"])
            V(lambda e: e.reciprocal(out=R_[:, 3:4], in_=R_[:, 2:3]), ["R2"], ["R3"])
            V(lambda e: e.tensor_tensor(out=T48[:], in0=LG[:, 4:36].rearrange("p (g x) -> p g x", g=4),
                                        in1=GOH[:].unsqueeze(2).to_broadcast([128, 4, 8]), op=ALU.mult),
              ["LG", "GOH"], ["T48"])
            V(lambda e: e.tensor_reduce(out=ESEL[:], in_=T48[:].rearrange("p g x -> p x g"), axis=AX.X, op=ALU.add),
              ["T48"], ["ESEL"])
            V(lambda e: e.tensor_reduce(out=R_[:, 4:5], in_=ESEL[:], axis=AX.X, op=ALU.max), ["ESEL"], ["R4"])
            V(lambda e: e.tensor_scalar(out=OHa[:], in0=ESEL[:], scalar1=R_[:, 4:5], scalar2=None, op0=ALU.is_equal),
              ["ESEL", "R4"], ["OHa"])
            V(lambda e: e.scalar_tensor_tensor(out=ES2[:], in0=OHa[:], scalar=-1e30, in1=ESEL[:], op0=ALU.mult,
                                               op1=ALU.add), ["OHa", "ESEL"], ["ES2"])
            V(lambda e: e.tensor_reduce(out=R_[:, 5:6], in_=ES2[:], axis=AX.X, op=ALU.max), ["ES2"], ["R5"])
            V(lambda e: e.tensor_scalar(out=OHb[:], in0=ES2[:], scalar1=R_[:, 5:6], scalar2=None, op0=ALU.is_equal),
              ["ES2", "R5"], ["OHb"])
            V(lambda e: e.tensor_tensor(out=R_[:, 6:7], in0=R_[:, 5:6], in1=R_[:, 4:5], op=ALU.subtract),
              ["R4", "R5"], ["R6"])
            op("act", lambda e: e.activation(out=R_[:, 7:8], in_=R_[:, 6:7], func=AF.Exp), r=["R6"], w=["R7"])
            V(lambda e: e.tensor_scalar(out=R_[:, 8:9], in0=R_[:, 7:8], scalar1=1.0, scalar2=None, op0=ALU.add),
              ["R7"], ["R8"])
            V(lambda e: e.reciprocal(out=R_[:, 9:10], in_=R_[:, 8:9]), ["R8"], ["R9"])
            V(lambda e: e.tensor_tensor(out=rw_f[:, 2 * ti:2 * ti + 1], in0=R_[:, 9:10], in1=R_[:, 3:4], op=ALU.mult),
              ["R9", "R3"], [f"rw{ti}a"])
            V(lambda e: e.tensor_tensor(out=rw_f[:, 2 * ti + 1:2 * ti + 2], in0=rw_f[:, 2 * ti:2 * ti + 1],
                                        in1=R_[:, 7:8], op=ALU.mult), [f"rw{ti}a", "R7"], [f"rw{ti}b"])
            gb = GOH[:].unsqueeze(2).to_broadcast([128, 4, 8])
            V(lambda e: e.tensor_tensor(out=OH1[:], in0=gb, in1=OHa[:].unsqueeze(1).to_broadcast([128, 4, 8]),
                                        op=ALU.mult), ["GOH", "OHa"], ["OH1"])
            V(lambda e: e.tensor_tensor(out=OH2[:], in0=gb, in1=OHb[:].unsqueeze(1).to_broadcast([128, 4, 8]),
                                        op=ALU.mult), ["GOH", "OHb"], ["OH2"])
            V(lambda e: e.tensor_tensor(out=OHSb[:], in0=g2(OH1), in1=g2(OH2), op=ALU.add), ["OH1", "OH2"], ["OHSb"])
            pc = PS()
            op("pe", lambda e: e.matmul(pc[0][:, 0:32], ustrb[:], OHSb[:], start=True, stop=True),
               r=["ustrb", "OHSb"], w=[pc[1]])
            op("pe", lambda e: e.matmul(pc[0][:, 32:64], onesb[:], OHSb[:], start=True, stop=True),
               r=["onesb", "OHSb"], w=[pc[1]])
            V(lambda e: e.tensor_tensor(out=POS[:], in0=pc[0][:, 0:32], in1=CNT[:], op=ALU.add), [pc[1], "CNT"], ["POS"])
            V(lambda e: e.tensor_tensor(out=CNT[:], in0=pc[0][:, 32:64], in1=CNT[:], op=ALU.add), [pc[1], "CNT"], ["CNT"])
            V(lambda e: e.tensor_scalar(out=POS[:], in0=POS[:], scalar1=float(CAP - 1), scalar2=None, op0=ALU.min),
              ["POS"], ["POS"])
            V(lambda e: e.tensor_tensor(out=POS[:], in0=POS[:], in1=EOFF, op=ALU.add), ["POS", "cst"], ["POS"])
            for j, OHx in enumerate((OH1, OH2)):
                V(lambda e: e.tensor_tensor(out=PT2[:], in0=POS[:], in1=g2(OHx), op=ALU.mult),
                  ["POS", f"OH{j + 1}"], ["PT2"])
                V(lambda e: e.tensor_reduce(out=SL[:, j:j + 1], in_=PT2[:], axis=AX.X, op=ALU.add), ["PT2"], [f"SL{j}"])
            V(lambda e: e.tensor_copy(out=slot_i[:, 2 * ti:2 * ti + 2], in_=SL[:]), ["SL0", "SL1"], [f"slot{ti}"])
            for j in range(2):
                dma("pool", lambda e: e.indirect_dma_start(
                    out=hs_d[:, :], out_offset=bass.IndirectOffsetOnAxis(ap=slot_i[:, 2 * ti + j:2 * ti + j + 1], axis=0),
                    in_=hm[:, :], in_offset=None), r=[hmk, f"slot{ti}", "hs_all"], w=[f"hs_sc{ti}_{j}"], key=f"sc{sl}{j}")
        if debug:
            RT = salloc("RT", [128, NTILE * 4], F32)
            op("dve", lambda e: e.tensor_copy(out=RT[:, 0:2 * NTILE], in_=slot_i[:]),
               r=[f"slot{t}" for t in range(NTILE)], w=["RT"])
            op("dve", lambda e: e.tensor_copy(out=RT[:, 2 * NTILE:4 * NTILE], in_=rw_f[:]),
               r=[f"rw{t}a" for t in range(NTILE)] + [f"rw{t}b" for t in range(NTILE)] + ["RT"], w=["RT"])
            dma("sp", lambda e: e.dma_start(out=rt_d, in_=RT[:]), r=["RT"], w=["rt_d"], key="RT")

    if "2" in PH:
        phase_reset()
        WG = [salloc(f"WG{i}", [128, 8, DE], BF16) for i in range(2)]
        WU = [salloc(f"WU{i}", [128, 8, DE], BF16) for i in range(2)]
        WD = [salloc(f"WD{i}", [128, 4, D], BF16) for i in range(2)]
        NB = 4
        XGb = [salloc(f"XGb{i}", [128, NB, D], BF16) for i in range(2)]
        XGT = salloc("XGT", [128, 8, NB * 128], BF16)
        SGf = [salloc(f"SGf{i}", [128, NB * 128], F32) for i in range(2)]
        HID = salloc("HID", [128, 4, NB * 128], BF16)
        YOb = [salloc(f"YOb{i}", [128, NB, D], F32) for i in range(2)]
        all_sc = [f"hs_sc{t}_{j}" for t in range(NTILE) for j in range(2)] + ["hs_all"]
        if CT <= 4:
            batches = [(0, CT)]
        elif CT == 5:
            batches = [(0, 3), (3, 2)]
        else:
            batches = [(r0, min(4, CT - r0)) for r0 in range(0, CT, 4)]
        it = 0
        for ex in range(NE):
            b = ex % 2
            dma("pool", lambda e: e.dma_start(out=WG[b][:], in_=wg_d[ex].rearrange("(p k) n -> p k n", k=8),
                                              max_dma_last_dim=8192), w=[f"WG{b}"], key=f"WG{b}")
            dma("pool", lambda e: e.dma_start(out=WU[b][:], in_=wu_d[ex].rearrange("(p k) n -> p k n", k=8),
                                              max_dma_last_dim=8192), w=[f"WU{b}"], key=f"WU{b}")
            dma("pool", lambda e: e.dma_start(out=WD[b][:], in_=wd_d[ex].rearrange("(k p) n -> p k n", p=128)),
                w=[f"WD{b}"], key=f"WD{b}")
            for (r0, nt) in batches:
                row0 = ex * CAP + r0 * 128
                xb = it % 2
                it += 1
                xgk = f"XGb{xb}"
                W_ = nt * 128
                dma("sp", lambda e: e.dma_start(
                    out=XGb[xb][:, 0:nt, :], in_=hs_d[row0:row0 + W_, :].rearrange("(t p) d -> p t d", p=128)),
                    r=(all_sc if "1b" in PH else []), w=[xgk], key=xgk)
                for t in range(nt):
                    pb = PSB()
                    xv = XGb[xb][:, t, :].rearrange("p (m j) -> p j m", j=8)
                    for j in range(8):
                        op("pe", lambda e: e.transpose(pb[0][:, j * 128:(j + 1) * 128], xv[:, j, :], identb[:]),
                           r=[xgk, "identb"], w=[pb[1]])
                    op("act", lambda e: e.activation(out=XGT[:, :, t * 128:(t + 1) * 128],
                                                     in_=pb[0][:, :].rearrange("p (j m) -> p j m", j=8), func=AF.Copy),
                       r=[pb[1]], w=[f"XGT{t}"])
                xgt_keys = [f"XGT{t}" for t in range(nt)]
                for hc in range(4):
                    pG = PS()
                    pU_ = PS()
                    for j in range(8):
                        op("pe", lambda e: e.matmul(pG[0][:, 0:W_], WG[b][:, j, hc * 128:(hc + 1) * 128],
                                                    XGT[:, j, 0:W_], start=(j == 0), stop=(j == 7)),
                           r=[f"WG{b}"] + xgt_keys, w=[pG[1]])
                    for j in range(8):
                        op("pe", lambda e: e.matmul(pU_[0][:, 0:W_], WU[b][:, j, hc * 128:(hc + 1) * 128],
                                                    XGT[:, j, 0:W_], start=(j == 0), stop=(j == 7)),
                           r=[f"WU{b}"] + xgt_keys, w=[pU_[1]])
                    sg = SGf[hc % 2]
                    sgk = f"SGf{hc % 2}"
                    op("act", lambda e: e.activation(out=sg[:, 0:W_], in_=pG[0][:, 0:W_], func=AF.Silu),
                       r=[pG[1]], w=[sgk])
                    op("dve", lambda e: e.tensor_tensor(out=HID[:, hc, 0:W_], in0=pU_[0][:, 0:W_], in1=sg[:, 0:W_],
                                                        op=ALU.mult), r=[pU_[1], sgk], w=[f"HID{hc}"])
                yok = f"YOb{xb}"
                for t in range(nt):
                    for half in range(2):
                        py = PS()
                        for hc in range(4):
                            op("pe", lambda e: e.matmul(py[0][:, 0:512], HID[:, hc, t * 128:(t + 1) * 128],
                                                        WD[b][:, hc, half * 512:(half + 1) * 512], start=(hc == 0),
                                                        stop=(hc == 3)), r=[f"HID{hc_}" for hc_ in range(4)] + [f"WD{b}"],
                               w=[py[1]])
                        if half:
                            op("act", lambda e: e.activation(out=YOb[xb][:, t, 512:1024], in_=py[0][:, 0:512],
                                                             func=AF.Copy), r=[py[1]], w=[yok + f"_{t}_1"])
                        else:
                            op("dve", lambda e: e.tensor_copy(out=YOb[xb][:, t, 0:512], in_=py[0][:, 0:512]),
                               r=[py[1]], w=[yok + f"_{t}_0"])
                dma("sp", lambda e: e.dma_start(
                    out=ys_d[row0:row0 + W_, :].rearrange("(t p) d -> p t d", p=128), in_=YOb[xb][:, 0:nt, :]),
                    r=[yok + f"_{t}_{hf}" for t in range(nt) for hf in range(2)], w=["ys_all"], key=yok)

    if "3" in PH:
        phase_reset()
        Wpg = salloc("Wpg", [128, 8, D], BF16)
        Wpp = salloc("Wpp", [128, 2, D], BF16)
        LNF = salloc("LNF", [128, D], F32)
        stgC = [salloc(f"stgC{i}", [128, D], F32) for i in range(2)]
        for kc in range(8):
            k_ = f"stgC{kc % 2}"
            t_ = stgC[kc % 2]
            dma("sp", lambda e: e.dma_start(out=t_[:], in_=wpg_d[kc * 128:(kc + 1) * 128, :]), w=[k_], key=k_)
            op("dve" if kc % 2 else "pool", lambda e: e.tensor_scalar(out=Wpg[:, kc, :], in0=t_[:],
                                                                      scalar1=vcol(V_LNPLE + kc), scalar2=1.0,
                                                                      op0=ALU.mult, op1=ALU.mult),
               r=[k_, "vec"], w=["Wpg"])
        for kc in range(2):
            k_ = f"stgC{kc % 2}"
            t_ = stgC[kc % 2]
            dma("sp", lambda e: e.dma_start(out=t_[:], in_=wpp_d[kc * 128:(kc + 1) * 128, :]), w=[k_], key=k_)
            op("dve", lambda e: e.tensor_copy(out=Wpp[:, kc, :], in_=t_[:]), r=[k_], w=["Wpp"])
        dma("sp", lambda e: e.dma_start(out=LNF[:], in_=lnf_d.to_broadcast([128, D])), w=["LNF"], key="LNF")
        X1c = [salloc(f"X1c{i}", [128, D], F32) for i in range(2)]
        Y1 = [salloc(f"Y1_{i}", [128, D], F32) for i in range(2)]
        Y2 = [salloc(f"Y2_{i}", [128, D], F32) for i in range(2)]
        Pin = [salloc(f"Pin{i}", [128, 256], F32) for i in range(2)]
        Pb2 = [salloc(f"Pb{i}", [128, 256], BF16) for i in range(2)]
        PTt2 = [salloc(f"PTt{i}", [128, 2, 128], BF16) for i in range(2)]
        X22 = [salloc(f"X2{i}", [128, D], F32) for i in range(2)]
        tmp2 = [dict(junk=salloc(f"junkc{i}", [128, D], BF16), ss=salloc(f"ssc{i}", [128, 4], F32),
                     rstd=salloc(f"rstdc{i}", [128, 1], F32), xbf=salloc(f"xbfc{i}", [128, D], BF16))
                for i in range(2)]
        hTc2 = [salloc(f"hTc{i}", [128, 8, 128], BF16) for i in range(2)]
        GP2 = [salloc(f"GP{i}", [128, D], F32) for i in range(2)]
        X32 = [salloc(f"X3{i}", [128, D], F32) for i in range(2)]
        rf2 = [salloc(f"rf{i}", [128, 4], F32) for i in range(2)]
        OUTt = [salloc(f"OUT{i}", [128, D], F32) for i in range(2)]

        def body3(ti):
            sl = ti % 2
            tl.pool = "E" if sl == 0 else "O"
            op, dma = stream(f"@{sl}")
            Pb, PTt, X2, tmp, hTc, GP, X3, rf = Pb2[sl], PTt2[sl], X22[sl], tmp2[sl], hTc2[sl], GP2[sl], X32[sl], rf2[sl]
            x1k, y1k, y2k, pk = f"X1c{sl}", f"Y1_{sl}", f"Y2_{sl}", f"Pin{sl}"
            dma("sp", lambda e: e.dma_start(out=X1c[sl][:], in_=x1_d[ti * 128:(ti + 1) * 128, :]),
                r=([f"x1_d{ti}"] if "1b" in PH else []), w=[x1k], key=x1k)
            dma("sp", lambda e: e.dma_start(out=Pin[sl][:], in_=p_d[ti * 128:(ti + 1) * 128, :]), w=[pk], key=pk)
            for (Yt, ykk, j) in ((Y1[sl], y1k, 0), (Y2[sl], y2k, 1)):
                dma("pool", lambda e: e.indirect_dma_start(
                    out=Yt[:, :], out_offset=None, in_=ys_d[:, :],
                    in_offset=bass.IndirectOffsetOnAxis(ap=slot_i[:, 2 * ti + j:2 * ti + j + 1], axis=0)),
                    r=(["ys_all", f"slot{ti}"] if "2" in PH else []), w=[ykk], key=ykk)
            op("dve", lambda e: e.scalar_tensor_tensor(out=X2[:], in0=Y1[sl][:], scalar=rw_f[:, 2 * ti:2 * ti + 1],
                                                       in1=X1c[sl][:], op0=ALU.mult, op1=ALU.add),
               r=[y1k, x1k, f"rw{ti}a"], w=["X2"])
            op("dve", lambda e: e.scalar_tensor_tensor(out=X2[:], in0=Y2[sl][:], scalar=rw_f[:, 2 * ti + 1:2 * ti + 2],
                                                       in1=X2[:], op0=ALU.mult, op1=ALU.add),
               r=[y2k, "X2", f"rw{ti}b"], w=["X2"])
            rms_to_hT(ti, X2, "X2", hTc, "hTc", tmp, normalize=False, op=op)
            op("pool", lambda e: e.tensor_copy(out=Pb[:], in_=Pin[sl][:]), r=[pk], w=["Pb"])
            pb = PSB()
            for kc in range(2):
                op("pe", lambda e: e.transpose(pb[0][:, kc * 128:(kc + 1) * 128], Pb[:, kc * 128:(kc + 1) * 128],
                                               identb[:]), r=["Pb", "identb"], w=[pb[1]])
            op("act", lambda e: e.activation(out=PTt[:].rearrange("p a b -> p (a b)"), in_=pb[0][:, 0:256],
                                             func=AF.Copy), r=[pb[1]], w=["PTt"])
            for half in range(2):
                pgm = PS()
                for kc in range(8):
                    op("pe", lambda e: e.matmul(pgm[0][:, 0:512], hTc[:, kc, :], Wpg[:, kc, half * 512:(half + 1) * 512],
                                                start=(kc == 0), stop=(kc == 7)), r=["hTc", "Wpg"], w=[pgm[1]])
                op("act", lambda e: e.activation(out=GP[:, half * 512:(half + 1) * 512], in_=pgm[0][:, 0:512],
                                                 func=AF.Sigmoid, scale=tmp["rstd"][:, 0:1]),
                   r=[pgm[1], "rstd"], w=[f"GP{half}"])
                ppm = PS()
                for kc in range(2):
                    op("pe", lambda e: e.matmul(ppm[0][:, 0:512], PTt[:, kc, :], Wpp[:, kc, half * 512:(half + 1) * 512],
                                                start=(kc == 0), stop=(kc == 1)), r=["PTt", "Wpp"], w=[ppm[1]])
                op("dve", lambda e: e.tensor_tensor(out=GP[:, half * 512:(half + 1) * 512], in0=ppm[0][:, 0:512],
                                                    in1=GP[:, half * 512:(half + 1) * 512], op=ALU.mult),
                   r=[ppm[1], f"GP{half}"], w=[f"GP{half}"])
                op("pool", lambda e: e.tensor_tensor(out=X3[:, half * 512:(half + 1) * 512],
                                                     in0=X2[:, half * 512:(half + 1) * 512],
                                                     in1=GP[:, half * 512:(half + 1) * 512], op=ALU.add),
                   r=["X2", f"GP{half}"], w=[f"X3{half}"])
            op("act", lambda e: e.activation(out=tmp["junk"][:], in_=X3[:], func=AF.Square, accum_out=rf[:, 0:1]),
               r=["X30", "X31"], w=["junk", "rf0"])
            op("dve", lambda e: e.tensor_scalar(out=rf[:, 1:2], in0=rf[:, 0:1], scalar1=1.0 / D, scalar2=1e-6,
                                                op0=ALU.mult, op1=ALU.add), r=["rf0"], w=["rf1"])
            op("act", lambda e: e.activation(out=rf[:, 2:3], in_=rf[:, 1:2], func=AF.Sqrt), r=["rf1"], w=["rf2"])
            op("dve", lambda e: e.reciprocal(out=rf[:, 3:4], in_=rf[:, 2:3]), r=["rf2"], w=["rf3"])
            ok = f"OUT{sl}"
            op("dve", lambda e: e.scalar_tensor_tensor(out=OUTt[sl][:], in0=X3[:], scalar=rf[:, 3:4], in1=LNF[:],
                                                       op0=ALU.mult, op1=ALU.mult), r=["X30", "X31", "rf3", "LNF"],
               w=[ok])
            dma("sp", lambda e: e.dma_start(out=out_d[ti * 128:(ti + 1) * 128, :], in_=OUTt[sl][:]),
                r=[ok], w=[f"out_d{ti}"], key=ok)

        for t0 in range(0, NTILE, 2):
            WV.run([lambda t0=t0: body3(t0), lambda t0=t0: body3(t0 + 1)], [1, 1], seq=not cfg.get("weave23", False))
        tl.pool = None

    S_.op("sp", nop_fns["sp"], r=[], w=[])
    return nc, S_


def emit(nc, S_):
    pref = S_.finish(nc, None, None, None)
    import contextlib
    with contextlib.ExitStack() as es:
        sems = {e: es.enter_context(nc.semaphore("s_" + e)) for e in ENGS}
        dsem = {k: es.enter_context(nc.semaphore("d_" + str(i))) for i, k in enumerate(S_.dma_cnt)}
        bsem = [es.enter_context(nc.semaphore(f"bar{i}")) for i in range(2)]
        block = es.enter_context(nc.Block())

        def run(ename, eh):
            for o in S_.ops[ename]:
                for (key, val) in o["waits"]:
                    if key[0] == "eng":
                        eh.wait_ge(sems[key[1]], pref[key[1]][val])
                    else:
                        eh.wait_ge(dsem[key[1]], val)
                ins = o["fn"](eh)
                if o.get("bar"):
                    nb = o["bar"]
                    ins.then_inc(bsem[nb % 2], 1)
                    eh.wait_ge(bsem[nb % 2], len(ENGS) * ((nb + 1) // 2 if nb % 2 else nb // 2))
                    continue
                if o["dma"] is not None:
                    ins.then_inc(dsem[o["dma"]], 16)
                elif o["inc"]:
                    ins.then_inc(sems[ename], 1)
            if ename == "sp":
                for k, c in S_.dma_cnt.items():
                    eh.wait_ge(dsem[k], c)

        @block.tensor
        def _(t):
            run("pe", t)

        @block.scalar
        def _(a):
            run("act", a)

        @block.vector
        def _(v):
            run("dve", v)

        @block.gpsimd
        def _(g):
            run("pool", g)

        @block.sync
        def _(s):
            run("sp", s)
    return nc


def _perm_q():
    idx = []
    for c in range(4):
        idx += list(range(c * 64, c * 64 + 64)) + list(range((c + 4) * 64, (c + 4) * 64 + 64))
    return np.array(idx)


def _consts(cap):
    c = np.zeros((128, 832), np.float32)
    c[:, 768:800] = (np.arange(32) * cap)[None, :]
    i = np.arange(128)
    c[:, 0:128] = np.eye(128)
    c[:, 128:256] = (i[:, None] < i[None, :])
    c[:, 256:384] = (i[:, None] <= i[None, :])
    c[:, 384:512] = (i[:, None] > i[None, :])
    invf = (10000.0 ** (-np.arange(32, dtype=np.float64) / 32.0)) / (2 * np.pi)
    c[:, 512:544] = invf[None, :]
    c[:, 544:576] = invf[None, :]
    c[:, 576:608] = 0.0
    c[:, 608:640] = 0.25
    c[:, 640:768] = ((i[:, None] // 64) == (i[None, :] // 64))
    return c


def prep_shared(inp, cap):
    f = lambda a: np.ascontiguousarray(np.asarray(a, dtype=np.float32))
    pq = _perm_q()
    w_in = f(inp["w_in"][0])
    cols = np.concatenate([pq, np.arange(512, 4608)])
    w_in = np.ascontiguousarray(w_in[:, cols])
    vec = np.zeros((128, 70), np.float32)
    vec[:, 0:8] = f(inp["ln_mix"][0]).reshape(8, 128).T
    vec[:, 8:16] = f(inp["ln_moe"][0]).reshape(8, 128).T
    vec[:, 16:24] = f(inp["ln_ple"][0]).reshape(8, 128).T
    vec[:, 24:38] = f(inp["mu_shift"][0]).reshape(14, 128).T
    for j, nm in enumerate(["w0", "a0", "k_k", "k_a", "r_k", "ln_x_w", "ln_x_b"]):
        vec[:, 38 + 4 * j:42 + 4 * j] = f(inp[nm][0]).reshape(4, 128).T
    sk = f(inp["sinks"][0])
    for c in range(4):
        vec[0:64, 66 + c] = sk[c]
        vec[64:128, 66 + c] = sk[c + 4]
    sh = dict(
        w_in=w_in, vecs=vec, cst=_consts(cap), ln_moe_row=f(inp['ln_moe'][0])[None, :],
        wlora=np.ascontiguousarray(np.concatenate([f(inp["w_decay_up"][0]), f(inp["w_aaa_up"][0])], 0)),
        wgu=f(inp["w_gate_up"][0]),
        w_ba=np.ascontiguousarray(f(inp["w_branch_att"][0])[pq, :]),
        w_bb=f(inp["w_branch_rwkv"][0]),
        w_out=f(inp["w_out"][0]),
        w_r=np.ascontiguousarray(np.concatenate([f(inp["w_group"][0]), f(inp["w_expert"][0])], 1)),
        b_r=np.ascontiguousarray(np.concatenate([f(inp["b_group"][0]), f(inp["b_expert"][0])])[None, :]),
        w_gate_e=f(inp["w_gate_e"][0]), w_up_e=f(inp["w_up_e"][0]), w_down_e=f(inp["w_down_e"][0]),
        w_pg=f(inp["w_ple_gate"][0]), w_pp=f(inp["w_ple_proj"][0]),
        ln_final=f(inp["ln_final"])[None, :],
    )
    return sh


def prep_core(inp, sh, b0, nseq):
    x = np.asarray(inp["x"], np.float32)[b0:b0 + nseq]
    S = x.shape[1]
    m = dict(sh)
    m["x"] = np.ascontiguousarray(x.reshape(nseq * S, D))
    m["p"] = np.ascontiguousarray(np.asarray(inp["p"], np.float32)[0, b0:b0 + nseq].reshape(nseq * S, 256))
    pos = np.asarray(inp["positions"], np.int32)[b0:b0 + nseq].reshape(-1)
    m["posT"] = np.ascontiguousarray(pos.reshape(-1, 128).T)
    return m


FULL_CFG = dict(NSEQ=4, S=2048, CAP=640)


def kernel(**inputs):
    cfg = FULL_CFG
    nc, S_ = build(cfg)
    emit(nc, S_)
    sh = prep_shared(inputs, cfg["CAP"])
    in_maps = [prep_core(inputs, sh, c * cfg["NSEQ"], cfg["NSEQ"]) for c in range(8)]
    res = run_bass_kernel_spmd(nc, in_maps, core_ids=list(range(8)))
    outs = [r["out"].reshape(cfg["NSEQ"], cfg["S"], D) for r in res.results]
    return np.concatenate(outs, 0).astype(np.float32)
```

```python
import numpy as np
import ml_dtypes
import concourse.bass as bass
import concourse.mybir as mybir
from concourse.bass_utils import run_bass_kernel_spmd

F32 = mybir.dt.float32
BF16 = mybir.dt.bfloat16
I32 = mybir.dt.int32
AF = mybir.ActivationFunctionType
ALU = mybir.AluOpType
AX = mybir.AxisListType

D = 1024
NE = 32
DE = 512
ENGS = ("pe", "act", "dve", "pool", "sp")
SYNC_SAME = ("act", "dve", "pool")


class _Rec:
    def __init__(self):
        self.call = None

    def __getattr__(self, name):
        def f(*a, **k):
            self.call = (name, a, k)
            return self
        return f


def _bind(fn):
    r = _Rec()
    fn(r)
    name, a, k = r.call
    return lambda e: getattr(e, name)(*a, **k)


import threading


class Weaver:
    def __init__(self):
        self.active = False

    def tick(self):
        if not self.active or threading.current_thread() is not self.cur_thread():
            return
        i = self.cur
        self.count[i] += 1
        if self.count[i] >= self.quota[i]:
            self.count[i] = 0
            self._handoff(i)

    def cur_thread(self):
        return self.threads[self.cur]

    def _next_live(self, i):
        n = len(self.threads)
        for d in range(1, n + 1):
            j = (i + d) % n
            if not self.done[j]:
                return j
        return None

    def _handoff(self, i):
        j = self._next_live(i)
        if j is None or j == i:
            return
        self.cur = j
        self.sems[j].release()
        self.sems[i].acquire()

    def run(self, fns, quota, seq=False):
        n = len(fns)
        if n == 1 or seq:
            for f in fns:
                f()
            return
        self.sems = [threading.Semaphore(0) for _ in range(n)]
        self.done = [False] * n
        self.count = [0] * n
        self.quota = list(quota)
        self.err = None
        fin = threading.Semaphore(0)

        def wrap(i):
            self.sems[i].acquire()
            try:
                fns[i]()
            except BaseException as ex:
                self.err = ex
            self.done[i] = True
            j = self._next_live(i)
            if j is None:
                fin.release()
            else:
                self.cur = j
                self.sems[j].release()

        self.threads = [threading.Thread(target=wrap, args=(i,)) for i in range(n)]
        for t in self.threads:
            t.start()
        self.active = True
        self.cur = 0
        self.sems[0].release()
        fin.acquire()
        self.active = False
        for t in self.threads:
            t.join()
        if self.err is not None:
            raise self.err


class Sched:
    def __init__(self):
        self.ops = {e: [] for e in ENGS}
        self.last_w = {}
        self.readers = {}
        self.waited = {e: {} for e in ENGS}
        self.dma_cnt = {}

    def _deps(self, eng, reads, writes):
        deps = []
        for r in reads:
            if r in self.last_w:
                deps.append((self.last_w[r], True))
        for w in writes:
            if w in self.last_w:
                deps.append((self.last_w[w], False))
            for t in self.readers.get(w, {}).values():
                deps.append((t, False))
        waits = []
        for d, raw in deps:
            if d[0] == "eng":
                if d[1] == eng and eng not in SYNC_SAME:
                    continue
                key = ("eng", d[1])
            else:
                key = ("dma", d[1])
            val = d[2]
            if self.waited[eng].get(key, -1) >= val:
                continue
            self.waited[eng][key] = val
            waits.append((key, val))
            if d[0] == "eng":
                self.ops[d[1]][val]["inc"] = True
        return waits

    def op(self, eng, fn, r=(), w=()):
        waits = self._deps(eng, r, w)
        idx = len(self.ops[eng])
        self.ops[eng].append(dict(fn=_bind(fn), waits=waits, inc=False, dma=None))
        tok = ("eng", eng, idx)
        for x in w:
            self.last_w[x] = tok
            self.readers[x] = {}
        for x in r:
            self.readers.setdefault(x, {})[("eng", eng)] = tok

    def dma(self, eng, fn, r=(), w=(), key=None):
        waits = self._deps(eng, r, w)
        prev = self.dma_cnt.get(key, 0)
        if prev and self.waited[eng].get(("dma", key), -1) < prev:
            self.waited[eng][("dma", key)] = prev
            waits.append((("dma", key), prev))
        cnt = prev + 16
        self.dma_cnt[key] = cnt
        self.ops[eng].append(dict(fn=_bind(fn), waits=waits, inc=False, dma=key))
        tok = ("dma", key, cnt)
        for x in w:
            self.last_w[x] = tok
            self.readers[x] = {}
        for x in r:
            self.readers.setdefault(x, {})[("dma", key)] = tok

    def barrier(self, nop_fns):
        self.nbar = getattr(self, "nbar", 0) + 1
        for e in ENGS:
            waits = []
            if e != "sp" and self.ops[e]:
                last = len(self.ops[e]) - 1
                while last >= 0 and (self.ops[e][last].get("bar") or self.ops[e][last]["dma"] is not None):
                    last -= 1
                if last >= 0:
                    self.ops[e][last]["inc"] = True
                    waits.append((("eng", e), last))
            if e == "sp":
                for k, c in self.dma_cnt.items():
                    if self.waited[e].get(("dma", k), -1) < c:
                        self.waited[e][("dma", k)] = c
                        waits.append((("dma", k), c))
            self.ops[e].append(dict(fn=nop_fns[e], waits=waits, inc=False, dma=None, bar=self.nbar))
        self.last_w = {}
        self.readers = {}

    def finish(self, nc, engines, sems, dma_sems):
        pref = {}
        for e in ENGS:
            c = 0
            arr = []
            for o in self.ops[e]:
                if o["inc"] and o["dma"] is None and not o.get("bar"):
                    c += 1
                arr.append(c)
            pref[e] = arr
        return pref


def build(cfg, debug=False):
    NSEQ, S, CAP = cfg["NSEQ"], cfg["S"], cfg["CAP"]
    TPS = S // 128
    NTILE = NSEQ * TPS
    TPC = NTILE * 128
    NSLOT = NE * CAP
    NST = NSLOT // 128
    CT = CAP // 128
    PH = cfg.get("phases", "1a,1b,2,3").split(",")
    CUT = cfg.get("cut", 99)

    nc = bass.Bass("TRN2", target_bir_lowering=False)
    dr = {}

    def din(name, shape, dt=F32):
        dr[name] = nc.dram_tensor(name, list(shape), dt, kind="ExternalInput").ap()
        return dr[name]

    def dscr(name, shape, dt=F32, out=False):
        dr[name] = nc.dram_tensor(name, list(shape), dt, kind=("ExternalOutput" if out else "Internal")).ap()
        return dr[name]

    x_d = din("x", [TPC, D])
    p_d = din("p", [TPC, 256])
    pos_d = din("posT", [128, NTILE], I32)
    win_d = din("w_in", [D, 4608])
    vec_d = din("vecs", [128, 70])
    cst_d = din("cst", [128, 832])
    wlora_d = din("wlora", [128, 512])
    wgu_d = din("wgu", [128, 512])
    wba_d = din("w_ba", [512, D])
    wbb_d = din("w_bb", [512, D])
    wout_d = din("w_out", [D, D])
    wr_d = din("w_r", [D, 36])
    br_d = din("b_r", [1, 36])
    wg_d = din("w_gate_e", [NE, D, DE])
    wu_d = din("w_up_e", [NE, D, DE])
    wd_d = din("w_down_e", [NE, DE, D])
    wpg_d = din("w_pg", [D, D])
    wpp_d = din("w_pp", [256, D])
    lnf_d = din("ln_final", [1, D])
    lnmoe_d = din("ln_moe_row", [1, D])
    out_d = dscr("out", [TPC, D], F32, out=True)
    yab_d = dscr("yab", [NTILE, 128, 8 * 128], BF16, out=debug)
    x1_d = dscr("x1s", [TPC, D], F32, out=debug)
    hs_d = dscr("hslots", [NSLOT, D], BF16, out=debug)
    ys_d = dscr("yslots", [NSLOT, D], F32, out=debug)
    if debug:
        rt_d = dscr("route", [128, NTILE * 4], F32, out=True)

    S_ = Sched()
    base0 = 229376 - int(nc.sbuf_bytes_remaining)
    base0 = (base0 + 63) // 64 * 64
    st = {"p": base0, "ph": None}

    def salloc(name, shape, dt):
        nb = int(np.prod(shape[1:])) * (4 if dt in (F32, I32) else 2)
        nb = (nb + 31) // 32 * 32
        t = nc.alloc_sbuf_tensor_at(name, list(shape), dt, offset=st["p"])
        st["p"] += nb
        assert st["p"] <= 229376 - 64, ("SBUF overflow", name, st["p"])
        return t

    psb = [nc.alloc_psum_tensor(f"psb{i}", [128, 1024], BF16) for i in range(2)]
    psf = [nc.alloc_psum_tensor(f"psf{i}", [128, 512], F32) for i in range(6)]
    rr = {"f": 0, "b": 0, "fA": 0, "fB": 0}
    tl = threading.local()

    def PS():
        pool = getattr(tl, "pool", None)
        if pool == "A":
            i = rr["fA"] % 2
            rr["fA"] += 1
        elif pool == "B":
            i = 2 + rr["fB"] % 4
            rr["fB"] += 1
        elif pool == "E":
            i = rr["fA"] % 3
            rr["fA"] += 1
        elif pool == "O":
            i = 3 + rr["fB"] % 3
            rr["fB"] += 1
        else:
            i = rr["f"] % 6
            rr["f"] += 1
        return psf[i], f"psf{i}"

    def PSB():
        pool = getattr(tl, "pool", None)
        if pool in ("A", "E"):
            i = 0
        elif pool in ("B", "O"):
            i = 1
        else:
            i = rr["b"] % 2
            rr["b"] += 1
        return psb[i], f"psb{i}"

    vec = salloc("vec", [128, 70], F32)
    cst = salloc("cstf", [128, 832], F32)
    identb = salloc("identb", [128, 128], BF16)
    bones = salloc("bones", [128, 128], BF16)
    onesb = salloc("onesb", [128, 128], BF16)
    onesf = salloc("onesf", [128, 128], F32)
    derived = salloc("derived", [128, 32], F32)
    posi = salloc("posi", [128, NTILE], I32)
    posf = salloc("posf", [128, NTILE], F32)
    slot_i = salloc("slot_i", [128, NTILE * 2], I32)
    rw_f = salloc("rw_f", [128, NTILE * 2], F32)
    scr = salloc("scr", [128, 64], F32)
    ph_base = st["p"]
    IDENTF = cst[:, 0:128]
    USTR = cst[:, 128:256]
    UINC = cst[:, 256:384]
    LSTR = cst[:, 384:512]
    INVF = cst[:, 512:576]
    OFFS = cst[:, 576:640]
    MASK2 = cst[:, 128:384]
    V_LNMIX, V_LNMOE, V_LNPLE, V_MU = 0, 8, 16, 24
    V_W0, V_A0, V_KK, V_KA, V_RK, V_LNW, V_LNB, V_SINK = 38, 42, 46, 50, 54, 58, 62, 66

    def bc(ap, shape):
        return ap.to_broadcast(list(shape))

    def vcol(c, n=1):
        return vec[:, c:c + n]

    WV = Weaver()
    GLOBAL_KEYS = {"vec", "cst", "identb", "bones", "onesb", "onesf", "derived", "posf", "posi",
                   "Wgt", "WbA", "WbB", "Wout", "Wr", "BR", "GMOE", "ustrb", "CNT", "hs_all", "ys_all",
                   "Wpg", "Wpp", "LNF", "WG0", "WG1", "WU0", "WU1", "WD0", "WD1"}
    GLOBAL_PREF = ("psf", "psb", "yab_d", "x1_d", "slot", "rw", "hs_sc", "out_d")

    def stream(sfx):
        def m(keys):
            return [k if (k in GLOBAL_KEYS or k.startswith(GLOBAL_PREF)) else k + sfx for k in keys]

        def op_(eng, fn, r=(), w=()):
            op(eng, fn, m(r), m(w))

        def dma_(eng, fn, r=(), w=(), key=None):
            dma(eng, fn, m(r), m(w), key)
        return op_, dma_

    def op(eng, fn, r=(), w=()):
        S_.op(eng, fn, r, w)
        WV.tick()
    op_glob = op

    def dma(eng, fn, r=(), w=(), key=None):
        S_.dma(eng, fn, r, w, key)
        WV.tick()

    dma("sp", lambda e: e.dma_start(out=vec[:], in_=vec_d), w=["vec"], key="vec")
    dma("sp", lambda e: e.dma_start(out=cst[:], in_=cst_d), w=["cst"], key="cst")
    dma("sp", lambda e: e.dma_start(out=posi[:], in_=pos_d), w=["posi"], key="posi")
    op("dve", lambda e: e.tensor_copy(out=identb[:], in_=IDENTF), r=["cst"], w=["identb"])
    op("dve", lambda e: e.tensor_copy(out=bones[:], in_=cst[:, 640:768]), r=["cst"], w=["bones"])
    op("dve", lambda e: e.memset(onesb[:], 1.0), w=["onesb"])
    op("dve", lambda e: e.memset(onesf[:], 1.0), w=["onesf"])
    op("dve", lambda e: e.tensor_copy(out=posf[:], in_=posi[:]), r=["posi"], w=["posf"])
    op("dve", lambda e: e.tensor_scalar(out=derived[:, 0:14], in0=vcol(V_MU, 14), scalar1=-1.0, scalar2=1.0,
                                        op0=ALU.mult, op1=ALU.add), r=["vec"], w=["derived"])
    op("dve", lambda e: e.tensor_scalar(out=derived[:, 14:18], in0=vcol(V_KA, 4), scalar1=-1.0, scalar2=1.0,
                                        op0=ALU.mult, op1=ALU.add), r=["vec"], w=["derived"])
    op("act", lambda e: e.activation(out=derived[:, 18:22], in_=vcol(V_SINK, 4), func=AF.Exp), r=["vec", "derived"],
       w=["derived"])
    OMU = lambda c, n=1: derived[:, c:c + n]
    OMKA = lambda c, n=1: derived[:, 14 + c:14 + c + n]
    ESINK = derived[:, 18:22]

    nop_fns = {
        "pe": lambda e: e.nop(), "act": lambda e: e.nop(), "dve": lambda e: e.nop(),
        "pool": lambda e: e.nop(), "sp": lambda e: e.nop(),
    }

    def phase_reset():
        S_.barrier(nop_fns)
        st["p"] = ph_base

    def load_cast_weight(dst, dst_key, src_ap, rows, cols, gcol, stage, stage_key, kchunks, eng_cycle):
        for kc in range(kchunks):
            sl = stage[kc % len(stage)]
            sk = stage_key[kc % len(stage)]
            dma("sp", lambda e, sl=sl, kc=kc: e.dma_start(out=sl[:, 0:cols], in_=src_ap[kc * 128:(kc + 1) * 128, :]),
                w=[sk], key=sk)
            eng = eng_cycle[kc % len(eng_cycle)]
            if gcol is None:
                op(eng, lambda e, sl=sl, kc=kc: e.tensor_copy(out=dst[:, kc, :], in_=sl[:, 0:cols]),
                   r=[sk], w=[dst_key])
            else:
                op(eng, lambda e, sl=sl, kc=kc: e.tensor_scalar(out=dst[:, kc, :], in0=sl[:, 0:cols],
                                                                 scalar1=vcol(gcol + kc), scalar2=None, op0=ALU.mult),
                   r=[sk, "vec"], w=[dst_key])

    def rms_to_hT(ti, xin, xin_key, hT, hT_key, tmp, normalize=True, op=None):
        op = op or op_glob
        junk, ss, rstd, xbf = tmp["junk"], tmp["ss"], tmp["rstd"], tmp["xbf"]
        op("act", lambda e: e.activation(out=junk[:], in_=xin[:], func=AF.Square, accum_out=ss[:, 0:1]),
           r=[xin_key], w=["junk", "ss"])
        op("dve", lambda e: e.tensor_scalar(out=ss[:, 1:2], in0=ss[:, 0:1], scalar1=1.0 / D, scalar2=1e-6,
                                            op0=ALU.mult, op1=ALU.add), r=["ss"], w=["ss1"])
        op("act", lambda e: e.activation(out=ss[:, 2:3], in_=ss[:, 1:2], func=AF.Sqrt), r=["ss1"], w=["ss2"])
        op("dve", lambda e: e.reciprocal(out=rstd[:, 0:1], in_=ss[:, 2:3]), r=["ss2"], w=["rstd"])
        if normalize:
            op("dve", lambda e: e.tensor_scalar(out=xbf[:], in0=xin[:], scalar1=rstd[:, 0:1], scalar2=None,
                                                op0=ALU.mult), r=[xin_key, "rstd"], w=["xbf"])
        else:
            op("act", lambda e: e.activation(out=xbf[:], in_=xin[:], func=AF.Copy), r=[xin_key], w=["xbf"])
        pb, pk = PSB()
        for kc in range(8):
            op("pe", lambda e, kc=kc: e.transpose(pb[:, kc * 128:(kc + 1) * 128], xbf[:, kc * 128:(kc + 1) * 128],
                                                  identb[:]), r=["xbf", "identb"], w=[pk])
        op("act", lambda e: e.activation(out=hT[:].rearrange("p k t -> p (k t)"), in_=pb[:, :], func=AF.Copy),
           r=[pk], w=[hT_key])

    if "1a" in PH:
        Wqkv = salloc("Wqkv", [128, 8, 768], BF16)
        Wrw = salloc("Wrw", [128, 8, 1792], BF16)
        Wlora = salloc("Wlora", [128, 512], BF16)
        Wgu = salloc("Wgu", [128, 512], BF16)
        stg = [salloc("stgA", [128, 2560], F32)]
        for kc in range(8):
            dma("sp", lambda e, kc=kc: e.dma_start(out=stg[0][:, 0:2560], in_=win_d[kc * 128:(kc + 1) * 128, 0:2560]),
                w=["stgA"], key="stgA")
            op("dve", lambda e, kc=kc: e.tensor_scalar(out=Wqkv[:, kc, :], in0=stg[0][:, 0:768],
                                                       scalar1=vcol(V_LNMIX + kc), scalar2=None, op0=ALU.mult),
               r=["stgA", "vec"], w=["Wqkv"])
            op("pool", lambda e, kc=kc: e.tensor_scalar(out=Wrw[:, kc, :], in0=stg[0][:, 768:2560],
                                                        scalar1=vcol(V_LNMIX + kc), scalar2=1.0, op0=ALU.mult,
                                                        op1=ALU.mult),
               r=["stgA", "vec"], w=["Wrw"])
        dma("sp", lambda e: e.dma_start(out=stg[0][:, 0:512], in_=wlora_d), w=["stgA"], key="stgA")
        op("dve", lambda e: e.tensor_copy(out=Wlora[:], in_=stg[0][:, 0:512]), r=["stgA"], w=["Wlora"])
        dma("sp", lambda e: e.dma_start(out=stg[0][:, 512:1024], in_=wgu_d), w=["stgA"], key="stgA")
        op("dve", lambda e: e.tensor_copy(out=Wgu[:], in_=stg[0][:, 512:1024]), r=["stgA"], w=["Wgu"])

        xin = [salloc(f"xin{i}", [128, D], F32) for i in range(2)]
        tmp = dict(junk=salloc("junk", [128, D], BF16), ss=salloc("ss", [128, 4], F32),
                   rstd=salloc("rstd", [128, 1], F32), xbf=salloc("xbf", [128, D], BF16))
        hT = [salloc(f"hT{i}", [128, 8, 128], BF16) for i in range(2)]
        ropeT = salloc("ropeT", [128, 64], F32)
        ropeN = salloc("ropeN", [128, 64], I32)
        ropeF = salloc("ropeF", [128, 64], F32)
        ropeG = salloc("ropeG", [128, 64], F32)
        CS = salloc("CS", [128, 64], F32)
        ropA = salloc("ropA", [128, 640], F32)
        ropB = salloc("ropB", [128, 640], F32)
        qkr = salloc("qkr", [128, 640], BF16)
        qT = salloc("qT", [128, 4, 128], BF16)
        kTs = [salloc(f"kT{i}", [128, 128], BF16) for i in range(2)]
        vts = [salloc(f"vtok{i}", [128, 128], BF16) for i in range(2)]
        Eb = [salloc(f"Eb{i}", [128, 512], BF16) for i in range(4)]
        dent = salloc("dent", [128, 4, 128], F32)
        yab = [salloc(f"yabs{i}", [128, 8, 128], BF16) for i in range(2)]
        zb = [salloc(f"zb{i}", [128, 4, 129], F32) for i in range(2)]
        zt1 = salloc("zt1", [128, 4, 128], F32)
        zt2 = salloc("zt2", [128, 4, 128], F32)
        carry = salloc("carry", [128, 16], F32)
        Rr = salloc("Rr", [128, 4, 128], F32)
        Kr = salloc("Kr", [128, 4, 128], F32)
        Vr = salloc("Vr", [128, 4, 128], F32)
        XM = salloc("XM", [128, 2, 128], F32)
        LIN = salloc("LIN", [128, 128], BF16)
        SXG = salloc("SXG", [128, 128], BF16)
        SG = salloc("SG", [128, 4, 128], F32)
        Aa = salloc("Aa", [128, 4, 128], F32)
        Gg = salloc("Gg", [128, 4, 128], F32)
        LW = salloc("LW", [128, 4, 128], F32)
        CUM = salloc("CUM", [128, 4, 128], F32)
        CX = salloc("CX", [128, 4, 128], F32)
        E1 = salloc("E1", [128, 4, 128], F32)
        E2 = salloc("E2", [128, 4, 128], F32)
        E3 = salloc("E3", [128, 4, 128], F32)
        E4 = salloc("E4", [128, 4, 128], F32)
        KKR = salloc("KKR", [128, 4, 128], F32)
        SQb = salloc("SQb", [128, 4, 128], BF16)
        RN = salloc("RN", [128, 4, 128], F32)
        KK = salloc("KK", [128, 4, 128], F32)
        T1 = salloc("T1", [128, 4, 128], F32)
        KP = salloc("KP", [128, 4, 128], F32)
        Bb = salloc("Bb", [128, 4, 128], F32)
        AR = salloc("AR", [128, 4, 2, 128], BF16)
        BK = salloc("BK", [128, 4, 2, 128], BF16)
        BKS = salloc("BKS", [128, 4, 2, 128], BF16)
        ARm = [salloc(f"ARm{i}", [128, 4, 2, 128], BF16) for i in range(2)]
        BKm = [salloc(f"BKm{i}", [128, 4, 2, 128], BF16) for i in range(2)]
        RK = salloc("RK", [128, 4, 128], F32)
        RK2 = salloc("RK2", [128, 4, 128], BF16)
        BV = salloc("BV", [128, 4, 128], F32)
        VB = salloc("VB", [128, 4, 128], BF16)
        BKT = salloc("BKT", [128, 1024], BF16)
        VT = salloc("VT", [128, 512], BF16)
        MA = salloc("MA", [128, 8, 256], BF16)
        MB = salloc("MB", [128, 8, 256], BF16)
        Qm = [salloc(f"Qm{i}", [128, 8, 128], BF16) for i in range(2)]
        PX = [salloc(f"PX{i}", [128, 8, 256], BF16) for i in range(2)]
        XF = salloc("XF", [128, 8, 128], BF16)
        SF = salloc("SF", [128, 4, 64], F32)
        SBs = salloc("SBs", [128, 4, 64], BF16)
        TMPS = salloc("TMPS", [128, 4, 64], F32)
        RH = salloc("RH", [128, 512], BF16)
        UT = salloc("UT", [128, 512], BF16)
        Yf = salloc("Yf", [128, 4, 128], F32)
        YB = salloc("YB", [128, 4, 128], BF16)
        YSQ = salloc("YSQ", [128, 4, 128], BF16)
        MEAN = salloc("MEAN", [128, 4, 128], F32)
        M2 = salloc("M2", [128, 4, 128], F32)
        VAR = salloc("VAR", [128, 4, 128], F32)
        Dd = salloc("Dd", [128, 4, 128], F32)

        def f2(t):
            return t[:].rearrange("p a b -> p (a b)")

        def genA(ti):
            tl.pool = "A"
            tj = ti % TPS
            sl = ti % 2
            xk = f"xin{sl}"
            dma("sp", lambda e, ti=ti, sl=sl: e.dma_start(out=xin[sl][:], in_=x_d[ti * 128:(ti + 1) * 128, :]),
                w=[xk], key=xk)
            hk = f"hT{sl}"
            rms_to_hT(ti, xin[sl], xk, hT[sl], hk, tmp)
            h = hT[sl]
            if CUT <= 1:
                return
            pq, pqk = PS()
            pkv, pkvk = PS()
            for kc in range(8):
                op("pe", lambda e, kc=kc: e.matmul(pq[:, 0:512], h[:, kc, :], Wqkv[:, kc, 0:512],
                                                   start=(kc == 0), stop=(kc == 7)), r=[hk, "Wqkv"], w=[pqk])
            for kc in range(8):
                op("pe", lambda e, kc=kc: e.matmul(pkv[:, 0:256], h[:, kc, :], Wqkv[:, kc, 512:768],
                                                   start=(kc == 0), stop=(kc == 7)), r=[hk, "Wqkv"], w=[pkvk])
            op("dve", lambda e, ti=ti: e.scalar_tensor_tensor(out=ropeT[:], in0=INVF, scalar=posf[:, ti:ti + 1],
                                                              in1=OFFS, op0=ALU.mult, op1=ALU.add),
               r=["cst", "posf"], w=["ropeT"])
            op("dve", lambda e: e.tensor_copy(out=ropeN[:], in_=ropeT[:]), r=["ropeT"], w=["ropeN"])
            op("dve", lambda e: e.tensor_copy(out=ropeF[:], in_=ropeN[:]), r=["ropeN"], w=["ropeF"])
            op("dve", lambda e: e.tensor_tensor(out=ropeF[:], in0=ropeT[:], in1=ropeF[:], op=ALU.subtract),
               r=["ropeT", "ropeF"], w=["ropeF"])
            op("dve", lambda e: e.tensor_single_scalar(out=ropeG[:], in_=ropeF[:], scalar=0.5, op=ALU.is_gt),
               r=["ropeF"], w=["ropeG"])
            op("dve", lambda e: e.tensor_tensor(out=ropeF[:], in0=ropeF[:], in1=ropeG[:], op=ALU.subtract),
               r=["ropeF", "ropeG"], w=["ropeF"])
            op("act", lambda e: e.activation(out=CS[:], in_=ropeF[:], func=AF.Sin, scale=2.0 * np.pi),
               r=["ropeF"], w=["CS"])
            if CUT <= 2:
                return
            for (src, skey, c0, H) in ((pq, pqk, 0, 8), (pkv, pkvk, 512, 2)):
                W_ = H * 64
                s4 = src[:, 0:W_].rearrange("p (h t d) -> p h t d", h=H, t=2)
                A4 = ropA[:, c0:c0 + W_].rearrange("p (h t d) -> p h t d", h=H, t=2)
                B4 = ropB[:, c0:c0 + W_].rearrange("p (h t d) -> p h t d", h=H, t=2)
                O4 = qkr[:, c0:c0 + W_].rearrange("p (h t d) -> p h t d", h=H, t=2)
                cosb = CS[:, 32:64].unsqueeze(1).unsqueeze(1).to_broadcast([128, H, 2, 32])
                sinb = CS[:, 0:32].unsqueeze(1).to_broadcast([128, H, 32])
                op("dve", lambda e, s4=s4, A4=A4, cosb=cosb: e.tensor_tensor(out=A4, in0=s4, in1=cosb, op=ALU.mult),
                   r=[skey, "CS"], w=["ropA"])
                op("dve", lambda e, s4=s4, B4=B4, sinb=sinb: e.tensor_tensor(out=B4[:, :, 0, :], in0=s4[:, :, 1, :],
                                                                           in1=sinb, op=ALU.mult),
                   r=[skey, "CS"], w=["ropB"])
                op("dve", lambda e, s4=s4, B4=B4, sinb=sinb: e.tensor_tensor(out=B4[:, :, 1, :], in0=s4[:, :, 0, :],
                                                                           in1=sinb, op=ALU.mult),
                   r=[skey, "CS"], w=["ropB"])
                op("pool", lambda e, A4=A4, B4=B4, O4=O4: e.tensor_tensor(out=O4[:, :, 0, :], in0=A4[:, :, 0, :],
                                                                         in1=B4[:, :, 0, :], op=ALU.subtract),
                   r=["ropA", "ropB"], w=["qkr"])
                op("pool", lambda e, A4=A4, B4=B4, O4=O4: e.tensor_tensor(out=O4[:, :, 1, :], in0=A4[:, :, 1, :],
                                                                         in1=B4[:, :, 1, :], op=ALU.add),
                   r=["ropA", "ropB"], w=["qkr"])
            vk = f"vtok{sl}"
            kk_ = f"kT{sl}"
            op("act", lambda e, sl=sl: e.activation(out=vts[sl][:], in_=pkv[:, 128:256], func=AF.Copy),
               r=[pkvk], w=[vk])
            pb, pbk = PSB()
            for c in range(5):
                op("pe", lambda e, c=c: e.transpose(pb[:, c * 128:(c + 1) * 128], qkr[:, c * 128:(c + 1) * 128],
                                                    identb[:]), r=["qkr", "identb"], w=[pbk])
            op("act", lambda e: e.activation(out=f2(qT), in_=pb[:, 0:512], func=AF.Copy), r=[pbk], w=["qT"])
            op("act", lambda e, sl=sl: e.activation(out=kTs[sl][:], in_=pb[:, 512:640], func=AF.Copy),
               r=[pbk], w=[kk_])
            if CUT <= 3:
                return
            kbs = ([1 - sl] if tj > 0 else []) + [sl]
            ei = 0
            Euse = {}
            for g in range(2):
                for kb in kbs:
                    pe_, pek = PS()
                    op("pe", lambda e, g=g, kb=kb, pe_=pe_: e.matmul(
                        pe_[:, 0:512], kTs[kb][g * 64:(g + 1) * 64, :], qT[g * 64:(g + 1) * 64, :, :],
                        start=True, stop=True), r=[f"kT{kb}", "qT"], w=[pek])
                    Et = Eb[ei]
                    ek = f"Eb{ei}"
                    ei += 1
                    op("act", lambda e, Et=Et, pe_=pe_: e.activation(out=Et[:], in_=pe_[:, 0:512], func=AF.Exp,
                                                                    scale=0.125), r=[pek], w=[ek])
                    msk = UINC if kb == sl else LSTR
                    op("pool", lambda e, Et=Et, msk=msk: e.tensor_tensor(
                        out=Et[:].rearrange("p (c q) -> p c q", c=4), in0=Et[:].rearrange("p (c q) -> p c q", c=4),
                        in1=msk.unsqueeze(1).to_broadcast([128, 4, 128]), op=ALU.mult), r=[ek, "cst"], w=[ek])
                    Euse[(g, kb)] = (Et, ek)
            po, pok = PS()
            pd, pdk = PS()
            for g in range(2):
                for i, kb in enumerate(kbs):
                    Et, ek = Euse[(g, kb)]
                    op("pe", lambda e, g=g, kb=kb, Et=Et, i=i: e.matmul(
                        po[g * 64:(g + 1) * 64, 0:512], vts[kb][:, g * 64:(g + 1) * 64], Et[:],
                        start=(i == 0), stop=(i == len(kbs) - 1)), r=[f"vtok{kb}", ek], w=[pok])
                for i, kb in enumerate(kbs):
                    Et, ek = Euse[(g, kb)]
                    op("pe", lambda e, g=g, Et=Et, i=i: e.matmul(
                        pd[g * 64:(g + 1) * 64, 0:512], onesb[:, 0:64], Et[:],
                        start=(i == 0), stop=(i == len(kbs) - 1)), r=["onesb", ek], w=[pdk])
            ys = yab[sl]
            yk = f"yabs{sl}"
            op("dve", lambda e: e.tensor_tensor(out=dent[:], in0=pd[:, 0:512].rearrange("p (c q) -> p c q", c=4),
                                                in1=ESINK.unsqueeze(2).to_broadcast([128, 4, 128]), op=ALU.add),
               r=[pdk, "derived"], w=["dent"])
            op("act", lambda e: e.activation(out=f2(dent), in_=f2(dent), func=AF.Ln), r=["dent"], w=["dent"])
            op("act", lambda e: e.activation(out=f2(dent), in_=f2(dent), func=AF.Exp, scale=-1.0), r=["dent"], w=["dent"])
            op("dve", lambda e, ys=ys: e.tensor_tensor(out=ys[:, 0:4, :],
                                                       in0=po[:, 0:512].rearrange("p (c q) -> p c q", c=4),
                                                       in1=dent[:], op=ALU.mult), r=[pok, "dent"], w=[yk + "a"])


        def genBC(ti):
            tl.pool = "B"
            tj = ti % TPS
            sl = ti % 2
            hk = f"hT{sl}"
            h = hT[sl]
            ys = yab[sl]
            yk = f"yabs{sl}"
            if CUT <= 4:
                return
            if tj == 0:
                op("pool", lambda e: e.memset(carry[:], 0.0), w=["carry"])
                op("pool", lambda e: e.memset(SF[:], 0.0), w=["SF"])
                op("pool", lambda e: e.memset(SBs[:], 0.0), w=["SBs"])
            groups = [(0, 4, Rr, "Rr"), (4, 4, Kr, "Kr"), (8, 4, Vr, "Vr"), (12, 2, XM, "XM")]
            for gi, (z0, n, dst, dk) in enumerate(groups):
                pz, pzk = PS()
                for j in range(n):
                    zc = z0 + j
                    for kc in range(8):
                        op("pe", lambda e, j=j, zc=zc, kc=kc, pz=pz: e.matmul(
                            pz[:, j * 128:(j + 1) * 128], Wrw[:, kc, zc * 128:(zc + 1) * 128], h[:, kc, :],
                            start=(kc == 0), stop=(kc == 7)), r=[hk, "Wrw"], w=[pzk])
                zbt = zb[gi % 2]
                zk = f"zb{gi % 2}"
                op("act", lambda e, zbt=zbt, pz=pz, n=n: e.activation(
                    out=zbt[:, 0:n, 1:129], in_=pz[:, 0:n * 128].rearrange("p (c t) -> p c t", c=n), func=AF.Copy),
                   r=[pzk], w=[zk])
                op("pool", lambda e, zbt=zbt, z0=z0, n=n: e.tensor_copy(out=zbt[:, 0:n, 0], in_=carry[:, z0:z0 + n]),
                   r=["carry"], w=[zk])
                op("dve", lambda e, zbt=zbt, z0=z0, n=n: e.tensor_tensor(
                    out=zt1[:, 0:n, :], in0=zbt[:, 0:n, 0:128],
                    in1=vcol(V_MU + z0, n).unsqueeze(2).to_broadcast([128, n, 128]), op=ALU.mult),
                   r=[zk, "vec"], w=["zt1"])
                op("pool", lambda e, zbt=zbt, z0=z0, n=n: e.tensor_tensor(
                    out=zt2[:, 0:n, :], in0=zbt[:, 0:n, 1:129],
                    in1=OMU(z0, n).unsqueeze(2).to_broadcast([128, n, 128]), op=ALU.mult),
                   r=[zk, "derived"], w=["zt2"])
                op("dve", lambda e, dst=dst, n=n: e.tensor_tensor(out=dst[:, 0:n, :], in0=zt1[:, 0:n, :],
                                                                  in1=zt2[:, 0:n, :], op=ALU.add),
                   r=["zt1", "zt2"], w=[dk])
                op("pool", lambda e, zbt=zbt, z0=z0, n=n: e.tensor_copy(out=carry[:, z0:z0 + n], in_=zbt[:, 0:n, 128]),
                   r=[zk], w=["carry"])
            if CUT <= 5:
                return
            op("act", lambda e: e.activation(out=LIN[0:64, :], in_=XM[0:64, 0, :], func=AF.Tanh), r=["XM"], w=["LINa"])
            op("pool", lambda e: e.tensor_copy(out=LIN[64:128, :], in_=XM[64:128, 0, :]), r=["XM"], w=["LINb"])
            op("act", lambda e: e.activation(out=SXG[:], in_=XM[:, 1, :], func=AF.Sigmoid), r=["XM"], w=["SXG"])
            pu, puk = PS()
            pa, pak = PS()
            pg, pgk = PS()
            for cc in range(4):
                op("pe", lambda e, cc=cc: e.matmul(pu[:, cc * 128:(cc + 1) * 128], Wlora[0:64, cc * 128:(cc + 1) * 128],
                                                   LIN[0:64, :], start=True, stop=True),
                   r=["Wlora", "LINa"], w=[puk])
            for cc in range(4):
                op("pe", lambda e, cc=cc: e.matmul(pa[:, cc * 128:(cc + 1) * 128],
                                                   Wlora[64:128, cc * 128:(cc + 1) * 128],
                                                   LIN[64:128, :], start=True, stop=True),
                   r=["Wlora", "LINb"], w=[pak])
            for cc in range(4):
                op("pe", lambda e, cc=cc: e.matmul(pg[:, cc * 128:(cc + 1) * 128], Wgu[:, cc * 128:(cc + 1) * 128],
                                                   SXG[:], start=True, stop=True), r=["Wgu", "SXG"], w=[pgk])
            for cc in range(4):
                op("act", lambda e, cc=cc: e.activation(out=SG[:, cc, :], in_=pu[:, cc * 128:(cc + 1) * 128],
                                                        func=AF.Sigmoid, bias=vcol(V_W0 + cc)),
                   r=[puk, "vec"], w=["SG"])
            for cc in range(4):
                op("act", lambda e, cc=cc: e.activation(out=Aa[:, cc, :], in_=pa[:, cc * 128:(cc + 1) * 128],
                                                        func=AF.Sigmoid, bias=vcol(V_A0 + cc)),
                   r=[pak, "vec"], w=["Aa"])
            op("act", lambda e: e.activation(out=f2(Gg), in_=pg[:, 0:512], func=AF.Copy), r=[pgk], w=["Gg"])
            op("act", lambda e: e.activation(out=f2(LW), in_=f2(SG), func=AF.Copy, scale=-0.6065306597126334),
               r=["SG"], w=["LW"])
            for cc in range(4):
                op("dve", lambda e, cc=cc: e.tensor_tensor_scan(out=CUM[:, cc, :], data0=onesf[:], data1=LW[:, cc, :],
                                                                initial=0.0, op0=ALU.mult, op1=ALU.add),
                   r=["onesf", "LW"], w=["CUM"])
            op("dve", lambda e: e.tensor_tensor(out=f2(CX), in0=f2(CUM), in1=f2(LW), op=ALU.subtract),
               r=["CUM", "LW"], w=["CX"])
            op("act", lambda e: e.activation(out=f2(E1), in_=f2(CUM), func=AF.Exp), r=["CUM"], w=["E1"])
            op("act", lambda e: e.activation(out=f2(E2), in_=f2(CUM), func=AF.Exp, scale=-1.0), r=["CUM"], w=["E2"])
            op("act", lambda e: e.activation(out=f2(E3), in_=f2(CX), func=AF.Exp), r=["CX"], w=["E3"])
            for cc in range(4):
                op("act", lambda e, cc=cc: e.activation(out=E4[:, cc, :], in_=CUM[:, cc, :], func=AF.Exp, scale=-1.0,
                                                        bias=CUM[:, cc, 127:128]), r=["CUM"], w=["E4"])
            op("dve", lambda e: e.tensor_tensor(out=KKR[:], in0=Kr[:],
                                                 in1=vcol(V_KK, 4).unsqueeze(2).to_broadcast([128, 4, 128]),
                                                 op=ALU.mult), r=["Kr", "vec"], w=["KKR"])
            op("act", lambda e: e.activation(out=f2(SQb), in_=f2(KKR), func=AF.Square), r=["KKR"], w=["SQb"])
            pss, pssk = PS()
            op("pe", lambda e: e.matmul(pss[:, 0:512], bones[:], f2(SQb), start=True, stop=True),
               r=["bones", "SQb"], w=[pssk])
            op("act", lambda e: e.activation(out=f2(RN), in_=pss[:, 0:512], func=AF.Ln, bias=1e-19),
               r=[pssk], w=["RN"])
            op("act", lambda e: e.activation(out=f2(RN), in_=f2(RN), func=AF.Exp, scale=-0.5), r=["RN"], w=["RN"])
            op("dve", lambda e: e.tensor_tensor(out=f2(KK), in0=f2(KKR), in1=f2(RN), op=ALU.mult),
               r=["KKR", "RN"], w=["KK"])
            for cc in range(4):
                op("dve", lambda e, cc=cc: e.tensor_scalar(out=T1[:, cc, :], in0=Aa[:, cc, :],
                                                           scalar1=vcol(V_KA + cc), scalar2=OMKA(cc),
                                                           op0=ALU.mult, op1=ALU.add),
                   r=["Aa", "vec", "derived"], w=["T1"])
            op("dve", lambda e: e.tensor_tensor(out=f2(KP), in0=f2(Kr), in1=f2(T1), op=ALU.mult),
               r=["Kr", "T1"], w=["KP"])
            op("dve", lambda e: e.tensor_tensor(out=f2(Bb), in0=f2(KK), in1=f2(Aa), op=ALU.mult),
               r=["KK", "Aa"], w=["Bb"])
            op("dve", lambda e: e.scalar_tensor_tensor(out=AR[:, :, 0, :], in0=E3[:], scalar=-1.0, in1=KK[:],
                                                       op0=ALU.mult, op1=ALU.mult), r=["E3", "KK"], w=["AR"])
            op("dve", lambda e: e.tensor_tensor(out=AR[:, :, 1, :], in0=E1[:], in1=Rr[:], op=ALU.mult),
               r=["E1", "Rr", "AR"], w=["AR"])
            op("dve", lambda e: e.tensor_tensor(out=BK[:, :, 0, :], in0=E2[:], in1=Bb[:], op=ALU.mult),
               r=["E2", "Bb"], w=["BK"])
            op("dve", lambda e: e.tensor_tensor(out=BK[:, :, 1, :], in0=E2[:], in1=KP[:], op=ALU.mult),
               r=["E2", "KP", "BK"], w=["BK"])
            op("dve", lambda e: e.tensor_tensor(out=BKS[:, :, 0, :], in0=E4[:], in1=Bb[:], op=ALU.mult),
               r=["E4", "Bb"], w=["BKS"])
            op("dve", lambda e: e.tensor_tensor(out=BKS[:, :, 1, :], in0=E4[:], in1=KP[:], op=ALU.mult),
               r=["E4", "KP", "BKS"], w=["BKS"])
            for par in range(2):
                pmc = cst[:, 640 + 64 * par:641 + 64 * par]
                op("act", lambda e: e.activation(
                    out=ARm[par][:].rearrange("p a b c -> p (a b c)"), in_=AR[:].rearrange("p a b c -> p (a b c)"),
                    func=AF.Copy, scale=pmc), r=["AR", "cst"], w=[f"ARm{par}"])
                op("act" if par else "dve", (lambda e: e.activation(
                    out=BKm[par][:].rearrange("p a b c -> p (a b c)"), in_=BK[:].rearrange("p a b c -> p (a b c)"),
                    func=AF.Copy, scale=pmc)) if par else (lambda e: e.tensor_scalar(
                    out=BKm[par][:].rearrange("p a b c -> p (a b c)"), in0=BK[:].rearrange("p a b c -> p (a b c)"),
                    scalar1=pmc, scalar2=None, op0=ALU.mult)), r=["BK", "cst"], w=[f"BKm{par}"])
            op("pool", lambda e: e.tensor_tensor(out=f2(RK), in0=f2(Rr), in1=f2(KP), op=ALU.mult),
               r=["Rr", "KP"], w=["RK"])
            op("pool", lambda e: e.tensor_tensor(out=RK2[:], in0=RK[:],
                                                 in1=vcol(V_RK, 4).unsqueeze(2).to_broadcast([128, 4, 128]),
                                                 op=ALU.mult), r=["RK", "vec"], w=["RK2"])
            pbn, pbnk = PS()
            op("pe", lambda e: e.matmul(pbn[:, 0:512], bones[:], f2(RK2), start=True, stop=True),
               r=["bones", "RK2"], w=[pbnk])
            op("dve", lambda e: e.tensor_tensor(out=f2(BV), in0=pbn[:, 0:512], in1=f2(Vr), op=ALU.mult),
               r=[pbnk, "Vr"], w=["BV"])
            op("act", lambda e: e.activation(out=f2(VB), in_=f2(Vr), func=AF.Copy), r=["Vr"], w=["VB"])
            pb1, pb1k = PSB()
            for j in range(2):
                for cc in range(4):
                    op("pe", lambda e, j=j, cc=cc: e.transpose(pb1[:, j * 512 + cc * 128: j * 512 + (cc + 1) * 128],
                                                               BKS[:, cc, j, :], identb[:]),
                       r=["BKS", "identb"], w=[pb1k])
            op("act", lambda e: e.activation(out=BKT[:], in_=pb1[:, :], func=AF.Copy), r=[pb1k], w=["BKT"])
            pb2, pb2k = PSB()
            for cc in range(4):
                op("pe", lambda e, cc=cc: e.transpose(pb2[:, cc * 128:(cc + 1) * 128], VB[:, cc, :], identb[:]),
                   r=["VB", "identb"], w=[pb2k])
            op("act", lambda e: e.activation(out=VT[:], in_=pb2[:, 0:512], func=AF.Copy), r=[pb2k], w=["VT"])
            if CUT <= 6:
                return
            def inv_s0(hh):
                heads = list(range(4 * hh, 4 * hh + 4))
                hs = slice(4 * hh, 4 * hh + 4)
                mk2 = MASK2.unsqueeze(1).to_broadcast([128, 2, 256])
                for (which, dstM) in ((0, MA), (1, MB)):
                    pM_ = [PS(), PS()]
                    for i, hd in enumerate(heads):
                        cc = hd // 2
                        c0 = (i % 2) * 256
                        par = hd % 2
                        op("pe", lambda e: e.matmul(
                            pM_[i // 2][0][:, c0:c0 + 256], BKm[par][:, cc, which, :],
                            AR[:, cc, :, :].rearrange("p a t -> p (a t)"), start=True, stop=True),
                           r=[f"BKm{par}", "AR"], w=[pM_[i // 2][1]])
                    for b2 in range(2):
                        h2 = slice(4 * hh + 2 * b2, 4 * hh + 2 * b2 + 2)
                        op("dve", lambda e: e.tensor_tensor(
                            out=dstM[:, h2, :], in0=pM_[b2][0][:, 0:512].rearrange("p (h c) -> p h c", h=2), in1=mk2,
                            op=ALU.mult), r=[pM_[b2][1], "cst"], w=[("MA" if which == 0 else "MB") + str(hh)])
                pQ0 = PS()
                for i, hd in enumerate(heads):
                    cc = hd // 2
                    par = hd % 2
                    op("pe", lambda e: e.matmul(
                        pQ0[0][:, i * 128:(i + 1) * 128], ARm[par][:, cc, 0, :], BK[:, cc, 0, :], start=True, stop=True),
                       r=["BK", f"ARm{par}"], w=[pQ0[1]])
                op("dve", lambda e: e.tensor_tensor(
                    out=Qm[0][:, hs, :], in0=pQ0[0][:, 0:512].rearrange("p (h c) -> p h c", h=4),
                    in1=LSTR.unsqueeze(1).to_broadcast([128, 4, 128]), op=ALU.mult),
                   r=[pQ0[1], "cst"], w=[f"Q0_{hh}"])

            def inv_l0(hh):
                heads = list(range(4 * hh, 4 * hh + 4))
                hs = slice(4 * hh, 4 * hh + 4)
                pP = PS()
                pQn = PS()
                for i, hd in enumerate(heads):
                    op("pe", lambda e, i=i, hd=hd: e.matmul(pP[0][:, i * 128:(i + 1) * 128], Qm[0][:, hd, :],
                                                            MA[:, hd, 0:128], start=True, stop=True),
                       r=[f"Q0_{hh}", f"MA{hh}"], w=[pP[1]])
                    op("pe", lambda e, i=i, hd=hd: e.matmul(pQn[0][:, i * 128:(i + 1) * 128], MA[:, hd, 0:128],
                                                            Qm[0][:, hd, :], start=True, stop=True),
                       r=[f"Q0_{hh}", f"MA{hh}"], w=[pQn[1]])
                op("act", lambda e, hs=hs: e.activation(out=PX[1][:, hs, 0:128],
                                                        in_=pP[0][:, 0:512].rearrange("p (h c) -> p h c", h=4),
                                                        func=AF.Copy), r=[pP[1]], w=[f"PX1_{hh}"])
                op("act", lambda e, hs=hs: e.activation(out=Qm[1][:, hs, :],
                                                        in_=pQn[0][:, 0:512].rearrange("p (h c) -> p h c", h=4),
                                                        func=AF.Copy), r=[pQn[1]], w=[f"Q1_{hh}"])
                op("pool", lambda e, hs=hs: e.tensor_tensor(out=PX[1][:, hs, 128:256], in0=MA[:, hs, 0:128],
                                                            in1=IDENTF.unsqueeze(1).to_broadcast([128, 4, 128]),
                                                            op=ALU.add),
                   r=[f"MA{hh}", "cst", f"PX1_{hh}"], w=[f"PX1_{hh}"])

            def inv_lv(hh, lv):
                heads = list(range(4 * hh, 4 * hh + 4))
                hs = slice(4 * hh, 4 * hh + 4)
                if True:
                    cur, nxt = lv % 2, 1 - (lv % 2)
                    pA = [PS(), PS()]
                    pQn = PS()
                    for i, hd in enumerate(heads):
                        c0 = (i % 2) * 256
                        op("pe", lambda e, i=i, hd=hd, c0=c0, cur=cur: e.matmul(
                            pA[i // 2][0][:, c0:c0 + 256], Qm[cur][:, hd, :], PX[cur][:, hd, :], start=True, stop=True),
                           r=[f"Q{cur}_{hh}", f"PX{cur}_{hh}"], w=[pA[i // 2][1]])
                        op("pe", lambda e, i=i, hd=hd, cur=cur: e.matmul(
                            pQn[0][:, i * 128:(i + 1) * 128], PX[cur][:, hd, 0:128], Qm[cur][:, hd, :],
                            start=True, stop=True), r=[f"Q{cur}_{hh}", f"PX{cur}_{hh}"], w=[pQn[1]])
                    for b2 in range(2):
                        h2 = slice(4 * hh + 2 * b2, 4 * hh + 2 * b2 + 2)
                        v3 = pA[b2][0][:, 0:512].rearrange("p (h c) -> p h c", h=2)
                        op("act", lambda e, h2=h2, v3=v3, nxt=nxt: e.activation(out=PX[nxt][:, h2, 0:128],
                                                                               in_=v3[:, :, 0:128], func=AF.Copy),
                           r=[pA[b2][1]], w=[f"PX{nxt}_{hh}"])
                        op("dve", lambda e, h2=h2, v3=v3, nxt=nxt, cur=cur: e.tensor_tensor(
                            out=PX[nxt][:, h2, 128:256], in0=v3[:, :, 128:256], in1=PX[cur][:, h2, 128:256],
                            op=ALU.add), r=[pA[b2][1], f"PX{cur}_{hh}", f"PX{nxt}_{hh}"], w=[f"PX{nxt}_{hh}"])
                    op("act", lambda e, hs=hs, nxt=nxt, pQn=pQn: e.activation(
                        out=Qm[nxt][:, hs, :], in_=pQn[0][:, 0:512].rearrange("p (h c) -> p h c", h=4), func=AF.Copy),
                       r=[pQn[1]], w=[f"Q{nxt}_{hh}"])

            def inv_fin(hh):
                heads = list(range(4 * hh, 4 * hh + 4))
                hs = slice(4 * hh, 4 * hh + 4)
                pX = PS()
                for i, hd in enumerate(heads):
                    op("pe", lambda e, i=i, hd=hd: e.matmul(pX[0][:, i * 128:(i + 1) * 128], Qm[0][:, hd, :],
                                                            PX[0][:, hd, 128:256], start=True, stop=True),
                       r=[f"Q0_{hh}", f"PX0_{hh}"], w=[pX[1]])
                op("dve", lambda e, hs=hs, pX=pX: e.tensor_tensor(
                    out=XF[:, hs, :], in0=pX[0][:, 0:512].rearrange("p (h c) -> p h c", h=4),
                    in1=PX[0][:, hs, 128:256], op=ALU.add), r=[pX[1], f"PX0_{hh}"], w=[f"XF{hh}"])

            for hh in range(2):
                inv_s0(hh)
            for hh in range(2):
                inv_l0(hh)
            for lv in range(1, 6):
                for hh in range(2):
                    inv_lv(hh, lv)
            for hh in range(2):
                inv_fin(hh)
            if CUT <= 7:
                return
            pR = PS()
            for hd in range(8):
                cc = hd // 2
                pr = slice((hd % 2) * 64, (hd % 2) * 64 + 64)
                op("pe", lambda e: e.matmul(pR[0][:, hd * 64:(hd + 1) * 64], ARm[hd % 2][:, cc, 0, :],
                                            SBs[:, cc, :], start=True, stop=False),
                   r=[f"ARm{hd % 2}", "SBs"], w=[pR[1]])
                op("pe", lambda e, hd=hd: e.matmul(pR[0][:, hd * 64:(hd + 1) * 64], MB[:, hd, 0:128],
                                                   VT[:, hd * 64:(hd + 1) * 64], start=False, stop=True),
                   r=[f"MB{hd // 4}", "VT"], w=[pR[1]])
            op("act", lambda e: e.activation(out=RH[:], in_=pR[0][:, 0:512], func=AF.Copy), r=[pR[1]], w=["RH"])
            pU = PS()
            for hd in range(8):
                op("pe", lambda e, hd=hd: e.matmul(pU[0][:, hd * 64:(hd + 1) * 64], XF[:, hd, :],
                                                   RH[:, hd * 64:(hd + 1) * 64], start=True, stop=True),
                   r=[f"XF{hd // 4}", "RH"], w=[pU[1]])
            op("act", lambda e: e.activation(out=UT[:], in_=pU[0][:, 0:512], func=AF.Copy), r=[pU[1]], w=["UT"])
            pY = PS()
            pS_ = PS()
            for hd in range(8):
                cc = hd // 2
                pr = slice((hd % 2) * 64, (hd % 2) * 64 + 64)
                oy = pY[0][pr, cc * 128:(cc + 1) * 128]
                op("pe", lambda e: e.matmul(oy, SBs[:, cc, :], ARm[hd % 2][:, cc, 1, :],
                                            start=True, stop=False), r=["SBs", f"ARm{hd % 2}"], w=[pY[1]])
                op("pe", lambda e, oy=oy, hd=hd: e.matmul(oy, UT[:, hd * 64:(hd + 1) * 64], MA[:, hd, 128:256],
                                                          start=False, stop=False),
                   r=["UT", f"MA{hd // 4}"], w=[pY[1]])
                op("pe", lambda e, oy=oy, hd=hd: e.matmul(oy, VT[:, hd * 64:(hd + 1) * 64], MB[:, hd, 128:256],
                                                          start=False, stop=True),
                   r=["VT", f"MB{hd // 4}"], w=[pY[1]])
            for hd in range(8):
                cc = hd // 2
                pr = slice((hd % 2) * 64, (hd % 2) * 64 + 64)
                os_ = pS_[0][pr, cc * 64:(cc + 1) * 64]
                op("pe", lambda e, os_=os_, hd=hd: e.matmul(os_, BKT[:, hd * 64:(hd + 1) * 64],
                                                            UT[:, hd * 64:(hd + 1) * 64], start=True, stop=False),
                   r=["BKT", "UT"], w=[pS_[1]])
                op("pe", lambda e, os_=os_, hd=hd: e.matmul(os_, BKT[:, 512 + hd * 64:512 + (hd + 1) * 64],
                                                            VT[:, hd * 64:(hd + 1) * 64], start=False, stop=True),
                   r=["BKT", "VT"], w=[pS_[1]])
            op("act", lambda e: e.activation(out=f2(Yf), in_=pY[0][:, 0:512], func=AF.Copy), r=[pY[1]], w=["Yf"])
            op("dve", lambda e: e.tensor_tensor(out=TMPS[:], in0=SF[:],
                                                in1=E1[:, :, 127:128].to_broadcast([128, 4, 64]), op=ALU.mult),
               r=["SF", "E1"], w=["TMPS"])
            op("dve", lambda e: e.tensor_tensor(out=SF[:], in0=pS_[0][:, 0:256].rearrange("p (c v) -> p c v", c=4),
                                                in1=TMPS[:], op=ALU.add), r=[pS_[1], "TMPS"], w=["SF"])
            op("act", lambda e: e.activation(out=SBs[:], in_=SF[:], func=AF.Copy), r=["SF"], w=["SBs"])
            if CUT <= 8:
                return
            op("act", lambda e: e.activation(out=f2(YB), in_=pY[0][:, 0:512], func=AF.Copy), r=[pY[1]], w=["YB"])
            op("act", lambda e: e.activation(out=f2(YSQ), in_=pY[0][:, 0:512], func=AF.Square), r=[pY[1]], w=["YSQ"])
            pM = PS()
            pV = PS()
            op("pe", lambda e: e.matmul(pM[0][:, 0:512], bones[:], f2(YB), start=True, stop=True),
               r=["bones", "YB"], w=[pM[1]])
            op("pe", lambda e: e.matmul(pV[0][:, 0:512], bones[:], f2(YSQ), start=True, stop=True),
               r=["bones", "YSQ"], w=[pV[1]])
            op("act", lambda e: e.activation(out=f2(MEAN), in_=pM[0][:, 0:512], func=AF.Copy, scale=1.0 / 64),
               r=[pM[1]], w=["MEAN"])
            op("pool", lambda e: e.tensor_tensor(out=f2(M2), in0=f2(MEAN), in1=f2(MEAN), op=ALU.mult),
               r=["MEAN"], w=["M2"])
            op("dve", lambda e: e.scalar_tensor_tensor(out=f2(VAR), in0=pV[0][:, 0:512], scalar=1.0 / 64, in1=f2(M2),
                                                       op0=ALU.mult, op1=ALU.subtract), r=[pV[1], "M2"], w=["VAR"])
            op("act", lambda e: e.activation(out=f2(VAR), in_=f2(VAR), func=AF.Ln, bias=64e-5), r=["VAR"], w=["VAR"])
            op("act", lambda e: e.activation(out=f2(VAR), in_=f2(VAR), func=AF.Exp, scale=-0.5), r=["VAR"], w=["VAR"])
            op("dve", lambda e: e.tensor_tensor(out=f2(Dd), in0=f2(Yf), in1=f2(MEAN), op=ALU.subtract),
               r=["Yf", "MEAN"], w=["Dd"])
            op("dve", lambda e: e.tensor_tensor(out=f2(Dd), in0=f2(Dd), in1=f2(VAR), op=ALU.mult),
               r=["Dd", "VAR"], w=["Dd"])
            for cc in range(4):
                op("dve", lambda e, cc=cc: e.tensor_scalar(out=Dd[:, cc, :], in0=Dd[:, cc, :], scalar1=vcol(V_LNW + cc),
                                                           scalar2=vcol(V_LNB + cc), op0=ALU.mult, op1=ALU.add),
                   r=["Dd", "vec"], w=["Dd"])
            op("dve", lambda e: e.tensor_tensor(out=f2(Dd), in0=f2(Dd), in1=f2(BV), op=ALU.add),
               r=["Dd", "BV"], w=["Dd"])
            op("dve", lambda e, ys=ys: e.tensor_tensor(out=ys[:, 4:8, :], in0=Dd[:], in1=Gg[:], op=ALU.mult),
               r=["Dd", "Gg"], w=[yk + "b"])
            dma("sp", lambda e, ys=ys, ti=ti: e.dma_start(out=yab_d[ti], in_=ys[:].rearrange("p a b -> p (a b)")),
                r=[yk + "a", yk + "b"], w=[f"yab_d{ti}"], key=yk)

        genA(0)
        for ti in range(NTILE):
            fns = [lambda ti=ti: genBC(ti)]
            q = [cfg.get("qBC", 5)]
            if ti + 1 < NTILE:
                fns.append(lambda ti=ti: genA(ti + 1))
                q.append(1)
            WV.run(fns, q)
        tl.pool = None


    if "1b" in PH:
        phase_reset()
        Wgt = salloc("Wgt", [128, 8, 2048], BF16)
        WbA = salloc("WbA", [128, 4, 1024], BF16)
        WbB = salloc("WbB", [128, 4, 1024], BF16)
        Wout = salloc("Wout", [128, 8, 1024], BF16)
        Wr = salloc("Wr", [128, 8, 36], F32)
        BR = salloc("BR", [128, 36], F32)
        GMOE = salloc("GMOE", [128, D], F32)
        ustrb = salloc("ustrb", [128, 128], BF16)
        CNT = salloc("CNT", [128, 32], F32)
        stgB = [salloc(f"stgB{i}", [128, 2048], F32) for i in range(2)]
        sB = 0
        for kc in range(8):
            k_ = f"stgB{sB % 2}"
            t_ = stgB[sB % 2]
            sB += 1
            dma("sp", lambda e: e.dma_start(out=t_[:, 0:2048], in_=win_d[kc * 128:(kc + 1) * 128, 2560:4608]),
                w=[k_], key=k_)
            op("dve" if kc % 2 else "pool", lambda e: e.tensor_scalar(out=Wgt[:, kc, :], in0=t_[:, 0:2048],
                                                                      scalar1=vcol(V_LNMIX + kc), scalar2=1.0,
                                                                      op0=ALU.mult, op1=ALU.mult),
               r=[k_, "vec"], w=["Wgt"])
        for (dst, dk, src, nk) in ((WbA, "WbA", wba_d, 4), (WbB, "WbB", wbb_d, 4), (Wout, "Wout", wout_d, 8)):
            for kc in range(nk):
                k_ = f"stgB{sB % 2}"
                t_ = stgB[sB % 2]
                sB += 1
                dma("sp", lambda e: e.dma_start(out=t_[:, 0:1024], in_=src[kc * 128:(kc + 1) * 128, :]),
                    w=[k_], key=k_)
                op("dve" if kc % 2 else "pool", lambda e: e.tensor_copy(out=dst[:, kc, :], in_=t_[:, 0:1024]),
                   r=[k_], w=[dk])
        dma("sp", lambda e: e.dma_start(out=Wr[:], in_=wr_d.rearrange("(k p) n -> p k n", p=128)), w=["Wr"], key="Wr")
        for kc in range(8):
            op("dve", lambda e: e.tensor_scalar(out=Wr[:, kc, :], in0=Wr[:, kc, :], scalar1=vcol(V_LNMOE + kc),
                                                scalar2=None, op0=ALU.mult), r=["Wr", "vec"], w=["Wr"])
        dma("sp", lambda e: e.dma_start(out=BR[:], in_=br_d.to_broadcast([128, 36])), w=["BR"], key="BR")
        dma("sp", lambda e: e.dma_start(out=GMOE[:], in_=lnmoe_d.to_broadcast([128, D])), w=["GMOE"], key="GMOE")
        op("dve", lambda e: e.tensor_copy(out=ustrb[:], in_=USTR), r=["cst"], w=["ustrb"])
        op("dve", lambda e: e.memset(CNT[:], 0.0), w=["CNT"])
        ZR = salloc("ZR", [128, D], BF16)
        op("pool", lambda e: e.memset(ZR[:], 0.0), w=["ZR"])
        hs_v = hs_d.rearrange("(r p) d -> p r d", p=128)
        RCH = 8
        for r0 in range(0, NST, RCH):
            rn = min(RCH, NST - r0)
            dma("sp", lambda e: e.dma_start(out=hs_v[:, r0:r0 + rn, :],
                                            in_=ZR[:].unsqueeze(1).to_broadcast([128, rn, D])),
                r=["ZR"], w=["hs_all"], key="ZRst")

        xin = [salloc(f"xinb{i}", [128, D], F32) for i in range(2)]
        tmp = dict(junk=salloc("junkb", [128, D], BF16), ss=salloc("ssb", [128, 4], F32),
                   rstd=salloc("rstdb", [128, 1], F32), xbf=salloc("xbfb", [128, D], BF16))
        hT = [salloc(f"hTb{i}", [128, 8, 128], BF16) for i in range(2)]
        yin = [salloc(f"yin{i}", [128, 8, 128], BF16) for i in range(2)]
        GT = salloc("GT", [128, 2048], F32)
        MAf = salloc("MAf", [128, D], F32)
        MBf = salloc("MBf", [128, D], F32)
        MG = salloc("MG", [128, D], BF16)
        MGT = salloc("MGT", [128, 8, 128], BF16)
        X1 = [salloc(f"X1_{i}", [128, D], F32) for i in range(2)]
        HM = [salloc(f"HM{i}", [128, D], BF16) for i in range(2)]
        X1T = salloc("X1T", [128, 8, 128], F32)
        rs = salloc("rs", [128, 8], F32)
        LG = salloc("LG", [128, 36], F32)
        R_ = salloc("Rsm", [128, 16], F32)
        GOH = salloc("GOH", [128, 4], F32)
        EG = salloc("EG", [128, 4], F32)
        T48 = salloc("T48", [128, 4, 8], F32)
        ESEL = salloc("ESEL", [128, 8], F32)
        ES2 = salloc("ES2", [128, 8], F32)
        OHa = salloc("OHa", [128, 8], F32)
        OHb = salloc("OHb", [128, 8], F32)
        OH1 = salloc("OH1", [128, 4, 8], F32)
        OH2 = salloc("OH2", [128, 4, 8], F32)
        OHSb = salloc("OHSb", [128, 32], BF16)
        POS = salloc("POS", [128, 32], F32)
        PT2 = salloc("PT2", [128, 32], F32)
        SL = salloc("SL", [128, 2], F32)
        EOFF = cst[:, 768:800]

        def g2(t):
            return t[:].rearrange("p a b -> p (a b)")

        for ti in range(NTILE):
            sl = ti % 2
            xk = f"xinb{sl}"
            dma("sp", lambda e: e.dma_start(out=xin[sl][:], in_=x_d[ti * 128:(ti + 1) * 128, :]), w=[xk], key=xk)
            yk = f"yin{sl}"
            dma("sp", lambda e: e.dma_start(out=yin[sl][:].rearrange("p a b -> p (a b)"), in_=yab_d[ti]),
                r=[f"yab_d{ti}"], w=[yk], key=yk)
            hk = f"hTb{sl}"
            rms_to_hT(ti, xin[sl], xk, hT[sl], hk, tmp)
            h = hT[sl]
            for nb in range(4):
                pgt = PS()
                for kc in range(8):
                    op("pe", lambda e: e.matmul(pgt[0][:, 0:512], h[:, kc, :], Wgt[:, kc, nb * 512:(nb + 1) * 512],
                                                start=(kc == 0), stop=(kc == 7)), r=[hk, "Wgt"], w=[pgt[1]])
                op("act", lambda e: e.activation(out=GT[:, nb * 512:(nb + 1) * 512], in_=pgt[0][:, 0:512],
                                                 func=AF.Sigmoid), r=[pgt[1]], w=[f"GT{nb}"])
            for half in range(2):
                pba = PS()
                for c in range(4):
                    op("pe", lambda e: e.matmul(pba[0][:, 0:512], yin[sl][:, c, :],
                                                WbA[:, c, half * 512:(half + 1) * 512], start=(c == 0), stop=(c == 3)),
                       r=[yk, "WbA"], w=[pba[1]])
                op("dve", lambda e: e.tensor_tensor(out=MAf[:, half * 512:(half + 1) * 512], in0=pba[0][:, 0:512],
                                                    in1=GT[:, half * 512:(half + 1) * 512], op=ALU.mult),
                   r=[pba[1], f"GT{half}"], w=[f"MAf{half}"])
                pbb = PS()
                for c in range(4):
                    op("pe", lambda e: e.matmul(pbb[0][:, 0:512], yin[sl][:, 4 + c, :],
                                                WbB[:, c, half * 512:(half + 1) * 512], start=(c == 0), stop=(c == 3)),
                       r=[yk, "WbB"], w=[pbb[1]])
                op("dve", lambda e: e.tensor_tensor(out=MBf[:, half * 512:(half + 1) * 512], in0=pbb[0][:, 0:512],
                                                    in1=GT[:, 1024 + half * 512:1024 + (half + 1) * 512], op=ALU.mult),
                   r=[pbb[1], f"GT{2 + half}"], w=[f"MBf{half}"])
                op("dve", lambda e: e.tensor_tensor(out=MG[:, half * 512:(half + 1) * 512],
                                                     in0=MAf[:, half * 512:(half + 1) * 512],
                                                     in1=MBf[:, half * 512:(half + 1) * 512], op=ALU.add),
                   r=[f"MAf{half}", f"MBf{half}"], w=[f"MG{half}"])
            pb = PSB()
            for kc in range(8):
                op("pe", lambda e: e.transpose(pb[0][:, kc * 128:(kc + 1) * 128], MG[:, kc * 128:(kc + 1) * 128],
                                               identb[:]), r=["MG0", "MG1", "identb"], w=[pb[1]])
            op("act", lambda e: e.activation(out=g2(MGT), in_=pb[0][:, :], func=AF.Copy), r=[pb[1]], w=["MGT"])
            x1 = X1[sl]
            x1k = f"X1_{sl}"
            for half in range(2):
                po = PS()
                for kc in range(8):
                    op("pe", lambda e: e.matmul(po[0][:, 0:512], MGT[:, kc, :], Wout[:, kc, half * 512:(half + 1) * 512],
                                                start=(kc == 0), stop=(kc == 7)), r=["MGT", "Wout"], w=[po[1]])
                op("dve", lambda e: e.tensor_tensor(out=x1[:, half * 512:(half + 1) * 512], in0=po[0][:, 0:512],
                                                    in1=xin[sl][:, half * 512:(half + 1) * 512], op=ALU.add),
                   r=[po[1], xk], w=[x1k + f"h{half}"])
            dma("sp", lambda e: e.dma_start(out=x1_d[ti * 128:(ti + 1) * 128, :], in_=x1[:]),
                r=[x1k + "h0", x1k + "h1"], w=[f"x1_d{ti}"], key=x1k)
            op("act", lambda e: e.activation(out=tmp["junk"][:], in_=x1[:], func=AF.Square, accum_out=rs[:, 0:1]),
               r=[x1k + "h0", x1k + "h1"], w=["junk", "rs0"])
            op("dve", lambda e: e.tensor_scalar(out=rs[:, 1:2], in0=rs[:, 0:1], scalar1=1.0 / D, scalar2=1e-6,
                                                op0=ALU.mult, op1=ALU.add), r=["rs0"], w=["rs1"])
            op("act", lambda e: e.activation(out=rs[:, 2:3], in_=rs[:, 1:2], func=AF.Sqrt), r=["rs1"], w=["rs2"])
            op("dve", lambda e: e.reciprocal(out=rs[:, 3:4], in_=rs[:, 2:3]), r=["rs2"], w=["rs3"])
            hm = HM[sl]
            hmk = f"HM{sl}"
            op("dve", lambda e: e.scalar_tensor_tensor(out=hm[:], in0=x1[:], scalar=rs[:, 3:4], in1=GMOE[:],
                                                       op0=ALU.mult, op1=ALU.mult),
               r=[x1k + "h0", x1k + "h1", "rs3", "GMOE"], w=[hmk])
            for half in range(2):
                ptx = PS()
                for j in range(4):
                    kc = half * 4 + j
                    op("pe", lambda e: e.transpose(ptx[0][:, j * 128:(j + 1) * 128], x1[:, kc * 128:(kc + 1) * 128],
                                                   IDENTF), r=[x1k + "h0", x1k + "h1", "cst"], w=[ptx[1]])
                op("act", lambda e: e.activation(out=X1T[:, half * 4:half * 4 + 4, :].rearrange("p a b -> p (a b)"),
                                                 in_=ptx[0][:, 0:512], func=AF.Copy), r=[ptx[1]], w=[f"X1T{half}"])
            pl = PS()
            for kc in range(8):
                op("pe", lambda e: e.matmul(pl[0][:, 0:36], X1T[:, kc, :], Wr[:, kc, :], start=(kc == 0), stop=(kc == 7)),
                   r=["X1T0", "X1T1", "Wr"], w=[pl[1]])
            op("dve", lambda e: e.scalar_tensor_tensor(out=LG[:], in0=pl[0][:, 0:36], scalar=rs[:, 3:4], in1=BR[:],
                                                       op0=ALU.mult, op1=ALU.add), r=[pl[1], "rs3", "BR"], w=["LG"])
            V = lambda f, r, w: op("dve", f, r=r, w=w)
            V(lambda e: e.tensor_reduce(out=R_[:, 0:1], in_=LG[:, 0:4], axis=AX.X, op=ALU.max), ["LG"], ["R0"])
            V(lambda e: e.tensor_scalar(out=GOH[:], in0=LG[:, 0:4], scalar1=R_[:, 0:1], scalar2=None, op0=ALU.is_equal),
              ["LG", "R0"], ["GOH"])
            V(lambda e: e.tensor_scalar(out=R_[:, 1:2], in0=R_[:, 0:1], scalar1=-1.0, scalar2=None, op0=ALU.mult),
              ["R0"], ["R1"])
            op("act", lambda e: e.activation(out=EG[:], in_=LG[:, 0:4], func=AF.Exp, bias=R_[:, 1:2],
                                             accum_out=R_[:, 2:3]), r=["LG", "R1"], w=["EG", "R2"])
            V(lambda e: e.reciprocal(out=R_[:, 3:4], in_=R_[:, 2:3]), ["R2"], ["R3"])
            V(lambda e: e.tensor_tensor(out=T48[:], in0=LG[:, 4:36].rearrange("p (g x) -> p g x", g=4),
                                        in1=GOH[:].unsqueeze(2).to_broadcast([128, 4, 8]), op=ALU.mult),
              ["LG", "GOH"], ["T48"])
            V(lambda e: e.tensor_reduce(out=ESEL[:], in_=T48[:].rearrange("p g x -> p x g"), axis=AX.X, op=ALU.add),
              ["T48"], ["ESEL"])
            V(lambda e: e.tensor_reduce(out=R_[:, 4:5], in_=ESEL[:], axis=AX.X, op=ALU.max), ["ESEL"], ["R4"])
            V(lambda e: e.tensor_scalar(out=OHa[:], in0=ESEL[:], scalar1=R_[:, 4:5], scalar2=None, op0=ALU.is_equal),
              ["ESEL", "R4"], ["OHa"])
            V(lambda e: e.scalar_tensor_tensor(out=ES2[:], in0=OHa[:], scalar=-1e30, in1=ESEL[:], op0=ALU.mult,
                                               op1=ALU.add), ["OHa", "ESEL"], ["ES2"])
            V(lambda e: e.tensor_reduce(out=R_[:, 5:6], in_=ES2[:], axis=AX.X, op=ALU.max), ["ES2"], ["R5"])
            V(lambda e: e.tensor_scalar(out=OHb[:], in0=ES2[:], scalar1=R_[:, 5:6], scalar2=None, op0=ALU.is_equal),
              ["ES2", "R5"], ["OHb"])
            V(lambda e: e.tensor_tensor(out=R_[:, 6:7], in0=R_[:, 5:6], in1=R_[:, 4:5], op=ALU.subtract),
              ["R4", "R5"], ["R6"])
            op("act", lambda e: e.activation(out=R_[:, 7:8], in_=R_[:, 6:7], func=AF.Exp), r=["R6"], w=["R7"])
            V(lambda e: e.tensor_scalar(out=R_[:, 8:9], in0=R_[:, 7:8], scalar1=1.0, scalar2=None, op0=ALU.add),
              ["R7"], ["R8"])
            V(lambda e: e.reciprocal(out=R_[:, 9:10], in_=R_[:, 8:9]), ["R8"], ["R9"])
            V(lambda e: e.tensor_tensor(out=rw_f[:, 2 * ti:2 * ti + 1], in0=R_[:, 9:10], in1=R_[:, 3:4], op=ALU.mult),
              ["R9", "R3"], [f"rw{ti}a"])
            V(lambda e: e.tensor_tensor(out=rw_f[:, 2 * ti + 1:2 * ti + 2], in0=rw_f[:, 2 * ti:2 * ti + 1],
                                        in1=R_[:, 7:8], op=ALU.mult), [f"rw{ti}a", "R7"], [f"rw{ti}b"])
            gb = GOH[:].unsqueeze(2).to_broadcast([128, 4, 8])
            V(lambda e: e.tensor_tensor(out=OH1[:], in0=gb, in1=OHa[:].unsqueeze(1).to_broadcast([128, 4, 8]),
                                        op=ALU.mult), ["GOH", "OHa"], ["OH1"])
            V(lambda e: e.tensor_tensor(out=OH2[:], in0=gb, in1=OHb[:].unsqueeze(1).to_broadcast([128, 4, 8]),
                                        op=ALU.mult), ["GOH", "OHb"], ["OH2"])
            V(lambda e: e.tensor_tensor(out=OHSb[:], in0=g2(OH1), in1=g2(OH2), op=ALU.add), ["OH1", "OH2"], ["OHSb"])
            pc = PS()
            op("pe", lambda e: e.matmul(pc[0][:, 0:32], ustrb[:], OHSb[:], start=True, stop=True),
               r=["ustrb", "OHSb"], w=[pc[1]])
            op("pe", lambda e: e.matmul(pc[0][:, 32:64], onesb[:], OHSb[:], start=True, stop=True),
               r=["onesb", "OHSb"], w=[pc[1]])
            V(lambda e: e.tensor_tensor(out=POS[:], in0=pc[0][:, 0:32], in1=CNT[:], op=ALU.add), [pc[1], "CNT"], ["POS"])
            V(lambda e: e.tensor_tensor(out=CNT[:], in0=pc[0][:, 32:64], in1=CNT[:], op=ALU.add), [pc[1], "CNT"], ["CNT"])
            V(lambda e: e.tensor_scalar(out=POS[:], in0=POS[:], scalar1=float(CAP - 1), scalar2=None, op0=ALU.min),
              ["POS"], ["POS"])
            V(lambda e: e.tensor_tensor(out=POS[:], in0=POS[:], in1=EOFF, op=ALU.add), ["POS", "cst"], ["POS"])
            for j, OHx in enumerate((OH1, OH2)):
                V(lambda e: e.tensor_tensor(out=PT2[:], in0=POS[:], in1=g2(OHx), op=ALU.mult),
                  ["POS", f"OH{j + 1}"], ["PT2"])
                V(lambda e: e.tensor_reduce(out=SL[:, j:j + 1], in_=PT2[:], axis=AX.X, op=ALU.add), ["PT2"], [f"SL{j}"])
            V(lambda e: e.tensor_copy(out=slot_i[:, 2 * ti:2 * ti + 2], in_=SL[:]), ["SL0", "SL1"], [f"slot{ti}"])
            for j in range(2):
                dma("pool", lambda e: e.indirect_dma_start(
                    out=hs_d[:, :], out_offset=bass.IndirectOffsetOnAxis(ap=slot_i[:, 2 * ti + j:2 * ti + j + 1], axis=0),
                    in_=hm[:, :], in_offset=None), r=[hmk, f"slot{ti}", "hs_all"], w=[f"hs_sc{ti}_{j}"], key=f"sc{sl}{j}")
        if debug:
            RT = salloc("RT", [128, NTILE * 4], F32)
            op("dve", lambda e: e.tensor_copy(out=RT[:, 0:2 * NTILE], in_=slot_i[:]),
               r=[f"slot{t}" for t in range(NTILE)], w=["RT"])
            op("dve", lambda e: e.tensor_copy(out=RT[:, 2 * NTILE:4 * NTILE], in_=rw_f[:]),
               r=[f"rw{t}a" for t in range(NTILE)] + [f"rw{t}b" for t in range(NTILE)] + ["RT"], w=["RT"])
            dma("sp", lambda e: e.dma_start(out=rt_d, in_=RT[:]), r=["RT"], w=["rt_d"], key="RT")

    if "2" in PH:
        phase_reset()
        WG = [salloc(f"WG{i}", [128, 8, DE], BF16) for i in range(2)]
        WU = [salloc(f"WU{i}", [128, 8, DE], BF16) for i in range(2)]
        WD = [salloc(f"WD{i}", [128, 4, D], BF16) for i in range(2)]
        NB = 4
        XGb = [salloc(f"XGb{i}", [128, NB, D], BF16) for i in range(2)]
        XGT = salloc("XGT", [128, 8, NB * 128], BF16)
        SGf = [salloc(f"SGf{i}", [128, NB * 128], F32) for i in range(2)]
        HID = salloc("HID", [128, 4, NB * 128], BF16)
        YOb = [salloc(f"YOb{i}", [128, NB, D], F32) for i in range(2)]
        all_sc = [f"hs_sc{t}_{j}" for t in range(NTILE) for j in range(2)] + ["hs_all"]
        if CT <= 4:
            batches = [(0, CT)]
        elif CT == 5:
            batches = [(0, 3), (3, 2)]
        else:
            batches = [(r0, min(4, CT - r0)) for r0 in range(0, CT, 4)]
        it = 0
        for ex in range(NE):
            b = ex % 2
            dma("pool", lambda e: e.dma_start(out=WG[b][:], in_=wg_d[ex].rearrange("(p k) n -> p k n", k=8),
                                              max_dma_last_dim=8192), w=[f"WG{b}"], key=f"WG{b}")
            dma("pool", lambda e: e.dma_start(out=WU[b][:], in_=wu_d[ex].rearrange("(p k) n -> p k n", k=8),
                                              max_dma_last_dim=8192), w=[f"WU{b}"], key=f"WU{b}")
            dma("pool", lambda e: e.dma_start(out=WD[b][:], in_=wd_d[ex].rearrange("(k p) n -> p k n", p=128)),
                w=[f"WD{b}"], key=f"WD{b}")
            for (r0, nt) in batches:
                row0 = ex * CAP + r0 * 128
                xb = it % 2
                it += 1
                xgk = f"XGb{xb}"
                W_ = nt * 128
                dma("sp", lambda e: e.dma_start(
                    out=XGb[xb][:, 0:nt, :], in_=hs_d[row0:row0 + W_, :].rearrange("(t p) d -> p t d", p=128)),
                    r=(all_sc if "1b" in PH else []), w=[xgk], key=xgk)
                for t in range(nt):
                    pb = PSB()
                    xv = XGb[xb][:, t, :].rearrange("p (m j) -> p j m", j=8)
                    for j in range(8):
                        op("pe", lambda e: e.transpose(pb[0][:, j * 128:(j + 1) * 128], xv[:, j, :], identb[:]),
                           r=[xgk, "identb"], w=[pb[1]])
                    op("act", lambda e: e.activation(out=XGT[:, :, t * 128:(t + 1) * 128],
                                                     in_=pb[0][:, :].rearrange("p (j m) -> p j m", j=8), func=AF.Copy),
                       r=[pb[1]], w=[f"XGT{t}"])
                xgt_keys = [f"XGT{t}" for t in range(nt)]
                for hc in range(4):
                    pG = PS()
                    pU_ = PS()
                    for j in range(8):
                        op("pe", lambda e: e.matmul(pG[0][:, 0:W_], WG[b][:, j, hc * 128:(hc + 1) * 128],
                                                    XGT[:, j, 0:W_], start=(j == 0), stop=(j == 7)),
                           r=[f"WG{b}"] + xgt_keys, w=[pG[1]])
                    for j in range(8):
                        op("pe", lambda e: e.matmul(pU_[0][:, 0:W_], WU[b][:, j, hc * 128:(hc + 1) * 128],
                                                    XGT[:, j, 0:W_], start=(j == 0), stop=(j == 7)),
                           r=[f"WU{b}"] + xgt_keys, w=[pU_[1]])
                    sg = SGf[hc % 2]
                    sgk = f"SGf{hc % 2}"
                    op("act", lambda e: e.activation(out=sg[:, 0:W_], in_=pG[0][:, 0:W_], func=AF.Silu),
                       r=[pG[1]], w=[sgk])
                    op("dve", lambda e: e.tensor_tensor(out=HID[:, hc, 0:W_], in0=pU_[0][:, 0:W_], in1=sg[:, 0:W_],
                                                        op=ALU.mult), r=[pU_[1], sgk], w=[f"HID{hc}"])
                yok = f"YOb{xb}"
                for t in range(nt):
                    for half in range(2):
                        py = PS()
                        for hc in range(4):
                            op("pe", lambda e: e.matmul(py[0][:, 0:512], HID[:, hc, t * 128:(t + 1) * 128],
                                                        WD[b][:, hc, half * 512:(half + 1) * 512], start=(hc == 0),
                                                        stop=(hc == 3)), r=[f"HID{hc_}" for hc_ in range(4)] + [f"WD{b}"],
                               w=[py[1]])
                        if half:
                            op("act", lambda e: e.activation(out=YOb[xb][:, t, 512:1024], in_=py[0][:, 0:512],
                                                             func=AF.Copy), r=[py[1]], w=[yok + f"_{t}_1"])
                        else:
                            op("dve", lambda e: e.tensor_copy(out=YOb[xb][:, t, 0:512], in_=py[0][:, 0:512]),
                               r=[py[1]], w=[yok + f"_{t}_0"])
                dma("sp", lambda e: e.dma_start(
                    out=ys_d[row0:row0 + W_, :].rearrange("(t p) d -> p t d", p=128), in_=YOb[xb][:, 0:nt, :]),
                    r=[yok + f"_{t}_{hf}" for t in range(nt) for hf in range(2)], w=["ys_all"], key=yok)

    if "3" in PH:
        phase_reset()
        Wpg = salloc("Wpg", [128, 8, D], BF16)
        Wpp = salloc("Wpp", [128, 2, D], BF16)
        LNF = salloc("LNF", [128, D], F32)
        stgC = [salloc(f"stgC{i}", [128, D], F32) for i in range(2)]
        for kc in range(8):
            k_ = f"stgC{kc % 2}"
            t_ = stgC[kc % 2]
            dma("sp", lambda e: e.dma_start(out=t_[:], in_=wpg_d[kc * 128:(kc + 1) * 128, :]), w=[k_], key=k_)
            op("dve" if kc % 2 else "pool", lambda e: e.tensor_scalar(out=Wpg[:, kc, :], in0=t_[:],
                                                                      scalar1=vcol(V_LNPLE + kc), scalar2=1.0,
                                                                      op0=ALU.mult, op1=ALU.mult),
               r=[k_, "vec"], w=["Wpg"])
        for kc in range(2):
            k_ = f"stgC{kc % 2}"
            t_ = stgC[kc % 2]
            dma("sp", lambda e: e.dma_start(out=t_[:], in_=wpp_d[kc * 128:(kc + 1) * 128, :]), w=[k_], key=k_)
            op("dve", lambda e: e.tensor_copy(out=Wpp[:, kc, :], in_=t_[:]), r=[k_], w=["Wpp"])
        dma("sp", lambda e: e.dma_start(out=LNF[:], in_=lnf_d.to_broadcast([128, D])), w=["LNF"], key="LNF")
        X1c = [salloc(f"X1c{i}", [128, D], F32) for i in range(2)]
        Y1 = [salloc(f"Y1_{i}", [128, D], F32) for i in range(2)]
        Y2 = [salloc(f"Y2_{i}", [128, D], F32) for i in range(2)]
        Pin = [salloc(f"Pin{i}", [128, 256], F32) for i in range(2)]
        Pb2 = [salloc(f"Pb{i}", [128, 256], BF16) for i in range(2)]
        PTt2 = [salloc(f"PTt{i}", [128, 2, 128], BF16) for i in range(2)]
        X22 = [salloc(f"X2{i}", [128, D], F32) for i in range(2)]
        tmp2 = [dict(junk=salloc(f"junkc{i}", [128, D], BF16), ss=salloc(f"ssc{i}", [128, 4], F32),
                     rstd=salloc(f"rstdc{i}", [128, 1], F32), xbf=salloc(f"xbfc{i}", [128, D], BF16))
                for i in range(2)]
        hTc2 = [salloc(f"hTc{i}", [128, 8, 128], BF16) for i in range(2)]
        GP2 = [salloc(f"GP{i}", [128, D], F32) for i in range(2)]
        X32 = [salloc(f"X3{i}", [128, D], F32) for i in range(2)]
        rf2 = [salloc(f"rf{i}", [128, 4], F32) for i in range(2)]
        OUTt = [salloc(f"OUT{i}", [128, D], F32) for i in range(2)]

        def body3(ti):
            sl = ti % 2
            tl.pool = "E" if sl == 0 else "O"
            op, dma = stream(f"@{sl}")
            Pb, PTt, X2, tmp, hTc, GP, X3, rf = Pb2[sl], PTt2[sl], X22[sl], tmp2[sl], hTc2[sl], GP2[sl], X32[sl], rf2[sl]
            x1k, y1k, y2k, pk = f"X1c{sl}", f"Y1_{sl}", f"Y2_{sl}", f"Pin{sl}"
            dma("sp", lambda e: e.dma_start(out=X1c[sl][:], in_=x1_d[ti * 128:(ti + 1) * 128, :]),
                r=([f"x1_d{ti}"] if "1b" in PH else []), w=[x1k], key=x1k)
            dma("sp", lambda e: e.dma_start(out=Pin[sl][:], in_=p_d[ti * 128:(ti + 1) * 128, :]), w=[pk], key=pk)
            for (Yt, ykk, j) in ((Y1[sl], y1k, 0), (Y2[sl], y2k, 1)):
                dma("pool", lambda e: e.indirect_dma_start(
                    out=Yt[:, :], out_offset=None, in_=ys_d[:, :],
                    in_offset=bass.IndirectOffsetOnAxis(ap=slot_i[:, 2 * ti + j:2 * ti + j + 1], axis=0)),
                    r=(["ys_all", f"slot{ti}"] if "2" in PH else []), w=[ykk], key=ykk)
            op("dve", lambda e: e.scalar_tensor_tensor(out=X2[:], in0=Y1[sl][:], scalar=rw_f[:, 2 * ti:2 * ti + 1],
                                                       in1=X1c[sl][:], op0=ALU.mult, op1=ALU.add),
               r=[y1k, x1k, f"rw{ti}a"], w=["X2"])
            op("dve", lambda e: e.scalar_tensor_tensor(out=X2[:], in0=Y2[sl][:], scalar=rw_f[:, 2 * ti + 1:2 * ti + 2],
                                                       in1=X2[:], op0=ALU.mult, op1=ALU.add),
               r=[y2k, "X2", f"rw{ti}b"], w=["X2"])
            rms_to_hT(ti, X2, "X2", hTc, "hTc", tmp, normalize=False, op=op)
            op("pool", lambda e: e.tensor_copy(out=Pb[:], in_=Pin[sl][:]), r=[pk], w=["Pb"])
            pb = PSB()
            for kc in range(2):
                op("pe", lambda e: e.transpose(pb[0][:, kc * 128:(kc + 1) * 128], Pb[:, kc * 128:(kc + 1) * 128],
                                               identb[:]), r=["Pb", "identb"], w=[pb[1]])
            op("act", lambda e: e.activation(out=PTt[:].rearrange("p a b -> p (a b)"), in_=pb[0][:, 0:256],
                                             func=AF.Copy), r=[pb[1]], w=["PTt"])
            for half in range(2):
                pgm = PS()
                for kc in range(8):
                    op("pe", lambda e: e.matmul(pgm[0][:, 0:512], hTc[:, kc, :], Wpg[:, kc, half * 512:(half + 1) * 512],
                                                start=(kc == 0), stop=(kc == 7)), r=["hTc", "Wpg"], w=[pgm[1]])
                op("act", lambda e: e.activation(out=GP[:, half * 512:(half + 1) * 512], in_=pgm[0][:, 0:512],
                                                 func=AF.Sigmoid, scale=tmp["rstd"][:, 0:1]),
                   r=[pgm[1], "rstd"], w=[f"GP{half}"])
                ppm = PS()
                for kc in range(2):
                    op("pe", lambda e: e.matmul(ppm[0][:, 0:512], PTt[:, kc, :], Wpp[:, kc, half * 512:(half + 1) * 512],
                                                start=(kc == 0), stop=(kc == 1)), r=["PTt", "Wpp"], w=[ppm[1]])
                op("dve", lambda e: e.tensor_tensor(out=GP[:, half * 512:(half + 1) * 512], in0=ppm[0][:, 0:512],
                                                    in1=GP[:, half * 512:(half + 1) * 512], op=ALU.mult),
                   r=[ppm[1], f"GP{half}"], w=[f"GP{half}"])
                op("dve", lambda e: e.tensor_tensor(out=X3[:, half * 512:(half + 1) * 512],
                                                     in0=X2[:, half * 512:(half + 1) * 512],
                                                     in1=GP[:, half * 512:(half + 1) * 512], op=ALU.add),
                   r=["X2", f"GP{half}"], w=[f"X3{half}"])
            op("act", lambda e: e.activation(out=tmp["junk"][:], in_=X3[:], func=AF.Square, accum_out=rf[:, 0:1]),
               r=["X30", "X31"], w=["junk", "rf0"])
            op("dve", lambda e: e.tensor_scalar(out=rf[:, 1:2], in0=rf[:, 0:1], scalar1=1.0 / D, scalar2=1e-6,
                                                op0=ALU.mult, op1=ALU.add), r=["rf0"], w=["rf1"])
            op("act", lambda e: e.activation(out=rf[:, 2:3], in_=rf[:, 1:2], func=AF.Sqrt), r=["rf1"], w=["rf2"])
            op("dve", lambda e: e.reciprocal(out=rf[:, 3:4], in_=rf[:, 2:3]), r=["rf2"], w=["rf3"])
            ok = f"OUT{sl}"
            op("dve", lambda e: e.scalar_tensor_tensor(out=OUTt[sl][:], in0=X3[:], scalar=rf[:, 3:4], in1=LNF[:],
                                                       op0=ALU.mult, op1=ALU.mult), r=["X30", "X31", "rf3", "LNF"],
               w=[ok])
            dma("sp", lambda e: e.dma_start(out=out_d[ti * 128:(ti + 1) * 128, :], in_=OUTt[sl][:]),
                r=[ok], w=[f"out_d{ti}"], key=ok)

        for t0 in range(0, NTILE, 2):
            WV.run([lambda t0=t0: body3(t0), lambda t0=t0: body3(t0 + 1)], [1, 1], seq=not cfg.get("weave23", False))
        tl.pool = None

    S_.op("sp", nop_fns["sp"], r=[], w=[])
    return nc, S_


def emit(nc, S_):
    pref = S_.finish(nc, None, None, None)
    import contextlib
    with contextlib.ExitStack() as es:
        sems = {e: es.enter_context(nc.semaphore("s_" + e)) for e in ENGS}
        dsem = {k: es.enter_context(nc.semaphore("d_" + str(i))) for i, k in enumerate(S_.dma_cnt)}
        bsem = [es.enter_context(nc.semaphore(f"bar{i}")) for i in range(2)]
        block = es.enter_context(nc.Block())

        def run(ename, eh):
            for o in S_.ops[ename]:
                for (key, val) in o["waits"]:
                    if key[0] == "eng":
                        eh.wait_ge(sems[key[1]], pref[key[1]][val])
                    else:
                        eh.wait_ge(dsem[key[1]], val)
                ins = o["fn"](eh)
                if o.get("bar"):
                    nb = o["bar"]
                    ins.then_inc(bsem[nb % 2], 1)
                    eh.wait_ge(bsem[nb % 2], len(ENGS) * ((nb + 1) // 2 if nb % 2 else nb // 2))
                    continue
                if o["dma"] is not None:
                    ins.then_inc(dsem[o["dma"]], 16)
                elif o["inc"]:
                    ins.then_inc(sems[ename], 1)
            if ename == "sp":
                for k, c in S_.dma_cnt.items():
                    eh.wait_ge(dsem[k], c)

        @block.tensor
        def _(t):
            run("pe", t)

        @block.scalar
        def _(a):
            run("act", a)

        @block.vector
        def _(v):
            run("dve", v)

        @block.gpsimd
        def _(g):
            run("pool", g)

        @block.sync
        def _(s):
            run("sp", s)
    return nc


def _perm_q():
    idx = []
    for c in range(4):
        idx += list(range(c * 64, c * 64 + 64)) + list(range((c + 4) * 64, (c + 4) * 64 + 64))
    return np.array(idx)


def _consts(cap):
    c = np.zeros((128, 832), np.float32)
    c[:, 768:800] = (np.arange(32) * cap)[None, :]
    i = np.arange(128)
    c[:, 0:128] = np.eye(128)
    c[:, 128:256] = (i[:, None] < i[None, :])
    c[:, 256:384] = (i[:, None] <= i[None, :])
    c[:, 384:512] = (i[:, None] > i[None, :])
    invf = (10000.0 ** (-np.arange(32, dtype=np.float64) / 32.0)) / (2 * np.pi)
    c[:, 512:544] = invf[None, :]
    c[:, 544:576] = invf[None, :]
    c[:, 576:608] = 0.0
    c[:, 608:640] = 0.25
    c[:, 640:768] = ((i[:, None] // 64) == (i[None, :] // 64))
    return c


def prep_shared(inp, cap):
    f = lambda a: np.ascontiguousarray(np.asarray(a, dtype=np.float32))
    pq = _perm_q()
    w_in = f(inp["w_in"][0])
    cols = np.concatenate([pq, np.arange(512, 4608)])
    w_in = np.ascontiguousarray(w_in[:, cols])
    vec = np.zeros((128, 70), np.float32)
    vec[:, 0:8] = f(inp["ln_mix"][0]).reshape(8, 128).T
    vec[:, 8:16] = f(inp["ln_moe"][0]).reshape(8, 128).T
    vec[:, 16:24] = f(inp["ln_ple"][0]).reshape(8, 128).T
    vec[:, 24:38] = f(inp["mu_shift"][0]).reshape(14, 128).T
    for j, nm in enumerate(["w0", "a0", "k_k", "k_a", "r_k", "ln_x_w", "ln_x_b"]):
        vec[:, 38 + 4 * j:42 + 4 * j] = f(inp[nm][0]).reshape(4, 128).T
    sk = f(inp["sinks"][0])
    for c in range(4):
        vec[0:64, 66 + c] = sk[c]
        vec[64:128, 66 + c] = sk[c + 4]
    sh = dict(
        w_in=w_in, vecs=vec, cst=_consts(cap), ln_moe_row=f(inp['ln_moe'][0])[None, :],
        wlora=np.ascontiguousarray(np.concatenate([f(inp["w_decay_up"][0]), f(inp["w_aaa_up"][0])], 0)),
        wgu=f(inp["w_gate_up"][0]),
        w_ba=np.ascontiguousarray(f(inp["w_branch_att"][0])[pq, :]),
        w_bb=f(inp["w_branch_rwkv"][0]),
        w_out=f(inp["w_out"][0]),
        w_r=np.ascontiguousarray(np.concatenate([f(inp["w_group"][0]), f(inp["w_expert"][0])], 1)),
        b_r=np.ascontiguousarray(np.concatenate([f(inp["b_group"][0]), f(inp["b_expert"][0])])[None, :]),
        w_gate_e=f(inp["w_gate_e"][0]), w_up_e=f(inp["w_up_e"][0]), w_down_e=f(inp["w_down_e"][0]),
        w_pg=f(inp["w_ple_gate"][0]), w_pp=f(inp["w_ple_proj"][0]),
        ln_final=f(inp["ln_final"])[None, :],
    )
    return sh


def prep_core(inp, sh, b0, nseq):
    x = np.asarray(inp["x"], np.float32)[b0:b0 + nseq]
    S = x.shape[1]
    m = dict(sh)
    m["x"] = np.ascontiguousarray(x.reshape(nseq * S, D))
    m["p"] = np.ascontiguousarray(np.asarray(inp["p"], np.float32)[0, b0:b0 + nseq].reshape(nseq * S, 256))
    pos = np.asarray(inp["positions"], np.int32)[b0:b0 + nseq].reshape(-1)
    m["posT"] = np.ascontiguousarray(pos.reshape(-1, 128).T)
    return m


FULL_CFG = dict(NSEQ=4, S=2048, CAP=640)


def kernel(**inputs):
    cfg = FULL_CFG
    nc, S_ = build(cfg)
    emit(nc, S_)
    sh = prep_shared(inputs, cfg["CAP"])
    in_maps = [prep_core(inputs, sh, c * cfg["NSEQ"], cfg["NSEQ"]) for c in range(8)]
    res = run_bass_kernel_spmd(nc, in_maps, core_ids=list(range(8)))
    outs = [r["out"].reshape(cfg["NSEQ"], cfg["S"], D) for r in res.results]
    return np.concatenate(outs, 0).astype(np.float32)
```
